# Optimizing a Trainium2 kernel written in Bass

```python
import math
import functools
import jax
import jax.numpy as jnp
from jax import lax
import numpy as np

D_MODEL = 1024
BATCH = 8
SEQ = 2048
DEPTH = 1
DEC_BATCH = 32
DEC_SEQ = 1
PAST_LEN = 16384
PAGE_SIZE = 128

H_A = 4
D_HA = 64
ROT_DIM = D_HA // 4
ROPE_THETA = 500000.0
Q_BLOCK = 128
H_M = 4
DK_M = 64
DV_M = 128
MLSTM_CHUNK = 64
GATE_SOFTCAP = 15.0
N_EXPERTS = 32
TOP_K = 4
D_FF = 1024
SWIGLU_LIMIT = 7.0
SWIGLU_ALPHA = 1.702
LN_EPS = 1e-5
DEEPNORM_ALPHA = (2.0 * DEPTH) ** 0.25
DEEPNORM_BETA = (8.0 * DEPTH) ** -0.25
QA_W = H_A * 2 * D_HA
VA_W = H_A * 2 * D_HA
QM_W = H_M * DK_M
VM_W = H_M * DV_M
N_IN = 2 * QA_W + VA_W + 2 * QM_W + 2 * VM_W + 2 * H_M + 2 * D_MODEL

F32 = jnp.float32

kernel_name = 'hybrid_diffattn_mlstm_moe_step'


def _split_points():
    sizes = (QA_W, QA_W, VA_W, QM_W, QM_W, VM_W, H_M, H_M, VM_W, D_MODEL, D_MODEL)
    pts, acc = [], 0
    for s in sizes[:-1]:
        acc += s
        pts.append(acc)
    return pts


def softcap(x):
    return GATE_SOFTCAP * jnp.tanh(x / GATE_SOFTCAP)


def rope(x, pos):
    inv = ROPE_THETA ** (-jnp.arange(0, ROT_DIM, 2, dtype=F32) / ROT_DIM)
    ang = pos.astype(F32)[:, None] * inv[None, :]
    cos = jnp.cos(ang)[None, :, None, None, :]
    sin = jnp.sin(ang)[None, :, None, None, :]
    xf = x.astype(F32)
    half = ROT_DIM // 2
    x1 = xf[..., :half]
    x2 = xf[..., half:ROT_DIM]
    out = jnp.concatenate([x1 * cos - x2 * sin, x2 * cos + x1 * sin, xf[..., ROT_DIM:]], axis=-1)
    return out.astype(x.dtype)


def rms_norm(x, g):
    xf = x.astype(F32)
    y = xf * lax.rsqrt(jnp.mean(xf * xf, axis=-1, keepdims=True) + LN_EPS)
    return (y * g.astype(F32)).astype(x.dtype)


def head_layer_norm(h, g):
    hf = h.astype(F32)
    mu = jnp.mean(hf, axis=-1, keepdims=True)
    var = jnp.mean(jnp.square(hf - mu), axis=-1, keepdims=True)
    y = (hf - mu) * lax.rsqrt(var + LN_EPS) * g.astype(F32).reshape(H_M, DV_M)
    return y.astype(h.dtype)


def layer_norm(x, g, b):
    xf = x.astype(F32)
    mu = jnp.mean(xf, axis=-1, keepdims=True)
    var = jnp.mean(jnp.square(xf - mu), axis=-1, keepdims=True)
    y = (xf - mu) * lax.rsqrt(var + LN_EPS) * g.astype(F32) + b.astype(F32)
    return y.astype(x.dtype)


def diff_lambda(lq1, lk1, lq2, lk2, lam_init):
    return (jnp.exp(jnp.sum(lq1.astype(F32) * lk1.astype(F32)))
            - jnp.exp(jnp.sum(lq2.astype(F32) * lk2.astype(F32))) + lam_init)


def project(x, w_in, b_igate, b_fgate, pos):
    B, L, _ = x.shape
    z = jnp.einsum('bld,dn->bln', x, w_in)
    qa, ka, va, qm, km, vm, ig, fg, og, ga, gm = jnp.split(z, _split_points(), axis=-1)
    qa = rope(qa.reshape(B, L, H_A, 2, D_HA), pos)
    ka = rope(ka.reshape(B, L, H_A, 2, D_HA), pos)
    va = va.reshape(B, L, H_A, 2 * D_HA)
    qm = qm.reshape(B, L, H_M, DK_M)
    km = km.reshape(B, L, H_M, DK_M) * (DK_M ** -0.5)
    vm = vm.reshape(B, L, H_M, DV_M)
    ig = softcap(ig.astype(F32) + b_igate.astype(F32))
    fg = softcap(fg.astype(F32) + b_fgate.astype(F32))
    return qa, ka, va, qm, km, vm, ig, fg, og, ga, gm


def diff_attn_prompt(q, k, v, lam):
    B, S = q.shape[:2]
    nb = S // Q_BLOCK
    qb = q.reshape(B, nb, Q_BLOCK, H_A, 2, D_HA).swapaxes(0, 1)
    kpos = jnp.arange(S)
    scale = D_HA ** -0.5

    def one_block(args):
        i, qi = args
        qpos = i * Q_BLOCK + jnp.arange(Q_BLOCK)
        mask = kpos[None, :] <= qpos[:, None]
        s = jnp.einsum('bqhcd,bkhcd->bchqk', qi, k).astype(F32) * scale
        p = jax.nn.softmax(jnp.where(mask, s, -jnp.inf), axis=-1)
        p = p[:, 0] - lam * p[:, 1]
        return jnp.einsum('bhqk,bkhe->bqhe', p.astype(v.dtype), v)

    o = lax.map(one_block, (jnp.arange(nb), qb))
    return o.swapaxes(0, 1).reshape(B, S, H_A, 2 * D_HA)


def diff_attn_paged(q, k, v, lam, cache_k, cache_v, page_table):
    B, L = q.shape[:2]
    kp = cache_k[page_table].reshape(B, -1, H_A, 2, D_HA)
    vp = cache_v[page_table].reshape(B, -1, H_A, 2 * D_HA)
    P = kp.shape[1]
    scale = D_HA ** -0.5
    s_past = jnp.einsum('bqhcd,bkhcd->bchqk', q, kp).astype(F32) * scale
    s_new = jnp.einsum('bqhcd,bkhcd->bchqk', q, k).astype(F32) * scale
    causal = jnp.tril(jnp.ones((L, L), dtype=bool))
    s_new = jnp.where(causal, s_new, -jnp.inf)
    p = jax.nn.softmax(jnp.concatenate([s_past, s_new], axis=-1), axis=-1)
    p = (p[:, 0] - lam * p[:, 1]).astype(v.dtype)
    return (jnp.einsum('bhqk,bkhe->bqhe', p[..., :P], vp)
            + jnp.einsum('bhqk,bkhe->bqhe', p[..., P:], v))


def mlstm(q, k, v, ig, fg, c0, n0, m0):
    B, L = q.shape[:2]
    lc = MLSTM_CHUNK if L % MLSTM_CHUNK == 0 else L
    nc = L // lc

    def chunks(t):
        t = t.astype(F32).reshape((B, nc, lc) + t.shape[2:])
        return jnp.moveaxis(jnp.moveaxis(t, 3, 2), 1, 0)

    lf = jax.nn.log_sigmoid(fg)
    xs = (chunks(q), chunks(k), chunks(v), chunks(ig), chunks(lf))
    causal = jnp.tril(jnp.ones((lc, lc), dtype=bool))

    def step(carry, inp):
        C, n, m = carry
        qc, kc, vc, ic, fc = inp
        b = jnp.cumsum(fc, axis=-1)
        D = jnp.where(causal, b[..., :, None] - b[..., None, :] + ic[..., None, :], -jnp.inf)
        m_inter = b + m[..., None]
        m_t = jnp.maximum(m_inter, jnp.max(D, axis=-1))
        w_inter = jnp.exp(m_inter - m_t)
        S = jnp.einsum('bhtd,bhsd->bhts', qc, kc) * jnp.exp(D - m_t[..., None])
        num = (w_inter[..., None] * jnp.einsum('bhtd,bhde->bhte', qc, C)
               + jnp.einsum('bhts,bhse->bhte', S, vc))
        den = w_inter * jnp.einsum('bhtd,bhd->bht', qc, n) + jnp.sum(S, axis=-1)
        h = num / jnp.maximum(jnp.abs(den), jnp.exp(-m_t))[..., None]
        m_new = m_t[..., -1]
        decay = jnp.exp(b[..., -1] + m - m_new)
        wk = jnp.exp(b[..., -1:] - b + ic - m_new[..., None])
        C_new = decay[..., None, None] * C + jnp.einsum('bhs,bhsd,bhse->bhde', wk, kc, vc)
        n_new = decay[..., None] * n + jnp.einsum('bhs,bhsd->bhd', wk, kc)
        return (C_new, n_new, m_new), h

    (C, n, m), hs = lax.scan(step, (c0.astype(F32), n0.astype(F32), m0.astype(F32)), xs)
    h = jnp.moveaxis(jnp.moveaxis(hs, 0, 1), 2, 3).reshape(B, L, H_M, DV_M)
    return h.astype(v.dtype), C, n, m


def moe(x, w_router, b_router, w_gate, b_gate, w_up, b_up, w_down, b_down):
    B, L, D = x.shape
    xt = x.reshape(-1, D)
    logits = jnp.einsum('td,de->te', xt, w_router).astype(F32) + b_router.astype(F32)
    topv, topi = lax.top_k(logits, TOP_K)
    probs = jax.nn.softmax(topv, axis=-1)
    comb = jnp.sum(jax.nn.one_hot(topi, N_EXPERTS, dtype=F32) * probs[..., None], axis=1)
    out = jnp.zeros(xt.shape, F32)
    for e in range(N_EXPERTS):
        g = jnp.minimum(xt @ w_gate[e] + b_gate[e], SWIGLU_LIMIT)
        u = jnp.clip(xt @ w_up[e] + b_up[e], -SWIGLU_LIMIT, SWIGLU_LIMIT)
        hid = g * jax.nn.sigmoid(SWIGLU_ALPHA * g) * (u + 1.0)
        out = out + comb[:, e:e + 1] * (hid @ w_down[e] + b_down[e]).astype(F32)
    return out.astype(x.dtype).reshape(B, L, D)


def decoder_layer(x, pos, attend, c0, n0, m0, lam, lam_init,
                  w_in, b_igate, b_fgate, subln_g, mh_norm_g, w_ba, w_bm, w_o, ln1_g, ln1_b,
                  w_router, b_router, w_gate, b_gate, w_up, b_up, w_down, b_down, ln2_g, ln2_b):
    B, L, _ = x.shape
    qa, ka, va, qm, km, vm, ig, fg, og, ga, gm = project(x, w_in, b_igate, b_fgate, pos)
    attn = attend(qa, ka, va, lam)
    hm, c, n, m = mlstm(qm, km, vm, ig, fg, c0, n0, m0)
    a_in = (rms_norm(attn, subln_g) * (1.0 - lam_init)).reshape(B, L, VA_W)
    a_branch = jnp.einsum('blf,fd->bld', a_in, w_ba)
    m_in = head_layer_norm(hm, mh_norm_g).reshape(B, L, VM_W) * jax.nn.sigmoid(og)
    m_branch = jnp.einsum('blf,fd->bld', m_in, w_bm)
    merged = jax.nn.sigmoid(ga) * a_branch + jax.nn.sigmoid(gm) * m_branch
    mix = jnp.einsum('bld,de->ble', merged, w_o)
    h = layer_norm(DEEPNORM_ALPHA * x + mix, ln1_g, ln1_b)
    ff = moe(h, w_router, b_router, w_gate, b_gate, w_up, b_up, w_down, b_down)
    y = layer_norm(DEEPNORM_ALPHA * h + ff, ln2_g, ln2_b)
    return y, ka.reshape(B, L, H_A, 2 * D_HA), va, c, n, m


def setup_inputs(seed: int = 0) -> dict:
    key = jax.random.key(seed)
    ks = iter(jax.random.split(key, 40))

    def nrm(shape, scale):
        return jax.random.normal(next(ks), shape, F32) * scale

    n_pages = PAST_LEN // PAGE_SIZE
    n_used = DEC_BATCH * n_pages
    n_pool = n_used + (n_used + 3) // 4
    perm = jax.random.permutation(next(ks), n_pool)
    page_table = perm[:n_used].reshape(DEC_BATCH, n_pages).astype(jnp.int32)

    x_prompt = nrm((BATCH, SEQ, D_MODEL), 1.0)
    x_sample = nrm((DEC_BATCH, DEC_SEQ, D_MODEL), 1.0)
    cache_k = nrm((DEPTH, n_pool, PAGE_SIZE, H_A, 2 * D_HA), 1.0)
    cache_v = nrm((DEPTH, n_pool, PAGE_SIZE, H_A, 2 * D_HA), 1.0)
    state_c = nrm((DEPTH, DEC_BATCH, H_M, DK_M, DV_M), 1.0)
    state_n = nrm((DEPTH, DEC_BATCH, H_M, DK_M), 1.0)
    state_m = nrm((DEPTH, DEC_BATCH, H_M), 0.5)

    w_in = nrm((DEPTH, D_MODEL, N_IN), D_MODEL ** -0.5)
    b_igate = nrm((DEPTH, H_M), 0.1)
    b_fgate = 3.0 + 3.0 * jax.random.uniform(next(ks), (DEPTH, H_M), F32)
    lambda_q1 = nrm((DEPTH, D_HA), 0.1)
    lambda_k1 = nrm((DEPTH, D_HA), 0.1)
    lambda_q2 = nrm((DEPTH, D_HA), 0.1)
    lambda_k2 = nrm((DEPTH, D_HA), 0.1)
    subln_g = 1.0 + nrm((DEPTH, 2 * D_HA), 0.01)
    mh_norm_g = 1.0 + nrm((DEPTH, VM_W), 0.01)
    w_ba = nrm((DEPTH, VA_W, D_MODEL), VA_W ** -0.5 * DEEPNORM_BETA)
    w_bm = nrm((DEPTH, VM_W, D_MODEL), VM_W ** -0.5 * DEEPNORM_BETA)
    w_o = nrm((DEPTH, D_MODEL, D_MODEL), D_MODEL ** -0.5 * DEEPNORM_BETA)
    ln1_g = 1.0 + nrm((DEPTH, D_MODEL), 0.01)
    ln1_b = nrm((DEPTH, D_MODEL), 0.01)
    w_router = nrm((DEPTH, D_MODEL, N_EXPERTS), D_MODEL ** -0.5)
    b_router = nrm((DEPTH, N_EXPERTS), 0.01)
    w_gate = nrm((DEPTH, N_EXPERTS, D_MODEL, D_FF), D_MODEL ** -0.5)
    b_gate = nrm((DEPTH, N_EXPERTS, D_FF), 0.01)
    w_up = nrm((DEPTH, N_EXPERTS, D_MODEL, D_FF), D_MODEL ** -0.5)
    b_up = nrm((DEPTH, N_EXPERTS, D_FF), 0.01)
    w_down = nrm((DEPTH, N_EXPERTS, D_FF, D_MODEL), D_FF ** -0.5 * DEEPNORM_BETA)
    b_down = nrm((DEPTH, N_EXPERTS, D_MODEL), 0.01)
    ln2_g = 1.0 + nrm((DEPTH, D_MODEL), 0.01)
    ln2_b = nrm((DEPTH, D_MODEL), 0.01)
    return {
        'x_prompt': x_prompt, 'x_sample': x_sample,
        'cache_k': cache_k, 'cache_v': cache_v,
        'state_c': state_c, 'state_n': state_n, 'state_m': state_m,
        'page_table': page_table,
        'w_in': w_in, 'b_igate': b_igate, 'b_fgate': b_fgate,
        'lambda_q1': lambda_q1, 'lambda_k1': lambda_k1, 'lambda_q2': lambda_q2, 'lambda_k2': lambda_k2,
        'subln_g': subln_g, 'mh_norm_g': mh_norm_g,
        'w_ba': w_ba, 'w_bm': w_bm, 'w_o': w_o, 'ln1_g': ln1_g, 'ln1_b': ln1_b,
        'w_router': w_router, 'b_router': b_router,
        'w_gate': w_gate, 'b_gate': b_gate, 'w_up': w_up, 'b_up': b_up,
        'w_down': w_down, 'b_down': b_down, 'ln2_g': ln2_g, 'ln2_b': ln2_b,
    }


def reference(x_prompt, x_sample, cache_k, cache_v, state_c, state_n, state_m, page_table,
              w_in, b_igate, b_fgate, lambda_q1, lambda_k1, lambda_q2, lambda_k2,
              subln_g, mh_norm_g, w_ba, w_bm, w_o, ln1_g, ln1_b,
              w_router, b_router, w_gate, b_gate, w_up, b_up, w_down, b_down, ln2_g, ln2_b):
    Bp, Lp = x_prompt.shape[:2]
    Ls = x_sample.shape[1]
    past_len = page_table.shape[1] * PAGE_SIZE
    pos_p = jnp.arange(Lp, dtype=jnp.int32)
    pos_s = past_len + jnp.arange(Ls, dtype=jnp.int32)
    y_p, y_s = x_prompt, x_sample
    kp_l, vp_l, cp_l, np_l, mp_l = [], [], [], [], []
    ks_l, vs_l, cs_l, ns_l, ms_l = [], [], [], [], []
    for l in range(DEPTH):
        lam_init = 0.8 - 0.6 * math.exp(-0.3 * l)
        lam = diff_lambda(lambda_q1[l], lambda_k1[l], lambda_q2[l], lambda_k2[l], lam_init)
        lw = (w_in[l], b_igate[l], b_fgate[l], subln_g[l], mh_norm_g[l], w_ba[l], w_bm[l], w_o[l],
              ln1_g[l], ln1_b[l], w_router[l], b_router[l], w_gate[l], b_gate[l], w_up[l], b_up[l],
              w_down[l], b_down[l], ln2_g[l], ln2_b[l])
        c0 = jnp.zeros((Bp, H_M, DK_M, DV_M), F32)
        n0 = jnp.zeros((Bp, H_M, DK_M), F32)
        m0 = jnp.zeros((Bp, H_M), F32)
        y_p, k_r, v_r, c_r, n_r, m_r = decoder_layer(
            y_p, pos_p, diff_attn_prompt, c0, n0, m0, lam, lam_init, *lw)
        kp_l.append(k_r); vp_l.append(v_r); cp_l.append(c_r); np_l.append(n_r); mp_l.append(m_r)
        attend_s = functools.partial(diff_attn_paged, cache_k=cache_k[l], cache_v=cache_v[l],
                                     page_table=page_table)
        y_s, k_r, v_r, c_r, n_r, m_r = decoder_layer(
            y_s, pos_s, attend_s, state_c[l], state_n[l], state_m[l], lam, lam_init, *lw)
        ks_l.append(k_r); vs_l.append(v_r); cs_l.append(c_r); ns_l.append(n_r); ms_l.append(m_r)
    dp, ds = x_prompt.dtype, x_sample.dtype
    y_prompt = y_p
    y_sample = y_s
    k_prompt = jnp.stack(kp_l)
    v_prompt = jnp.stack(vp_l)
    c_prompt = jnp.stack(cp_l).astype(dp)
    n_prompt = jnp.stack(np_l).astype(dp)
    m_prompt = jnp.stack(mp_l).astype(dp)
    k_sample = jnp.stack(ks_l)
    v_sample = jnp.stack(vs_l)
    c_sample = jnp.stack(cs_l).astype(ds)
    n_sample = jnp.stack(ns_l).astype(ds)
    m_sample = jnp.stack(ms_l).astype(ds)
    return (y_prompt, y_sample, k_prompt, v_prompt, c_prompt, n_prompt, m_prompt,
            k_sample, v_sample, c_sample, n_sample, m_sample)
```

```python
import math
from contextlib import ExitStack

import numpy as np
import concourse.bass as bass
import concourse.mybir as mybir
from concourse.bass_utils import run_bass_kernel_spmd

F32 = mybir.dt.float32
F32R = mybir.dt.float32r
BF16 = mybir.dt.bfloat16
I32 = mybir.dt.int32
ALU = mybir.AluOpType
AF = mybir.ActivationFunctionType
AX = mybir.AxisListType

FULL = dict(S=2048, PAST=16384, NPOOL=5120, E=32, FF=1024, CAP=512, D=1024)

HA, DH = 4, 64
HM, DK, DV = 4, 64, 128
ROT = 16
THETA = 500000.0
SOFTCAP = 15.0
LIMIT = 7.0
SALPHA = 1.702
EPS = 1e-5
DEPTH = 1
ALPHA = (2.0 * DEPTH) ** 0.25
LAM_INIT = 0.8 - 0.6 * math.exp(-0.3 * 0)
NSO = 4
NSG = 16
BIG = 1.0e6


def r_(ap):
    return ap.bitcast(F32R)


class Sync:
    def __init__(self, nc, es):
        self.nc = nc
        self.es = es
        self.engs = {"pe": nc.tensor, "act": nc.scalar, "dve": nc.vector, "pool": nc.gpsimd, "sp": nc.sync}
        self.sem = {}
        self.cnt = {}
        for n in ("pe", "act", "dve", "pool"):
            self.sem[n] = es.enter_context(nc.semaphore("s_" + n))
            self.cnt[n] = 0
        self.seen = {n: {} for n in self.engs}
        self.last_w = {}
        self.readers = {}
        self.dsem = {}
        self.dcnt = {}
        self.out_tokens = []
        self.extra_tokens = []

    def _deps(self, r, w):
        deps = []
        for k in r:
            deps += self.last_w.get(k, [])
        for k in w:
            deps += self.readers.get(k, [])
            deps += self.last_w.get(k, [])
        return deps

    def barrier(self):
        toks = [("s_" + n, self.sem[n], self.cnt[n]) for n in self.sem if self.cnt[n] > 0]
        toks += [("d_" + k, self.dsem[k], self.dcnt[k]) for k in self.dsem if self.dcnt[k] > 0]
        toks += list(self.extra_tokens)
        for eng in self.engs:
            self._wait(eng, toks)

    def _wait(self, eng, deps):
        best = {}
        for (sn, sem, val) in deps:
            if best.get(sn, (None, 0))[1] < val:
                best[sn] = (sem, val)
        for sn, (sem, val) in best.items():
            if self.seen[eng].get(sn, 0) < val:
                self.engs[eng].wait_ge(sem, val)
                self.seen[eng][sn] = val

    def _record(self, tok, r, w, wa=()):
        for k in w:
            self.last_w[k] = [tok]
            self.readers[k] = []
        for k in wa:
            self.last_w.setdefault(k, []).append(tok)
        for k in r:
            if k not in w:
                self.readers.setdefault(k, []).append(tok)

    def op(self, eng, fn, r=(), w=()):
        self._wait(eng, self._deps(r, w))
        ins = fn(self.engs[eng])
        self.cnt[eng] += 1
        ins.then_inc(self.sem[eng], 1)
        tok = ("s_" + eng, self.sem[eng], self.cnt[eng])
        self._record(tok, r, w)
        return tok

    def mm(self, fn, r=(), w=(), last=True):
        self._wait("pe", self._deps(r, w))
        ins = fn(self.engs["pe"])
        if last:
            self.cnt["pe"] += 1
            ins.then_inc(self.sem["pe"], 1)
            tok = ("s_pe", self.sem["pe"], self.cnt["pe"])
            pr, pw = getattr(self, "_pend", ([], []))
            self._record(tok, list(r) + pr, list(w) + pw)
            self._pend = ([], [])
        else:
            pr, pw = getattr(self, "_pend", ([], []))
            self._pend = (pr + list(r), pw + list(w))

    def dma(self, q, key, fn, r=(), w=(), wa=(), is_out=False):
        if key not in self.dsem:
            self.dsem[key] = self.es.enter_context(self.nc.semaphore("d_" + key))
            self.dcnt[key] = 0
        self._wait(q, self._deps(r, w))
        ins = fn(self.engs[q])
        self.dcnt[key] += 16
        ins.then_inc(self.dsem[key], 16)
        tok = ("d_" + key, self.dsem[key], self.dcnt[key])
        self._record(tok, r, w, wa)
        if is_out:
            self.out_tokens.append(tok)
        return tok

    def finish(self):
        best = {}
        for (sn, sem, val) in self.out_tokens:
            if best.get(sn, (None, 0))[1] < val:
                best[sn] = (sem, val)
        for sn, (sem, val) in best.items():
            self.engs["sp"].wait_ge(sem, val)


def build(cfg):
    S, PAST, NPOOL, E, FF, CAP, D = (cfg[k] for k in ("S", "PAST", "NPOOL", "E", "FF", "CAP", "D"))
    KC = D // 128
    FC = FF // 128
    NT = S // 128
    T = S + 128
    NTT = NT + 1
    PAGES = PAST // 128
    assert PAGES <= 128
    NIN = 5128
    NQB = S // 512 if S >= 512 else 1
    QW = 512 if S >= 512 else S
    CT = CAP // 128
    RB = 32
    NRB = 128 // RB

    nc = bass.Bass("TRN2", target_bir_lowering=False)

    def din(name, shape, dt=F32):
        return nc.dram_tensor(name, list(shape), dt, kind="ExternalInput")

    def dout(name, shape, dt=F32):
        return nc.dram_tensor(name, list(shape), dt, kind="ExternalOutput")

    def dscr(name, shape, dt=F32):
        return nc.dram_tensor(name, list(shape), dt)

    xT_d = din("xT", [128, KC, T])
    xtok_d = din("xtok", [T, D])
    xgT_d = din("xgT", [128, 4, KC, NSG])
    win_d = din("win", [128, KC, NIN])
    whs_d = din("whs", [128, 4, KC, 384])
    ck_d = [[din("ck%d_%d" % (h, i), [NPOOL, RB * 128]) for i in range(NRB)] for h in range(HA)]
    cv_d = [[din("cv%d_%d" % (h, i), [NPOOL, RB * 128]) for i in range(NRB)] for h in range(HA)]
    pt_d = din("pt", [128, NSG], I32)
    sel_d = din("selm", [128, 16])
    stc_d = din("stc", [64, NSO * HM, 129])
    stm_d = din("stm", [NSO, HM])
    big_d = din("bigate", [8, 1])
    lamp_d = din("lamp", [4, 64])
    subg_d = din("subg", [128, 1])
    mhg_d = din("mhg", [512])
    wba_d = din("wba", [128, 4, D])
    wbm_d = din("wbm", [128, 4, D])
    wo_d = din("wo", [128, KC, D])
    ln_d = din("lnp", [4, D])
    wr_d = din("wr", [128, KC, E])
    br_d = din("br", [E])
    wg_d = din("wg", [E, 128, KC, FF])
    wu_d = din("wu", [E, 128, KC, FF])
    wd_d = din("wd", [E, 128, FC, D])
    bgu_d = din("bgu", [128, 2, E, FC])
    bd_d = din("bd", [E, D])
    cst_d = din("cst", [128, 1024])
    cm_d = din("cm", [128, 4, 512])
    rope_d = din("rope", [128, NTT, 16])
    ropes_d = din("ropes", [128, 16])
    ecap_d = din("ecap", [128, E])
    i4_d = din("i4", [128, 4, 4])

    y_o = dout("y", [S, D])
    ys_o = dout("ysamp", [NSO, D])
    k_o = dout("k", [S, 512])
    v_o = dout("v", [S, 512])
    cp_o = dout("cp", [HM, 64, 128])
    np_o = dout("npr", [HM, 64])
    mp_o = dout("mp", [HM, 1])
    ks_o = dout("ksamp", [NSO, 512])
    vs_o = dout("vsamp", [NSO, 512])
    cs_o = dout("csamp", [NSO * HM, 64, 128])
    ns_o = dout("nsamp", [NSO * HM, 64])
    ms_o = dout("msamp", [NSO, HM])

    qT_s = dscr("qT_s", [HA, 128, T])
    kT_s = dscr("kT_s", [HA, 128, T])
    v_s = dscr("v_s", [T, 512])
    qmT_s = dscr("qmT_s", [HM, 64, T])
    kmT_s = dscr("kmT_s", [HM, 64, T])
    km_s = dscr("km_s", [T, 256])
    qms_s = dscr("qms_s", [128, 256])
    vm_s = dscr("vm_s", [T, 512])
    ig_s = dscr("ig_s", [4, T])
    lf_s = dscr("lf_s", [4, T])
    sog_s = dscr("sog_s", [T, 512])
    sga_s = dscr("sga_s", [KC, 128, T])
    sgm_s = dscr("sgm_s", [KC, 128, T])
    ainT_s = dscr("ainT_s", [HA, 128, T])
    minT_s = dscr("minT_s", [4, 128, T])
    M_s = dscr("M_s", [4, S])
    qsb_s = dscr("qsb_s", [NSG, 128])
    agi_s = dscr("agi_s", [NSG, 128])
    ago_s = dscr("ago_s", [8 * NSG, 128])
    hs_s = dscr("hs_s", [T, D])
    xg_s = dscr("xg_s", [E * CAP, D])
    yx_s = dscr("yx_s", [E * CAP, D])

    es = ExitStack()
    sy = Sync(nc, es)

    def sb(name, shape, dt=F32, st=None):
        return (st or es).enter_context(nc.sbuf_tensor("t_" + name, list(shape), dt))

    def ps(name, shape, dt=F32, st=None):
        return (st or es).enter_context(nc.psum_tensor("p_" + name, list(shape), dt))

    cst = sb("cst", [128, 1024])
    ident = cst[:, 0:128]
    ones = cst[:, 128:256]
    ltri = cst[:, 256:384]
    maskD = cst[:, 384:512]
    rowm = cst[:, 512:513]
    zeros = cst[:, 640:1024]
    cm = sb("cm", [128, 4, 512])
    rope = sb("rope", [128, NTT, 16])
    ropes = sb("ropes", [128, 16])
    ecap = sb("ecap", [128, E])
    i4 = sb("i4", [128, 4, 4])
    lnp = sb("lnp", [128, 4, D])
    brb = sb("brb", [128, E])
    mhg = sb("mhg", [128, 512])
    subg = sb("subg", [128, 1])
    bigt = sb("bigt", [8, 1])
    lamp = sb("lamp", [128, 4, 64])
    bgu = sb("bgu", [128, 2, E, FC])
    stm = sb("stm", [NSO, HM])
    selm = sb("selm", [128, 16])
    ptt = sb("ptt", [128, NSG], I32)
    ainS = sb("ainS", [128, 16])
    wts = sb("wts", [128, NTT, 4])
    sli = sb("sli", [128, NTT, 4], I32)

    CK = ["cst", "cm", "rope", "ropes", "ecap", "i4", "lnp", "brb", "mhg", "subg", "bigt", "lamp", "bgu",
          "stm", "selm", "ptt"]
    loads = [
        (r_(cst[:]), r_(cst_d.ap())), (cm[:], cm_d.ap()), (rope[:], rope_d.ap()), (ropes[:], ropes_d.ap()),
        (ecap[:], ecap_d.ap()), (i4[:], i4_d.ap()),
        (lnp[:].rearrange("p a d -> p (a d)"), ln_d.ap().rearrange("a d -> (a d)").partition_broadcast(128)),
        (brb[:], br_d.ap().partition_broadcast(128)), (mhg[:], mhg_d.ap().partition_broadcast(128)),
        (subg[:], subg_d.ap()), (bigt[:], big_d.ap()),
        (lamp[:].rearrange("p a d -> p (a d)"), lamp_d.ap().rearrange("a d -> (a d)").partition_broadcast(128)),
        (bgu[:], bgu_d.ap()), (stm[:], stm_d.ap()), (selm[:], sel_d.ap()), (ptt[:], pt_d.ap()),
    ]
    for li, (o, i) in enumerate(loads):
        bc = li in (0, 6, 7, 8, 11)
        sy.dma("pool" if bc else "sp", "constb" if bc else "const", lambda e, o=o, i=i: e.dma_start(out=o, in_=i), w=[])
    ctok = [("d_const", sy.dsem["const"], sy.dcnt["const"]), ("d_constb", sy.dsem["constb"], sy.dcnt["constb"])]
    for k in CK:
        sy.last_w[k] = list(ctok)

    lam = sb("lam", [128, 4])
    ltmp = sb("ltmp", [128, 2, 64])
    sy.op("dve", lambda e: e.tensor_tensor(out=ltmp[:, 0, :], in0=lamp[:, 0, :], in1=lamp[:, 1, :], op=ALU.mult),
          r=["lamp"], w=["ltmp"])
    sy.op("dve", lambda e: e.tensor_tensor(out=ltmp[:, 1, :], in0=lamp[:, 2, :], in1=lamp[:, 3, :], op=ALU.mult),
          r=["lamp"], w=["ltmp"])
    sy.op("dve", lambda e: e.tensor_reduce(out=lam[:, 2:4], in_=ltmp[:], axis=AX.X, op=ALU.add),
          r=["ltmp"], w=["lam"])
    sy.op("act", lambda e: e.activation(out=lam[:, 2:4], in_=lam[:, 2:4], func=AF.Exp), r=["lam"], w=["lam"])
    sy.op("dve", lambda e: e.tensor_tensor(out=lam[:, 0:1], in0=lam[:, 2:3], in1=lam[:, 3:4], op=ALU.subtract),
          r=["lam"], w=["lam"])
    sy.op("dve", lambda e: e.tensor_scalar(out=lam[:, 0:1], in0=lam[:, 0:1], scalar1=LAM_INIT, scalar2=None,
                                           op0=ALU.add), r=["lam"], w=["lam"])
    sy.op("dve", lambda e: e.tensor_scalar(out=lam[:, 1:2], in0=lam[:, 0:1], scalar1=-1.0, scalar2=None,
                                           op0=ALU.mult), r=["lam"], w=["lam"])
    gsc = sb("gsc", [128, 1])
    sy.op("dve", lambda e: e.tensor_scalar(out=gsc[:], in0=subg[:], scalar1=(1.0 - LAM_INIT), scalar2=None,
                                           op0=ALU.mult), r=["subg"], w=["gsc"])
    bu1 = sb("bu1", [128, E, FC])
    sy.op("dve", lambda e: e.tensor_scalar(out=bu1[:], in0=bgu[:, 1, :, :], scalar1=1.0, scalar2=None, op0=ALU.add),
          r=["bgu"], w=["bu1"])
    bsc = sb("bsc", [8, 1])
    sy.op("dve", lambda e: e.tensor_scalar(out=bsc[:], in0=bigt[:], scalar1=1.0 / SOFTCAP, scalar2=None,
                                           op0=ALU.mult), r=["bigt"], w=["bsc"])

    zt = sb("zt", [128, D])
    sy.op("pool", lambda e: e.memset(zt[:], 0.0), w=["zt"])
    nz = (E * CAP) // 128
    for z0 in range(0, nz, 32):
        z1 = min(nz, z0 + 32)
        sy.dma("pool", "zero", lambda e, z0=z0, z1=z1: e.dma_start(
            out=xg_s.ap()[z0 * 128:z1 * 128, :].rearrange("(a p) d -> p a d", p=128),
            in_=zt[:].unsqueeze(1).to_broadcast([128, z1 - z0, D])), r=["zt"], wa=["xg_zero"])

    for h in range(HA):
        sy.dma("pool", "zero", lambda e, h=h: e.dma_start(out=ainT_s.ap()[h, :, S + NSO:T], in_=zt[:, 0:128 - NSO]),
               r=["zt"], wa=["ainT_s"])

    st = ExitStack()
    xgT = sb("xgT", [128, 4, KC, NSG], st=st)
    whs = sb("whs", [128, 4, KC, 384], st=st)
    sy.dma("sp", "ld0", lambda e: e.dma_start(out=xgT[:], in_=xgT_d.ap()), w=["xgT"])
    sy.dma("sp", "ld1", lambda e: e.dma_start(out=whs[:], in_=whs_d.ap()), w=["whs"])
    zs_ps = ps("zs_ps", [NSG, 384], st=st)
    for h in range(4):
        for kc in range(KC):
            sy.mm(lambda e, kc=kc, h=h: e.matmul(zs_ps[:], lhsT=xgT[:, h, kc, :], rhs=whs[:, h, kc, :],
                                                 start=(h == 0 and kc == 0), stop=(h == 3 and kc == KC - 1)),
                  r=["xgT", "whs"], w=["zs_ps"], last=(h == 3 and kc == KC - 1))
    zs = sb("zs", [NSG, 384], st=st)
    zv = zs_ps[:].rearrange("p (g d) -> p g d", d=64)
    zo = zs[:].rearrange("p (g d) -> p g d", d=64)
    cosb = ropes[0:NSG, 0:8].unsqueeze(1).to_broadcast([NSG, 4, 8])
    sinb = ropes[0:NSG, 8:16].unsqueeze(1).to_broadcast([NSG, 4, 8])
    rt = sb("rt", [NSG, 4, 4, 8], st=st)

    def rope_ops(src, dst, cosv, sinv, tmp, n, rk, wk_, tk):
        sy.op("dve", lambda e: e.tensor_tensor(out=tmp[:, 0], in0=src[:, :, 0:8], in1=cosv, op=ALU.mult),
              r=[rk], w=[tk])
        sy.op("dve", lambda e: e.tensor_tensor(out=tmp[:, 1], in0=src[:, :, 8:16], in1=sinv, op=ALU.mult),
              r=[rk], w=[tk])
        sy.op("dve", lambda e: e.tensor_tensor(out=tmp[:, 2], in0=src[:, :, 8:16], in1=cosv, op=ALU.mult),
              r=[rk], w=[tk])
        sy.op("dve", lambda e: e.tensor_tensor(out=tmp[:, 3], in0=src[:, :, 0:8], in1=sinv, op=ALU.mult),
              r=[rk], w=[tk])
        sy.op("dve", lambda e: e.tensor_tensor(out=dst[:, :, 0:8], in0=tmp[:, 0], in1=tmp[:, 1], op=ALU.subtract),
              r=[tk], w=[wk_])
        sy.op("dve", lambda e: e.tensor_tensor(out=dst[:, :, 8:16], in0=tmp[:, 2], in1=tmp[:, 3], op=ALU.add),
              r=[tk], w=[wk_])
        sy.op("act", lambda e: e.copy(out=dst[:, :, 16:64], in_=src[:, :, 16:64]), r=[rk], w=[wk_])

    rope_ops(zv[:, 0:4, :], zo[:, 0:4, :], cosb, sinb, rt[:], NSG, "zs_ps", "zs", "rt")
    sy.op("act", lambda e: e.copy(out=zs[:, 256:384], in_=zs_ps[:, 256:384]), r=["zs_ps"], w=["zs"])
    sy.dma("sp", "ld0", lambda e: e.dma_start(out=qsb_s.ap(), in_=zs[:, 0:128]), r=["zs"], w=["qsb_s"])
    qb = sb("qb", [128, NSG, 128], st=st)
    sy.dma("pool", "ldb", lambda e: e.dma_start(
        out=qb[:].rearrange("p j d -> p (j d)"),
        in_=qsb_s.ap().rearrange("j d -> (j d)").partition_broadcast(128)), r=["qsb_s"], w=["qb"])
    pn = sb("pn", [NSG, 2], st=st)
    pnt = sb("pnt", [NSG, 128], st=st)
    sy.op("dve", lambda e: e.tensor_tensor(out=pnt[:], in0=zs[:, 0:128], in1=zs[:, 128:256], op=ALU.mult),
          r=["zs"], w=["pnt"])
    sy.op("dve", lambda e: e.tensor_reduce(out=pn[:], in_=pnt[:].rearrange("p (c d) -> p c d", d=64), axis=AX.X,
                                           op=ALU.add), r=["pnt"], w=["pn"])
    sy.op("act", lambda e: e.activation(out=pn[:], in_=pn[:], func=AF.Exp, scale=DH ** -0.5), r=["pn"], w=["pn"])
    pnd = sb("pnd", [NSG, NSG, 2], st=st)
    sy.op("dve", lambda e: e.tensor_tensor(out=pnd[:], in0=ident[0:NSG, 0:NSG].unsqueeze(2).to_broadcast([NSG, NSG, 2]),
                                           in1=pn[:].unsqueeze(1).to_broadcast([NSG, NSG, 2]), op=ALU.mult),
          r=["pn", "cst"], w=["pnd"])

    kb = [sb("kb%d" % i, [128, RB, 128], st=st) for i in range(2)]
    vb = [sb("vb%d" % i, [128, RB, 128], st=st) for i in range(2)]
    ktmp = sb("ktmp", [128, RB, 128], st=st)
    vbb = [sb("vbb%d" % i, [128, RB, 128], BF16, st=st) for i in range(2)]
    scb = sb("scb", [128, 128, 2], BF16, st=st)
    sc_s = sb("sc_s", [128, 128, 2], st=st)
    prs = sb("prs", [128, 2], st=st)
    pv_ps = ps("pv_ps", [128, 2], st=st)
    dn_ps = ps("dn_ps", [128, 2], st=st)
    oS = sb("oS", [128, NSG], st=st)
    osm = sb("osm", [128, 8], st=st)
    blk = 0
    for j in range(NSG):
        for rb in range(NRB):
            b = blk % 2
            blk += 1
            sy.dma("pool", "kb%d" % b, lambda e, b=b, rb=rb, j=j: e.indirect_dma_start(
                out=kb[b][:].rearrange("p r d -> p (r d)"), out_offset=None, in_=ck_d[j // 4][rb].ap(),
                in_offset=bass.IndirectOffsetOnAxis(ap=ptt[:, j:j + 1], axis=0)), r=["ptt"], w=["kb%d" % b])
            sy.dma("pool", "vb%d" % b, lambda e, b=b, rb=rb, j=j: e.indirect_dma_start(
                out=vb[b][:].rearrange("p r d -> p (r d)"), out_offset=None, in_=cv_d[j // 4][rb].ap(),
                in_offset=bass.IndirectOffsetOnAxis(ap=ptt[:, j:j + 1], axis=0)), r=["ptt"], w=["vb%d" % b])
            sy.op("act", lambda e, b=b: e.copy(out=vbb[b][:], in_=vb[b][:]), r=["vb%d" % b], w=["vbb%d" % b])
            sy.op("dve", lambda e, b=b, j=j: e.tensor_tensor(
                out=ktmp[:], in0=kb[b][:], in1=qb[:, j, :].unsqueeze(1).to_broadcast([128, RB, 128]), op=ALU.mult),
                r=["kb%d" % b, "qb"], w=["ktmp"])
            sy.op("dve", lambda e, rb=rb: e.tensor_reduce(
                out=sc_s[:, rb * RB:(rb + 1) * RB, :], in_=ktmp[:].rearrange("p r (c d) -> p r c d", d=64),
                axis=AX.X, op=ALU.add), r=["ktmp"], w=["sc_s%d" % rb])
            sy.op("act", lambda e, rb=rb: e.activation(
                out=sc_s[:, rb * RB:(rb + 1) * RB, :], in_=sc_s[:, rb * RB:(rb + 1) * RB, :], func=AF.Exp,
                scale=DH ** -0.5), r=["sc_s%d" % rb], w=["sc_s%d" % rb])
            if PAGES < 128:
                sy.op("dve", lambda e, rb=rb: e.tensor_scalar(
                    out=sc_s[:, rb * RB:(rb + 1) * RB, :], in0=sc_s[:, rb * RB:(rb + 1) * RB, :],
                    scalar1=cst[:, 513:514], scalar2=None, op0=ALU.mult), r=["sc_s%d" % rb, "cst"], w=["sc_s%d" % rb])
            sy.op("act", lambda e, rb=rb: e.copy(out=scb[:, rb * RB:(rb + 1) * RB, :],
                                                 in_=sc_s[:, rb * RB:(rb + 1) * RB, :]),
                  r=["sc_s%d" % rb], w=["scb%d" % rb])
            for rr in range(RB):
                row = rb * RB + rr
                sy.mm(lambda e, b=b, rr=rr, row=row: e.matmul(
                    pv_ps[:], lhsT=vbb[b][:, rr, :], rhs=scb[:, row, :], start=(row == 0), stop=False),
                    r=["vbb%d" % b, "scb%d" % rb], w=["pv_ps"], last=(rr == RB - 1))
        sy.mm(lambda e, j=j: e.matmul(pv_ps[:], lhsT=zs[:, 256:384], rhs=pnd[:, j, :], start=False, stop=True),
              r=["zs", "pnd"], w=["pv_ps"])
        sy.op("dve", lambda e: e.tensor_reduce(out=prs[:], in_=sc_s[:].rearrange("p r c -> p c r"), axis=AX.X,
                                               op=ALU.add), r=["sc_s%d" % i for i in range(NRB)], w=["prs"])
        sy.mm(lambda e: e.matmul(dn_ps[:], lhsT=ones, rhs=prs[:], start=True, stop=False),
              r=["cst", "prs"], w=["dn_ps"], last=False)
        sy.mm(lambda e, j=j: e.matmul(dn_ps[:], lhsT=ones[0:NSG, :], rhs=pnd[:, j, :], start=False, stop=True),
              r=["cst", "pnd"], w=["dn_ps"])
        sy.op("dve", lambda e: e.reciprocal(out=osm[:, 0:2], in_=dn_ps[:]), r=["dn_ps"], w=["osm"])
        sy.op("dve", lambda e: e.tensor_tensor(out=osm[:, 2:4], in0=pv_ps[:], in1=osm[:, 0:2], op=ALU.mult),
              r=["pv_ps", "osm"], w=["osm"])
        sy.op("dve", lambda e, j=j: e.scalar_tensor_tensor(
            out=oS[:, j:j + 1], in0=osm[:, 3:4], scalar=lam[:, 1:2], in1=osm[:, 2:3], op0=ALU.mult, op1=ALU.add),
            r=["osm", "lam"], w=["oS"])
    osq = sb("osq", [128, NSG], st=st)
    ss_ps = ps("ss_ps", [128, NSG], st=st)
    sy.op("act", lambda e: e.activation(out=osq[:], in_=oS[:], func=AF.Square), r=["oS"], w=["osq"])
    sy.mm(lambda e: e.matmul(ss_ps[:], lhsT=ones, rhs=osq[:], start=True, stop=True), r=["cst", "osq"], w=["ss_ps"])
    sy.op("act", lambda e: e.activation(out=osq[:], in_=ss_ps[:], func=AF.Ln, bias=EPS, scale=1.0 / 128),
          r=["ss_ps"], w=["osq"])
    sy.op("act", lambda e: e.activation(out=osq[:], in_=osq[:], func=AF.Exp, scale=-0.5), r=["osq"], w=["osq"])
    sy.op("dve", lambda e: e.scalar_tensor_tensor(out=oS[:], in0=oS[:], scalar=gsc[:, 0:1], in1=osq[:],
                                                  op0=ALU.mult, op1=ALU.mult), r=["oS", "gsc", "osq"], w=["oS"])
    sy.op("act", lambda e: e.copy(out=ainS[:], in_=oS[:]), r=["oS"], w=["ainS"])
    st.close()
    if cfg.get('stop') == 'S':
        sy.finish()
        es.close()
        return nc
    for h in range(HA):
        sy.dma("sp", "st0", lambda e, h=h: e.dma_start(out=ainT_s.ap()[h, :, S:S + NSO], in_=ainS[:, h * 4:h * 4 + 4]),
               r=["ainS"], wa=["ainT_s"])

    sy.barrier()
    st = ExitStack()
    xT = sb("xT", [128, KC, T], st=st)
    sy.dma("pool", "ld0", lambda e: e.dma_start(out=r_(xT[:]), in_=(xT_d.ap())), w=["xT"])
    BW = 520
    wb = [sb("wb%d" % i, [128, KC, BW], st=st) for i in range(2)]
    blocks = [(0, 512), (512, 512), (1024, 512), (1536, 512), (2048, 512), (2560, 520), (3080, 512), (3592, 512),
              (4104, 512), (4616, 512)]
    zp = [ps("zp%d" % i, [128, 512], st=st) for i in range(4)]
    zsb = [sb("zsb%d" % i, [128, 512], st=st) for i in range(4)]
    rtt = sb("rtt", [128, 4, 8, 8], st=st)
    tp = [ps("tp%d" % i, [128, 4, 128], st=st) for i in range(2)]
    tsb = [sb("tsb%d" % i, [128, 4, 128], st=st) for i in range(2)]
    cnt = {"z": 0, "t": 0}
    tgroups = [(g * 512, min(512, S - g * 512)) for g in range((S + 511) // 512)] + [(S, 128)]

    def tok_major(bi, i, c0, n, post):
        zi = cnt["z"] % 4
        cnt["z"] += 1
        for kc in range(KC):
            sy.mm(lambda e, kc=kc: e.matmul(zp[zi][:, 0:n], lhsT=r_(xT[:, kc, i * 128:(i + 1) * 128]),
                                            rhs=r_(wb[bi % 2][:, kc, c0:c0 + n]), start=(kc == 0), stop=(kc == KC - 1)),
                  r=["xT", "wb%d" % (bi % 2)], w=["zp%d" % zi], last=(kc == KC - 1))
        post(zp[zi], zi)

    def feat_major(bi, c0, m, t0, n, post, cast=r_):
        zi = cnt["z"] % 4
        cnt["z"] += 1
        for kc in range(KC):
            sy.mm(lambda e, kc=kc: e.matmul(zp[zi][0:m, 0:n], lhsT=cast(wb[bi % 2][:, kc, c0:c0 + m]),
                                            rhs=cast(xT[:, kc, t0:t0 + n]), start=(kc == 0), stop=(kc == KC - 1)),
                  r=["xT", "wb%d" % (bi % 2)], w=["zp%d" % zi], last=(kc == KC - 1))
        post(zp[zi], zi)

    def transposes_out(src_tile, zi, dst, i):
        ti = cnt["t"] % 2
        cnt["t"] += 1
        for h in range(4):
            sy.mm(lambda e, h=h: e.transpose(out=tp[ti][:, h, :], in_=src_tile[:, h * 128:(h + 1) * 128],
                                             identity=ident), r=["zsb%d" % zi, "cst"], w=["tp%d" % ti], last=(h == 3))
        sy.op("act", lambda e: e.copy(out=tsb[ti][:], in_=tp[ti][:]), r=["tp%d" % ti], w=["tsb%d" % ti])
        sy.dma("sp", "st%d" % ti, lambda e: e.dma_start(
            out=dst.ap()[:, :, i * 128:(i + 1) * 128].rearrange("h p t -> p h t"), in_=tsb[ti][:]),
            r=["tsb%d" % ti], wa=[dst.name])

    for bi, (c_off, c_w) in enumerate(blocks):
        if bi >= cfg.get('nblk', 99):
            break
        sy.dma("pool", "wb%d" % (bi % 2), lambda e, bi=bi, c_off=c_off, c_w=c_w: e.dma_start(
            out=r_(wb[bi % 2][:, :, 0:c_w]), in_=(win_d.ap()[:, :, c_off:c_off + c_w])), w=["wb%d" % (bi % 2)])
        if bi in (0, 1):
            for i in range(NTT):
                def post(zpt, zi, i=i, bi=bi):
                    src = zpt[:].rearrange("p (g d) -> p g d", d=64)
                    dst = zsb[zi][:].rearrange("p (g d) -> p g d", d=64)
                    cosv = rope[:, i, 0:8].unsqueeze(1).to_broadcast([128, 8, 8])
                    sinv = rope[:, i, 8:16].unsqueeze(1).to_broadcast([128, 8, 8])
                    rope_ops(src, dst, cosv, sinv, rtt[:], 128, "zp%d" % zi, "zsb%d" % zi, "rtt")
                    if bi == 1:
                        if i < NT:
                            sy.dma("sp", "zo%d" % zi, lambda e: e.dma_start(out=k_o.ap()[i * 128:(i + 1) * 128, :],
                                                                            in_=zsb[zi][:]), r=["zsb%d" % zi], is_out=True)
                        else:
                            sy.dma("sp", "zo%d" % zi, lambda e: e.dma_start(out=ks_o.ap(), in_=zsb[zi][0:NSO, :]),
                                   r=["zsb%d" % zi], is_out=True)
                    transposes_out(zsb[zi], zi, qT_s if bi == 0 else kT_s, i)
                tok_major(bi, i, 0, 512, post)
        elif bi in (2, 4):
            for i in range(NTT):
                def post(zpt, zi, i=i, bi=bi):
                    sy.op("act", lambda e: e.copy(out=zsb[zi][:], in_=zpt[:]), r=["zp%d" % zi], w=["zsb%d" % zi])
                    dst = v_s if bi == 2 else vm_s
                    sy.dma("sp", "zo%d" % zi, lambda e: e.dma_start(out=dst.ap()[i * 128:(i + 1) * 128, :], in_=zsb[zi][:]),
                           r=["zsb%d" % zi], wa=[dst.name])
                    if bi == 2:
                        if i < NT:
                            sy.dma("sp", "zq%d" % zi, lambda e: e.dma_start(out=v_o.ap()[i * 128:(i + 1) * 128, :],
                                                                            in_=zsb[zi][:]), r=["zsb%d" % zi], is_out=True)
                        else:
                            sy.dma("sp", "zq%d" % zi, lambda e: e.dma_start(out=vs_o.ap(), in_=zsb[zi][0:NSO, :]),
                                   r=["zsb%d" % zi], is_out=True)
                tok_major(bi, i, 0, 512, post)
        elif bi == 3:
            for (t0, n) in tgroups:
                for ch in range(4):
                    def post(zpt, zi, ch=ch, t0=t0, n=n):
                        if ch < 2:
                            sy.op("act", lambda e: e.copy(out=zsb[zi][:, 0:n], in_=zpt[:, 0:n]), r=["zp%d" % zi],
                                  w=["zsb%d" % zi])
                            dst = qmT_s
                        else:
                            sy.op("act", lambda e: e.mul(out=zsb[zi][:, 0:n], in_=zpt[:, 0:n], mul=DK ** -0.5),
                                  r=["zp%d" % zi], w=["zsb%d" % zi])
                            dst = kmT_s
                        hh = (ch % 2) * 2
                        sy.dma("sp", "zo%d" % zi, lambda e: e.dma_start(
                            out=dst.ap()[hh:hh + 2, :, t0:t0 + n].rearrange("h p t -> (h p) t"), in_=zsb[zi][:, 0:n]),
                            r=["zsb%d" % zi], wa=[dst.name])
                    feat_major(bi, ch * 128, 128, t0, n, post)
            for i in range(NTT):
                def post(zpt, zi, i=i):
                    sy.op("act", lambda e: e.mul(out=zsb[zi][:, 0:256], in_=zpt[:, 0:256], mul=DK ** -0.5),
                          r=["zp%d" % zi], w=["zsb%d" % zi])
                    sy.dma("sp", "zo%d" % zi, lambda e: e.dma_start(out=km_s.ap()[i * 128:(i + 1) * 128, :],
                                                                    in_=zsb[zi][:, 0:256]), r=["zsb%d" % zi], wa=["km_s"])
                tok_major(bi, i, 256, 256, post)

            def post(zpt, zi):
                sy.op("act", lambda e: e.copy(out=zsb[zi][:, 0:256], in_=zpt[:, 0:256]), r=["zp%d" % zi], w=["zsb%d" % zi])
                sy.dma("sp", "zo%d" % zi, lambda e: e.dma_start(out=qms_s.ap(), in_=zsb[zi][:, 0:256]),
                       r=["zsb%d" % zi], wa=["qms_s"])
            tok_major(bi, NT, 0, 256, post)
        elif bi == 5:
            for (t0, n) in tgroups:
                def post(zpt, zi, t0=t0, n=n):
                    a = zsb[zi]
                    sy.op("act", lambda e: e.activation(out=a[0:8, 0:n], in_=zpt[0:8, 0:n], func=AF.Tanh,
                                                        bias=bsc[:, 0:1], scale=1.0 / SOFTCAP),
                          r=["zp%d" % zi, "bsc"], w=["zsb%d" % zi])
                    sy.op("dve", lambda e: e.tensor_scalar(out=a[0:8, 0:n], in0=a[0:8, 0:n], scalar1=SOFTCAP,
                                                           scalar2=None, op0=ALU.mult), r=["zsb%d" % zi], w=["zsb%d" % zi])
                    sy.dma("sp", "zo%d" % zi, lambda e: e.dma_start(out=ig_s.ap()[:, t0:t0 + n], in_=a[0:4, 0:n]),
                           r=["zsb%d" % zi], wa=["ig_s"])
                    sy.op("act", lambda e: e.activation(out=a[0:8, 0:n], in_=a[0:8, 0:n], func=AF.Exp, scale=-1.0),
                          r=["zsb%d" % zi], w=["zsb%d" % zi])
                    sy.op("act", lambda e: e.activation(out=a[0:8, 0:n], in_=a[0:8, 0:n], func=AF.Ln, bias=1.0),
                          r=["zsb%d" % zi], w=["zsb%d" % zi])
                    sy.op("dve", lambda e: e.tensor_scalar(out=a[0:8, 0:n], in0=a[0:8, 0:n], scalar1=-1.0,
                                                           scalar2=None, op0=ALU.mult), r=["zsb%d" % zi], w=["zsb%d" % zi])
                    sy.dma("sp", "zq%d" % zi, lambda e: e.dma_start(out=lf_s.ap()[:, t0:t0 + n], in_=a[4:8, 0:n]),
                           r=["zsb%d" % zi], wa=["lf_s"])
                feat_major(bi, 0, 8, t0, n, post, cast=lambda a: a)
            for i in range(NTT):
                def post(zpt, zi, i=i):
                    sy.op("act", lambda e: e.activation(out=zsb[zi][:], in_=zpt[:], func=AF.Sigmoid),
                          r=["zp%d" % zi], w=["zsb%d" % zi])
                    sy.dma("sp", "zo%d" % zi, lambda e: e.dma_start(out=sog_s.ap()[i * 128:(i + 1) * 128, :],
                                                                    in_=zsb[zi][:]), r=["zsb%d" % zi], wa=["sog_s"])
                tok_major(bi, i, 8, 512, post)
        else:
            dst = sga_s if bi in (6, 7) else sgm_s
            base = ((bi - 6) % 2) * 4
            for (t0, n) in tgroups:
                for ch in range(4):
                    def post(zpt, zi, ch=ch, t0=t0, n=n, dst=dst, base=base):
                        sy.op("act", lambda e: e.activation(out=zsb[zi][:, 0:n], in_=zpt[:, 0:n], func=AF.Sigmoid),
                              r=["zp%d" % zi], w=["zsb%d" % zi])
                        sy.dma("sp", "zo%d" % zi, lambda e: e.dma_start(out=dst.ap()[base + ch, :, t0:t0 + n],
                                                                        in_=zsb[zi][:, 0:n]), r=["zsb%d" % zi], wa=[dst.name])
                    feat_major(bi, ch * 128, 128, t0, n, post)
    st.close()
    if cfg.get('stop') == '1':
        sy.finish()
        es.close()
        return nc

    sy.barrier()
    st = ExitStack()
    qTh = sb("qTh", [128, S], st=st)
    kTh = sb("kTh", [128, S], st=st)
    vh = sb("vh", [128, NT, 128], st=st)
    scp = [ps("scp%d" % i, [128, 512], st=st) for i in range(4)]
    dnp = [ps("dnp%d" % i, [128, 512], st=st) for i in range(2)]
    pvp = [ps("pvp%d" % i, [128, 512], st=st) for i in range(2)]
    ptl = [sb("ptl%d" % i, [128, 512], st=st) for i in range(4)]
    o_t = sb("o_t", [128, 512], st=st)
    t_t = sb("t_t", [128, 512], st=st)
    rd_t = sb("rd_t", [128, 512], st=st)
    sq_t = sb("sq_t", [128, 512], st=st)
    it = 0
    for h in range(HA):
        sy.dma("pool", "ld0", lambda e, h=h: e.dma_start(out=r_(qTh[:]), in_=(qT_s.ap()[h, :, 0:S])), r=["qT_s"], w=["qTh"])
        sy.dma("pool", "ld1", lambda e, h=h: e.dma_start(out=r_(kTh[:]), in_=(kT_s.ap()[h, :, 0:S])), r=["kT_s"], w=["kTh"])
        sy.dma("pool", "ld2", lambda e, h=h: e.dma_start(
            out=r_(vh[:]), in_=(v_s.ap()[0:S, h * 128:(h + 1) * 128].rearrange("(i p) d -> p i d", p=128))),
            r=["v_s"], w=["vh"])
        for qblk in range(NQB):
            q0 = qblk * QW
            nkt = (q0 + QW) // 128
            for kt in range(nkt):
                for c in range(2):
                    bi_ = it % 4
                    it += 1
                    sy.mm(lambda e, c=c, kt=kt, bi_=bi_: e.matmul(
                        scp[bi_][:, 0:QW], lhsT=r_(kTh[c * 64:(c + 1) * 64, kt * 128:(kt + 1) * 128]),
                        rhs=r_(qTh[c * 64:(c + 1) * 64, q0:q0 + QW]), start=True, stop=True),
                        r=["qTh", "kTh"], w=["scp%d" % bi_])
                    sy.op("act", lambda e, bi_=bi_: e.activation(out=r_(ptl[bi_][:, 0:QW]), in_=scp[bi_][:, 0:QW],
                                                                 func=AF.Exp, scale=DH ** -0.5),
                          r=["scp%d" % bi_], w=["ptl%d" % bi_])
                    off = kt - q0 // 128
                    if off >= 0:
                        sy.op("dve", lambda e, bi_=bi_, off=off: e.tensor_tensor(
                            out=r_(ptl[bi_][:, 0:QW]), in0=ptl[bi_][:, 0:QW], in1=cm[:, off, 0:QW], op=ALU.mult),
                            r=["ptl%d" % bi_, "cm"], w=["ptl%d" % bi_])
                    sy.mm(lambda e, c=c, kt=kt, bi_=bi_: e.matmul(
                        dnp[c][:, 0:QW], lhsT=r_(ones), rhs=r_(ptl[bi_][:, 0:QW]), start=(kt == 0), stop=(kt == nkt - 1)),
                        r=["cst", "ptl%d" % bi_], w=["dnp%d" % c], last=False)
                    sy.mm(lambda e, c=c, kt=kt, bi_=bi_: e.matmul(
                        pvp[c][:, 0:QW], lhsT=r_(vh[:, kt, :]), rhs=r_(ptl[bi_][:, 0:QW]), start=(kt == 0),
                        stop=(kt == nkt - 1)), r=["vh", "ptl%d" % bi_], w=["pvp%d" % c])
            W_ = QW
            sy.op("dve", lambda e: e.reciprocal(out=rd_t[:, 0:W_], in_=dnp[0][:, 0:W_]), r=["dnp0"], w=["rd_t"])
            sy.op("dve", lambda e: e.tensor_tensor(out=o_t[:, 0:W_], in0=pvp[0][:, 0:W_], in1=rd_t[:, 0:W_], op=ALU.mult),
                  r=["pvp0", "rd_t"], w=["o_t"])
            sy.op("dve", lambda e: e.reciprocal(out=rd_t[:, 0:W_], in_=dnp[1][:, 0:W_]), r=["dnp1"], w=["rd_t"])
            sy.op("dve", lambda e: e.tensor_tensor(out=t_t[:, 0:W_], in0=pvp[1][:, 0:W_], in1=rd_t[:, 0:W_], op=ALU.mult),
                  r=["pvp1", "rd_t"], w=["t_t"])
            sy.op("dve", lambda e: e.scalar_tensor_tensor(out=o_t[:, 0:W_], in0=t_t[:, 0:W_], scalar=lam[:, 1:2],
                                                          in1=o_t[:, 0:W_], op0=ALU.mult, op1=ALU.add),
                  r=["t_t", "o_t", "lam"], w=["o_t"])
            sy.op("act", lambda e: e.activation(out=r_(sq_t[:, 0:W_]), in_=o_t[:, 0:W_], func=AF.Square), r=["o_t"], w=["sq_t"])
            sy.mm(lambda e: e.matmul(dnp[0][:, 0:W_], lhsT=r_(ones), rhs=r_(sq_t[:, 0:W_]), start=True, stop=True),
                  r=["cst", "sq_t"], w=["dnp0"])
            sy.op("act", lambda e: e.activation(out=rd_t[:, 0:W_], in_=dnp[0][:, 0:W_], func=AF.Ln, bias=EPS,
                                                scale=1.0 / 128), r=["dnp0"], w=["rd_t"])
            sy.op("act", lambda e: e.activation(out=rd_t[:, 0:W_], in_=rd_t[:, 0:W_], func=AF.Exp, scale=-0.5),
                  r=["rd_t"], w=["rd_t"])
            sy.op("dve", lambda e: e.scalar_tensor_tensor(out=o_t[:, 0:W_], in0=o_t[:, 0:W_], scalar=gsc[:, 0:1],
                                                          in1=rd_t[:, 0:W_], op0=ALU.mult, op1=ALU.mult),
                  r=["o_t", "gsc", "rd_t"], w=["o_t"])
            sy.dma("sp", "st0", lambda e, h=h, q0=q0: e.dma_start(out=ainT_s.ap()[h, :, q0:q0 + W_], in_=o_t[:, 0:W_]),
                   r=["o_t"], wa=["ainT_s"])
    st.close()
    if cfg.get('stop') == '2':
        sy.finish()
        es.close()
        return nc

    sy.barrier()
    st = ExitStack()
    igt = sb("igt", [4, T], st=st)
    lft = sb("lft", [4, T], st=st)
    sy.dma("sp", "ld0", lambda e: e.dma_start(out=igt[:], in_=ig_s.ap()), r=["ig_s"], w=["igt"])
    sy.dma("sp", "ld1", lambda e: e.dma_start(out=lft[:], in_=lf_s.ap()), r=["lf_s"], w=["lft"])
    Bt = sb("Bt", [4, S], st=st)
    at = sb("at", [4, S], st=st)
    Mt = sb("Mt", [4, S], st=st)
    mt = sb("mtot", [4, S], st=st)
    o4 = sb("o4", [4, S], st=st)
    sy.op("pool", lambda e: e.memset(o4[:], 1.0), w=["o4"])
    sy.op("dve", lambda e: e.tensor_tensor_scan(out=Bt[:], data0=o4[:], data1=lft[:, 0:S],
                                                initial=0.0, op0=ALU.mult, op1=ALU.add), r=["lft", "o4"], w=["Bt"])
    sy.op("dve", lambda e: e.tensor_tensor(out=at[:], in0=igt[:, 0:S], in1=Bt[:], op=ALU.subtract),
          r=["igt", "Bt"], w=["at"])
    sy.op("dve", lambda e: e.tensor_tensor_scan(out=Mt[:], data0=at[:], data1=at[:], initial=0.0, op0=ALU.max,
                                                op1=ALU.max), r=["at"], w=["Mt"])
    sy.op("dve", lambda e: e.tensor_tensor(out=mt[:], in0=Bt[:], in1=Mt[:], op=ALU.add), r=["Bt", "Mt"], w=["mtot"])
    sy.dma("sp", "st0", lambda e: e.dma_start(out=mp_o.ap(), in_=mt[:, S - 1:S]), r=["mtot"], is_out=True)
    sy.dma("sp", "st1", lambda e: e.dma_start(out=M_s.ap(), in_=Mt[:]), r=["Mt"], w=["M_s"])
    MB = sb("MB", [128, 4, S], st=st)
    sy.dma("pool", "ldb", lambda e: e.dma_start(out=MB[:].rearrange("p h t -> p (h t)"),
                                              in_=M_s.ap().rearrange("h t -> (h t)").partition_broadcast(128)),
           r=["M_s"], w=["MB"])
    acol = sb("acol", [128, NT, 4], st=st)
    ecol = sb("ecol", [128, NT, 4], st=st)
    colp = ps("colp", [128, 2, NT * 4], st=st)
    for i in range(NT):
        sy.mm(lambda e, i=i: e.transpose(out=colp[:, 0, i * 4:(i + 1) * 4], in_=at[0:4, i * 128:(i + 1) * 128],
                                         identity=ident[0:4, 0:4]), r=["at", "cst"], w=["colp"], last=False)
        sy.mm(lambda e, i=i: e.transpose(out=colp[:, 1, i * 4:(i + 1) * 4], in_=mt[0:4, i * 128:(i + 1) * 128],
                                         identity=ident[0:4, 0:4]), r=["mtot", "cst"], w=["colp"], last=(i == NT - 1))
    sy.op("act", lambda e: e.copy(out=acol[:].rearrange("p i h -> p (i h)"), in_=colp[:, 0, :]), r=["colp"], w=["acol"])
    sy.op("act", lambda e: e.activation(out=ecol[:].rearrange("p i h -> p (i h)"), in_=colp[:, 1, :], func=AF.Exp,
                                        scale=-1.0), r=["colp"], w=["ecol"])
    qmc = [sb("qmc%d" % i, [64, 4, 128], st=st) for i in range(2)]
    kmc = [sb("kmc%d" % i, [64, 4, 128], st=st) for i in range(2)]
    kmt = [sb("kmt%d" % i, [128, 256], st=st) for i in range(2)]
    vm1 = [sb("vm1%d" % i, [128, 4, 129], st=st) for i in range(2)]
    for b in range(2):
        sy.op("pool", lambda e, b=b: e.memset(vm1[b][:], 1.0), w=["vm1%d" % b])

    def load_chunk(i):
        b = i % 2
        t0 = i * 128
        sy.dma("sp", "lq%d" % b, lambda e: e.dma_start(out=qmc[b][:], in_=qmT_s.ap()[:, :, t0:t0 + 128].rearrange("h p t -> p h t")),
               r=["qmT_s"], w=["qmc%d" % b])
        sy.dma("sp", "lk%d" % b, lambda e: e.dma_start(out=kmc[b][:], in_=kmT_s.ap()[:, :, t0:t0 + 128].rearrange("h p t -> p h t")),
               r=["kmT_s"], w=["kmc%d" % b])
        sy.dma("sp", "lt%d" % b, lambda e: e.dma_start(out=kmt[b][:], in_=km_s.ap()[t0:t0 + 128, :]), r=["km_s"], w=["kmt%d" % b])
        sy.dma("sp", "lv%d" % b, lambda e: e.dma_start(out=vm1[b][:, :, 0:128],
                                                       in_=vm_s.ap()[t0:t0 + 128, :].rearrange("p (h d) -> p h d", d=128)),
               r=["vm_s"], w=["vm1%d" % b])

    Cst = sb("Cst", [64, 4, 129], st=st)
    WtL = [sb("Wt%d" % i, [128, 128], st=st) for i in range(2)]
    WItL = [sb("WIt%d" % i, [128, 128], st=st) for i in range(2)]
    SttL = [sb("Stt%d" % i, [128, 128], st=st) for i in range(2)]
    qstL = [sb("qst%d" % i, [64, 128], st=st) for i in range(2)]
    kwtL = [sb("kwt%d" % i, [128, 64], st=st) for i in range(2)]
    hm = sb("hm", [128, 512], st=st)
    sogt = sb("sogt", [128, 512], st=st)
    smalL = [sb("smal%d" % i, [128, 16], st=st) for i in range(2)]
    st6L = [sb("st6%d" % i, [128, 6], st=st) for i in range(2)]
    scmL = [ps("scm%d" % i, [128, 128], st=st) for i in range(2)]
    ndpL = [ps("ndp%d" % i, [128, 129], st=st) for i in range(2)]
    uppL = [ps("upp%d" % i, [64, 129], st=st) for i in range(2)]
    PB = {"p": 0}
    mtp = ps("mtp", [128, 4, 128], st=st)
    mts = sb("mts", [128, 4, 128], st=st)

    def head_norm(src, npart, h, keys_r, emt, emt_keys):
        P = npart
        smal, st6 = smalL[PB["p"]], st6L[PB["p"]]
        SM, S6 = "smal%d" % PB["p"], "st6%d" % PB["p"]
        sy.op("dve", lambda e: e.tensor_scalar(out=smal[0:P, 9:10], in0=src[0:P, 128:129], scalar1=-1.0, scalar2=None,
                                               op0=ALU.mult), r=keys_r, w=[SM])
        sy.op("dve", lambda e: e.tensor_tensor(out=smal[0:P, 10:11], in0=src[0:P, 128:129], in1=smal[0:P, 9:10],
                                               op=ALU.max), r=keys_r + [SM], w=[SM])
        sy.op("dve", lambda e: e.tensor_scalar(out=smal[0:P, 0:1], in0=smal[0:P, 10:11], scalar1=emt, scalar2=None,
                                               op0=ALU.max), r=[SM] + emt_keys, w=[SM])
        sy.op("dve", lambda e: e.reciprocal(out=smal[0:P, 1:2], in_=smal[0:P, 0:1]), r=[SM], w=[SM])
        sy.op("dve", lambda e: e.bn_stats(out=st6[0:P, :], in_=src[0:P, 0:128]), r=keys_r, w=[S6])
        sy.op("dve", lambda e: e.bn_aggr(out=smal[0:P, 2:4], in_=st6[0:P, :]), r=[S6], w=[SM])
        sy.op("dve", lambda e: e.tensor_tensor(out=smal[0:P, 4:5], in0=smal[0:P, 1:2], in1=smal[0:P, 1:2], op=ALU.mult),
              r=[SM], w=[SM])
        sy.op("dve", lambda e: e.tensor_tensor(out=smal[0:P, 6:7], in0=smal[0:P, 4:5], in1=smal[0:P, 3:4], op=ALU.mult),
              r=[SM], w=[SM])
        sy.op("act", lambda e: e.activation(out=smal[0:P, 7:8], in_=smal[0:P, 6:7], func=AF.Ln, bias=EPS),
              r=[SM], w=[SM])
        sy.op("act", lambda e: e.activation(out=smal[0:P, 7:8], in_=smal[0:P, 7:8], func=AF.Exp, scale=-0.5),
              r=[SM], w=[SM])
        sy.op("dve", lambda e: e.tensor_tensor(out=smal[0:P, 5:6], in0=smal[0:P, 7:8], in1=smal[0:P, 1:2], op=ALU.mult),
              r=[SM], w=[SM])
        sy.op("dve", lambda e: e.tensor_scalar(out=hm[0:P, h * 128:(h + 1) * 128], in0=src[0:P, 0:128],
                                               scalar1=smal[0:P, 2:3], scalar2=smal[0:P, 5:6], op0=ALU.subtract,
                                               op1=ALU.mult), r=keys_r + [SM], w=["hm"])

    def gate_and_store(i):
        sy.dma("sp", "ld4", lambda e: e.dma_start(out=sogt[:], in_=sog_s.ap()[i * 128:(i + 1) * 128, :]),
               r=["sog_s"], w=["sogt"])
        sy.op("pool", lambda e: e.tensor_tensor(out=sogt[:], in0=sogt[:], in1=mhg[:], op=ALU.mult),
              r=["sogt", "mhg"], w=["sogt"])
        sy.op("dve", lambda e: e.tensor_tensor(out=hm[:], in0=hm[:], in1=sogt[:], op=ALU.mult), r=["hm", "sogt"], w=["hm"])
        for h in range(4):
            sy.mm(lambda e, h=h: e.transpose(out=mtp[:, h, :], in_=hm[:, h * 128:(h + 1) * 128], identity=ident),
                  r=["hm", "cst"], w=["mtp"], last=(h == 3))
        sy.op("act", lambda e: e.copy(out=mts[:], in_=mtp[:]), r=["mtp"], w=["mts"])
        sy.dma("sp", "st2", lambda e: e.dma_start(out=minT_s.ap()[:, :, i * 128:(i + 1) * 128].rearrange("h p t -> p h t"),
                                                  in_=mts[:]), r=["mts"], wa=["minT_s"])

    load_chunk(0)
    for i in range(NT):
        t0 = i * 128
        b = i % 2
        load_chunk(i + 1)
        qk_, kk_, tk_, vk_ = "qmc%d" % b, "kmc%d" % b, "kmt%d" % b, "vm1%d" % b
        for h in range(4):
            p_ = h % 2
            PB["p"] = p_
            Wt, WIt, Stt, qst, kwt = WtL[p_], WItL[p_], SttL[p_], qstL[p_], kwtL[p_]
            scm, ndp, upp = scmL[p_], ndpL[p_], uppL[p_]
            sy.op("act", lambda e, h=h: e.activation(out=Wt[:], in_=MB[:, h, t0:t0 + 128], func=AF.Exp,
                                                     bias=acol[:, i, h:h + 1], scale=-1.0), r=["MB", "acol"], w=["Wt%d" % p_])
            sy.op("pool", lambda e: e.tensor_tensor(out=Wt[:], in0=Wt[:], in1=maskD, op=ALU.mult), r=["Wt%d" % p_, "cst"], w=["Wt%d" % p_])
            if i > 0:
                sy.op("act", lambda e, h=h: e.activation(out=WIt[:], in_=MB[:, h, t0:t0 + 128], func=AF.Exp,
                                                         bias=MB[:, h, t0 - 1:t0], scale=-1.0), r=["MB"], w=["WIt%d" % p_])
                sy.op("dve", lambda e, h=h: e.tensor_tensor(out=qst[:], in0=qmc[b][:, h, :], in1=WIt[0:64, :],
                                                            op=ALU.mult), r=[qk_, "WIt%d" % p_], w=["qst%d" % p_])
            sy.mm(lambda e, h=h: e.matmul(scm[:], lhsT=kmc[b][:, h, :], rhs=qmc[b][:, h, :],
                                          start=True, stop=True), r=[kk_, qk_], w=["scm%d" % p_])
            sy.op("dve", lambda e: e.tensor_tensor(out=Stt[:], in0=scm[:], in1=Wt[:], op=ALU.mult), r=["scm%d" % p_, "Wt%d" % p_], w=["Stt%d" % p_])
            sy.mm(lambda e, h=h: e.matmul(ndp[:], lhsT=Stt[:], rhs=vm1[b][:, h, :], start=True, stop=(i == 0)),
                  r=["Stt%d" % p_, vk_], w=["ndp%d" % p_], last=(i == 0))
            if i > 0:
                sy.mm(lambda e, h=h: e.matmul(ndp[:], lhsT=qst[:], rhs=Cst[:, h, :], start=False, stop=True),
                      r=["qst%d" % p_, "Cst%d" % h], w=["ndp%d" % p_])
            head_norm(ndp, 128, h, ["ndp%d" % p_], ecol[:, i, h:h + 1], ["ecol"])
            sy.op("dve", lambda e, h=h: e.tensor_scalar(out=kwt[:], in0=kmt[b][:, h * 64:(h + 1) * 64],
                                                        scalar1=Wt[:, 127:128], scalar2=None, op0=ALU.mult),
                  r=[tk_, "Wt%d" % p_], w=["kwt%d" % p_])
            sy.mm(lambda e, h=h: e.matmul(upp[:], lhsT=kwt[:], rhs=vm1[b][:, h, :], start=True, stop=True),
                  r=["kwt%d" % p_, vk_], w=["upp%d" % p_])
            if i == 0:
                sy.op("act", lambda e, h=h: e.copy(out=Cst[:, h, :], in_=upp[:]), r=["upp%d" % p_], w=["Cst%d" % h])
            else:
                sy.op("dve", lambda e, h=h: e.scalar_tensor_tensor(out=Cst[:, h, :], in0=Cst[:, h, :],
                                                                   scalar=WIt[0:64, 127:128], in1=upp[:], op0=ALU.mult,
                                                                   op1=ALU.add), r=["Cst%d" % h, "WIt%d" % p_, "upp%d" % p_], w=["Cst%d" % h])
        gate_and_store(i)
    ck_ = ["Cst%d" % h for h in range(4)]
    sy.dma("sp", "st0", lambda e: e.dma_start(out=cp_o.ap().rearrange("h p d -> p h d"), in_=Cst[:, :, 0:128]),
           r=ck_, is_out=True)
    sy.dma("sp", "st1", lambda e: e.dma_start(out=np_o.ap().rearrange("h p -> p h"), in_=Cst[:, :, 128], allow_slow_non_contiguous=True),
           r=ck_, is_out=True)

    PB["p"] = 0
    scm, ndp, upp = scmL[0], ndpL[0], uppL[0]
    bS = NT % 2
    qS, kS, tS, vS = "qmc%d" % bS, "kmc%d" % bS, "kmt%d" % bS, "vm1%d" % bS
    C0 = sb("C0", [64, NSO * HM, 129], st=st)
    sy.dma("sp", "ld0", lambda e: e.dma_start(out=C0[:], in_=stc_d.ap()), w=["C0"])
    qms = sb("qms", [NSO, 256], st=st)
    sy.dma("sp", "ld1", lambda e: e.dma_start(out=qms[:], in_=qms_s.ap()[0:NSO, :]), r=["qms_s"], w=["qms"])
    sg = sb("sg", [NSO, 12, 4], st=st)
    sy.mm(lambda e: e.transpose(out=scm[0:NSO, 0:4], in_=igt[0:4, S:S + NSO], identity=ident[0:4, 0:4]),
          r=["igt", "cst"], w=["scm0"], last=False)
    sy.mm(lambda e: e.transpose(out=scm[0:NSO, 4:8], in_=lft[0:4, S:S + NSO], identity=ident[0:4, 0:4]),
          r=["lft", "cst"], w=["scm0"])
    sy.op("act", lambda e: e.copy(out=sg[:, 0:2, :].rearrange("p a b -> p (a b)"), in_=scm[0:NSO, 0:8]), r=["scm0"], w=["sg"])
    A = lambda k: sg[:, k, :]
    sy.op("dve", lambda e: e.tensor_tensor(out=A(8), in0=A(1), in1=stm[:], op=ALU.add), r=["sg", "stm"], w=["sg"])
    sy.op("dve", lambda e: e.tensor_tensor(out=A(2), in0=A(8), in1=A(0), op=ALU.max), r=["sg"], w=["sg"])
    sy.op("dve", lambda e: e.tensor_tensor(out=A(9), in0=A(8), in1=A(2), op=ALU.subtract), r=["sg"], w=["sg"])
    sy.op("act", lambda e: e.activation(out=A(3), in_=A(9), func=AF.Exp), r=["sg"], w=["sg"])
    sy.op("dve", lambda e: e.tensor_tensor(out=A(10), in0=A(0), in1=A(2), op=ALU.subtract), r=["sg"], w=["sg"])
    sy.op("act", lambda e: e.activation(out=A(4), in_=A(10), func=AF.Exp), r=["sg"], w=["sg"])
    sy.op("act", lambda e: e.activation(out=A(5), in_=A(2), func=AF.Exp, scale=-1.0), r=["sg"], w=["sg"])
    sy.dma("sp", "st2", lambda e: e.dma_start(out=ms_o.ap(), in_=A(2)), r=["sg"], is_out=True)
    qkt = sb("qkt", [NSO, 256], st=st)
    sy.op("dve", lambda e: e.tensor_tensor(out=qkt[:], in0=qms[:], in1=kmt[bS][0:NSO, :], op=ALU.mult),
          r=["qms", tS], w=["qkt"])
    sy.op("dve", lambda e: e.tensor_reduce(out=A(6), in_=qkt[:].rearrange("p (h d) -> p h d", d=64), axis=AX.X,
                                           op=ALU.add), r=["qkt"], w=["sg"])
    sy.op("dve", lambda e: e.tensor_tensor(out=A(7), in0=A(6), in1=A(4), op=ALU.mult), r=["sg"], w=["sg"])
    wdg = sb("wdg", [NSO, NSO, 4], st=st)
    sy.op("dve", lambda e: e.tensor_tensor(out=wdg[:], in0=ident[0:NSO, 0:NSO].unsqueeze(2).to_broadcast([NSO, NSO, 4]),
                                           in1=A(3).unsqueeze(1).to_broadcast([NSO, NSO, 4]), op=ALU.mult),
          r=["sg", "cst"], w=["wdg"])
    sy.mm(lambda e: e.matmul(scm[0:64, 0:NSO * 4], lhsT=ones[0:NSO, 0:64], rhs=wdg[:].rearrange("p a b -> p (a b)"), start=True,
                             stop=True), r=["cst", "wdg"], w=["scm0"])
    WB = sb("WB", [64, NSO * 4], st=st)
    sy.op("act", lambda e: e.copy(out=WB[:], in_=scm[0:64, 0:NSO * 4]), r=["scm0"], w=["WB"])
    Qd = sb("Qd", [64, 4, NSO, NSO], st=st)
    sy.op("dve", lambda e: e.tensor_tensor(out=Qd[:], in0=qmc[bS][:, :, 0:NSO].unsqueeze(2).to_broadcast([64, 4, NSO, NSO]),
                                           in1=i4[0:64, :, :].unsqueeze(1).to_broadcast([64, 4, NSO, NSO]), op=ALU.mult),
          r=[qS, "i4"], w=["Qd"])
    sy.op("pool", lambda e: e.memset(hm[:], 0.0), w=["hm"])
    nds = sb("nds", [NSO, 129], st=st)
    cj = sb("cj", [NSO, 1], st=st)
    kws = sb("kws", [NSO, 64], st=st)
    Cn = sb("Cn", [64, NSO * 4, 129], st=st)
    for h in range(4):
        for j in range(NSO):
            sy.mm(lambda e, h=h, j=j: e.matmul(ndp[0:NSO, :], lhsT=Qd[:, h, j, :], rhs=C0[:, j * 4 + h, :],
                                               start=(j == 0), stop=(j == NSO - 1)), r=["Qd", "C0"], w=["ndp0"],
                  last=(j == NSO - 1))
        sy.op("dve", lambda e, h=h: e.tensor_scalar(out=nds[:], in0=ndp[0:NSO, :], scalar1=sg[:, 3, h:h + 1], scalar2=None,
                                                    op0=ALU.mult), r=["ndp0", "sg"], w=["nds"])
        sy.op("dve", lambda e, h=h: e.scalar_tensor_tensor(out=nds[:], in0=vm1[bS][0:NSO, h, :], scalar=sg[:, 7, h:h + 1],
                                                           in1=nds[:], op0=ALU.mult, op1=ALU.add),
              r=[vS, "sg", "nds"], w=["nds"])
        head_norm(nds, NSO, h, ["nds"], sg[:, 5, h:h + 1], ["sg"])
        for j in range(NSO):
            sy.op("dve", lambda e, h=h, j=j: e.tensor_tensor(out=cj[:], in0=ident[0:NSO, j:j + 1], in1=sg[:, 4, h:h + 1],
                                                             op=ALU.mult), r=["cst", "sg"], w=["cj"])
            sy.op("dve", lambda e, h=h: e.tensor_scalar(out=kws[:], in0=kmt[bS][0:NSO, h * 64:(h + 1) * 64],
                                                        scalar1=cj[:, 0:1], scalar2=None, op0=ALU.mult),
                  r=[tS, "cj"], w=["kws"])
            sy.mm(lambda e, h=h: e.matmul(upp[:], lhsT=kws[:], rhs=vm1[bS][0:NSO, h, :], start=True, stop=True),
                  r=["kws", vS], w=["upp0"])
            jh = j * 4 + h
            sy.op("dve", lambda e, jh=jh: e.scalar_tensor_tensor(out=Cn[:, jh, :], in0=C0[:, jh, :],
                                                                 scalar=WB[:, jh:jh + 1], in1=upp[:], op0=ALU.mult,
                                                                 op1=ALU.add), r=["C0", "WB", "upp0"], w=["Cn"])
    gate_and_store(NT)
    sy.dma("sp", "st0", lambda e: e.dma_start(out=cs_o.ap().rearrange("a p d -> p a d"), in_=Cn[:, :, 0:128]),
           r=["Cn"], is_out=True)
    sy.dma("sp", "st1", lambda e: e.dma_start(out=ns_o.ap().rearrange("a p -> p a"), in_=Cn[:, :, 128], allow_slow_non_contiguous=True),
           r=["Cn"], is_out=True)
    st.close()
    if cfg.get('stop') == '3':
        sy.finish()
        es.close()
        return nc

    sy.barrier()
    st = ExitStack()
    wba = sb("wba", [128, 4, D], st=st)
    wbm = sb("wbm", [128, 4, D], st=st)
    wo = sb("wo", [128, KC, D], st=st)
    wr = sb("wr", [128, KC, E], st=st)
    sy.dma("pool", "ld0", lambda e: e.dma_start(out=r_(wba[:]), in_=(wba_d.ap())), w=["wba"])
    sy.dma("pool", "ld1", lambda e: e.dma_start(out=r_(wbm[:]), in_=(wbm_d.ap())), w=["wbm"])
    sy.dma("pool", "ld2", lambda e: e.dma_start(out=r_(wo[:]), in_=(wo_d.ap())), w=["wo"])
    sy.dma("sp", "ld3", lambda e: e.dma_start(out=wr[:], in_=wr_d.ap()), w=["wr"])
    ain = sb("ain", [128, 4, 512], st=st)
    mi_ = sb("mi_", [128, 4, 512], st=st)
    sgat = sb("sgat", [128, KC, 512], st=st)
    sgmt = sb("sgmt", [128, KC, 512], st=st)
    mg = sb("mg", [128, KC, 512], st=st)
    t1 = sb("t1", [128, 512], st=st)
    abp = [ps("abp%d" % i, [128, 512], st=st) for i in range(2)]
    mxp = [ps("mxp%d" % i, [128, 512], st=st) for i in range(2)]
    htp = [ps("htp%d" % i, [128, 4, 128], st=st) for i in range(2)]
    lgp = ps("lgp", [128, 2, E], st=st)
    xtk = sb("xtk", [128, D], st=st)
    pre = sb("pre", [128, D], st=st)
    hT = sb("hT", [128, KC, 128], st=st)
    st12 = sb("st12", [128, 2, 6], st=st)
    mv = sb("mv", [128, 4], st=st)
    lg = sb("lg", [128, E], st=st)
    top8 = sb("top8", [128, 8], st=st)
    rt_ = sb("rt_", [128, 8], st=st)
    selt = sb("selt", [128, E], st=st)
    cntb = sb("cntb", [128, E], st=st)
    slp = sb("slp", [128, E], st=st)
    tmpE = sb("tmpE", [128, E], st=st)
    slf = sb("slf", [128, 4], st=st)
    sy.op("pool", lambda e: e.memset(cntb[:], 0.0), w=["cntb"])
    bc_reg = nc.gpsimd.alloc_register("bc_reg")
    nc.gpsimd.reg_mov(bc_reg, E * CAP - 1)

    def layer_norm(src, dst, gi, keys):
        for hf in range(2):
            sy.op("dve", lambda e, hf=hf: e.bn_stats(out=st12[:, hf, :], in_=src[:, hf * 512:(hf + 1) * 512]),
                  r=keys, w=["st12"])
        sy.op("dve", lambda e: e.bn_aggr(out=mv[:, 0:2], in_=st12[:].rearrange("p a b -> p (a b)")), r=["st12"], w=["mv"])
        sy.op("act", lambda e: e.activation(out=mv[:, 2:3], in_=mv[:, 1:2], func=AF.Ln, bias=EPS), r=["mv"], w=["mv"])
        sy.op("act", lambda e: e.activation(out=mv[:, 2:3], in_=mv[:, 2:3], func=AF.Exp, scale=-0.5), r=["mv"], w=["mv"])
        sy.op("dve", lambda e: e.tensor_scalar(out=dst[:], in0=src[:], scalar1=mv[:, 0:1], scalar2=mv[:, 2:3],
                                               op0=ALU.subtract, op1=ALU.mult), r=keys + ["mv"], w=keys)
        sy.op("pool", lambda e: e.tensor_tensor(out=dst[:], in0=dst[:], in1=lnp[:, gi, :], op=ALU.mult),
              r=keys + ["lnp"], w=keys)
        sy.op("pool", lambda e: e.tensor_tensor(out=dst[:], in0=dst[:], in1=lnp[:, gi + 1, :], op=ALU.add),
              r=keys + ["lnp"], w=keys)

    for (g0, gn) in tgroups:
        for (dst_t, src_s, nm, nch) in ((ain, ainT_s, "ain", 4), (mi_, minT_s, "mi_", 4), (sgat, sga_s, "sgat", KC),
                                        (sgmt, sgm_s, "sgmt", KC)):
            sy.dma("pool", "l" + nm, lambda e, dst_t=dst_t, src_s=src_s: e.dma_start(
                out=r_(dst_t[:, :, 0:gn]), in_=(src_s.ap()[:, :, g0:g0 + gn].rearrange("h p t -> p h t"))),
                r=[src_s.name], w=[nm])
        for dc in range(KC):
            pa, pm = abp[dc % 2], mxp[dc % 2]
            for h in range(4):
                sy.mm(lambda e, h=h, dc=dc: e.matmul(pa[:, 0:gn], lhsT=r_(wba[:, h, dc * 128:(dc + 1) * 128]),
                                                     rhs=r_(ain[:, h, 0:gn]), start=(h == 0), stop=(h == 3)),
                      r=["wba", "ain"], w=["abp%d" % (dc % 2)], last=(h == 3))
            for h in range(4):
                sy.mm(lambda e, h=h, dc=dc: e.matmul(pm[:, 0:gn], lhsT=r_(wbm[:, h, dc * 128:(dc + 1) * 128]),
                                                     rhs=r_(mi_[:, h, 0:gn]), start=(h == 0), stop=(h == 3)),
                      r=["wbm", "mi_"], w=["mxp%d" % (dc % 2)], last=(h == 3))
            sy.op("dve", lambda e, dc=dc: e.tensor_tensor(out=t1[:, 0:gn], in0=pa[:, 0:gn], in1=sgat[:, dc, 0:gn],
                                                          op=ALU.mult), r=["abp%d" % (dc % 2), "sgat"], w=["t1"])
            sy.op("dve", lambda e, dc=dc: e.tensor_tensor(out=r_(mg[:, dc, 0:gn]), in0=pm[:, 0:gn], in1=sgmt[:, dc, 0:gn],
                                                          op=ALU.mult), r=["mxp%d" % (dc % 2), "sgmt"], w=["mg%d" % dc])
            sy.op("pool", lambda e, dc=dc: e.tensor_tensor(out=r_(mg[:, dc, 0:gn]), in0=mg[:, dc, 0:gn], in1=t1[:, 0:gn],
                                                           op=ALU.add), r=["mg%d" % dc, "t1"], w=["mg%d" % dc])
        mgk = ["mg%d" % dc for dc in range(KC)]
        for tl in range(gn // 128):
            i = (g0 // 128) + tl
            sy.dma("sp", "lx", lambda e, i=i: e.dma_start(out=xtk[:], in_=xtok_d.ap()[i * 128:(i + 1) * 128, :]), w=["xtk"])
            for hf in range(2):
                for dc in range(KC):
                    sy.mm(lambda e, dc=dc, hf=hf: e.matmul(abp[hf][:], lhsT=r_(mg[:, dc, tl * 128:(tl + 1) * 128]),
                                                           rhs=r_(wo[:, dc, hf * 512:(hf + 1) * 512]), start=(dc == 0),
                                                           stop=(dc == KC - 1)), r=mgk + ["wo"], w=["abp%d" % hf],
                          last=(dc == KC - 1))
                sy.op("dve", lambda e, hf=hf: e.scalar_tensor_tensor(out=pre[:, hf * 512:(hf + 1) * 512],
                                                                     in0=xtk[:, hf * 512:(hf + 1) * 512], scalar=ALPHA,
                                                                     in1=abp[hf][:], op0=ALU.mult, op1=ALU.add),
                      r=["xtk", "abp%d" % hf], w=["pre"])
            layer_norm(pre, pre, 0, ["pre"])
            sy.dma("sp", "sh", lambda e, i=i: e.dma_start(out=hs_s.ap()[i * 128:(i + 1) * 128, :], in_=pre[:]),
                   r=["pre"], wa=["hs_s"])
            for q4 in range(2):
                for k4 in range(4):
                    kc = q4 * 4 + k4
                    sy.mm(lambda e, kc=kc, k4=k4, q4=q4: e.transpose(out=htp[q4][:, k4, :],
                                                                    in_=pre[:, kc * 128:(kc + 1) * 128], identity=ident),
                          r=["pre", "cst"], w=["htp%d" % q4], last=(k4 == 3))
                sy.op("act", lambda e, q4=q4: e.copy(out=hT[:, q4 * 4:(q4 + 1) * 4, :], in_=htp[q4][:]),
                      r=["htp%d" % q4], w=["hT"])
            for kc in range(KC):
                sy.mm(lambda e, kc=kc: e.matmul(lgp[:, 0, :], lhsT=hT[:, kc, :], rhs=wr[:, kc, :], start=(kc == 0),
                                                stop=(kc == KC - 1)), r=["hT", "wr"], w=["lgp0"], last=(kc == KC - 1))
            sy.op("dve", lambda e: e.tensor_tensor(out=lg[:], in0=lgp[:, 0, :], in1=brb[:], op=ALU.add),
                  r=["lgp0", "brb"], w=["lg"])
            sy.op("dve", lambda e: e.max(out=top8[:], in_=lg[:]), r=["lg"], w=["top8"])
            sy.op("dve", lambda e: e.tensor_scalar(out=rt_[:, 0:1], in0=top8[:, 0:1], scalar1=-1.0, scalar2=None,
                                                   op0=ALU.mult), r=["top8"], w=["rt_"])
            sy.op("act", lambda e: e.activation(out=rt_[:, 4:8], in_=top8[:, 0:4], func=AF.Exp, bias=rt_[:, 0:1]),
                  r=["top8", "rt_"], w=["rt_"])
            sy.op("dve", lambda e: e.tensor_reduce(out=rt_[:, 1:2], in_=rt_[:, 4:8], axis=AX.X, op=ALU.add),
                  r=["rt_"], w=["rt_"])
            sy.op("dve", lambda e: e.reciprocal(out=rt_[:, 2:3], in_=rt_[:, 1:2]), r=["rt_"], w=["rt_"])
            sy.op("dve", lambda e, i=i: e.tensor_scalar(out=wts[:, i, :], in0=rt_[:, 4:8], scalar1=rt_[:, 2:3],
                                                        scalar2=None, op0=ALU.mult), r=["rt_"], w=["wts"])
            sy.op("dve", lambda e: e.tensor_scalar(out=selt[:], in0=lg[:], scalar1=top8[:, 3:4], scalar2=None,
                                                   op0=ALU.is_ge), r=["lg", "top8"], w=["selt"])
            if i == NT:
                sy.op("dve", lambda e: e.tensor_scalar(out=selt[:], in0=selt[:], scalar1=rowm, scalar2=None,
                                                       op0=ALU.mult), r=["selt", "cst"], w=["selt"])
            sy.mm(lambda e: e.matmul(lgp[:, 1, :], lhsT=ltri, rhs=selt[:], start=True, stop=True),
                  r=["cst", "selt"], w=["lgp1"])
            sy.op("dve", lambda e: e.tensor_tensor(out=slp[:], in0=lgp[:, 1, :], in1=cntb[:], op=ALU.add),
                  r=["lgp1", "cntb"], w=["slp"])
            sy.op("dve", lambda e: e.tensor_scalar(out=tmpE[:], in0=slp[:], scalar1=float(CAP) - 0.5, scalar2=BIG,
                                                   op0=ALU.is_ge, op1=ALU.mult), r=["slp"], w=["tmpE"])
            sy.op("dve", lambda e: e.tensor_tensor(out=slp[:], in0=slp[:], in1=tmpE[:], op=ALU.add),
                  r=["slp", "tmpE"], w=["slp"])
            sy.op("dve", lambda e: e.tensor_scalar(out=tmpE[:], in0=selt[:], scalar1=-BIG, scalar2=BIG, op0=ALU.mult,
                                                   op1=ALU.add), r=["selt"], w=["tmpE"])
            sy.op("dve", lambda e: e.tensor_tensor(out=slp[:], in0=slp[:], in1=tmpE[:], op=ALU.add),
                  r=["slp", "tmpE"], w=["slp"])
            sy.op("dve", lambda e: e.tensor_tensor(out=slp[:], in0=slp[:], in1=ecap[:], op=ALU.add),
                  r=["slp", "ecap"], w=["slp"])
            sy.mm(lambda e: e.matmul(lgp[:, 1, :], lhsT=ones, rhs=selt[:], start=True, stop=True),
                  r=["cst", "selt", "slp"], w=["lgp1"])
            sy.op("dve", lambda e: e.tensor_tensor(out=cntb[:], in0=cntb[:], in1=lgp[:, 1, :], op=ALU.add),
                  r=["cntb", "lgp1"], w=["cntb"])
            for j in range(4):
                sy.op("dve", lambda e, j=j: e.tensor_scalar(out=tmpE[:], in0=lg[:], scalar1=top8[:, j:j + 1], scalar2=None,
                                                            op0=ALU.is_equal), r=["lg", "top8"], w=["tmpE"])
                sy.op("dve", lambda e: e.tensor_tensor(out=tmpE[:], in0=tmpE[:], in1=slp[:], op=ALU.mult),
                      r=["tmpE", "slp"], w=["tmpE"])
                sy.op("dve", lambda e, j=j: e.tensor_reduce(out=slf[:, j:j + 1], in_=tmpE[:], axis=AX.X, op=ALU.add),
                      r=["tmpE"], w=["slf"])
            sy.op("dve", lambda e: e.tensor_scalar(out=slf[:], in0=slf[:], scalar1=float(E * CAP + 64), scalar2=None,
                                                   op0=ALU.min), r=["slf"], w=["slf"])
            sy.op("dve", lambda e, i=i: e.tensor_copy(out=sli[:, i, :], in_=slf[:]), r=["slf"], w=["sli"])
            for j in range(4):
                sy.dma("pool", "scat%d" % j, lambda e, i=i, j=j: e.indirect_dma_start(
                    out=xg_s.ap(), out_offset=bass.IndirectOffsetOnAxis(ap=sli[:, i, j:j + 1], axis=0), in_=pre[:],
                    in_offset=None, bounds_check=bc_reg, oob_is_err=False), r=["pre", "sli", "xg_zero"], wa=["xg_s"])
    st.close()
    if cfg.get('stop') == '4':
        sy.finish()
        es.close()
        return nc

    sy.barrier()
    st = ExitStack()
    NWB = 6
    wbuf = [sb("wbuf%d" % i, [128, KC, 512], BF16, st=st) for i in range(NWB)]
    xe2 = [sb("xe%d" % i, [128, CT, D], st=st) for i in range(2)]
    xeT2 = [sb("xeT%d" % i, [128, KC, CAP], BF16, st=st) for i in range(2)]
    hid = sb("hid", [128, FC, CAP], BF16, st=st)
    gt2 = [sb("gt%d" % i, [128, CAP], st=st) for i in range(2)]
    ut2 = [sb("ut%d" % i, [128, CAP], st=st) for i in range(2)]
    sgt2 = [sb("sgt%d" % i, [128, CAP], st=st) for i in range(2)]
    bdb = sb("bdb", [128, D], st=st)
    yst = [sb("yst%d" % i, [128, D], st=st) for i in range(2)]
    ep = [ps("ep%d" % i, [128, 512], st=st) for i in range(6)]
    tpp = [ps("tpp%d" % i, [128, 4, 128], st=st) for i in range(2)]
    wi = 0
    pi_ = 0
    ti_ = 0
    yi_ = 0
    assert FF % 512 == 0 or FF < 512
    FH = max(1, FF // 512)
    FW = min(512, FF)
    for e_ in range(E):
        xe, xeT = xe2[e_ % 2], xeT2[e_ % 2]
        xek, xetk = "xe%d" % (e_ % 2), "xeT%d" % (e_ % 2)
        sy.dma("sp", "lxe%d" % (e_ % 2), lambda e, e_=e_, xe=xe: e.dma_start(
            out=xe[:], in_=xg_s.ap()[e_ * CAP:(e_ + 1) * CAP, :].rearrange("(a p) d -> p a d", p=128)),
            r=["xg_s", "xg_zero"], w=[xek])
        sy.dma("pool", "lbd", lambda e, e_=e_: e.dma_start(out=bdb[:], in_=bd_d.ap()[e_].partition_broadcast(128)), w=["bdb"])
        for a in range(CT):
            for q4 in range(KC // 4):
                tb = ti_ % 2
                ti_ += 1
                for k4 in range(4):
                    kc = q4 * 4 + k4
                    sy.mm(lambda e, a=a, kc=kc, k4=k4, tb=tb, xe=xe: e.transpose(out=tpp[tb][:, k4, :],
                                                                                 in_=xe[:, a, kc * 128:(kc + 1) * 128],
                                                                                 identity=ident),
                          r=[xek, "cst"], w=["tpp%d" % tb], last=(k4 == 3))
                sy.op("act", lambda e, a=a, q4=q4, tb=tb, xeT=xeT: e.copy(out=xeT[:, q4 * 4:(q4 + 1) * 4, a * 128:(a + 1) * 128],
                                                                          in_=tpp[tb][:]), r=["tpp%d" % tb], w=[xetk])
        for fh in range(FH):
            wg_b = wi % NWB
            wi += 1
            wu_b = wi % NWB
            wi += 1
            sy.dma("pool", "lw%d" % wg_b, lambda e, e_=e_, fh=fh, wg_b=wg_b: e.dma_start(
                out=wbuf[wg_b][:, :, 0:FW], in_=(wg_d.ap()[e_, :, :, fh * FW:(fh + 1) * FW])), w=["wbuf%d" % wg_b])
            sy.dma("pool", "lw%d" % wu_b, lambda e, e_=e_, fh=fh, wu_b=wu_b: e.dma_start(
                out=wbuf[wu_b][:, :, 0:FW], in_=(wu_d.ap()[e_, :, :, fh * FW:(fh + 1) * FW])), w=["wbuf%d" % wu_b])
            for f4 in range(FW // 128):
                fc = fh * (FW // 128) + f4
                gt, ut, sgt = gt2[fc % 2], ut2[fc % 2], sgt2[fc % 2]
                gk, uk, sk = "gt%d" % (fc % 2), "ut%d" % (fc % 2), "sgt%d" % (fc % 2)
                pg = pi_ % 6
                pi_ += 1
                pu = pi_ % 6
                pi_ += 1
                for kc in range(KC):
                    sy.mm(lambda e, kc=kc, f4=f4, pg=pg, wg_b=wg_b, xeT=xeT: e.matmul(
                        ep[pg][:, 0:CAP], lhsT=wbuf[wg_b][:, kc, f4 * 128:(f4 + 1) * 128], rhs=xeT[:, kc, :],
                        start=(kc == 0), stop=(kc == KC - 1)), r=["wbuf%d" % wg_b, xetk], w=["ep%d" % pg],
                        last=(kc == KC - 1))
                for kc in range(KC):
                    sy.mm(lambda e, kc=kc, f4=f4, pu=pu, wu_b=wu_b, xeT=xeT: e.matmul(
                        ep[pu][:, 0:CAP], lhsT=wbuf[wu_b][:, kc, f4 * 128:(f4 + 1) * 128], rhs=xeT[:, kc, :],
                        start=(kc == 0), stop=(kc == KC - 1)), r=["wbuf%d" % wu_b, xetk], w=["ep%d" % pu],
                        last=(kc == KC - 1))
                sy.op("dve", lambda e, pg=pg, fc=fc, e_=e_, gt=gt: e.tensor_scalar(
                    out=gt[:], in0=ep[pg][:, 0:CAP], scalar1=bgu[:, 0, e_, fc:fc + 1], scalar2=LIMIT, op0=ALU.add,
                    op1=ALU.min), r=["ep%d" % pg, "bgu"], w=[gk])
                sy.op("act", lambda e, gt=gt, sgt=sgt: e.activation(out=sgt[:], in_=gt[:], func=AF.Sigmoid, scale=SALPHA),
                      r=[gk], w=[sk])
                sy.op("dve", lambda e, pu=pu, fc=fc, e_=e_, ut=ut: e.tensor_scalar(
                    out=ut[:], in0=ep[pu][:, 0:CAP], scalar1=bu1[:, e_, fc:fc + 1], scalar2=LIMIT + 1.0, op0=ALU.add,
                    op1=ALU.min), r=["ep%d" % pu, "bu1"], w=[uk])
                sy.op("dve", lambda e, gt=gt, sgt=sgt: e.tensor_tensor(out=gt[:], in0=gt[:], in1=sgt[:], op=ALU.mult),
                      r=[gk, sk], w=[gk])
                sy.op("dve", lambda e, fc=fc, gt=gt, ut=ut: e.scalar_tensor_tensor(out=hid[:, fc, :], in0=ut[:],
                                                                                   scalar=1.0 - LIMIT, in1=gt[:],
                                                                                   op0=ALU.max, op1=ALU.mult),
                      r=[gk, uk], w=["hid%d" % fc])
        hk = ["hid%d" % fc for fc in range(FC)]
        wds = []
        for dh in range(D // 512):
            wd_b = wi % NWB
            wi += 1
            wds.append(wd_b)
            sy.dma("pool", "lw%d" % wd_b, lambda e, e_=e_, dh=dh, wd_b=wd_b: e.dma_start(
                out=wbuf[wd_b][:, 0:FC, :], in_=(wd_d.ap()[e_, :, :, dh * 512:(dh + 1) * 512])), w=["wbuf%d" % wd_b])
            if dh == 0 and NWB < 4:
                pass
        for a in range(CT):
            yb = yi_ % 2
            yi_ += 1
            for dh in range(D // 512):
                pd = pi_ % 6
                pi_ += 1
                for fc in range(FC):
                    sy.mm(lambda e, fc=fc, a=a, dh=dh, pd=pd: e.matmul(
                        ep[pd][:], lhsT=hid[:, fc, a * 128:(a + 1) * 128], rhs=wbuf[wds[dh]][:, fc, :],
                        start=(fc == 0), stop=(fc == FC - 1)), r=hk + ["wbuf%d" % wds[dh]], w=["ep%d" % pd],
                        last=(fc == FC - 1))
                sy.op("dve", lambda e, dh=dh, pd=pd, yb=yb: e.tensor_tensor(
                    out=yst[yb][:, dh * 512:(dh + 1) * 512], in0=ep[pd][:], in1=bdb[:, dh * 512:(dh + 1) * 512],
                    op=ALU.add), r=["ep%d" % pd, "bdb"], w=["yst%d" % yb])
            sy.dma("sp", "sy%d" % yb, lambda e, e_=e_, a=a, yb=yb: e.dma_start(
                out=yx_s.ap()[e_ * CAP + a * 128:e_ * CAP + (a + 1) * 128, :], in_=yst[yb][:]),
                r=["yst%d" % yb], wa=["yx_s"])
    st.close()
    if cfg.get('stop') == '5':
        sy.finish()
        es.close()
        return nc

    sy.barrier()
    st = ExitStack()
    yg = [sb("yg%d" % i, [128, 4, D], st=st) for i in range(2)]
    hb = [sb("hb%d" % i, [128, D], st=st) for i in range(2)]
    st12 = sb("st12b", [128, 2, 6], st=st)
    mv = sb("mvb", [128, 4], st=st)
    for b in range(2):
        sy.op("pool", lambda e, b=b: e.memset(yg[b][:], 0.0), w=["yg%d" % b])
    for i in range(NTT):
        b = i % 2
        for j in range(4):
            sy.dma("pool", "gat%d_%d" % (b, j), lambda e, i=i, j=j, b=b: e.indirect_dma_start(
                out=yg[b][:, j, :], out_offset=None, in_=yx_s.ap(),
                in_offset=bass.IndirectOffsetOnAxis(ap=sli[:, i, j:j + 1], axis=0), bounds_check=bc_reg,
                oob_is_err=False), r=["yx_s", "sli"], w=["yg%d" % b])
        sy.dma("sp", "lh%d" % b, lambda e, i=i, b=b: e.dma_start(out=hb[b][:], in_=hs_s.ap()[i * 128:(i + 1) * 128, :]),
               r=["hs_s"], w=["hb%d" % b])
        sy.op("dve", lambda e, b=b: e.tensor_scalar(out=hb[b][:], in0=hb[b][:], scalar1=ALPHA, scalar2=None, op0=ALU.mult),
              r=["hb%d" % b], w=["hb%d" % b])
        for j in range(4):
            sy.op("dve", lambda e, b=b, j=j, i=i: e.scalar_tensor_tensor(out=hb[b][:], in0=yg[b][:, j, :],
                                                                         scalar=wts[:, i, j:j + 1], in1=hb[b][:],
                                                                         op0=ALU.mult, op1=ALU.add),
                  r=["yg%d" % b, "wts", "hb%d" % b], w=["hb%d" % b])
        layer_norm(hb[b], hb[b], 2, ["hb%d" % b])
        if i < NT:
            sy.dma("sp", "so%d" % b, lambda e, i=i, b=b: e.dma_start(out=y_o.ap()[i * 128:(i + 1) * 128, :], in_=hb[b][:]),
                   r=["hb%d" % b], is_out=True)
        else:
            sy.dma("sp", "so%d" % b, lambda e, b=b: e.dma_start(out=ys_o.ap(), in_=hb[b][0:NSO, :]),
                   r=["hb%d" % b], is_out=True)
    sy.finish()
    st.close()
    es.close()
    return nc


def consts(cfg):
    S, PAST, E, CAP = cfg["S"], cfg["PAST"], cfg["E"], cfg["CAP"]
    NT = S // 128
    c = np.zeros((128, 1024), np.float32)
    c[:, 0:128] = np.eye(128, dtype=np.float32)
    c[:, 128:256] = 1.0
    p = np.arange(128)
    c[:, 256:384] = (p[:, None] < p[None, :]).astype(np.float32)
    c[:, 384:512] = (p[:, None] <= p[None, :]).astype(np.float32)
    c[:NSO, 512] = 1.0
    c[:PAST // 128, 513] = 1.0
    q = np.arange(512)
    cm = np.zeros((128, 4, 512), np.float32)
    for off in range(4):
        cm[:, off, :] = (q[None, :] >= off * 128 + p[:, None]).astype(np.float32)
    inv = (np.float32(THETA) ** (-np.arange(0, ROT, 2, dtype=np.float32) / np.float32(ROT))).astype(np.float32)
    pos = np.concatenate([np.arange(S), np.full(128, PAST)]).astype(np.float32)
    ang = pos[:, None] * inv[None, :]
    tab = np.concatenate([np.cos(ang), np.sin(ang)], axis=1).astype(np.float32)
    rope = np.ascontiguousarray(tab.reshape(NT + 1, 128, 16).transpose(1, 0, 2))
    ropes = np.ascontiguousarray(np.broadcast_to(tab[S], (128, 16))).astype(np.float32)
    ecap = np.ascontiguousarray(np.broadcast_to((np.arange(E) * CAP).astype(np.float32), (128, E)))
    i4 = np.ascontiguousarray(np.broadcast_to(np.eye(4, dtype=np.float32), (128, 4, 4)))
    return dict(cst=c, cm=cm, rope=rope, ropes=ropes, ecap=ecap, i4=i4)


def prep(cfg, inp):
    S, PAST, NPOOL, E, FF, CAP, D = (cfg[k] for k in ("S", "PAST", "NPOOL", "E", "FF", "CAP", "D"))
    KC, FC = D // 128, FF // 128
    T = S + 128
    f = lambda a: np.ascontiguousarray(np.asarray(a, dtype=np.float32))
    cs = consts(cfg)
    w_in = np.asarray(inp["w_in"][0], np.float32)
    win = f(w_in.reshape(KC, 128, -1).transpose(1, 0, 2))
    shared = dict(
        win=win,
        bigate=f(np.concatenate([inp["b_igate"][0], inp["b_fgate"][0]]).reshape(8, 1)),
        lamp=f(np.stack([inp["lambda_q1"][0], inp["lambda_k1"][0], inp["lambda_q2"][0], inp["lambda_k2"][0]])),
        subg=f(np.asarray(inp["subln_g"][0]).reshape(128, 1)),
        mhg=f(inp["mh_norm_g"][0]),
        wba=f(np.asarray(inp["w_ba"][0]).reshape(4, 128, D).transpose(1, 0, 2)),
        wbm=f(np.asarray(inp["w_bm"][0]).reshape(4, 128, D).transpose(1, 0, 2)),
        wo=f(np.asarray(inp["w_o"][0]).reshape(KC, 128, D).transpose(1, 0, 2)),
        lnp=f(np.stack([inp["ln1_g"][0], inp["ln1_b"][0], inp["ln2_g"][0], inp["ln2_b"][0]])),
        wr=f(np.asarray(inp["w_router"][0]).reshape(KC, 128, E).transpose(1, 0, 2)),
        br=f(inp["b_router"][0]),
        wg=f(np.asarray(inp["w_gate"][0]).reshape(E, KC, 128, FF).transpose(0, 2, 1, 3)),
        wu=f(np.asarray(inp["w_up"][0]).reshape(E, KC, 128, FF).transpose(0, 2, 1, 3)),
        wd=f(np.asarray(inp["w_down"][0]).reshape(E, FC, 128, D).transpose(0, 2, 1, 3)),
        bgu=f(np.stack([np.asarray(inp["b_gate"][0]).reshape(E, FC, 128), np.asarray(inp["b_up"][0]).reshape(E, FC, 128)])
              .transpose(3, 0, 1, 2)),
        bd=f(inp["b_down"][0]),
        **cs,
    )
    xp = np.asarray(inp["x_prompt"], np.float32)
    xs = np.asarray(inp["x_sample"], np.float32)[:, 0, :]
    ck = np.asarray(inp["cache_k"][0])
    cv = np.asarray(inp["cache_v"][0])
    pt = np.asarray(inp["page_table"]).astype(np.int32)
    ckh, cvh = {}, {}
    for h in range(HA):
        ckh[h] = np.ascontiguousarray(ck[:, :, h, :].reshape(NPOOL, 128 // 32, 32 * 128).transpose(1, 0, 2))
        cvh[h] = np.ascontiguousarray(cv[:, :, h, :].reshape(NPOOL, 128 // 32, 32 * 128).transpose(1, 0, 2))
    cols = {}
    for h in range(HA):
        cols[h] = np.concatenate([w_in[:, h * 128:(h + 1) * 128], w_in[:, 512 + h * 128:512 + (h + 1) * 128],
                                  w_in[:, 1024 + h * 128:1024 + (h + 1) * 128]], axis=1)
    whs_all = f(np.stack([cols[hh].reshape(KC, 128, 384).transpose(1, 0, 2) for hh in range(4)], axis=1))
    maps = []
    for c in range(8):
        h, g = c % 4, c // 4
        xt = np.zeros((T, D), np.float32)
        xt[:S] = xp[c]
        xt[S:S + NSO] = xs[4 * c:4 * c + 4]
        xT = f(xt.T.reshape(KC, 128, T).transpose(1, 0, 2))
        xo = xs[4 * c:4 * c + 4]
        xgT = np.zeros((128, 4, KC, NSG), np.float32)
        xoT = xo.T.reshape(KC, 128, NSO).transpose(1, 0, 2)
        for hh in range(4):
            xgT[:, hh, :, hh * 4:hh * 4 + 4] = xoT
        ptg = np.zeros((128, NSG), np.int32)
        for hh in range(4):
            ptg[:pt.shape[1], hh * 4:hh * 4 + 4] = pt[4 * c:4 * c + 4].T
        sel = np.zeros((128, 16), np.float32)
        for hh in range(4):
            for j in range(4):
                rank = g * 4 + hh
                sel[rank * 16 + (c % 4) * 4 + j, hh * 4 + j] = 1.0
        sc = np.asarray(inp["state_c"][0][4 * c:4 * c + 4], np.float32)
        sn = np.asarray(inp["state_n"][0][4 * c:4 * c + 4], np.float32)
        stc = np.concatenate([sc, sn[..., None]], axis=-1).reshape(NSO * HM, 64, 129).transpose(1, 0, 2)
        m = dict(shared)
        m.update(xT=xT, xtok=f(xt), xgT=xgT, whs=whs_all,
                 pt=ptg, selm=sel, stc=f(stc),
                 stm=f(inp["state_m"][0][4 * c:4 * c + 4]))
        for hh in range(4):
            for i in range(4):
                m["ck%d_%d" % (hh, i)] = ckh[hh][i]
                m["cv%d_%d" % (hh, i)] = cvh[hh][i]
        maps.append(m)
    return maps


def assemble(cfg, res):
    S, D = cfg["S"], cfg["D"]
    g = lambda n: np.stack([np.asarray(r[n]) for r in res])
    y_p = g("y")
    y_s = g("ysamp").reshape(32, 1, D)
    k_p = g("k").reshape(1, 8, S, HA, 128)
    v_p = g("v").reshape(1, 8, S, HA, 128)
    c_p = g("cp").reshape(1, 8, HM, 64, 128)
    n_p = g("npr").reshape(1, 8, HM, 64)
    m_p = g("mp").reshape(1, 8, HM)
    k_s = g("ksamp").reshape(1, 32, 1, HA, 128)
    v_s = g("vsamp").reshape(1, 32, 1, HA, 128)
    c_s = g("csamp").reshape(1, 32, HM, 64, 128)
    n_s = g("nsamp").reshape(1, 32, HM, 64)
    m_s = g("msamp").reshape(1, 32, HM)
    return tuple(np.ascontiguousarray(a, dtype=np.float32) for a in
                 (y_p, y_s, k_p, v_p, c_p, n_p, m_p, k_s, v_s, c_s, n_s, m_s))


def run(cfg, inp):
    nc = build(cfg)
    maps = prep(cfg, inp)
    res = run_bass_kernel_spmd(nc, maps, core_ids=list(range(8)))
    return assemble(cfg, res.results)


def kernel(**inputs):
    return run(FULL, inputs)
```

```python
import math
from contextlib import ExitStack

import numpy as np
import concourse.bass as bass
import concourse.mybir as mybir
from concourse.bass_utils import run_bass_kernel_spmd

F32 = mybir.dt.float32
F32R = mybir.dt.float32r
BF16 = mybir.dt.bfloat16
I32 = mybir.dt.int32
ALU = mybir.AluOpType
AF = mybir.ActivationFunctionType
AX = mybir.AxisListType

FULL = dict(S=2048, PAST=16384, NPOOL=5120, E=32, FF=1024, CAP=512, D=1024)

HA, DH = 4, 64
HM, DK, DV = 4, 64, 128
ROT = 16
THETA = 500000.0
SOFTCAP = 15.0
LIMIT = 7.0
SALPHA = 1.702
EPS = 1e-5
DEPTH = 1
ALPHA = (2.0 * DEPTH) ** 0.25
LAM_INIT = 0.8 - 0.6 * math.exp(-0.3 * 0)
NSO = 4
NSG = 16
BIG = 1.0e6


def r_(ap):
    return ap.bitcast(F32R)


class Sync:
    def __init__(self, nc, es):
        self.nc = nc
        self.es = es
        self.engs = {"pe": nc.tensor, "act": nc.scalar, "dve": nc.vector, "pool": nc.gpsimd, "sp": nc.sync}
        self.sem = {}
        self.cnt = {}
        for n in ("pe", "act", "dve", "pool"):
            self.sem[n] = es.enter_context(nc.semaphore("s_" + n))
            self.cnt[n] = 0
        self.seen = {n: {} for n in self.engs}
        self.last_w = {}
        self.readers = {}
        self.dsem = {}
        self.dcnt = {}
        self.out_tokens = []
        self.extra_tokens = []

    def _deps(self, r, w):
        deps = []
        for k in r:
            deps += self.last_w.get(k, [])
        for k in w:
            deps += self.readers.get(k, [])
            deps += self.last_w.get(k, [])
        return deps

    def barrier(self):
        toks = [("s_" + n, self.sem[n], self.cnt[n]) for n in self.sem if self.cnt[n] > 0]
        toks += [("d_" + k, self.dsem[k], self.dcnt[k]) for k in self.dsem if self.dcnt[k] > 0]
        toks += list(self.extra_tokens)
        for eng in self.engs:
            self._wait(eng, toks)

    def _wait(self, eng, deps):
        best = {}
        for (sn, sem, val) in deps:
            if best.get(sn, (None, 0))[1] < val:
                best[sn] = (sem, val)
        for sn, (sem, val) in best.items():
            if self.seen[eng].get(sn, 0) < val:
                self.engs[eng].wait_ge(sem, val)
                self.seen[eng][sn] = val

    def _record(self, tok, r, w, wa=()):
        for k in w:
            self.last_w[k] = [tok]
            self.readers[k] = []
        for k in wa:
            self.last_w.setdefault(k, []).append(tok)
        for k in r:
            if k not in w:
                self.readers.setdefault(k, []).append(tok)

    def op(self, eng, fn, r=(), w=()):
        self._wait(eng, self._deps(r, w))
        ins = fn(self.engs[eng])
        self.cnt[eng] += 1
        ins.then_inc(self.sem[eng], 1)
        tok = ("s_" + eng, self.sem[eng], self.cnt[eng])
        self._record(tok, r, w)
        return tok

    def mm(self, fn, r=(), w=(), last=True):
        self._wait("pe", self._deps(r, w))
        ins = fn(self.engs["pe"])
        if last:
            self.cnt["pe"] += 1
            ins.then_inc(self.sem["pe"], 1)
            tok = ("s_pe", self.sem["pe"], self.cnt["pe"])
            pr, pw = getattr(self, "_pend", ([], []))
            self._record(tok, list(r) + pr, list(w) + pw)
            self._pend = ([], [])
        else:
            pr, pw = getattr(self, "_pend", ([], []))
            self._pend = (pr + list(r), pw + list(w))

    def dma(self, q, key, fn, r=(), w=(), wa=(), is_out=False):
        if key not in self.dsem:
            self.dsem[key] = self.es.enter_context(self.nc.semaphore("d_" + key))
            self.dcnt[key] = 0
        self._wait(q, self._deps(r, w))
        ins = fn(self.engs[q])
        self.dcnt[key] += 16
        ins.then_inc(self.dsem[key], 16)
        tok = ("d_" + key, self.dsem[key], self.dcnt[key])
        self._record(tok, r, w, wa)
        if is_out:
            self.out_tokens.append(tok)
        return tok

    def finish(self):
        best = {}
        for (sn, sem, val) in self.out_tokens:
            if best.get(sn, (None, 0))[1] < val:
                best[sn] = (sem, val)
        for sn, (sem, val) in best.items():
            self.engs["sp"].wait_ge(sem, val)


def build(cfg):
    S, PAST, NPOOL, E, FF, CAP, D = (cfg[k] for k in ("S", "PAST", "NPOOL", "E", "FF", "CAP", "D"))
    KC = D // 128
    FC = FF // 128
    NT = S // 128
    T = S + 128
    NTT = NT + 1
    PAGES = PAST // 128
    assert PAGES <= 128
    NIN = 5128
    NQB = S // 512 if S >= 512 else 1
    QW = 512 if S >= 512 else S
    CT = CAP // 128
    RB = 32
    NRB = 128 // RB

    nc = bass.Bass("TRN2", target_bir_lowering=False)

    def din(name, shape, dt=F32):
        return nc.dram_tensor(name, list(shape), dt, kind="ExternalInput")

    def dout(name, shape, dt=F32):
        return nc.dram_tensor(name, list(shape), dt, kind="ExternalOutput")

    def dscr(name, shape, dt=F32):
        return nc.dram_tensor(name, list(shape), dt)

    xT_d = din("xT", [128, KC, T])
    xtok_d = din("xtok", [T, D])
    xgT_d = din("xgT", [128, 4, KC, NSG])
    win_d = din("win", [128, KC, NIN])
    whs_d = din("whs", [128, 4, KC, 384])
    ck_d = [[din("ck%d_%d" % (h, i), [NPOOL, RB * 128]) for i in range(NRB)] for h in range(HA)]
    cv_d = [[din("cv%d_%d" % (h, i), [NPOOL, RB * 128]) for i in range(NRB)] for h in range(HA)]
    pt_d = din("pt", [128, NSG], I32)
    sel_d = din("selm", [128, 16])
    stc_d = din("stc", [64, NSO * HM, 129])
    stm_d = din("stm", [NSO, HM])
    big_d = din("bigate", [8, 1])
    lamp_d = din("lamp", [4, 64])
    subg_d = din("subg", [128, 1])
    mhg_d = din("mhg", [512])
    wba_d = din("wba", [128, 4, D])
    wbm_d = din("wbm", [128, 4, D])
    wo_d = din("wo", [128, KC, D])
    ln_d = din("lnp", [4, D])
    wr_d = din("wr", [128, KC, E])
    br_d = din("br", [E])
    wg_d = din("wg", [E, 128, KC, FF])
    wu_d = din("wu", [E, 128, KC, FF])
    wd_d = din("wd", [E, 128, FC, D])
    bgu_d = din("bgu", [128, 2, E, FC])
    bd_d = din("bd", [E, D])
    cst_d = din("cst", [128, 1024])
    cm_d = din("cm", [128, 4, 512])
    rope_d = din("rope", [128, NTT, 16])
    ropes_d = din("ropes", [128, 16])
    ecap_d = din("ecap", [128, E])
    i4_d = din("i4", [128, 4, 4])

    y_o = dout("y", [S, D])
    ys_o = dout("ysamp", [NSO, D])
    k_o = dout("k", [S, 512])
    v_o = dout("v", [S, 512])
    cp_o = dout("cp", [HM, 64, 128])
    np_o = dout("npr", [HM, 64])
    mp_o = dout("mp", [HM, 1])
    ks_o = dout("ksamp", [NSO, 512])
    vs_o = dout("vsamp", [NSO, 512])
    cs_o = dout("csamp", [NSO * HM, 64, 128])
    ns_o = dout("nsamp", [NSO * HM, 64])
    ms_o = dout("msamp", [NSO, HM])

    qT_s = dscr("qT_s", [HA, 128, T])
    kT_s = dscr("kT_s", [HA, 128, T])
    v_s = dscr("v_s", [T, 512])
    qmT_s = dscr("qmT_s", [HM, 64, T])
    kmT_s = dscr("kmT_s", [HM, 64, T])
    km_s = dscr("km_s", [T, 256])
    qms_s = dscr("qms_s", [128, 256])
    vm_s = dscr("vm_s", [T, 512])
    ig_s = dscr("ig_s", [4, T])
    lf_s = dscr("lf_s", [4, T])
    sog_s = dscr("sog_s", [T, 512])
    sga_s = dscr("sga_s", [KC, 128, T])
    sgm_s = dscr("sgm_s", [KC, 128, T])
    ainT_s = dscr("ainT_s", [HA, 128, T])
    minT_s = dscr("minT_s", [4, 128, T])
    M_s = dscr("M_s", [4, S])
    qsb_s = dscr("qsb_s", [NSG, 128])
    agi_s = dscr("agi_s", [NSG, 128])
    ago_s = dscr("ago_s", [8 * NSG, 128])
    hs_s = dscr("hs_s", [T, D])
    xg_s = dscr("xg_s", [E * CAP, D])
    yx_s = dscr("yx_s", [E * CAP, D])

    es = ExitStack()
    sy = Sync(nc, es)

    def sb(name, shape, dt=F32, st=None):
        return (st or es).enter_context(nc.sbuf_tensor("t_" + name, list(shape), dt))

    def ps(name, shape, dt=F32, st=None):
        return (st or es).enter_context(nc.psum_tensor("p_" + name, list(shape), dt))

    cst = sb("cst", [128, 1024])
    ident = cst[:, 0:128]
    ones = cst[:, 128:256]
    ltri = cst[:, 256:384]
    maskD = cst[:, 384:512]
    rowm = cst[:, 512:513]
    zeros = cst[:, 640:1024]
    cm = sb("cm", [128, 4, 512])
    rope = sb("rope", [128, NTT, 16])
    ropes = sb("ropes", [128, 16])
    ecap = sb("ecap", [128, E])
    i4 = sb("i4", [128, 4, 4])
    lnp = sb("lnp", [128, 4, D])
    brb = sb("brb", [128, E])
    mhg = sb("mhg", [128, 512])
    subg = sb("subg", [128, 1])
    bigt = sb("bigt", [8, 1])
    lamp = sb("lamp", [128, 4, 64])
    bgu = sb("bgu", [128, 2, E, FC])
    stm = sb("stm", [NSO, HM])
    selm = sb("selm", [128, 16])
    ptt = sb("ptt", [128, NSG], I32)
    ainS = sb("ainS", [128, 16])
    wts = sb("wts", [128, NTT, 4])
    sli = sb("sli", [128, NTT, 4], I32)

    CK = ["cst", "cm", "rope", "ropes", "ecap", "i4", "lnp", "brb", "mhg", "subg", "bigt", "lamp", "bgu",
          "stm", "selm", "ptt"]
    loads = [
        (r_(cst[:]), r_(cst_d.ap())), (cm[:], cm_d.ap()), (rope[:], rope_d.ap()), (ropes[:], ropes_d.ap()),
        (ecap[:], ecap_d.ap()), (i4[:], i4_d.ap()),
        (lnp[:].rearrange("p a d -> p (a d)"), ln_d.ap().rearrange("a d -> (a d)").partition_broadcast(128)),
        (brb[:], br_d.ap().partition_broadcast(128)), (mhg[:], mhg_d.ap().partition_broadcast(128)),
        (subg[:], subg_d.ap()), (bigt[:], big_d.ap()),
        (lamp[:].rearrange("p a d -> p (a d)"), lamp_d.ap().rearrange("a d -> (a d)").partition_broadcast(128)),
        (bgu[:], bgu_d.ap()), (stm[:], stm_d.ap()), (selm[:], sel_d.ap()), (ptt[:], pt_d.ap()),
    ]
    for li, (o, i) in enumerate(loads):
        bc = li in (0, 6, 7, 8, 11)
        sy.dma("pool" if bc else "sp", "constb" if bc else "const", lambda e, o=o, i=i: e.dma_start(out=o, in_=i), w=[])
    ctok = [("d_const", sy.dsem["const"], sy.dcnt["const"]), ("d_constb", sy.dsem["constb"], sy.dcnt["constb"])]
    for k in CK:
        sy.last_w[k] = list(ctok)

    lam = sb("lam", [128, 4])
    ltmp = sb("ltmp", [128, 2, 64])
    sy.op("dve", lambda e: e.tensor_tensor(out=ltmp[:, 0, :], in0=lamp[:, 0, :], in1=lamp[:, 1, :], op=ALU.mult),
          r=["lamp"], w=["ltmp"])
    sy.op("dve", lambda e: e.tensor_tensor(out=ltmp[:, 1, :], in0=lamp[:, 2, :], in1=lamp[:, 3, :], op=ALU.mult),
          r=["lamp"], w=["ltmp"])
    sy.op("dve", lambda e: e.tensor_reduce(out=lam[:, 2:4], in_=ltmp[:], axis=AX.X, op=ALU.add),
          r=["ltmp"], w=["lam"])
    sy.op("act", lambda e: e.activation(out=lam[:, 2:4], in_=lam[:, 2:4], func=AF.Exp), r=["lam"], w=["lam"])
    sy.op("dve", lambda e: e.tensor_tensor(out=lam[:, 0:1], in0=lam[:, 2:3], in1=lam[:, 3:4], op=ALU.subtract),
          r=["lam"], w=["lam"])
    sy.op("dve", lambda e: e.tensor_scalar(out=lam[:, 0:1], in0=lam[:, 0:1], scalar1=LAM_INIT, scalar2=None,
                                           op0=ALU.add), r=["lam"], w=["lam"])
    sy.op("dve", lambda e: e.tensor_scalar(out=lam[:, 1:2], in0=lam[:, 0:1], scalar1=-1.0, scalar2=None,
                                           op0=ALU.mult), r=["lam"], w=["lam"])
    gsc = sb("gsc", [128, 1])
    sy.op("dve", lambda e: e.tensor_scalar(out=gsc[:], in0=subg[:], scalar1=(1.0 - LAM_INIT), scalar2=None,
                                           op0=ALU.mult), r=["subg"], w=["gsc"])
    bu1 = sb("bu1", [128, E, FC])
    sy.op("dve", lambda e: e.tensor_scalar(out=bu1[:], in0=bgu[:, 1, :, :], scalar1=1.0, scalar2=None, op0=ALU.add),
          r=["bgu"], w=["bu1"])
    bsc = sb("bsc", [8, 1])
    sy.op("dve", lambda e: e.tensor_scalar(out=bsc[:], in0=bigt[:], scalar1=1.0 / SOFTCAP, scalar2=None,
                                           op0=ALU.mult), r=["bigt"], w=["bsc"])

    zt = sb("zt", [128, D])
    sy.op("pool", lambda e: e.memset(zt[:], 0.0), w=["zt"])
    nz = (E * CAP) // 128
    for z0 in range(0, nz, 32):
        z1 = min(nz, z0 + 32)
        sy.dma("pool", "zero", lambda e, z0=z0, z1=z1: e.dma_start(
            out=xg_s.ap()[z0 * 128:z1 * 128, :].rearrange("(a p) d -> p a d", p=128),
            in_=zt[:].unsqueeze(1).to_broadcast([128, z1 - z0, D])), r=["zt"], wa=["xg_zero"])

    for h in range(HA):
        sy.dma("pool", "zero", lambda e, h=h: e.dma_start(out=ainT_s.ap()[h, :, S + NSO:T], in_=zt[:, 0:128 - NSO]),
               r=["zt"], wa=["ainT_s"])

    st = ExitStack()
    xgT = sb("xgT", [128, 4, KC, NSG], st=st)
    whs = sb("whs", [128, 4, KC, 384], st=st)
    sy.dma("sp", "ld0", lambda e: e.dma_start(out=xgT[:], in_=xgT_d.ap()), w=["xgT"])
    sy.dma("sp", "ld1", lambda e: e.dma_start(out=whs[:], in_=whs_d.ap()), w=["whs"])
    zs_ps = ps("zs_ps", [NSG, 384], st=st)
    for h in range(4):
        for kc in range(KC):
            sy.mm(lambda e, kc=kc, h=h: e.matmul(zs_ps[:], lhsT=xgT[:, h, kc, :], rhs=whs[:, h, kc, :],
                                                 start=(h == 0 and kc == 0), stop=(h == 3 and kc == KC - 1)),
                  r=["xgT", "whs"], w=["zs_ps"], last=(h == 3 and kc == KC - 1))
    zs = sb("zs", [NSG, 384], st=st)
    zv = zs_ps[:].rearrange("p (g d) -> p g d", d=64)
    zo = zs[:].rearrange("p (g d) -> p g d", d=64)
    cosb = ropes[0:NSG, 0:8].unsqueeze(1).to_broadcast([NSG, 4, 8])
    sinb = ropes[0:NSG, 8:16].unsqueeze(1).to_broadcast([NSG, 4, 8])
    rt = sb("rt", [NSG, 4, 4, 8], st=st)

    def rope_ops(src, dst, cosv, sinv, tmp, n, rk, wk_, tk):
        sy.op("dve", lambda e: e.tensor_tensor(out=tmp[:, 0], in0=src[:, :, 0:8], in1=cosv, op=ALU.mult),
              r=[rk], w=[tk])
        sy.op("dve", lambda e: e.tensor_tensor(out=tmp[:, 1], in0=src[:, :, 8:16], in1=sinv, op=ALU.mult),
              r=[rk], w=[tk])
        sy.op("dve", lambda e: e.tensor_tensor(out=tmp[:, 2], in0=src[:, :, 8:16], in1=cosv, op=ALU.mult),
              r=[rk], w=[tk])
        sy.op("dve", lambda e: e.tensor_tensor(out=tmp[:, 3], in0=src[:, :, 0:8], in1=sinv, op=ALU.mult),
              r=[rk], w=[tk])
        sy.op("dve", lambda e: e.tensor_tensor(out=dst[:, :, 0:8], in0=tmp[:, 0], in1=tmp[:, 1], op=ALU.subtract),
              r=[tk], w=[wk_])
        sy.op("dve", lambda e: e.tensor_tensor(out=dst[:, :, 8:16], in0=tmp[:, 2], in1=tmp[:, 3], op=ALU.add),
              r=[tk], w=[wk_])
        sy.op("act", lambda e: e.copy(out=dst[:, :, 16:64], in_=src[:, :, 16:64]), r=[rk], w=[wk_])

    rope_ops(zv[:, 0:4, :], zo[:, 0:4, :], cosb, sinb, rt[:], NSG, "zs_ps", "zs", "rt")
    sy.op("act", lambda e: e.copy(out=zs[:, 256:384], in_=zs_ps[:, 256:384]), r=["zs_ps"], w=["zs"])
    sy.dma("sp", "ld0", lambda e: e.dma_start(out=qsb_s.ap(), in_=zs[:, 0:128]), r=["zs"], w=["qsb_s"])
    qb = sb("qb", [128, NSG, 128], st=st)
    sy.dma("pool", "ldb", lambda e: e.dma_start(
        out=qb[:].rearrange("p j d -> p (j d)"),
        in_=qsb_s.ap().rearrange("j d -> (j d)").partition_broadcast(128)), r=["qsb_s"], w=["qb"])
    pn = sb("pn", [NSG, 2], st=st)
    pnt = sb("pnt", [NSG, 128], st=st)
    sy.op("dve", lambda e: e.tensor_tensor(out=pnt[:], in0=zs[:, 0:128], in1=zs[:, 128:256], op=ALU.mult),
          r=["zs"], w=["pnt"])
    sy.op("dve", lambda e: e.tensor_reduce(out=pn[:], in_=pnt[:].rearrange("p (c d) -> p c d", d=64), axis=AX.X,
                                           op=ALU.add), r=["pnt"], w=["pn"])
    sy.op("act", lambda e: e.activation(out=pn[:], in_=pn[:], func=AF.Exp, scale=DH ** -0.5), r=["pn"], w=["pn"])
    pnd = sb("pnd", [NSG, NSG, 2], st=st)
    sy.op("dve", lambda e: e.tensor_tensor(out=pnd[:], in0=ident[0:NSG, 0:NSG].unsqueeze(2).to_broadcast([NSG, NSG, 2]),
                                           in1=pn[:].unsqueeze(1).to_broadcast([NSG, NSG, 2]), op=ALU.mult),
          r=["pn", "cst"], w=["pnd"])

    kb = [sb("kb%d" % i, [128, RB, 128], st=st) for i in range(2)]
    vb = [sb("vb%d" % i, [128, RB, 128], st=st) for i in range(2)]
    ktmp = sb("ktmp", [128, RB, 128], st=st)
    vbb = [sb("vbb%d" % i, [128, RB, 128], BF16, st=st) for i in range(2)]
    scb = sb("scb", [128, 128, 2], BF16, st=st)
    sc_s = sb("sc_s", [128, 128, 2], st=st)
    prs = sb("prs", [128, 2], st=st)
    pv_ps = ps("pv_ps", [128, 2], st=st)
    dn_ps = ps("dn_ps", [128, 2], st=st)
    oS = sb("oS", [128, NSG], st=st)
    osm = sb("osm", [128, 8], st=st)
    blk = 0
    for j in range(NSG):
        for rb in range(NRB):
            b = blk % 2
            blk += 1
            sy.dma("pool", "kb%d" % b, lambda e, b=b, rb=rb, j=j: e.indirect_dma_start(
                out=kb[b][:].rearrange("p r d -> p (r d)"), out_offset=None, in_=ck_d[j // 4][rb].ap(),
                in_offset=bass.IndirectOffsetOnAxis(ap=ptt[:, j:j + 1], axis=0)), r=["ptt"], w=["kb%d" % b])
            sy.dma("pool", "vb%d" % b, lambda e, b=b, rb=rb, j=j: e.indirect_dma_start(
                out=vb[b][:].rearrange("p r d -> p (r d)"), out_offset=None, in_=cv_d[j // 4][rb].ap(),
                in_offset=bass.IndirectOffsetOnAxis(ap=ptt[:, j:j + 1], axis=0)), r=["ptt"], w=["vb%d" % b])
            sy.op("act", lambda e, b=b: e.copy(out=vbb[b][:], in_=vb[b][:]), r=["vb%d" % b], w=["vbb%d" % b])
            sy.op("dve", lambda e, b=b, j=j: e.tensor_tensor(
                out=ktmp[:], in0=kb[b][:], in1=qb[:, j, :].unsqueeze(1).to_broadcast([128, RB, 128]), op=ALU.mult),
                r=["kb%d" % b, "qb"], w=["ktmp"])
            sy.op("dve", lambda e, rb=rb: e.tensor_reduce(
                out=sc_s[:, rb * RB:(rb + 1) * RB, :], in_=ktmp[:].rearrange("p r (c d) -> p r c d", d=64),
                axis=AX.X, op=ALU.add), r=["ktmp"], w=["sc_s%d" % rb])
            sy.op("act", lambda e, rb=rb: e.activation(
                out=sc_s[:, rb * RB:(rb + 1) * RB, :], in_=sc_s[:, rb * RB:(rb + 1) * RB, :], func=AF.Exp,
                scale=DH ** -0.5), r=["sc_s%d" % rb], w=["sc_s%d" % rb])
            if PAGES < 128:
                sy.op("dve", lambda e, rb=rb: e.tensor_scalar(
                    out=sc_s[:, rb * RB:(rb + 1) * RB, :], in0=sc_s[:, rb * RB:(rb + 1) * RB, :],
                    scalar1=cst[:, 513:514], scalar2=None, op0=ALU.mult), r=["sc_s%d" % rb, "cst"], w=["sc_s%d" % rb])
            sy.op("act", lambda e, rb=rb: e.copy(out=scb[:, rb * RB:(rb + 1) * RB, :],
                                                 in_=sc_s[:, rb * RB:(rb + 1) * RB, :]),
                  r=["sc_s%d" % rb], w=["scb%d" % rb])
            for rr in range(RB):
                row = rb * RB + rr
                sy.mm(lambda e, b=b, rr=rr, row=row: e.matmul(
                    pv_ps[:], lhsT=vbb[b][:, rr, :], rhs=scb[:, row, :], start=(row == 0), stop=False),
                    r=["vbb%d" % b, "scb%d" % rb], w=["pv_ps"], last=(rr == RB - 1))
        sy.mm(lambda e, j=j: e.matmul(pv_ps[:], lhsT=zs[:, 256:384], rhs=pnd[:, j, :], start=False, stop=True),
              r=["zs", "pnd"], w=["pv_ps"])
        sy.op("dve", lambda e: e.tensor_reduce(out=prs[:], in_=sc_s[:].rearrange("p r c -> p c r"), axis=AX.X,
                                               op=ALU.add), r=["sc_s%d" % i for i in range(NRB)], w=["prs"])
        sy.mm(lambda e: e.matmul(dn_ps[:], lhsT=ones, rhs=prs[:], start=True, stop=False),
              r=["cst", "prs"], w=["dn_ps"], last=False)
        sy.mm(lambda e, j=j: e.matmul(dn_ps[:], lhsT=ones[0:NSG, :], rhs=pnd[:, j, :], start=False, stop=True),
              r=["cst", "pnd"], w=["dn_ps"])
        sy.op("dve", lambda e: e.reciprocal(out=osm[:, 0:2], in_=dn_ps[:]), r=["dn_ps"], w=["osm"])
        sy.op("dve", lambda e: e.tensor_tensor(out=osm[:, 2:4], in0=pv_ps[:], in1=osm[:, 0:2], op=ALU.mult),
              r=["pv_ps", "osm"], w=["osm"])
        sy.op("dve", lambda e, j=j: e.scalar_tensor_tensor(
            out=oS[:, j:j + 1], in0=osm[:, 3:4], scalar=lam[:, 1:2], in1=osm[:, 2:3], op0=ALU.mult, op1=ALU.add),
            r=["osm", "lam"], w=["oS"])
    osq = sb("osq", [128, NSG], st=st)
    ss_ps = ps("ss_ps", [128, NSG], st=st)
    sy.op("act", lambda e: e.activation(out=osq[:], in_=oS[:], func=AF.Square), r=["oS"], w=["osq"])
    sy.mm(lambda e: e.matmul(ss_ps[:], lhsT=ones, rhs=osq[:], start=True, stop=True), r=["cst", "osq"], w=["ss_ps"])
    sy.op("act", lambda e: e.activation(out=osq[:], in_=ss_ps[:], func=AF.Ln, bias=EPS, scale=1.0 / 128),
          r=["ss_ps"], w=["osq"])
    sy.op("act", lambda e: e.activation(out=osq[:], in_=osq[:], func=AF.Exp, scale=-0.5), r=["osq"], w=["osq"])
    sy.op("dve", lambda e: e.scalar_tensor_tensor(out=oS[:], in0=oS[:], scalar=gsc[:, 0:1], in1=osq[:],
                                                  op0=ALU.mult, op1=ALU.mult), r=["oS", "gsc", "osq"], w=["oS"])
    sy.op("act", lambda e: e.copy(out=ainS[:], in_=oS[:]), r=["oS"], w=["ainS"])
    st.close()
    if cfg.get('stop') == 'S':
        sy.finish()
        es.close()
        return nc
    for h in range(HA):
        sy.dma("sp", "st0", lambda e, h=h: e.dma_start(out=ainT_s.ap()[h, :, S:S + NSO], in_=ainS[:, h * 4:h * 4 + 4]),
               r=["ainS"], wa=["ainT_s"])

    sy.barrier()
    st = ExitStack()
    xT = sb("xT", [128, KC, T], st=st)
    sy.dma("pool", "ld0", lambda e: e.dma_start(out=r_(xT[:]), in_=(xT_d.ap())), w=["xT"])
    BW = 520
    wb = [sb("wb%d" % i, [128, KC, BW], st=st) for i in range(2)]
    blocks = [(0, 512), (512, 512), (1024, 512), (1536, 512), (2048, 512), (2560, 520), (3080, 512), (3592, 512),
              (4104, 512), (4616, 512)]
    zp = [ps("zp%d" % i, [128, 512], st=st) for i in range(4)]
    zsb = [sb("zsb%d" % i, [128, 512], st=st) for i in range(4)]
    rtt = sb("rtt", [128, 4, 8, 8], st=st)
    tp = [ps("tp%d" % i, [128, 4, 128], st=st) for i in range(2)]
    tsb = [sb("tsb%d" % i, [128, 4, 128], st=st) for i in range(2)]
    cnt = {"z": 0, "t": 0}
    tgroups = [(g * 512, min(512, S - g * 512)) for g in range((S + 511) // 512)] + [(S, 128)]

    def tok_major(bi, i, c0, n, post):
        zi = cnt["z"] % 4
        cnt["z"] += 1
        for kc in range(KC):
            sy.mm(lambda e, kc=kc: e.matmul(zp[zi][:, 0:n], lhsT=r_(xT[:, kc, i * 128:(i + 1) * 128]),
                                            rhs=r_(wb[bi % 2][:, kc, c0:c0 + n]), start=(kc == 0), stop=(kc == KC - 1)),
                  r=["xT", "wb%d" % (bi % 2)], w=["zp%d" % zi], last=(kc == KC - 1))
        post(zp[zi], zi)

    def feat_major(bi, c0, m, t0, n, post, cast=r_):
        zi = cnt["z"] % 4
        cnt["z"] += 1
        for kc in range(KC):
            sy.mm(lambda e, kc=kc: e.matmul(zp[zi][0:m, 0:n], lhsT=cast(wb[bi % 2][:, kc, c0:c0 + m]),
                                            rhs=cast(xT[:, kc, t0:t0 + n]), start=(kc == 0), stop=(kc == KC - 1)),
                  r=["xT", "wb%d" % (bi % 2)], w=["zp%d" % zi], last=(kc == KC - 1))
        post(zp[zi], zi)

    def transposes_out(src_tile, zi, dst, i):
        ti = cnt["t"] % 2
        cnt["t"] += 1
        for h in range(4):
            sy.mm(lambda e, h=h: e.transpose(out=tp[ti][:, h, :], in_=src_tile[:, h * 128:(h + 1) * 128],
                                             identity=ident), r=["zsb%d" % zi, "cst"], w=["tp%d" % ti], last=(h == 3))
        sy.op("act", lambda e: e.copy(out=tsb[ti][:], in_=tp[ti][:]), r=["tp%d" % ti], w=["tsb%d" % ti])
        sy.dma("sp", "st%d" % ti, lambda e: e.dma_start(
            out=dst.ap()[:, :, i * 128:(i + 1) * 128].rearrange("h p t -> p h t"), in_=tsb[ti][:]),
            r=["tsb%d" % ti], wa=[dst.name])

    for bi, (c_off, c_w) in enumerate(blocks):
        if bi >= cfg.get('nblk', 99):
            break
        sy.dma("pool", "wb%d" % (bi % 2), lambda e, bi=bi, c_off=c_off, c_w=c_w: e.dma_start(
            out=r_(wb[bi % 2][:, :, 0:c_w]), in_=(win_d.ap()[:, :, c_off:c_off + c_w])), w=["wb%d" % (bi % 2)])
        if bi in (0, 1):
            for i in range(NTT):
                def post(zpt, zi, i=i, bi=bi):
                    src = zpt[:].rearrange("p (g d) -> p g d", d=64)
                    dst = zsb[zi][:].rearrange("p (g d) -> p g d", d=64)
                    cosv = rope[:, i, 0:8].unsqueeze(1).to_broadcast([128, 8, 8])
                    sinv = rope[:, i, 8:16].unsqueeze(1).to_broadcast([128, 8, 8])
                    rope_ops(src, dst, cosv, sinv, rtt[:], 128, "zp%d" % zi, "zsb%d" % zi, "rtt")
                    if bi == 1:
                        if i < NT:
                            sy.dma("sp", "zo%d" % zi, lambda e: e.dma_start(out=k_o.ap()[i * 128:(i + 1) * 128, :],
                                                                            in_=zsb[zi][:]), r=["zsb%d" % zi], is_out=True)
                        else:
                            sy.dma("sp", "zo%d" % zi, lambda e: e.dma_start(out=ks_o.ap(), in_=zsb[zi][0:NSO, :]),
                                   r=["zsb%d" % zi], is_out=True)
                    transposes_out(zsb[zi], zi, qT_s if bi == 0 else kT_s, i)
                tok_major(bi, i, 0, 512, post)
        elif bi in (2, 4):
            for i in range(NTT):
                def post(zpt, zi, i=i, bi=bi):
                    sy.op("act", lambda e: e.copy(out=zsb[zi][:], in_=zpt[:]), r=["zp%d" % zi], w=["zsb%d" % zi])
                    dst = v_s if bi == 2 else vm_s
                    sy.dma("sp", "zo%d" % zi, lambda e: e.dma_start(out=dst.ap()[i * 128:(i + 1) * 128, :], in_=zsb[zi][:]),
                           r=["zsb%d" % zi], wa=[dst.name])
                    if bi == 2:
                        if i < NT:
                            sy.dma("sp", "zq%d" % zi, lambda e: e.dma_start(out=v_o.ap()[i * 128:(i + 1) * 128, :],
                                                                            in_=zsb[zi][:]), r=["zsb%d" % zi], is_out=True)
                        else:
                            sy.dma("sp", "zq%d" % zi, lambda e: e.dma_start(out=vs_o.ap(), in_=zsb[zi][0:NSO, :]),
                                   r=["zsb%d" % zi], is_out=True)
                tok_major(bi, i, 0, 512, post)
        elif bi == 3:
            for (t0, n) in tgroups:
                for ch in range(4):
                    def post(zpt, zi, ch=ch, t0=t0, n=n):
                        if ch < 2:
                            sy.op("act", lambda e: e.copy(out=zsb[zi][:, 0:n], in_=zpt[:, 0:n]), r=["zp%d" % zi],
                                  w=["zsb%d" % zi])
                            dst = qmT_s
                        else:
                            sy.op("act", lambda e: e.mul(out=zsb[zi][:, 0:n], in_=zpt[:, 0:n], mul=DK ** -0.5),
                                  r=["zp%d" % zi], w=["zsb%d" % zi])
                            dst = kmT_s
                        hh = (ch % 2) * 2
                        sy.dma("sp", "zo%d" % zi, lambda e: e.dma_start(
                            out=dst.ap()[hh:hh + 2, :, t0:t0 + n].rearrange("h p t -> (h p) t"), in_=zsb[zi][:, 0:n]),
                            r=["zsb%d" % zi], wa=[dst.name])
                    feat_major(bi, ch * 128, 128, t0, n, post)
            for i in range(NTT):
                def post(zpt, zi, i=i):
                    sy.op("act", lambda e: e.mul(out=zsb[zi][:, 0:256], in_=zpt[:, 0:256], mul=DK ** -0.5),
                          r=["zp%d" % zi], w=["zsb%d" % zi])
                    sy.dma("sp", "zo%d" % zi, lambda e: e.dma_start(out=km_s.ap()[i * 128:(i + 1) * 128, :],
                                                                    in_=zsb[zi][:, 0:256]), r=["zsb%d" % zi], wa=["km_s"])
                tok_major(bi, i, 256, 256, post)

            def post(zpt, zi):
                sy.op("act", lambda e: e.copy(out=zsb[zi][:, 0:256], in_=zpt[:, 0:256]), r=["zp%d" % zi], w=["zsb%d" % zi])
                sy.dma("sp", "zo%d" % zi, lambda e: e.dma_start(out=qms_s.ap(), in_=zsb[zi][:, 0:256]),
                       r=["zsb%d" % zi], wa=["qms_s"])
            tok_major(bi, NT, 0, 256, post)
        elif bi == 5:
            for (t0, n) in tgroups:
                def post(zpt, zi, t0=t0, n=n):
                    a = zsb[zi]
                    sy.op("act", lambda e: e.activation(out=a[0:8, 0:n], in_=zpt[0:8, 0:n], func=AF.Tanh,
                                                        bias=bsc[:, 0:1], scale=1.0 / SOFTCAP),
                          r=["zp%d" % zi, "bsc"], w=["zsb%d" % zi])
                    sy.op("dve", lambda e: e.tensor_scalar(out=a[0:8, 0:n], in0=a[0:8, 0:n], scalar1=SOFTCAP,
                                                           scalar2=None, op0=ALU.mult), r=["zsb%d" % zi], w=["zsb%d" % zi])
                    sy.dma("sp", "zo%d" % zi, lambda e: e.dma_start(out=ig_s.ap()[:, t0:t0 + n], in_=a[0:4, 0:n]),
                           r=["zsb%d" % zi], wa=["ig_s"])
                    sy.op("act", lambda e: e.activation(out=a[0:8, 0:n], in_=a[0:8, 0:n], func=AF.Exp, scale=-1.0),
                          r=["zsb%d" % zi], w=["zsb%d" % zi])
                    sy.op("act", lambda e: e.activation(out=a[0:8, 0:n], in_=a[0:8, 0:n], func=AF.Ln, bias=1.0),
                          r=["zsb%d" % zi], w=["zsb%d" % zi])
                    sy.op("dve", lambda e: e.tensor_scalar(out=a[0:8, 0:n], in0=a[0:8, 0:n], scalar1=-1.0,
                                                           scalar2=None, op0=ALU.mult), r=["zsb%d" % zi], w=["zsb%d" % zi])
                    sy.dma("sp", "zq%d" % zi, lambda e: e.dma_start(out=lf_s.ap()[:, t0:t0 + n], in_=a[4:8, 0:n]),
                           r=["zsb%d" % zi], wa=["lf_s"])
                feat_major(bi, 0, 8, t0, n, post, cast=lambda a: a)
            for i in range(NTT):
                def post(zpt, zi, i=i):
                    sy.op("act", lambda e: e.activation(out=zsb[zi][:], in_=zpt[:], func=AF.Sigmoid),
                          r=["zp%d" % zi], w=["zsb%d" % zi])
                    sy.dma("sp", "zo%d" % zi, lambda e: e.dma_start(out=sog_s.ap()[i * 128:(i + 1) * 128, :],
                                                                    in_=zsb[zi][:]), r=["zsb%d" % zi], wa=["sog_s"])
                tok_major(bi, i, 8, 512, post)
        else:
            dst = sga_s if bi in (6, 7) else sgm_s
            base = ((bi - 6) % 2) * 4
            for (t0, n) in tgroups:
                for ch in range(4):
                    def post(zpt, zi, ch=ch, t0=t0, n=n, dst=dst, base=base):
                        sy.op("act", lambda e: e.activation(out=zsb[zi][:, 0:n], in_=zpt[:, 0:n], func=AF.Sigmoid),
                              r=["zp%d" % zi], w=["zsb%d" % zi])
                        sy.dma("sp", "zo%d" % zi, lambda e: e.dma_start(out=dst.ap()[base + ch, :, t0:t0 + n],
                                                                        in_=zsb[zi][:, 0:n]), r=["zsb%d" % zi], wa=[dst.name])
                    feat_major(bi, ch * 128, 128, t0, n, post)
    st.close()
    if cfg.get('stop') == '1':
        sy.finish()
        es.close()
        return nc

    sy.barrier()
    st = ExitStack()
    qTh = sb("qTh", [128, S], st=st)
    kTh = sb("kTh", [128, S], st=st)
    vh = sb("vh", [128, NT, 128], st=st)
    scp = [ps("scp%d" % i, [128, 512], st=st) for i in range(4)]
    dnp = [ps("dnp%d" % i, [128, 512], st=st) for i in range(2)]
    pvp = [ps("pvp%d" % i, [128, 512], st=st) for i in range(2)]
    ptl = [sb("ptl%d" % i, [128, 512], st=st) for i in range(4)]
    o_t = sb("o_t", [128, 512], st=st)
    t_t = sb("t_t", [128, 512], st=st)
    rd_t = sb("rd_t", [128, 512], st=st)
    sq_t = sb("sq_t", [128, 512], st=st)
    it = 0
    for h in range(HA):
        sy.dma("pool", "ld0", lambda e, h=h: e.dma_start(out=r_(qTh[:]), in_=(qT_s.ap()[h, :, 0:S])), r=["qT_s"], w=["qTh"])
        sy.dma("pool", "ld1", lambda e, h=h: e.dma_start(out=r_(kTh[:]), in_=(kT_s.ap()[h, :, 0:S])), r=["kT_s"], w=["kTh"])
        sy.dma("pool", "ld2", lambda e, h=h: e.dma_start(
            out=r_(vh[:]), in_=(v_s.ap()[0:S, h * 128:(h + 1) * 128].rearrange("(i p) d -> p i d", p=128))),
            r=["v_s"], w=["vh"])
        for qblk in range(NQB):
            q0 = qblk * QW
            nkt = (q0 + QW) // 128
            for kt in range(nkt):
                for c in range(2):
                    bi_ = it % 4
                    it += 1
                    sy.mm(lambda e, c=c, kt=kt, bi_=bi_: e.matmul(
                        scp[bi_][:, 0:QW], lhsT=r_(kTh[c * 64:(c + 1) * 64, kt * 128:(kt + 1) * 128]),
                        rhs=r_(qTh[c * 64:(c + 1) * 64, q0:q0 + QW]), start=True, stop=True),
                        r=["qTh", "kTh"], w=["scp%d" % bi_])
                    sy.op("act", lambda e, bi_=bi_: e.activation(out=r_(ptl[bi_][:, 0:QW]), in_=scp[bi_][:, 0:QW],
                                                                 func=AF.Exp, scale=DH ** -0.5),
                          r=["scp%d" % bi_], w=["ptl%d" % bi_])
                    off = kt - q0 // 128
                    if off >= 0:
                        sy.op("dve", lambda e, bi_=bi_, off=off: e.tensor_tensor(
                            out=r_(ptl[bi_][:, 0:QW]), in0=ptl[bi_][:, 0:QW], in1=cm[:, off, 0:QW], op=ALU.mult),
                            r=["ptl%d" % bi_, "cm"], w=["ptl%d" % bi_])
                    sy.mm(lambda e, c=c, kt=kt, bi_=bi_: e.matmul(
                        dnp[c][:, 0:QW], lhsT=r_(ones), rhs=r_(ptl[bi_][:, 0:QW]), start=(kt == 0), stop=(kt == nkt - 1)),
                        r=["cst", "ptl%d" % bi_], w=["dnp%d" % c], last=False)
                    sy.mm(lambda e, c=c, kt=kt, bi_=bi_: e.matmul(
                        pvp[c][:, 0:QW], lhsT=r_(vh[:, kt, :]), rhs=r_(ptl[bi_][:, 0:QW]), start=(kt == 0),
                        stop=(kt == nkt - 1)), r=["vh", "ptl%d" % bi_], w=["pvp%d" % c])
            W_ = QW
            sy.op("dve", lambda e: e.reciprocal(out=rd_t[:, 0:W_], in_=dnp[0][:, 0:W_]), r=["dnp0"], w=["rd_t"])
            sy.op("dve", lambda e: e.tensor_tensor(out=o_t[:, 0:W_], in0=pvp[0][:, 0:W_], in1=rd_t[:, 0:W_], op=ALU.mult),
                  r=["pvp0", "rd_t"], w=["o_t"])
            sy.op("dve", lambda e: e.reciprocal(out=rd_t[:, 0:W_], in_=dnp[1][:, 0:W_]), r=["dnp1"], w=["rd_t"])
            sy.op("dve", lambda e: e.tensor_tensor(out=t_t[:, 0:W_], in0=pvp[1][:, 0:W_], in1=rd_t[:, 0:W_], op=ALU.mult),
                  r=["pvp1", "rd_t"], w=["t_t"])
            sy.op("dve", lambda e: e.scalar_tensor_tensor(out=o_t[:, 0:W_], in0=t_t[:, 0:W_], scalar=lam[:, 1:2],
                                                          in1=o_t[:, 0:W_], op0=ALU.mult, op1=ALU.add),
                  r=["t_t", "o_t", "lam"], w=["o_t"])
            sy.op("act", lambda e: e.activation(out=r_(sq_t[:, 0:W_]), in_=o_t[:, 0:W_], func=AF.Square), r=["o_t"], w=["sq_t"])
            sy.mm(lambda e: e.matmul(dnp[0][:, 0:W_], lhsT=r_(ones), rhs=r_(sq_t[:, 0:W_]), start=True, stop=True),
                  r=["cst", "sq_t"], w=["dnp0"])
            sy.op("act", lambda e: e.activation(out=rd_t[:, 0:W_], in_=dnp[0][:, 0:W_], func=AF.Ln, bias=EPS,
                                                scale=1.0 / 128), r=["dnp0"], w=["rd_t"])
            sy.op("act", lambda e: e.activation(out=rd_t[:, 0:W_], in_=rd_t[:, 0:W_], func=AF.Exp, scale=-0.5),
                  r=["rd_t"], w=["rd_t"])
            sy.op("dve", lambda e: e.scalar_tensor_tensor(out=o_t[:, 0:W_], in0=o_t[:, 0:W_], scalar=gsc[:, 0:1],
                                                          in1=rd_t[:, 0:W_], op0=ALU.mult, op1=ALU.mult),
                  r=["o_t", "gsc", "rd_t"], w=["o_t"])
            sy.dma("sp", "st0", lambda e, h=h, q0=q0: e.dma_start(out=ainT_s.ap()[h, :, q0:q0 + W_], in_=o_t[:, 0:W_]),
                   r=["o_t"], wa=["ainT_s"])
    st.close()
    if cfg.get('stop') == '2':
        sy.finish()
        es.close()
        return nc

    sy.barrier()
    st = ExitStack()
    igt = sb("igt", [4, T], st=st)
    lft = sb("lft", [4, T], st=st)
    sy.dma("sp", "ld0", lambda e: e.dma_start(out=igt[:], in_=ig_s.ap()), r=["ig_s"], w=["igt"])
    sy.dma("sp", "ld1", lambda e: e.dma_start(out=lft[:], in_=lf_s.ap()), r=["lf_s"], w=["lft"])
    Bt = sb("Bt", [4, S], st=st)
    at = sb("at", [4, S], st=st)
    Mt = sb("Mt", [4, S], st=st)
    mt = sb("mtot", [4, S], st=st)
    o4 = sb("o4", [4, S], st=st)
    sy.op("pool", lambda e: e.memset(o4[:], 1.0), w=["o4"])
    sy.op("dve", lambda e: e.tensor_tensor_scan(out=Bt[:], data0=o4[:], data1=lft[:, 0:S],
                                                initial=0.0, op0=ALU.mult, op1=ALU.add), r=["lft", "o4"], w=["Bt"])
    sy.op("dve", lambda e: e.tensor_tensor(out=at[:], in0=igt[:, 0:S], in1=Bt[:], op=ALU.subtract),
          r=["igt", "Bt"], w=["at"])
    sy.op("dve", lambda e: e.tensor_tensor_scan(out=Mt[:], data0=at[:], data1=at[:], initial=0.0, op0=ALU.max,
                                                op1=ALU.max), r=["at"], w=["Mt"])
    sy.op("dve", lambda e: e.tensor_tensor(out=mt[:], in0=Bt[:], in1=Mt[:], op=ALU.add), r=["Bt", "Mt"], w=["mtot"])
    sy.dma("sp", "st0", lambda e: e.dma_start(out=mp_o.ap(), in_=mt[:, S - 1:S]), r=["mtot"], is_out=True)
    sy.dma("sp", "st1", lambda e: e.dma_start(out=M_s.ap(), in_=Mt[:]), r=["Mt"], w=["M_s"])
    MB = sb("MB", [128, 4, S], st=st)
    sy.dma("pool", "ldb", lambda e: e.dma_start(out=MB[:].rearrange("p h t -> p (h t)"),
                                              in_=M_s.ap().rearrange("h t -> (h t)").partition_broadcast(128)),
           r=["M_s"], w=["MB"])
    acol = sb("acol", [128, NT, 4], st=st)
    ecol = sb("ecol", [128, NT, 4], st=st)
    colp = ps("colp", [128, 2, NT * 4], st=st)
    for i in range(NT):
        sy.mm(lambda e, i=i: e.transpose(out=colp[:, 0, i * 4:(i + 1) * 4], in_=at[0:4, i * 128:(i + 1) * 128],
                                         identity=ident[0:4, 0:4]), r=["at", "cst"], w=["colp"], last=False)
        sy.mm(lambda e, i=i: e.transpose(out=colp[:, 1, i * 4:(i + 1) * 4], in_=mt[0:4, i * 128:(i + 1) * 128],
                                         identity=ident[0:4, 0:4]), r=["mtot", "cst"], w=["colp"], last=(i == NT - 1))
    sy.op("act", lambda e: e.copy(out=acol[:].rearrange("p i h -> p (i h)"), in_=colp[:, 0, :]), r=["colp"], w=["acol"])
    sy.op("act", lambda e: e.activation(out=ecol[:].rearrange("p i h -> p (i h)"), in_=colp[:, 1, :], func=AF.Exp,
                                        scale=-1.0), r=["colp"], w=["ecol"])
    qmc = [sb("qmc%d" % i, [64, 4, 128], st=st) for i in range(2)]
    kmc = [sb("kmc%d" % i, [64, 4, 128], st=st) for i in range(2)]
    kmt = [sb("kmt%d" % i, [128, 256], st=st) for i in range(2)]
    vm1 = [sb("vm1%d" % i, [128, 4, 129], st=st) for i in range(2)]
    for b in range(2):
        sy.op("pool", lambda e, b=b: e.memset(vm1[b][:], 1.0), w=["vm1%d" % b])

    def load_chunk(i):
        b = i % 2
        t0 = i * 128
        sy.dma("sp", "lq%d" % b, lambda e: e.dma_start(out=qmc[b][:], in_=qmT_s.ap()[:, :, t0:t0 + 128].rearrange("h p t -> p h t")),
               r=["qmT_s"], w=["qmc%d" % b])
        sy.dma("sp", "lk%d" % b, lambda e: e.dma_start(out=kmc[b][:], in_=kmT_s.ap()[:, :, t0:t0 + 128].rearrange("h p t -> p h t")),
               r=["kmT_s"], w=["kmc%d" % b])
        sy.dma("sp", "lt%d" % b, lambda e: e.dma_start(out=kmt[b][:], in_=km_s.ap()[t0:t0 + 128, :]), r=["km_s"], w=["kmt%d" % b])
        sy.dma("sp", "lv%d" % b, lambda e: e.dma_start(out=vm1[b][:, :, 0:128],
                                                       in_=vm_s.ap()[t0:t0 + 128, :].rearrange("p (h d) -> p h d", d=128)),
               r=["vm_s"], w=["vm1%d" % b])

    Cst = sb("Cst", [64, 4, 129], st=st)
    WtL = [sb("Wt%d" % i, [128, 128], st=st) for i in range(2)]
    WItL = [sb("WIt%d" % i, [128, 128], st=st) for i in range(2)]
    SttL = [sb("Stt%d" % i, [128, 128], st=st) for i in range(2)]
    qstL = [sb("qst%d" % i, [64, 128], st=st) for i in range(2)]
    kwtL = [sb("kwt%d" % i, [128, 64], st=st) for i in range(2)]
    hm = sb("hm", [128, 512], st=st)
    sogt = sb("sogt", [128, 512], st=st)
    smalL = [sb("smal%d" % i, [128, 16], st=st) for i in range(2)]
    st6L = [sb("st6%d" % i, [128, 6], st=st) for i in range(2)]
    scmL = [ps("scm%d" % i, [128, 128], st=st) for i in range(2)]
    ndpL = [ps("ndp%d" % i, [128, 129], st=st) for i in range(2)]
    uppL = [ps("upp%d" % i, [64, 129], st=st) for i in range(2)]
    PB = {"p": 0}
    mtp = ps("mtp", [128, 4, 128], st=st)
    mts = sb("mts", [128, 4, 128], st=st)

    def head_norm(src, npart, h, keys_r, emt, emt_keys):
        P = npart
        smal, st6 = smalL[PB["p"]], st6L[PB["p"]]
        SM, S6 = "smal%d" % PB["p"], "st6%d" % PB["p"]
        sy.op("dve", lambda e: e.tensor_scalar(out=smal[0:P, 9:10], in0=src[0:P, 128:129], scalar1=-1.0, scalar2=None,
                                               op0=ALU.mult), r=keys_r, w=[SM])
        sy.op("dve", lambda e: e.tensor_tensor(out=smal[0:P, 10:11], in0=src[0:P, 128:129], in1=smal[0:P, 9:10],
                                               op=ALU.max), r=keys_r + [SM], w=[SM])
        sy.op("dve", lambda e: e.tensor_scalar(out=smal[0:P, 0:1], in0=smal[0:P, 10:11], scalar1=emt, scalar2=None,
                                               op0=ALU.max), r=[SM] + emt_keys, w=[SM])
        sy.op("dve", lambda e: e.reciprocal(out=smal[0:P, 1:2], in_=smal[0:P, 0:1]), r=[SM], w=[SM])
        sy.op("dve", lambda e: e.bn_stats(out=st6[0:P, :], in_=src[0:P, 0:128]), r=keys_r, w=[S6])
        sy.op("dve", lambda e: e.bn_aggr(out=smal[0:P, 2:4], in_=st6[0:P, :]), r=[S6], w=[SM])
        sy.op("dve", lambda e: e.tensor_tensor(out=smal[0:P, 4:5], in0=smal[0:P, 1:2], in1=smal[0:P, 1:2], op=ALU.mult),
              r=[SM], w=[SM])
        sy.op("dve", lambda e: e.tensor_tensor(out=smal[0:P, 6:7], in0=smal[0:P, 4:5], in1=smal[0:P, 3:4], op=ALU.mult),
              r=[SM], w=[SM])
        sy.op("act", lambda e: e.activation(out=smal[0:P, 7:8], in_=smal[0:P, 6:7], func=AF.Ln, bias=EPS),
              r=[SM], w=[SM])
        sy.op("act", lambda e: e.activation(out=smal[0:P, 7:8], in_=smal[0:P, 7:8], func=AF.Exp, scale=-0.5),
              r=[SM], w=[SM])
        sy.op("dve", lambda e: e.tensor_tensor(out=smal[0:P, 5:6], in0=smal[0:P, 7:8], in1=smal[0:P, 1:2], op=ALU.mult),
              r=[SM], w=[SM])
        sy.op("dve", lambda e: e.tensor_scalar(out=hm[0:P, h * 128:(h + 1) * 128], in0=src[0:P, 0:128],
                                               scalar1=smal[0:P, 2:3], scalar2=smal[0:P, 5:6], op0=ALU.subtract,
                                               op1=ALU.mult), r=keys_r + [SM], w=["hm"])

    def gate_and_store(i):
        sy.dma("sp", "ld4", lambda e: e.dma_start(out=sogt[:], in_=sog_s.ap()[i * 128:(i + 1) * 128, :]),
               r=["sog_s"], w=["sogt"])
        sy.op("pool", lambda e: e.tensor_tensor(out=sogt[:], in0=sogt[:], in1=mhg[:], op=ALU.mult),
              r=["sogt", "mhg"], w=["sogt"])
        sy.op("dve", lambda e: e.tensor_tensor(out=hm[:], in0=hm[:], in1=sogt[:], op=ALU.mult), r=["hm", "sogt"], w=["hm"])
        for h in range(4):
            sy.mm(lambda e, h=h: e.transpose(out=mtp[:, h, :], in_=hm[:, h * 128:(h + 1) * 128], identity=ident),
                  r=["hm", "cst"], w=["mtp"], last=(h == 3))
        sy.op("act", lambda e: e.copy(out=mts[:], in_=mtp[:]), r=["mtp"], w=["mts"])
        sy.dma("sp", "st2", lambda e: e.dma_start(out=minT_s.ap()[:, :, i * 128:(i + 1) * 128].rearrange("h p t -> p h t"),
                                                  in_=mts[:]), r=["mts"], wa=["minT_s"])

    load_chunk(0)
    for i in range(NT):
        t0 = i * 128
        b = i % 2
        load_chunk(i + 1)
        qk_, kk_, tk_, vk_ = "qmc%d" % b, "kmc%d" % b, "kmt%d" % b, "vm1%d" % b
        for h in range(4):
            p_ = h % 2
            PB["p"] = p_
            Wt, WIt, Stt, qst, kwt = WtL[p_], WItL[p_], SttL[p_], qstL[p_], kwtL[p_]
            scm, ndp, upp = scmL[p_], ndpL[p_], uppL[p_]
            sy.op("act", lambda e, h=h: e.activation(out=Wt[:], in_=MB[:, h, t0:t0 + 128], func=AF.Exp,
                                                     bias=acol[:, i, h:h + 1], scale=-1.0), r=["MB", "acol"], w=["Wt%d" % p_])
            sy.op("pool", lambda e: e.tensor_tensor(out=Wt[:], in0=Wt[:], in1=maskD, op=ALU.mult), r=["Wt%d" % p_, "cst"], w=["Wt%d" % p_])
            if i > 0:
                sy.op("act", lambda e, h=h: e.activation(out=WIt[:], in_=MB[:, h, t0:t0 + 128], func=AF.Exp,
                                                         bias=MB[:, h, t0 - 1:t0], scale=-1.0), r=["MB"], w=["WIt%d" % p_])
                sy.op("dve", lambda e, h=h: e.tensor_tensor(out=qst[:], in0=qmc[b][:, h, :], in1=WIt[0:64, :],
                                                            op=ALU.mult), r=[qk_, "WIt%d" % p_], w=["qst%d" % p_])
            sy.mm(lambda e, h=h: e.matmul(scm[:], lhsT=kmc[b][:, h, :], rhs=qmc[b][:, h, :],
                                          start=True, stop=True), r=[kk_, qk_], w=["scm%d" % p_])
            sy.op("dve", lambda e: e.tensor_tensor(out=Stt[:], in0=scm[:], in1=Wt[:], op=ALU.mult), r=["scm%d" % p_, "Wt%d" % p_], w=["Stt%d" % p_])
            sy.mm(lambda e, h=h: e.matmul(ndp[:], lhsT=Stt[:], rhs=vm1[b][:, h, :], start=True, stop=(i == 0)),
                  r=["Stt%d" % p_, vk_], w=["ndp%d" % p_], last=(i == 0))
            if i > 0:
                sy.mm(lambda e, h=h: e.matmul(ndp[:], lhsT=qst[:], rhs=Cst[:, h, :], start=False, stop=True),
                      r=["qst%d" % p_, "Cst%d" % h], w=["ndp%d" % p_])
            head_norm(ndp, 128, h, ["ndp%d" % p_], ecol[:, i, h:h + 1], ["ecol"])
            sy.op("dve", lambda e, h=h: e.tensor_scalar(out=kwt[:], in0=kmt[b][:, h * 64:(h + 1) * 64],
                                                        scalar1=Wt[:, 127:128], scalar2=None, op0=ALU.mult),
                  r=[tk_, "Wt%d" % p_], w=["kwt%d" % p_])
            sy.mm(lambda e, h=h: e.matmul(upp[:], lhsT=kwt[:], rhs=vm1[b][:, h, :], start=True, stop=True),
                  r=["kwt%d" % p_, vk_], w=["upp%d" % p_])
            if i == 0:
                sy.op("act", lambda e, h=h: e.copy(out=Cst[:, h, :], in_=upp[:]), r=["upp%d" % p_], w=["Cst%d" % h])
            else:
                sy.op("dve", lambda e, h=h: e.scalar_tensor_tensor(out=Cst[:, h, :], in0=Cst[:, h, :],
                                                                   scalar=WIt[0:64, 127:128], in1=upp[:], op0=ALU.mult,
                                                                   op1=ALU.add), r=["Cst%d" % h, "WIt%d" % p_, "upp%d" % p_], w=["Cst%d" % h])
        gate_and_store(i)
    ck_ = ["Cst%d" % h for h in range(4)]
    sy.dma("sp", "st0", lambda e: e.dma_start(out=cp_o.ap().rearrange("h p d -> p h d"), in_=Cst[:, :, 0:128]),
           r=ck_, is_out=True)
    sy.dma("sp", "st1", lambda e: e.dma_start(out=np_o.ap().rearrange("h p -> p h"), in_=Cst[:, :, 128], allow_slow_non_contiguous=True),
           r=ck_, is_out=True)

    PB["p"] = 0
    scm, ndp, upp = scmL[0], ndpL[0], uppL[0]
    bS = NT % 2
    qS, kS, tS, vS = "qmc%d" % bS, "kmc%d" % bS, "kmt%d" % bS, "vm1%d" % bS
    C0 = sb("C0", [64, NSO * HM, 129], st=st)
    sy.dma("sp", "ld0", lambda e: e.dma_start(out=C0[:], in_=stc_d.ap()), w=["C0"])
    qms = sb("qms", [NSO, 256], st=st)
    sy.dma("sp", "ld1", lambda e: e.dma_start(out=qms[:], in_=qms_s.ap()[0:NSO, :]), r=["qms_s"], w=["qms"])
    sg = sb("sg", [NSO, 12, 4], st=st)
    sy.mm(lambda e: e.transpose(out=scm[0:NSO, 0:4], in_=igt[0:4, S:S + NSO], identity=ident[0:4, 0:4]),
          r=["igt", "cst"], w=["scm0"], last=False)
    sy.mm(lambda e: e.transpose(out=scm[0:NSO, 4:8], in_=lft[0:4, S:S + NSO], identity=ident[0:4, 0:4]),
          r=["lft", "cst"], w=["scm0"])
    sy.op("act", lambda e: e.copy(out=sg[:, 0:2, :].rearrange("p a b -> p (a b)"), in_=scm[0:NSO, 0:8]), r=["scm0"], w=["sg"])
    A = lambda k: sg[:, k, :]
    sy.op("dve", lambda e: e.tensor_tensor(out=A(8), in0=A(1), in1=stm[:], op=ALU.add), r=["sg", "stm"], w=["sg"])
    sy.op("dve", lambda e: e.tensor_tensor(out=A(2), in0=A(8), in1=A(0), op=ALU.max), r=["sg"], w=["sg"])
    sy.op("dve", lambda e: e.tensor_tensor(out=A(9), in0=A(8), in1=A(2), op=ALU.subtract), r=["sg"], w=["sg"])
    sy.op("act", lambda e: e.activation(out=A(3), in_=A(9), func=AF.Exp), r=["sg"], w=["sg"])
    sy.op("dve", lambda e: e.tensor_tensor(out=A(10), in0=A(0), in1=A(2), op=ALU.subtract), r=["sg"], w=["sg"])
    sy.op("act", lambda e: e.activation(out=A(4), in_=A(10), func=AF.Exp), r=["sg"], w=["sg"])
    sy.op("act", lambda e: e.activation(out=A(5), in_=A(2), func=AF.Exp, scale=-1.0), r=["sg"], w=["sg"])
    sy.dma("sp", "st2", lambda e: e.dma_start(out=ms_o.ap(), in_=A(2)), r=["sg"], is_out=True)
    qkt = sb("qkt", [NSO, 256], st=st)
    sy.op("dve", lambda e: e.tensor_tensor(out=qkt[:], in0=qms[:], in1=kmt[bS][0:NSO, :], op=ALU.mult),
          r=["qms", tS], w=["qkt"])
    sy.op("dve", lambda e: e.tensor_reduce(out=A(6), in_=qkt[:].rearrange("p (h d) -> p h d", d=64), axis=AX.X,
                                           op=ALU.add), r=["qkt"], w=["sg"])
    sy.op("dve", lambda e: e.tensor_tensor(out=A(7), in0=A(6), in1=A(4), op=ALU.mult), r=["sg"], w=["sg"])
    wdg = sb("wdg", [NSO, NSO, 4], st=st)
    sy.op("dve", lambda e: e.tensor_tensor(out=wdg[:], in0=ident[0:NSO, 0:NSO].unsqueeze(2).to_broadcast([NSO, NSO, 4]),
                                           in1=A(3).unsqueeze(1).to_broadcast([NSO, NSO, 4]), op=ALU.mult),
          r=["sg", "cst"], w=["wdg"])
    sy.mm(lambda e: e.matmul(scm[0:64, 0:NSO * 4], lhsT=ones[0:NSO, 0:64], rhs=wdg[:].rearrange("p a b -> p (a b)"), start=True,
                             stop=True), r=["cst", "wdg"], w=["scm0"])
    WB = sb("WB", [64, NSO * 4], st=st)
    sy.op("act", lambda e: e.copy(out=WB[:], in_=scm[0:64, 0:NSO * 4]), r=["scm0"], w=["WB"])
    Qd = sb("Qd", [64, 4, NSO, NSO], st=st)
    sy.op("dve", lambda e: e.tensor_tensor(out=Qd[:], in0=qmc[bS][:, :, 0:NSO].unsqueeze(2).to_broadcast([64, 4, NSO, NSO]),
                                           in1=i4[0:64, :, :].unsqueeze(1).to_broadcast([64, 4, NSO, NSO]), op=ALU.mult),
          r=[qS, "i4"], w=["Qd"])
    sy.op("pool", lambda e: e.memset(hm[:], 0.0), w=["hm"])
    nds = sb("nds", [NSO, 129], st=st)
    cj = sb("cj", [NSO, 1], st=st)
    kws = sb("kws", [NSO, 64], st=st)
    Cn = sb("Cn", [64, NSO * 4, 129], st=st)
    for h in range(4):
        for j in range(NSO):
            sy.mm(lambda e, h=h, j=j: e.matmul(ndp[0:NSO, :], lhsT=Qd[:, h, j, :], rhs=C0[:, j * 4 + h, :],
                                               start=(j == 0), stop=(j == NSO - 1)), r=["Qd", "C0"], w=["ndp0"],
                  last=(j == NSO - 1))
        sy.op("dve", lambda e, h=h: e.tensor_scalar(out=nds[:], in0=ndp[0:NSO, :], scalar1=sg[:, 3, h:h + 1], scalar2=None,
                                                    op0=ALU.mult), r=["ndp0", "sg"], w=["nds"])
        sy.op("dve", lambda e, h=h: e.scalar_tensor_tensor(out=nds[:], in0=vm1[bS][0:NSO, h, :], scalar=sg[:, 7, h:h + 1],
                                                           in1=nds[:], op0=ALU.mult, op1=ALU.add),
              r=[vS, "sg", "nds"], w=["nds"])
        head_norm(nds, NSO, h, ["nds"], sg[:, 5, h:h + 1], ["sg"])
        for j in range(NSO):
            sy.op("dve", lambda e, h=h, j=j: e.tensor_tensor(out=cj[:], in0=ident[0:NSO, j:j + 1], in1=sg[:, 4, h:h + 1],
                                                             op=ALU.mult), r=["cst", "sg"], w=["cj"])
            sy.op("dve", lambda e, h=h: e.tensor_scalar(out=kws[:], in0=kmt[bS][0:NSO, h * 64:(h + 1) * 64],
                                                        scalar1=cj[:, 0:1], scalar2=None, op0=ALU.mult),
                  r=[tS, "cj"], w=["kws"])
            sy.mm(lambda e, h=h: e.matmul(upp[:], lhsT=kws[:], rhs=vm1[bS][0:NSO, h, :], start=True, stop=True),
                  r=["kws", vS], w=["upp0"])
            jh = j * 4 + h
            sy.op("dve", lambda e, jh=jh: e.scalar_tensor_tensor(out=Cn[:, jh, :], in0=C0[:, jh, :],
                                                                 scalar=WB[:, jh:jh + 1], in1=upp[:], op0=ALU.mult,
                                                                 op1=ALU.add), r=["C0", "WB", "upp0"], w=["Cn"])
    gate_and_store(NT)
    sy.dma("sp", "st0", lambda e: e.dma_start(out=cs_o.ap().rearrange("a p d -> p a d"), in_=Cn[:, :, 0:128]),
           r=["Cn"], is_out=True)
    sy.dma("sp", "st1", lambda e: e.dma_start(out=ns_o.ap().rearrange("a p -> p a"), in_=Cn[:, :, 128], allow_slow_non_contiguous=True),
           r=["Cn"], is_out=True)
    st.close()
    if cfg.get('stop') == '3':
        sy.finish()
        es.close()
        return nc

    sy.barrier()
    st = ExitStack()
    wba = sb("wba", [128, 4, D], st=st)
    wbm = sb("wbm", [128, 4, D], st=st)
    wo = sb("wo", [128, KC, D], st=st)
    wr = sb("wr", [128, KC, E], st=st)
    sy.dma("pool", "ld0", lambda e: e.dma_start(out=r_(wba[:]), in_=(wba_d.ap())), w=["wba"])
    sy.dma("pool", "ld1", lambda e: e.dma_start(out=r_(wbm[:]), in_=(wbm_d.ap())), w=["wbm"])
    sy.dma("pool", "ld2", lambda e: e.dma_start(out=r_(wo[:]), in_=(wo_d.ap())), w=["wo"])
    sy.dma("sp", "ld3", lambda e: e.dma_start(out=wr[:], in_=wr_d.ap()), w=["wr"])
    ain = sb("ain", [128, 4, 512], st=st)
    mi_ = sb("mi_", [128, 4, 512], st=st)
    sgat = sb("sgat", [128, KC, 512], st=st)
    sgmt = sb("sgmt", [128, KC, 512], st=st)
    mg = sb("mg", [128, KC, 512], st=st)
    t1 = sb("t1", [128, 512], st=st)
    abp = [ps("abp%d" % i, [128, 512], st=st) for i in range(2)]
    mxp = [ps("mxp%d" % i, [128, 512], st=st) for i in range(2)]
    htp = [ps("htp%d" % i, [128, 4, 128], st=st) for i in range(2)]
    lgp = ps("lgp", [128, 2, E], st=st)
    xtk = sb("xtk", [128, D], st=st)
    pre2 = [sb("pre%d" % i, [128, D], st=st) for i in range(2)]
    hT = sb("hT", [128, KC, 128], st=st)
    st12 = sb("st12", [128, 2, 6], st=st)
    mv = sb("mv", [128, 4], st=st)
    lg = sb("lg", [128, E], st=st)
    top8 = sb("top8", [128, 8], st=st)
    rt_ = sb("rt_", [128, 8], st=st)
    selt = sb("selt", [128, E], st=st)
    cntb = sb("cntb", [128, E], st=st)
    slp = sb("slp", [128, E], st=st)
    tmpE = sb("tmpE", [128, E], st=st)
    slf = sb("slf", [128, 4], st=st)
    sy.op("pool", lambda e: e.memset(cntb[:], 0.0), w=["cntb"])
    bc_reg = nc.gpsimd.alloc_register("bc_reg")
    nc.gpsimd.reg_mov(bc_reg, E * CAP - 1)

    def layer_norm(src, dst, gi, keys):
        for hf in range(2):
            sy.op("dve", lambda e, hf=hf: e.bn_stats(out=st12[:, hf, :], in_=src[:, hf * 512:(hf + 1) * 512]),
                  r=keys, w=["st12"])
        sy.op("dve", lambda e: e.bn_aggr(out=mv[:, 0:2], in_=st12[:].rearrange("p a b -> p (a b)")), r=["st12"], w=["mv"])
        sy.op("act", lambda e: e.activation(out=mv[:, 2:3], in_=mv[:, 1:2], func=AF.Ln, bias=EPS), r=["mv"], w=["mv"])
        sy.op("act", lambda e: e.activation(out=mv[:, 2:3], in_=mv[:, 2:3], func=AF.Exp, scale=-0.5), r=["mv"], w=["mv"])
        sy.op("dve", lambda e: e.tensor_scalar(out=dst[:], in0=src[:], scalar1=mv[:, 0:1], scalar2=mv[:, 2:3],
                                               op0=ALU.subtract, op1=ALU.mult), r=keys + ["mv"], w=keys)
        sy.op("pool", lambda e: e.tensor_tensor(out=dst[:], in0=dst[:], in1=lnp[:, gi, :], op=ALU.mult),
              r=keys + ["lnp"], w=keys)
        sy.op("pool", lambda e: e.tensor_tensor(out=dst[:], in0=dst[:], in1=lnp[:, gi + 1, :], op=ALU.add),
              r=keys + ["lnp"], w=keys)

    for (g0, gn) in tgroups:
        for (dst_t, src_s, nm, nch) in ((ain, ainT_s, "ain", 4), (mi_, minT_s, "mi_", 4), (sgat, sga_s, "sgat", KC),
                                        (sgmt, sgm_s, "sgmt", KC)):
            sy.dma("pool", "l" + nm, lambda e, dst_t=dst_t, src_s=src_s: e.dma_start(
                out=r_(dst_t[:, :, 0:gn]), in_=(src_s.ap()[:, :, g0:g0 + gn].rearrange("h p t -> p h t"))),
                r=[src_s.name], w=[nm])
        for dc in range(KC):
            pa, pm = abp[dc % 2], mxp[dc % 2]
            for h in range(4):
                sy.mm(lambda e, h=h, dc=dc: e.matmul(pa[:, 0:gn], lhsT=r_(wba[:, h, dc * 128:(dc + 1) * 128]),
                                                     rhs=r_(ain[:, h, 0:gn]), start=(h == 0), stop=(h == 3)),
                      r=["wba", "ain"], w=["abp%d" % (dc % 2)], last=(h == 3))
            for h in range(4):
                sy.mm(lambda e, h=h, dc=dc: e.matmul(pm[:, 0:gn], lhsT=r_(wbm[:, h, dc * 128:(dc + 1) * 128]),
                                                     rhs=r_(mi_[:, h, 0:gn]), start=(h == 0), stop=(h == 3)),
                      r=["wbm", "mi_"], w=["mxp%d" % (dc % 2)], last=(h == 3))
            sy.op("dve", lambda e, dc=dc: e.tensor_tensor(out=t1[:, 0:gn], in0=pa[:, 0:gn], in1=sgat[:, dc, 0:gn],
                                                          op=ALU.mult), r=["abp%d" % (dc % 2), "sgat"], w=["t1"])
            sy.op("dve", lambda e, dc=dc: e.tensor_tensor(out=r_(mg[:, dc, 0:gn]), in0=pm[:, 0:gn], in1=sgmt[:, dc, 0:gn],
                                                          op=ALU.mult), r=["mxp%d" % (dc % 2), "sgmt"], w=["mg%d" % dc])
            sy.op("pool", lambda e, dc=dc: e.tensor_tensor(out=r_(mg[:, dc, 0:gn]), in0=mg[:, dc, 0:gn], in1=t1[:, 0:gn],
                                                           op=ALU.add), r=["mg%d" % dc, "t1"], w=["mg%d" % dc])
        mgk = ["mg%d" % dc for dc in range(KC)]
        for tl in range(gn // 128):
            i = (g0 // 128) + tl
            pre = pre2[i % 2]
            pk = "pre%d" % (i % 2)
            sy.dma("sp", "lx", lambda e, i=i: e.dma_start(out=xtk[:], in_=xtok_d.ap()[i * 128:(i + 1) * 128, :]), w=["xtk"])
            for hf in range(2):
                for dc in range(KC):
                    sy.mm(lambda e, dc=dc, hf=hf: e.matmul(abp[hf][:], lhsT=r_(mg[:, dc, tl * 128:(tl + 1) * 128]),
                                                           rhs=r_(wo[:, dc, hf * 512:(hf + 1) * 512]), start=(dc == 0),
                                                           stop=(dc == KC - 1)), r=mgk + ["wo"], w=["abp%d" % hf],
                          last=(dc == KC - 1))
                sy.op("dve", lambda e, hf=hf: e.scalar_tensor_tensor(out=pre[:, hf * 512:(hf + 1) * 512],
                                                                     in0=xtk[:, hf * 512:(hf + 1) * 512], scalar=ALPHA,
                                                                     in1=abp[hf][:], op0=ALU.mult, op1=ALU.add),
                      r=["xtk", "abp%d" % hf], w=[pk])
            layer_norm(pre, pre, 0, [pk])
            sy.dma("sp", "sh", lambda e, i=i: e.dma_start(out=hs_s.ap()[i * 128:(i + 1) * 128, :], in_=pre[:]),
                   r=[pk], wa=["hs_s"])
            for q4 in range(2):
                for k4 in range(4):
                    kc = q4 * 4 + k4
                    sy.mm(lambda e, kc=kc, k4=k4, q4=q4: e.transpose(out=htp[q4][:, k4, :],
                                                                    in_=pre[:, kc * 128:(kc + 1) * 128], identity=ident),
                          r=[pk, "cst"], w=["htp%d" % q4], last=(k4 == 3))
                sy.op("act", lambda e, q4=q4: e.copy(out=hT[:, q4 * 4:(q4 + 1) * 4, :], in_=htp[q4][:]),
                      r=["htp%d" % q4], w=["hT"])
            for kc in range(KC):
                sy.mm(lambda e, kc=kc: e.matmul(lgp[:, 0, :], lhsT=hT[:, kc, :], rhs=wr[:, kc, :], start=(kc == 0),
                                                stop=(kc == KC - 1)), r=["hT", "wr"], w=["lgp0"], last=(kc == KC - 1))
            sy.op("dve", lambda e: e.tensor_tensor(out=lg[:], in0=lgp[:, 0, :], in1=brb[:], op=ALU.add),
                  r=["lgp0", "brb"], w=["lg"])
            sy.op("dve", lambda e: e.max(out=top8[:], in_=lg[:]), r=["lg"], w=["top8"])
            sy.op("dve", lambda e: e.tensor_scalar(out=rt_[:, 0:1], in0=top8[:, 0:1], scalar1=-1.0, scalar2=None,
                                                   op0=ALU.mult), r=["top8"], w=["rt_"])
            sy.op("act", lambda e: e.activation(out=rt_[:, 4:8], in_=top8[:, 0:4], func=AF.Exp, bias=rt_[:, 0:1]),
                  r=["top8", "rt_"], w=["rt_"])
            sy.op("dve", lambda e: e.tensor_reduce(out=rt_[:, 1:2], in_=rt_[:, 4:8], axis=AX.X, op=ALU.add),
                  r=["rt_"], w=["rt_"])
            sy.op("dve", lambda e: e.reciprocal(out=rt_[:, 2:3], in_=rt_[:, 1:2]), r=["rt_"], w=["rt_"])
            sy.op("dve", lambda e, i=i: e.tensor_scalar(out=wts[:, i, :], in0=rt_[:, 4:8], scalar1=rt_[:, 2:3],
                                                        scalar2=None, op0=ALU.mult), r=["rt_"], w=["wts"])
            sy.op("dve", lambda e: e.tensor_scalar(out=selt[:], in0=lg[:], scalar1=top8[:, 3:4], scalar2=None,
                                                   op0=ALU.is_ge), r=["lg", "top8"], w=["selt"])
            if i == NT:
                sy.op("dve", lambda e: e.tensor_scalar(out=selt[:], in0=selt[:], scalar1=rowm, scalar2=None,
                                                       op0=ALU.mult), r=["selt", "cst"], w=["selt"])
            sy.mm(lambda e: e.matmul(lgp[:, 1, :], lhsT=ltri, rhs=selt[:], start=True, stop=True),
                  r=["cst", "selt"], w=["lgp1"])
            sy.op("dve", lambda e: e.tensor_tensor(out=slp[:], in0=lgp[:, 1, :], in1=cntb[:], op=ALU.add),
                  r=["lgp1", "cntb"], w=["slp"])
            sy.op("dve", lambda e: e.tensor_scalar(out=tmpE[:], in0=slp[:], scalar1=float(CAP) - 0.5, scalar2=BIG,
                                                   op0=ALU.is_ge, op1=ALU.mult), r=["slp"], w=["tmpE"])
            sy.op("dve", lambda e: e.tensor_tensor(out=slp[:], in0=slp[:], in1=tmpE[:], op=ALU.add),
                  r=["slp", "tmpE"], w=["slp"])
            sy.op("dve", lambda e: e.tensor_scalar(out=tmpE[:], in0=selt[:], scalar1=-BIG, scalar2=BIG, op0=ALU.mult,
                                                   op1=ALU.add), r=["selt"], w=["tmpE"])
            sy.op("dve", lambda e: e.tensor_tensor(out=slp[:], in0=slp[:], in1=tmpE[:], op=ALU.add),
                  r=["slp", "tmpE"], w=["slp"])
            sy.op("dve", lambda e: e.tensor_tensor(out=slp[:], in0=slp[:], in1=ecap[:], op=ALU.add),
                  r=["slp", "ecap"], w=["slp"])
            sy.mm(lambda e: e.matmul(lgp[:, 1, :], lhsT=ones, rhs=selt[:], start=True, stop=True),
                  r=["cst", "selt", "slp"], w=["lgp1"])
            sy.op("dve", lambda e: e.tensor_tensor(out=cntb[:], in0=cntb[:], in1=lgp[:, 1, :], op=ALU.add),
                  r=["cntb", "lgp1"], w=["cntb"])
            for j in range(4):
                sy.op("dve", lambda e, j=j: e.tensor_scalar(out=tmpE[:], in0=lg[:], scalar1=top8[:, j:j + 1], scalar2=None,
                                                            op0=ALU.is_equal), r=["lg", "top8"], w=["tmpE"])
                sy.op("dve", lambda e: e.tensor_tensor(out=tmpE[:], in0=tmpE[:], in1=slp[:], op=ALU.mult),
                      r=["tmpE", "slp"], w=["tmpE"])
                sy.op("dve", lambda e, j=j: e.tensor_reduce(out=slf[:, j:j + 1], in_=tmpE[:], axis=AX.X, op=ALU.add),
                      r=["tmpE"], w=["slf"])
            sy.op("dve", lambda e: e.tensor_scalar(out=slf[:], in0=slf[:], scalar1=float(E * CAP + 64), scalar2=None,
                                                   op0=ALU.min), r=["slf"], w=["slf"])
            sy.op("dve", lambda e, i=i: e.tensor_copy(out=sli[:, i, :], in_=slf[:]), r=["slf"], w=["sli"])
            for j in range(4):
                sy.dma("pool", "scat%d" % j, lambda e, i=i, j=j: e.indirect_dma_start(
                    out=xg_s.ap(), out_offset=bass.IndirectOffsetOnAxis(ap=sli[:, i, j:j + 1], axis=0), in_=pre[:],
                    in_offset=None, bounds_check=bc_reg, oob_is_err=False), r=[pk, "sli", "xg_zero"], wa=["xg_s"])
    st.close()
    if cfg.get('stop') == '4':
        sy.finish()
        es.close()
        return nc

    sy.barrier()
    st = ExitStack()
    NWB = 6
    wbuf = [sb("wbuf%d" % i, [128, KC, 512], BF16, st=st) for i in range(NWB)]
    xe2 = [sb("xe%d" % i, [128, CT, D], st=st) for i in range(2)]
    xeT2 = [sb("xeT%d" % i, [128, KC, CAP], BF16, st=st) for i in range(2)]
    hid = sb("hid", [128, FC, CAP], BF16, st=st)
    gt2 = [sb("gt%d" % i, [128, CAP], st=st) for i in range(2)]
    ut2 = [sb("ut%d" % i, [128, CAP], st=st) for i in range(2)]
    sgt2 = [sb("sgt%d" % i, [128, CAP], st=st) for i in range(2)]
    bdb = sb("bdb", [128, D], st=st)
    yst = [sb("yst%d" % i, [128, D], st=st) for i in range(2)]
    ep = [ps("ep%d" % i, [128, 512], st=st) for i in range(6)]
    tpp = [ps("tpp%d" % i, [128, 4, 128], st=st) for i in range(2)]
    wi = 0
    pi_ = 0
    ti_ = 0
    yi_ = 0
    assert FF % 512 == 0 or FF < 512
    FH = max(1, FF // 512)
    FW = min(512, FF)
    for e_ in range(E):
        xe, xeT = xe2[e_ % 2], xeT2[e_ % 2]
        xek, xetk = "xe%d" % (e_ % 2), "xeT%d" % (e_ % 2)
        sy.dma("sp", "lxe%d" % (e_ % 2), lambda e, e_=e_, xe=xe: e.dma_start(
            out=xe[:], in_=xg_s.ap()[e_ * CAP:(e_ + 1) * CAP, :].rearrange("(a p) d -> p a d", p=128)),
            r=["xg_s", "xg_zero"], w=[xek])
        sy.dma("pool", "lbd", lambda e, e_=e_: e.dma_start(out=bdb[:], in_=bd_d.ap()[e_].partition_broadcast(128)), w=["bdb"])
        for a in range(CT):
            for q4 in range(KC // 4):
                tb = ti_ % 2
                ti_ += 1
                for k4 in range(4):
                    kc = q4 * 4 + k4
                    sy.mm(lambda e, a=a, kc=kc, k4=k4, tb=tb, xe=xe: e.transpose(out=tpp[tb][:, k4, :],
                                                                                 in_=xe[:, a, kc * 128:(kc + 1) * 128],
                                                                                 identity=ident),
                          r=[xek, "cst"], w=["tpp%d" % tb], last=(k4 == 3))
                sy.op("act", lambda e, a=a, q4=q4, tb=tb, xeT=xeT: e.copy(out=xeT[:, q4 * 4:(q4 + 1) * 4, a * 128:(a + 1) * 128],
                                                                          in_=tpp[tb][:]), r=["tpp%d" % tb], w=[xetk])
        for fh in range(FH):
            wg_b = wi % NWB
            wi += 1
            wu_b = wi % NWB
            wi += 1
            sy.dma("pool", "lw%d" % wg_b, lambda e, e_=e_, fh=fh, wg_b=wg_b: e.dma_start(
                out=wbuf[wg_b][:, :, 0:FW], in_=(wg_d.ap()[e_, :, :, fh * FW:(fh + 1) * FW])), w=["wbuf%d" % wg_b])
            sy.dma("pool", "lw%d" % wu_b, lambda e, e_=e_, fh=fh, wu_b=wu_b: e.dma_start(
                out=wbuf[wu_b][:, :, 0:FW], in_=(wu_d.ap()[e_, :, :, fh * FW:(fh + 1) * FW])), w=["wbuf%d" % wu_b])
            for f4 in range(FW // 128):
                fc = fh * (FW // 128) + f4
                gt, ut, sgt = gt2[fc % 2], ut2[fc % 2], sgt2[fc % 2]
                gk, uk, sk = "gt%d" % (fc % 2), "ut%d" % (fc % 2), "sgt%d" % (fc % 2)
                pg = pi_ % 6
                pi_ += 1
                pu = pi_ % 6
                pi_ += 1
                for kc in range(KC):
                    sy.mm(lambda e, kc=kc, f4=f4, pg=pg, wg_b=wg_b, xeT=xeT: e.matmul(
                        ep[pg][:, 0:CAP], lhsT=wbuf[wg_b][:, kc, f4 * 128:(f4 + 1) * 128], rhs=xeT[:, kc, :],
                        start=(kc == 0), stop=(kc == KC - 1)), r=["wbuf%d" % wg_b, xetk], w=["ep%d" % pg],
                        last=(kc == KC - 1))
                for kc in range(KC):
                    sy.mm(lambda e, kc=kc, f4=f4, pu=pu, wu_b=wu_b, xeT=xeT: e.matmul(
                        ep[pu][:, 0:CAP], lhsT=wbuf[wu_b][:, kc, f4 * 128:(f4 + 1) * 128], rhs=xeT[:, kc, :],
                        start=(kc == 0), stop=(kc == KC - 1)), r=["wbuf%d" % wu_b, xetk], w=["ep%d" % pu],
                        last=(kc == KC - 1))
                sy.op("dve", lambda e, pg=pg, fc=fc, e_=e_, gt=gt: e.tensor_scalar(
                    out=gt[:], in0=ep[pg][:, 0:CAP], scalar1=bgu[:, 0, e_, fc:fc + 1], scalar2=LIMIT, op0=ALU.add,
                    op1=ALU.min), r=["ep%d" % pg, "bgu"], w=[gk])
                sy.op("act", lambda e, gt=gt, sgt=sgt: e.activation(out=sgt[:], in_=gt[:], func=AF.Sigmoid, scale=SALPHA),
                      r=[gk], w=[sk])
                sy.op("dve", lambda e, pu=pu, fc=fc, e_=e_, ut=ut: e.tensor_scalar(
                    out=ut[:], in0=ep[pu][:, 0:CAP], scalar1=bu1[:, e_, fc:fc + 1], scalar2=LIMIT + 1.0, op0=ALU.add,
                    op1=ALU.min), r=["ep%d" % pu, "bu1"], w=[uk])
                sy.op("dve", lambda e, gt=gt, sgt=sgt: e.tensor_tensor(out=gt[:], in0=gt[:], in1=sgt[:], op=ALU.mult),
                      r=[gk, sk], w=[gk])
                sy.op("dve", lambda e, fc=fc, gt=gt, ut=ut: e.scalar_tensor_tensor(out=hid[:, fc, :], in0=ut[:],
                                                                                   scalar=1.0 - LIMIT, in1=gt[:],
                                                                                   op0=ALU.max, op1=ALU.mult),
                      r=[gk, uk], w=["hid%d" % fc])
        hk = ["hid%d" % fc for fc in range(FC)]
        wds = []
        for dh in range(D // 512):
            wd_b = wi % NWB
            wi += 1
            wds.append(wd_b)
            sy.dma("pool", "lw%d" % wd_b, lambda e, e_=e_, dh=dh, wd_b=wd_b: e.dma_start(
                out=wbuf[wd_b][:, 0:FC, :], in_=(wd_d.ap()[e_, :, :, dh * 512:(dh + 1) * 512])), w=["wbuf%d" % wd_b])
            if dh == 0 and NWB < 4:
                pass
        for a in range(CT):
            yb = yi_ % 2
            yi_ += 1
            for dh in range(D // 512):
                pd = pi_ % 6
                pi_ += 1
                for fc in range(FC):
                    sy.mm(lambda e, fc=fc, a=a, dh=dh, pd=pd: e.matmul(
                        ep[pd][:], lhsT=hid[:, fc, a * 128:(a + 1) * 128], rhs=wbuf[wds[dh]][:, fc, :],
                        start=(fc == 0), stop=(fc == FC - 1)), r=hk + ["wbuf%d" % wds[dh]], w=["ep%d" % pd],
                        last=(fc == FC - 1))
                sy.op("dve", lambda e, dh=dh, pd=pd, yb=yb: e.tensor_tensor(
                    out=yst[yb][:, dh * 512:(dh + 1) * 512], in0=ep[pd][:], in1=bdb[:, dh * 512:(dh + 1) * 512],
                    op=ALU.add), r=["ep%d" % pd, "bdb"], w=["yst%d" % yb])
            sy.dma("sp", "sy%d" % yb, lambda e, e_=e_, a=a, yb=yb: e.dma_start(
                out=yx_s.ap()[e_ * CAP + a * 128:e_ * CAP + (a + 1) * 128, :], in_=yst[yb][:]),
                r=["yst%d" % yb], wa=["yx_s"])
    st.close()
    if cfg.get('stop') == '5':
        sy.finish()
        es.close()
        return nc

    sy.barrier()
    st = ExitStack()
    yg = [sb("yg%d" % i, [128, 4, D], st=st) for i in range(2)]
    hb = [sb("hb%d" % i, [128, D], st=st) for i in range(2)]
    st12 = sb("st12b", [128, 2, 6], st=st)
    mv = sb("mvb", [128, 4], st=st)
    for b in range(2):
        sy.op("pool", lambda e, b=b: e.memset(yg[b][:], 0.0), w=["yg%d" % b])
    for i in range(NTT):
        b = i % 2
        for j in range(4):
            sy.dma("pool", "gat%d_%d" % (b, j), lambda e, i=i, j=j, b=b: e.indirect_dma_start(
                out=yg[b][:, j, :], out_offset=None, in_=yx_s.ap(),
                in_offset=bass.IndirectOffsetOnAxis(ap=sli[:, i, j:j + 1], axis=0), bounds_check=bc_reg,
                oob_is_err=False), r=["yx_s", "sli"], w=["yg%d" % b])
        sy.dma("sp", "lh%d" % b, lambda e, i=i, b=b: e.dma_start(out=hb[b][:], in_=hs_s.ap()[i * 128:(i + 1) * 128, :]),
               r=["hs_s"], w=["hb%d" % b])
        sy.op("dve", lambda e, b=b: e.tensor_scalar(out=hb[b][:], in0=hb[b][:], scalar1=ALPHA, scalar2=None, op0=ALU.mult),
              r=["hb%d" % b], w=["hb%d" % b])
        for j in range(4):
            sy.op("dve", lambda e, b=b, j=j, i=i: e.scalar_tensor_tensor(out=hb[b][:], in0=yg[b][:, j, :],
                                                                         scalar=wts[:, i, j:j + 1], in1=hb[b][:],
                                                                         op0=ALU.mult, op1=ALU.add),
                  r=["yg%d" % b, "wts", "hb%d" % b], w=["hb%d" % b])
        layer_norm(hb[b], hb[b], 2, ["hb%d" % b])
        if i < NT:
            sy.dma("sp", "so%d" % b, lambda e, i=i, b=b: e.dma_start(out=y_o.ap()[i * 128:(i + 1) * 128, :], in_=hb[b][:]),
                   r=["hb%d" % b], is_out=True)
        else:
            sy.dma("sp", "so%d" % b, lambda e, b=b: e.dma_start(out=ys_o.ap(), in_=hb[b][0:NSO, :]),
                   r=["hb%d" % b], is_out=True)
    sy.finish()
    st.close()
    es.close()
    return nc


def consts(cfg):
    S, PAST, E, CAP = cfg["S"], cfg["PAST"], cfg["E"], cfg["CAP"]
    NT = S // 128
    c = np.zeros((128, 1024), np.float32)
    c[:, 0:128] = np.eye(128, dtype=np.float32)
    c[:, 128:256] = 1.0
    p = np.arange(128)
    c[:, 256:384] = (p[:, None] < p[None, :]).astype(np.float32)
    c[:, 384:512] = (p[:, None] <= p[None, :]).astype(np.float32)
    c[:NSO, 512] = 1.0
    c[:PAST // 128, 513] = 1.0
    q = np.arange(512)
    cm = np.zeros((128, 4, 512), np.float32)
    for off in range(4):
        cm[:, off, :] = (q[None, :] >= off * 128 + p[:, None]).astype(np.float32)
    inv = (np.float32(THETA) ** (-np.arange(0, ROT, 2, dtype=np.float32) / np.float32(ROT))).astype(np.float32)
    pos = np.concatenate([np.arange(S), np.full(128, PAST)]).astype(np.float32)
    ang = pos[:, None] * inv[None, :]
    tab = np.concatenate([np.cos(ang), np.sin(ang)], axis=1).astype(np.float32)
    rope = np.ascontiguousarray(tab.reshape(NT + 1, 128, 16).transpose(1, 0, 2))
    ropes = np.ascontiguousarray(np.broadcast_to(tab[S], (128, 16))).astype(np.float32)
    ecap = np.ascontiguousarray(np.broadcast_to((np.arange(E) * CAP).astype(np.float32), (128, E)))
    i4 = np.ascontiguousarray(np.broadcast_to(np.eye(4, dtype=np.float32), (128, 4, 4)))
    return dict(cst=c, cm=cm, rope=rope, ropes=ropes, ecap=ecap, i4=i4)


def prep(cfg, inp):
    S, PAST, NPOOL, E, FF, CAP, D = (cfg[k] for k in ("S", "PAST", "NPOOL", "E", "FF", "CAP", "D"))
    KC, FC = D // 128, FF // 128
    T = S + 128
    f = lambda a: np.ascontiguousarray(np.asarray(a, dtype=np.float32))
    cs = consts(cfg)
    w_in = np.asarray(inp["w_in"][0], np.float32)
    win = f(w_in.reshape(KC, 128, -1).transpose(1, 0, 2))
    shared = dict(
        win=win,
        bigate=f(np.concatenate([inp["b_igate"][0], inp["b_fgate"][0]]).reshape(8, 1)),
        lamp=f(np.stack([inp["lambda_q1"][0], inp["lambda_k1"][0], inp["lambda_q2"][0], inp["lambda_k2"][0]])),
        subg=f(np.asarray(inp["subln_g"][0]).reshape(128, 1)),
        mhg=f(inp["mh_norm_g"][0]),
        wba=f(np.asarray(inp["w_ba"][0]).reshape(4, 128, D).transpose(1, 0, 2)),
        wbm=f(np.asarray(inp["w_bm"][0]).reshape(4, 128, D).transpose(1, 0, 2)),
        wo=f(np.asarray(inp["w_o"][0]).reshape(KC, 128, D).transpose(1, 0, 2)),
        lnp=f(np.stack([inp["ln1_g"][0], inp["ln1_b"][0], inp["ln2_g"][0], inp["ln2_b"][0]])),
        wr=f(np.asarray(inp["w_router"][0]).reshape(KC, 128, E).transpose(1, 0, 2)),
        br=f(inp["b_router"][0]),
        wg=f(np.asarray(inp["w_gate"][0]).reshape(E, KC, 128, FF).transpose(0, 2, 1, 3)),
        wu=f(np.asarray(inp["w_up"][0]).reshape(E, KC, 128, FF).transpose(0, 2, 1, 3)),
        wd=f(np.asarray(inp["w_down"][0]).reshape(E, FC, 128, D).transpose(0, 2, 1, 3)),
        bgu=f(np.stack([np.asarray(inp["b_gate"][0]).reshape(E, FC, 128), np.asarray(inp["b_up"][0]).reshape(E, FC, 128)])
              .transpose(3, 0, 1, 2)),
        bd=f(inp["b_down"][0]),
        **cs,
    )
    xp = np.asarray(inp["x_prompt"], np.float32)
    xs = np.asarray(inp["x_sample"], np.float32)[:, 0, :]
    ck = np.asarray(inp["cache_k"][0])
    cv = np.asarray(inp["cache_v"][0])
    pt = np.asarray(inp["page_table"]).astype(np.int32)
    ckh, cvh = {}, {}
    for h in range(HA):
        ckh[h] = np.ascontiguousarray(ck[:, :, h, :].reshape(NPOOL, 128 // 32, 32 * 128).transpose(1, 0, 2))
        cvh[h] = np.ascontiguousarray(cv[:, :, h, :].reshape(NPOOL, 128 // 32, 32 * 128).transpose(1, 0, 2))
    cols = {}
    for h in range(HA):
        cols[h] = np.concatenate([w_in[:, h * 128:(h + 1) * 128], w_in[:, 512 + h * 128:512 + (h + 1) * 128],
                                  w_in[:, 1024 + h * 128:1024 + (h + 1) * 128]], axis=1)
    whs_all = f(np.stack([cols[hh].reshape(KC, 128, 384).transpose(1, 0, 2) for hh in range(4)], axis=1))
    maps = []
    for c in range(8):
        h, g = c % 4, c // 4
        xt = np.zeros((T, D), np.float32)
        xt[:S] = xp[c]
        xt[S:S + NSO] = xs[4 * c:4 * c + 4]
        xT = f(xt.T.reshape(KC, 128, T).transpose(1, 0, 2))
        xo = xs[4 * c:4 * c + 4]
        xgT = np.zeros((128, 4, KC, NSG), np.float32)
        xoT = xo.T.reshape(KC, 128, NSO).transpose(1, 0, 2)
        for hh in range(4):
            xgT[:, hh, :, hh * 4:hh * 4 + 4] = xoT
        ptg = np.zeros((128, NSG), np.int32)
        for hh in range(4):
            ptg[:pt.shape[1], hh * 4:hh * 4 + 4] = pt[4 * c:4 * c + 4].T
        sel = np.zeros((128, 16), np.float32)
        for hh in range(4):
            for j in range(4):
                rank = g * 4 + hh
                sel[rank * 16 + (c % 4) * 4 + j, hh * 4 + j] = 1.0
        sc = np.asarray(inp["state_c"][0][4 * c:4 * c + 4], np.float32)
        sn = np.asarray(inp["state_n"][0][4 * c:4 * c + 4], np.float32)
        stc = np.concatenate([sc, sn[..., None]], axis=-1).reshape(NSO * HM, 64, 129).transpose(1, 0, 2)
        m = dict(shared)
        m.update(xT=xT, xtok=f(xt), xgT=xgT, whs=whs_all,
                 pt=ptg, selm=sel, stc=f(stc),
                 stm=f(inp["state_m"][0][4 * c:4 * c + 4]))
        for hh in range(4):
            for i in range(4):
                m["ck%d_%d" % (hh, i)] = ckh[hh][i]
                m["cv%d_%d" % (hh, i)] = cvh[hh][i]
        maps.append(m)
    return maps


def assemble(cfg, res):
    S, D = cfg["S"], cfg["D"]
    g = lambda n: np.stack([np.asarray(r[n]) for r in res])
    y_p = g("y")
    y_s = g("ysamp").reshape(32, 1, D)
    k_p = g("k").reshape(1, 8, S, HA, 128)
    v_p = g("v").reshape(1, 8, S, HA, 128)
    c_p = g("cp").reshape(1, 8, HM, 64, 128)
    n_p = g("npr").reshape(1, 8, HM, 64)
    m_p = g("mp").reshape(1, 8, HM)
    k_s = g("ksamp").reshape(1, 32, 1, HA, 128)
    v_s = g("vsamp").reshape(1, 32, 1, HA, 128)
    c_s = g("csamp").reshape(1, 32, HM, 64, 128)
    n_s = g("nsamp").reshape(1, 32, HM, 64)
    m_s = g("msamp").reshape(1, 32, HM)
    return tuple(np.ascontiguousarray(a, dtype=np.float32) for a in
                 (y_p, y_s, k_p, v_p, c_p, n_p, m_p, k_s, v_s, c_s, n_s, m_s))


def run(cfg, inp):
    nc = build(cfg)
    maps = prep(cfg, inp)
    res = run_bass_kernel_spmd(nc, maps, core_ids=list(range(8)))
    return assemble(cfg, res.results)


def kernel(**inputs):
    return run(FULL, inputs)
```

```python
import math
from contextlib import ExitStack

import numpy as np
import concourse.bass as bass
import concourse.mybir as mybir
from concourse.bass_utils import run_bass_kernel_spmd

F32 = mybir.dt.float32
F32R = mybir.dt.float32r
BF16 = mybir.dt.bfloat16
I32 = mybir.dt.int32
ALU = mybir.AluOpType
AF = mybir.ActivationFunctionType
AX = mybir.AxisListType

FULL = dict(S=2048, PAST=16384, NPOOL=5120, E=32, FF=1024, CAP=512, D=1024)

HA, DH = 4, 64
HM, DK, DV = 4, 64, 128
ROT = 16
THETA = 500000.0
SOFTCAP = 15.0
LIMIT = 7.0
SALPHA = 1.702
EPS = 1e-5
DEPTH = 1
ALPHA = (2.0 * DEPTH) ** 0.25
LAM_INIT = 0.8 - 0.6 * math.exp(-0.3 * 0)
NSO = 4
NSG = 16
BIG = 1.0e6


def r_(ap):
    return ap.bitcast(F32R)


class Sync:
    def __init__(self, nc, es):
        self.nc = nc
        self.es = es
        self.engs = {"pe": nc.tensor, "act": nc.scalar, "dve": nc.vector, "pool": nc.gpsimd, "sp": nc.sync}
        self.sem = {}
        self.cnt = {}
        for n in ("pe", "act", "dve", "pool"):
            self.sem[n] = es.enter_context(nc.semaphore("s_" + n))
            self.cnt[n] = 0
        self.seen = {n: {} for n in self.engs}
        self.last_w = {}
        self.readers = {}
        self.dsem = {}
        self.dcnt = {}
        self.out_tokens = []
        self.extra_tokens = []

    def _deps(self, r, w):
        deps = []
        for k in r:
            deps += self.last_w.get(k, [])
        for k in w:
            deps += self.readers.get(k, [])
            deps += self.last_w.get(k, [])
        return deps

    def barrier(self):
        toks = [("s_" + n, self.sem[n], self.cnt[n]) for n in self.sem if self.cnt[n] > 0]
        toks += [("d_" + k, self.dsem[k], self.dcnt[k]) for k in self.dsem if self.dcnt[k] > 0]
        toks += list(self.extra_tokens)
        for eng in self.engs:
            self._wait(eng, toks)

    def _wait(self, eng, deps):
        best = {}
        for (sn, sem, val) in deps:
            if best.get(sn, (None, 0))[1] < val:
                best[sn] = (sem, val)
        for sn, (sem, val) in best.items():
            if self.seen[eng].get(sn, 0) < val:
                self.engs[eng].wait_ge(sem, val)
                self.seen[eng][sn] = val

    def _record(self, tok, r, w, wa=()):
        for k in w:
            self.last_w[k] = [tok]
            self.readers[k] = []
        for k in wa:
            self.last_w.setdefault(k, []).append(tok)
        for k in r:
            if k not in w:
                self.readers.setdefault(k, []).append(tok)

    def op(self, eng, fn, r=(), w=()):
        self._wait(eng, self._deps(r, w))
        ins = fn(self.engs[eng])
        self.cnt[eng] += 1
        ins.then_inc(self.sem[eng], 1)
        tok = ("s_" + eng, self.sem[eng], self.cnt[eng])
        self._record(tok, r, w)
        return tok

    def mm(self, fn, r=(), w=(), last=True):
        self._wait("pe", self._deps(r, w))
        ins = fn(self.engs["pe"])
        if last:
            self.cnt["pe"] += 1
            ins.then_inc(self.sem["pe"], 1)
            tok = ("s_pe", self.sem["pe"], self.cnt["pe"])
            pr, pw = getattr(self, "_pend", ([], []))
            self._record(tok, list(r) + pr, list(w) + pw)
            self._pend = ([], [])
        else:
            pr, pw = getattr(self, "_pend", ([], []))
            self._pend = (pr + list(r), pw + list(w))

    def dma(self, q, key, fn, r=(), w=(), wa=(), is_out=False):
        if key not in self.dsem:
            self.dsem[key] = self.es.enter_context(self.nc.semaphore("d_" + key))
            self.dcnt[key] = 0
        self._wait(q, self._deps(r, w))
        ins = fn(self.engs[q])
        self.dcnt[key] += 16
        ins.then_inc(self.dsem[key], 16)
        tok = ("d_" + key, self.dsem[key], self.dcnt[key])
        self._record(tok, r, w, wa)
        if is_out:
            self.out_tokens.append(tok)
        return tok

    def finish(self):
        best = {}
        for (sn, sem, val) in self.out_tokens:
            if best.get(sn, (None, 0))[1] < val:
                best[sn] = (sem, val)
        for sn, (sem, val) in best.items():
            self.engs["sp"].wait_ge(sem, val)


def build(cfg):
    S, PAST, NPOOL, E, FF, CAP, D = (cfg[k] for k in ("S", "PAST", "NPOOL", "E", "FF", "CAP", "D"))
    KC = D // 128
    FC = FF // 128
    NT = S // 128
    T = S + 128
    NTT = NT + 1
    PAGES = PAST // 128
    assert PAGES <= 128
    NIN = 5128
    NQB = S // 512 if S >= 512 else 1
    QW = 512 if S >= 512 else S
    CT = CAP // 128
    RB = 32
    NRB = 128 // RB

    nc = bass.Bass("TRN2", target_bir_lowering=False)

    def din(name, shape, dt=F32):
        return nc.dram_tensor(name, list(shape), dt, kind="ExternalInput")

    def dout(name, shape, dt=F32):
        return nc.dram_tensor(name, list(shape), dt, kind="ExternalOutput")

    def dscr(name, shape, dt=F32):
        return nc.dram_tensor(name, list(shape), dt)

    xT_d = din("xT", [128, KC, T])
    xtok_d = din("xtok", [T, D])
    xgT_d = din("xgT", [128, 4, KC, NSG])
    win_d = din("win", [128, KC, NIN])
    whs_d = din("whs", [128, 4, KC, 384])
    ck_d = [[din("ck%d_%d" % (h, i), [NPOOL, RB * 128]) for i in range(NRB)] for h in range(HA)]
    cv_d = [[din("cv%d_%d" % (h, i), [NPOOL, RB * 128]) for i in range(NRB)] for h in range(HA)]
    pt_d = din("pt", [128, NSG], I32)
    sel_d = din("selm", [128, 16])
    stc_d = din("stc", [64, NSO * HM, 129])
    stm_d = din("stm", [NSO, HM])
    big_d = din("bigate", [8, 1])
    lamp_d = din("lamp", [4, 64])
    subg_d = din("subg", [128, 1])
    mhg_d = din("mhg", [512])
    wba_d = din("wba", [128, 4, D])
    wbm_d = din("wbm", [128, 4, D])
    wo_d = din("wo", [128, KC, D])
    ln_d = din("lnp", [4, D])
    wr_d = din("wr", [128, KC, E])
    br_d = din("br", [E])
    wg_d = din("wg", [E, 128, KC, FF])
    wu_d = din("wu", [E, 128, KC, FF])
    wd_d = din("wd", [E, 128, FC, D])
    bgu_d = din("bgu", [128, 2, E, FC])
    bd_d = din("bd", [E, D])
    cst_d = din("cst", [128, 1024])
    cm_d = din("cm", [128, 4, 512])
    rope_d = din("rope", [128, NTT, 16])
    ropes_d = din("ropes", [128, 16])
    ecap_d = din("ecap", [128, E])
    i4_d = din("i4", [128, 4, 4])

    y_o = dout("y", [S, D])
    ys_o = dout("ysamp", [NSO, D])
    k_o = dout("k", [S, 512])
    v_o = dout("v", [S, 512])
    cp_o = dout("cp", [HM, 64, 128])
    np_o = dout("npr", [HM, 64])
    mp_o = dout("mp", [HM, 1])
    ks_o = dout("ksamp", [NSO, 512])
    vs_o = dout("vsamp", [NSO, 512])
    cs_o = dout("csamp", [NSO * HM, 64, 128])
    ns_o = dout("nsamp", [NSO * HM, 64])
    ms_o = dout("msamp", [NSO, HM])

    qT_s = dscr("qT_s", [HA, 128, T])
    kT_s = dscr("kT_s", [HA, 128, T])
    v_s = dscr("v_s", [T, 512])
    qmT_s = dscr("qmT_s", [HM, 64, T])
    kmT_s = dscr("kmT_s", [HM, 64, T])
    km_s = dscr("km_s", [T, 256])
    qms_s = dscr("qms_s", [128, 256])
    vm_s = dscr("vm_s", [T, 512])
    ig_s = dscr("ig_s", [4, T])
    lf_s = dscr("lf_s", [4, T])
    sog_s = dscr("sog_s", [T, 512])
    sga_s = dscr("sga_s", [KC, 128, T])
    sgm_s = dscr("sgm_s", [KC, 128, T])
    ainT_s = dscr("ainT_s", [HA, 128, T])
    minT_s = dscr("minT_s", [4, 128, T])
    M_s = dscr("M_s", [4, S])
    qsb_s = dscr("qsb_s", [NSG, 128])
    agi_s = dscr("agi_s", [NSG, 128])
    ago_s = dscr("ago_s", [8 * NSG, 128])
    hs_s = dscr("hs_s", [T, D])
    xg_s = dscr("xg_s", [E * CAP, D])
    yx_s = dscr("yx_s", [E * CAP, D])

    es = ExitStack()
    sy = Sync(nc, es)

    def sb(name, shape, dt=F32, st=None):
        return (st or es).enter_context(nc.sbuf_tensor("t_" + name, list(shape), dt))

    def ps(name, shape, dt=F32, st=None):
        return (st or es).enter_context(nc.psum_tensor("p_" + name, list(shape), dt))

    cst = sb("cst", [128, 1024])
    ident = cst[:, 0:128]
    ones = cst[:, 128:256]
    ltri = cst[:, 256:384]
    maskD = cst[:, 384:512]
    rowm = cst[:, 512:513]
    zeros = cst[:, 640:1024]
    cm = sb("cm", [128, 4, 512])
    rope = sb("rope", [128, NTT, 16])
    ropes = sb("ropes", [128, 16])
    ecap = sb("ecap", [128, E])
    i4 = sb("i4", [128, 4, 4])
    lnp = sb("lnp", [128, 4, D])
    brb = sb("brb", [128, E])
    mhg = sb("mhg", [128, 512])
    subg = sb("subg", [128, 1])
    bigt = sb("bigt", [8, 1])
    lamp = sb("lamp", [128, 4, 64])
    bgu = sb("bgu", [128, 2, E, FC])
    stm = sb("stm", [NSO, HM])
    selm = sb("selm", [128, 16])
    ptt = sb("ptt", [128, NSG], I32)
    ainS = sb("ainS", [128, 16])
    wts = sb("wts", [128, NTT, 4])
    sli = sb("sli", [128, NTT, 4], I32)

    CK = ["cst", "cm", "rope", "ropes", "ecap", "i4", "lnp", "brb", "mhg", "subg", "bigt", "lamp", "bgu",
          "stm", "selm", "ptt"]
    loads = [
        (r_(cst[:]), r_(cst_d.ap())), (cm[:], cm_d.ap()), (rope[:], rope_d.ap()), (ropes[:], ropes_d.ap()),
        (ecap[:], ecap_d.ap()), (i4[:], i4_d.ap()),
        (lnp[:].rearrange("p a d -> p (a d)"), ln_d.ap().rearrange("a d -> (a d)").partition_broadcast(128)),
        (brb[:], br_d.ap().partition_broadcast(128)), (mhg[:], mhg_d.ap().partition_broadcast(128)),
        (subg[:], subg_d.ap()), (bigt[:], big_d.ap()),
        (lamp[:].rearrange("p a d -> p (a d)"), lamp_d.ap().rearrange("a d -> (a d)").partition_broadcast(128)),
        (bgu[:], bgu_d.ap()), (stm[:], stm_d.ap()), (selm[:], sel_d.ap()), (ptt[:], pt_d.ap()),
    ]
    for li, (o, i) in enumerate(loads):
        bc = li in (0, 6, 7, 8, 11)
        sy.dma("pool" if bc else "sp", "constb" if bc else "const", lambda e, o=o, i=i: e.dma_start(out=o, in_=i), w=[])
    ctok = [("d_const", sy.dsem["const"], sy.dcnt["const"]), ("d_constb", sy.dsem["constb"], sy.dcnt["constb"])]
    for k in CK:
        sy.last_w[k] = list(ctok)

    lam = sb("lam", [128, 4])
    ltmp = sb("ltmp", [128, 2, 64])
    sy.op("dve", lambda e: e.tensor_tensor(out=ltmp[:, 0, :], in0=lamp[:, 0, :], in1=lamp[:, 1, :], op=ALU.mult),
          r=["lamp"], w=["ltmp"])
    sy.op("dve", lambda e: e.tensor_tensor(out=ltmp[:, 1, :], in0=lamp[:, 2, :], in1=lamp[:, 3, :], op=ALU.mult),
          r=["lamp"], w=["ltmp"])
    sy.op("dve", lambda e: e.tensor_reduce(out=lam[:, 2:4], in_=ltmp[:], axis=AX.X, op=ALU.add),
          r=["ltmp"], w=["lam"])
    sy.op("act", lambda e: e.activation(out=lam[:, 2:4], in_=lam[:, 2:4], func=AF.Exp), r=["lam"], w=["lam"])
    sy.op("dve", lambda e: e.tensor_tensor(out=lam[:, 0:1], in0=lam[:, 2:3], in1=lam[:, 3:4], op=ALU.subtract),
          r=["lam"], w=["lam"])
    sy.op("dve", lambda e: e.tensor_scalar(out=lam[:, 0:1], in0=lam[:, 0:1], scalar1=LAM_INIT, scalar2=None,
                                           op0=ALU.add), r=["lam"], w=["lam"])
    sy.op("dve", lambda e: e.tensor_scalar(out=lam[:, 1:2], in0=lam[:, 0:1], scalar1=-1.0, scalar2=None,
                                           op0=ALU.mult), r=["lam"], w=["lam"])
    gsc = sb("gsc", [128, 1])
    sy.op("dve", lambda e: e.tensor_scalar(out=gsc[:], in0=subg[:], scalar1=(1.0 - LAM_INIT), scalar2=None,
                                           op0=ALU.mult), r=["subg"], w=["gsc"])
    bu1 = sb("bu1", [128, E, FC])
    sy.op("dve", lambda e: e.tensor_scalar(out=bu1[:], in0=bgu[:, 1, :, :], scalar1=1.0, scalar2=None, op0=ALU.add),
          r=["bgu"], w=["bu1"])
    bsc = sb("bsc", [8, 1])
    sy.op("dve", lambda e: e.tensor_scalar(out=bsc[:], in0=bigt[:], scalar1=1.0 / SOFTCAP, scalar2=None,
                                           op0=ALU.mult), r=["bigt"], w=["bsc"])

    zt = sb("zt", [128, D])
    sy.op("pool", lambda e: e.memset(zt[:], 0.0), w=["zt"])
    nz = (E * CAP) // 128
    for z0 in range(0, nz, 32):
        z1 = min(nz, z0 + 32)
        sy.dma("pool", "zero", lambda e, z0=z0, z1=z1: e.dma_start(
            out=xg_s.ap()[z0 * 128:z1 * 128, :].rearrange("(a p) d -> p a d", p=128),
            in_=zt[:].unsqueeze(1).to_broadcast([128, z1 - z0, D])), r=["zt"], wa=["xg_zero"])

    for h in range(HA):
        sy.dma("pool", "zero", lambda e, h=h: e.dma_start(out=ainT_s.ap()[h, :, S + NSO:T], in_=zt[:, 0:128 - NSO]),
               r=["zt"], wa=["ainT_s"])

    st = ExitStack()
    xgT = sb("xgT", [128, 4, KC, NSG], st=st)
    whs = sb("whs", [128, 4, KC, 384], st=st)
    sy.dma("sp", "ld0", lambda e: e.dma_start(out=xgT[:], in_=xgT_d.ap()), w=["xgT"])
    sy.dma("sp", "ld1", lambda e: e.dma_start(out=whs[:], in_=whs_d.ap()), w=["whs"])
    zs_ps = ps("zs_ps", [NSG, 384], st=st)
    for h in range(4):
        for kc in range(KC):
            sy.mm(lambda e, kc=kc, h=h: e.matmul(zs_ps[:], lhsT=xgT[:, h, kc, :], rhs=whs[:, h, kc, :],
                                                 start=(h == 0 and kc == 0), stop=(h == 3 and kc == KC - 1)),
                  r=["xgT", "whs"], w=["zs_ps"], last=(h == 3 and kc == KC - 1))
    zs = sb("zs", [NSG, 384], st=st)
    zv = zs_ps[:].rearrange("p (g d) -> p g d", d=64)
    zo = zs[:].rearrange("p (g d) -> p g d", d=64)
    cosb = ropes[0:NSG, 0:8].unsqueeze(1).to_broadcast([NSG, 4, 8])
    sinb = ropes[0:NSG, 8:16].unsqueeze(1).to_broadcast([NSG, 4, 8])
    rt = sb("rt", [NSG, 4, 4, 8], st=st)

    def rope_ops(src, dst, cosv, sinv, tmp, n, rk, wk_, tk):
        sy.op("dve", lambda e: e.tensor_tensor(out=tmp[:, 0], in0=src[:, :, 0:8], in1=cosv, op=ALU.mult),
              r=[rk], w=[tk])
        sy.op("dve", lambda e: e.tensor_tensor(out=tmp[:, 1], in0=src[:, :, 8:16], in1=sinv, op=ALU.mult),
              r=[rk], w=[tk])
        sy.op("dve", lambda e: e.tensor_tensor(out=tmp[:, 2], in0=src[:, :, 8:16], in1=cosv, op=ALU.mult),
              r=[rk], w=[tk])
        sy.op("dve", lambda e: e.tensor_tensor(out=tmp[:, 3], in0=src[:, :, 0:8], in1=sinv, op=ALU.mult),
              r=[rk], w=[tk])
        sy.op("dve", lambda e: e.tensor_tensor(out=dst[:, :, 0:8], in0=tmp[:, 0], in1=tmp[:, 1], op=ALU.subtract),
              r=[tk], w=[wk_])
        sy.op("dve", lambda e: e.tensor_tensor(out=dst[:, :, 8:16], in0=tmp[:, 2], in1=tmp[:, 3], op=ALU.add),
              r=[tk], w=[wk_])
        sy.op("act", lambda e: e.copy(out=dst[:, :, 16:64], in_=src[:, :, 16:64]), r=[rk], w=[wk_])

    rope_ops(zv[:, 0:4, :], zo[:, 0:4, :], cosb, sinb, rt[:], NSG, "zs_ps", "zs", "rt")
    sy.op("act", lambda e: e.copy(out=zs[:, 256:384], in_=zs_ps[:, 256:384]), r=["zs_ps"], w=["zs"])
    sy.dma("sp", "ld0", lambda e: e.dma_start(out=qsb_s.ap(), in_=zs[:, 0:128]), r=["zs"], w=["qsb_s"])
    qb = sb("qb", [128, NSG, 128], st=st)
    sy.dma("pool", "ldb", lambda e: e.dma_start(
        out=qb[:].rearrange("p j d -> p (j d)"),
        in_=qsb_s.ap().rearrange("j d -> (j d)").partition_broadcast(128)), r=["qsb_s"], w=["qb"])
    pn = sb("pn", [NSG, 2], st=st)
    pnt = sb("pnt", [NSG, 128], st=st)
    sy.op("dve", lambda e: e.tensor_tensor(out=pnt[:], in0=zs[:, 0:128], in1=zs[:, 128:256], op=ALU.mult),
          r=["zs"], w=["pnt"])
    sy.op("dve", lambda e: e.tensor_reduce(out=pn[:], in_=pnt[:].rearrange("p (c d) -> p c d", d=64), axis=AX.X,
                                           op=ALU.add), r=["pnt"], w=["pn"])
    sy.op("act", lambda e: e.activation(out=pn[:], in_=pn[:], func=AF.Exp, scale=DH ** -0.5), r=["pn"], w=["pn"])
    pnd = sb("pnd", [NSG, NSG, 2], st=st)
    sy.op("dve", lambda e: e.tensor_tensor(out=pnd[:], in0=ident[0:NSG, 0:NSG].unsqueeze(2).to_broadcast([NSG, NSG, 2]),
                                           in1=pn[:].unsqueeze(1).to_broadcast([NSG, NSG, 2]), op=ALU.mult),
          r=["pn", "cst"], w=["pnd"])

    kb = [sb("kb%d" % i, [128, RB, 128], st=st) for i in range(2)]
    vb = [sb("vb%d" % i, [128, RB, 128], st=st) for i in range(2)]
    ktmp = sb("ktmp", [128, RB, 128], st=st)
    vbb = [sb("vbb%d" % i, [128, RB, 128], BF16, st=st) for i in range(2)]
    scb = sb("scb", [128, 128, 2], BF16, st=st)
    sc_s = sb("sc_s", [128, 128, 2], st=st)
    prs = sb("prs", [128, 2], st=st)
    pv_ps = ps("pv_ps", [128, 2], st=st)
    dn_ps = ps("dn_ps", [128, 2], st=st)
    oS = sb("oS", [128, NSG], st=st)
    osm = sb("osm", [128, 8], st=st)
    blk = 0
    for j in range(NSG):
        for rb in range(NRB):
            b = blk % 2
            blk += 1
            sy.dma("pool", "kb%d" % b, lambda e, b=b, rb=rb, j=j: e.indirect_dma_start(
                out=kb[b][:].rearrange("p r d -> p (r d)"), out_offset=None, in_=ck_d[j // 4][rb].ap(),
                in_offset=bass.IndirectOffsetOnAxis(ap=ptt[:, j:j + 1], axis=0)), r=["ptt"], w=["kb%d" % b])
            sy.dma("pool", "vb%d" % b, lambda e, b=b, rb=rb, j=j: e.indirect_dma_start(
                out=vb[b][:].rearrange("p r d -> p (r d)"), out_offset=None, in_=cv_d[j // 4][rb].ap(),
                in_offset=bass.IndirectOffsetOnAxis(ap=ptt[:, j:j + 1], axis=0)), r=["ptt"], w=["vb%d" % b])
            sy.op("act", lambda e, b=b: e.copy(out=vbb[b][:], in_=vb[b][:]), r=["vb%d" % b], w=["vbb%d" % b])
            sy.op("dve", lambda e, b=b, j=j: e.tensor_tensor(
                out=ktmp[:], in0=kb[b][:], in1=qb[:, j, :].unsqueeze(1).to_broadcast([128, RB, 128]), op=ALU.mult),
                r=["kb%d" % b, "qb"], w=["ktmp"])
            sy.op("dve", lambda e, rb=rb: e.tensor_reduce(
                out=sc_s[:, rb * RB:(rb + 1) * RB, :], in_=ktmp[:].rearrange("p r (c d) -> p r c d", d=64),
                axis=AX.X, op=ALU.add), r=["ktmp"], w=["sc_s%d" % rb])
            sy.op("act", lambda e, rb=rb: e.activation(
                out=sc_s[:, rb * RB:(rb + 1) * RB, :], in_=sc_s[:, rb * RB:(rb + 1) * RB, :], func=AF.Exp,
                scale=DH ** -0.5), r=["sc_s%d" % rb], w=["sc_s%d" % rb])
            if PAGES < 128:
                sy.op("dve", lambda e, rb=rb: e.tensor_scalar(
                    out=sc_s[:, rb * RB:(rb + 1) * RB, :], in0=sc_s[:, rb * RB:(rb + 1) * RB, :],
                    scalar1=cst[:, 513:514], scalar2=None, op0=ALU.mult), r=["sc_s%d" % rb, "cst"], w=["sc_s%d" % rb])
            sy.op("act", lambda e, rb=rb: e.copy(out=scb[:, rb * RB:(rb + 1) * RB, :],
                                                 in_=sc_s[:, rb * RB:(rb + 1) * RB, :]),
                  r=["sc_s%d" % rb], w=["scb%d" % rb])
            for rr in range(RB):
                row = rb * RB + rr
                sy.mm(lambda e, b=b, rr=rr, row=row: e.matmul(
                    pv_ps[:], lhsT=vbb[b][:, rr, :], rhs=scb[:, row, :], start=(row == 0), stop=False),
                    r=["vbb%d" % b, "scb%d" % rb], w=["pv_ps"], last=(rr == RB - 1))
        sy.mm(lambda e, j=j: e.matmul(pv_ps[:], lhsT=zs[:, 256:384], rhs=pnd[:, j, :], start=False, stop=True),
              r=["zs", "pnd"], w=["pv_ps"])
        sy.op("dve", lambda e: e.tensor_reduce(out=prs[:], in_=sc_s[:].rearrange("p r c -> p c r"), axis=AX.X,
                                               op=ALU.add), r=["sc_s%d" % i for i in range(NRB)], w=["prs"])
        sy.mm(lambda e: e.matmul(dn_ps[:], lhsT=ones, rhs=prs[:], start=True, stop=False),
              r=["cst", "prs"], w=["dn_ps"], last=False)
        sy.mm(lambda e, j=j: e.matmul(dn_ps[:], lhsT=ones[0:NSG, :], rhs=pnd[:, j, :], start=False, stop=True),
              r=["cst", "pnd"], w=["dn_ps"])
        sy.op("dve", lambda e: e.reciprocal(out=osm[:, 0:2], in_=dn_ps[:]), r=["dn_ps"], w=["osm"])
        sy.op("dve", lambda e: e.tensor_tensor(out=osm[:, 2:4], in0=pv_ps[:], in1=osm[:, 0:2], op=ALU.mult),
              r=["pv_ps", "osm"], w=["osm"])
        sy.op("dve", lambda e, j=j: e.scalar_tensor_tensor(
            out=oS[:, j:j + 1], in0=osm[:, 3:4], scalar=lam[:, 1:2], in1=osm[:, 2:3], op0=ALU.mult, op1=ALU.add),
            r=["osm", "lam"], w=["oS"])
    osq = sb("osq", [128, NSG], st=st)
    ss_ps = ps("ss_ps", [128, NSG], st=st)
    sy.op("act", lambda e: e.activation(out=osq[:], in_=oS[:], func=AF.Square), r=["oS"], w=["osq"])
    sy.mm(lambda e: e.matmul(ss_ps[:], lhsT=ones, rhs=osq[:], start=True, stop=True), r=["cst", "osq"], w=["ss_ps"])
    sy.op("act", lambda e: e.activation(out=osq[:], in_=ss_ps[:], func=AF.Ln, bias=EPS, scale=1.0 / 128),
          r=["ss_ps"], w=["osq"])
    sy.op("act", lambda e: e.activation(out=osq[:], in_=osq[:], func=AF.Exp, scale=-0.5), r=["osq"], w=["osq"])
    sy.op("dve", lambda e: e.scalar_tensor_tensor(out=oS[:], in0=oS[:], scalar=gsc[:, 0:1], in1=osq[:],
                                                  op0=ALU.mult, op1=ALU.mult), r=["oS", "gsc", "osq"], w=["oS"])
    sy.op("act", lambda e: e.copy(out=ainS[:], in_=oS[:]), r=["oS"], w=["ainS"])
    st.close()
    if cfg.get('stop') == 'S':
        sy.finish()
        es.close()
        return nc
    for h in range(HA):
        sy.dma("sp", "st0", lambda e, h=h: e.dma_start(out=ainT_s.ap()[h, :, S:S + NSO], in_=ainS[:, h * 4:h * 4 + 4]),
               r=["ainS"], wa=["ainT_s"])

    sy.barrier()
    st = ExitStack()
    xT = sb("xT", [128, KC, T], st=st)
    sy.dma("pool", "ld0", lambda e: e.dma_start(out=r_(xT[:]), in_=(xT_d.ap())), w=["xT"])
    BW = 520
    wb = [sb("wb%d" % i, [128, KC, BW], st=st) for i in range(2)]
    blocks = [(0, 512), (512, 512), (1024, 512), (1536, 512), (2048, 512), (2560, 520), (3080, 512), (3592, 512),
              (4104, 512), (4616, 512)]
    zp = [ps("zp%d" % i, [128, 512], st=st) for i in range(4)]
    zsb = [sb("zsb%d" % i, [128, 512], st=st) for i in range(4)]
    rtt = sb("rtt", [128, 4, 8, 8], st=st)
    tp = [ps("tp%d" % i, [128, 4, 128], st=st) for i in range(2)]
    tsb = [sb("tsb%d" % i, [128, 4, 128], st=st) for i in range(2)]
    cnt = {"z": 0, "t": 0}
    tgroups = [(g * 512, min(512, S - g * 512)) for g in range((S + 511) // 512)] + [(S, 128)]

    def tok_major(bi, i, c0, n, post):
        zi = cnt["z"] % 4
        cnt["z"] += 1
        for kc in range(KC):
            sy.mm(lambda e, kc=kc: e.matmul(zp[zi][:, 0:n], lhsT=r_(xT[:, kc, i * 128:(i + 1) * 128]),
                                            rhs=r_(wb[bi % 2][:, kc, c0:c0 + n]), start=(kc == 0), stop=(kc == KC - 1)),
                  r=["xT", "wb%d" % (bi % 2)], w=["zp%d" % zi], last=(kc == KC - 1))
        post(zp[zi], zi)

    def feat_major(bi, c0, m, t0, n, post, cast=r_):
        zi = cnt["z"] % 4
        cnt["z"] += 1
        for kc in range(KC):
            sy.mm(lambda e, kc=kc: e.matmul(zp[zi][0:m, 0:n], lhsT=cast(wb[bi % 2][:, kc, c0:c0 + m]),
                                            rhs=cast(xT[:, kc, t0:t0 + n]), start=(kc == 0), stop=(kc == KC - 1)),
                  r=["xT", "wb%d" % (bi % 2)], w=["zp%d" % zi], last=(kc == KC - 1))
        post(zp[zi], zi)

    def transposes_out(src_tile, zi, dst, i):
        ti = cnt["t"] % 2
        cnt["t"] += 1
        for h in range(4):
            sy.mm(lambda e, h=h: e.transpose(out=tp[ti][:, h, :], in_=src_tile[:, h * 128:(h + 1) * 128],
                                             identity=ident), r=["zsb%d" % zi, "cst"], w=["tp%d" % ti], last=(h == 3))
        sy.op("act", lambda e: e.copy(out=tsb[ti][:], in_=tp[ti][:]), r=["tp%d" % ti], w=["tsb%d" % ti])
        sy.dma("sp", "st%d" % ti, lambda e: e.dma_start(
            out=dst.ap()[:, :, i * 128:(i + 1) * 128].rearrange("h p t -> p h t"), in_=tsb[ti][:]),
            r=["tsb%d" % ti], wa=[dst.name])

    for bi, (c_off, c_w) in enumerate(blocks):
        if bi >= cfg.get('nblk', 99):
            break
        sy.dma("pool", "wb%d" % (bi % 2), lambda e, bi=bi, c_off=c_off, c_w=c_w: e.dma_start(
            out=r_(wb[bi % 2][:, :, 0:c_w]), in_=(win_d.ap()[:, :, c_off:c_off + c_w])), w=["wb%d" % (bi % 2)])
        if bi in (0, 1):
            for i in range(NTT):
                def post(zpt, zi, i=i, bi=bi):
                    src = zpt[:].rearrange("p (g d) -> p g d", d=64)
                    dst = zsb[zi][:].rearrange("p (g d) -> p g d", d=64)
                    cosv = rope[:, i, 0:8].unsqueeze(1).to_broadcast([128, 8, 8])
                    sinv = rope[:, i, 8:16].unsqueeze(1).to_broadcast([128, 8, 8])
                    rope_ops(src, dst, cosv, sinv, rtt[:], 128, "zp%d" % zi, "zsb%d" % zi, "rtt")
                    if bi == 1:
                        if i < NT:
                            sy.dma("sp", "zo%d" % zi, lambda e: e.dma_start(out=k_o.ap()[i * 128:(i + 1) * 128, :],
                                                                            in_=zsb[zi][:]), r=["zsb%d" % zi], is_out=True)
                        else:
                            sy.dma("sp", "zo%d" % zi, lambda e: e.dma_start(out=ks_o.ap(), in_=zsb[zi][0:NSO, :]),
                                   r=["zsb%d" % zi], is_out=True)
                    transposes_out(zsb[zi], zi, qT_s if bi == 0 else kT_s, i)
                tok_major(bi, i, 0, 512, post)
        elif bi in (2, 4):
            for i in range(NTT):
                def post(zpt, zi, i=i, bi=bi):
                    sy.op("act", lambda e: e.copy(out=zsb[zi][:], in_=zpt[:]), r=["zp%d" % zi], w=["zsb%d" % zi])
                    dst = v_s if bi == 2 else vm_s
                    sy.dma("sp", "zo%d" % zi, lambda e: e.dma_start(out=dst.ap()[i * 128:(i + 1) * 128, :], in_=zsb[zi][:]),
                           r=["zsb%d" % zi], wa=[dst.name])
                    if bi == 2:
                        if i < NT:
                            sy.dma("sp", "zq%d" % zi, lambda e: e.dma_start(out=v_o.ap()[i * 128:(i + 1) * 128, :],
                                                                            in_=zsb[zi][:]), r=["zsb%d" % zi], is_out=True)
                        else:
                            sy.dma("sp", "zq%d" % zi, lambda e: e.dma_start(out=vs_o.ap(), in_=zsb[zi][0:NSO, :]),
                                   r=["zsb%d" % zi], is_out=True)
                tok_major(bi, i, 0, 512, post)
        elif bi == 3:
            for (t0, n) in tgroups:
                for ch in range(4):
                    def post(zpt, zi, ch=ch, t0=t0, n=n):
                        if ch < 2:
                            sy.op("act", lambda e: e.copy(out=zsb[zi][:, 0:n], in_=zpt[:, 0:n]), r=["zp%d" % zi],
                                  w=["zsb%d" % zi])
                            dst = qmT_s
                        else:
                            sy.op("act", lambda e: e.mul(out=zsb[zi][:, 0:n], in_=zpt[:, 0:n], mul=DK ** -0.5),
                                  r=["zp%d" % zi], w=["zsb%d" % zi])
                            dst = kmT_s
                        hh = (ch % 2) * 2
                        sy.dma("sp", "zo%d" % zi, lambda e: e.dma_start(
                            out=dst.ap()[hh:hh + 2, :, t0:t0 + n].rearrange("h p t -> (h p) t"), in_=zsb[zi][:, 0:n]),
                            r=["zsb%d" % zi], wa=[dst.name])
                    feat_major(bi, ch * 128, 128, t0, n, post)
            for i in range(NTT):
                def post(zpt, zi, i=i):
                    sy.op("act", lambda e: e.mul(out=zsb[zi][:, 0:256], in_=zpt[:, 0:256], mul=DK ** -0.5),
                          r=["zp%d" % zi], w=["zsb%d" % zi])
                    sy.dma("sp", "zo%d" % zi, lambda e: e.dma_start(out=km_s.ap()[i * 128:(i + 1) * 128, :],
                                                                    in_=zsb[zi][:, 0:256]), r=["zsb%d" % zi], wa=["km_s"])
                tok_major(bi, i, 256, 256, post)

            def post(zpt, zi):
                sy.op("act", lambda e: e.copy(out=zsb[zi][:, 0:256], in_=zpt[:, 0:256]), r=["zp%d" % zi], w=["zsb%d" % zi])
                sy.dma("sp", "zo%d" % zi, lambda e: e.dma_start(out=qms_s.ap(), in_=zsb[zi][:, 0:256]),
                       r=["zsb%d" % zi], wa=["qms_s"])
            tok_major(bi, NT, 0, 256, post)
        elif bi == 5:
            for (t0, n) in tgroups:
                def post(zpt, zi, t0=t0, n=n):
                    a = zsb[zi]
                    sy.op("act", lambda e: e.activation(out=a[0:8, 0:n], in_=zpt[0:8, 0:n], func=AF.Tanh,
                                                        bias=bsc[:, 0:1], scale=1.0 / SOFTCAP),
                          r=["zp%d" % zi, "bsc"], w=["zsb%d" % zi])
                    sy.op("dve", lambda e: e.tensor_scalar(out=a[0:8, 0:n], in0=a[0:8, 0:n], scalar1=SOFTCAP,
                                                           scalar2=None, op0=ALU.mult), r=["zsb%d" % zi], w=["zsb%d" % zi])
                    sy.dma("sp", "zo%d" % zi, lambda e: e.dma_start(out=ig_s.ap()[:, t0:t0 + n], in_=a[0:4, 0:n]),
                           r=["zsb%d" % zi], wa=["ig_s"])
                    sy.op("act", lambda e: e.activation(out=a[0:8, 0:n], in_=a[0:8, 0:n], func=AF.Exp, scale=-1.0),
                          r=["zsb%d" % zi], w=["zsb%d" % zi])
                    sy.op("act", lambda e: e.activation(out=a[0:8, 0:n], in_=a[0:8, 0:n], func=AF.Ln, bias=1.0),
                          r=["zsb%d" % zi], w=["zsb%d" % zi])
                    sy.op("dve", lambda e: e.tensor_scalar(out=a[0:8, 0:n], in0=a[0:8, 0:n], scalar1=-1.0,
                                                           scalar2=None, op0=ALU.mult), r=["zsb%d" % zi], w=["zsb%d" % zi])
                    sy.dma("sp", "zq%d" % zi, lambda e: e.dma_start(out=lf_s.ap()[:, t0:t0 + n], in_=a[4:8, 0:n]),
                           r=["zsb%d" % zi], wa=["lf_s"])
                feat_major(bi, 0, 8, t0, n, post, cast=lambda a: a)
            for i in range(NTT):
                def post(zpt, zi, i=i):
                    sy.op("act", lambda e: e.activation(out=zsb[zi][:], in_=zpt[:], func=AF.Sigmoid),
                          r=["zp%d" % zi], w=["zsb%d" % zi])
                    sy.dma("sp", "zo%d" % zi, lambda e: e.dma_start(out=sog_s.ap()[i * 128:(i + 1) * 128, :],
                                                                    in_=zsb[zi][:]), r=["zsb%d" % zi], wa=["sog_s"])
                tok_major(bi, i, 8, 512, post)
        else:
            dst = sga_s if bi in (6, 7) else sgm_s
            base = ((bi - 6) % 2) * 4
            for (t0, n) in tgroups:
                for ch in range(4):
                    def post(zpt, zi, ch=ch, t0=t0, n=n, dst=dst, base=base):
                        sy.op("act", lambda e: e.activation(out=zsb[zi][:, 0:n], in_=zpt[:, 0:n], func=AF.Sigmoid),
                              r=["zp%d" % zi], w=["zsb%d" % zi])
                        sy.dma("sp", "zo%d" % zi, lambda e: e.dma_start(out=dst.ap()[base + ch, :, t0:t0 + n],
                                                                        in_=zsb[zi][:, 0:n]), r=["zsb%d" % zi], wa=[dst.name])
                    feat_major(bi, ch * 128, 128, t0, n, post)
    st.close()
    if cfg.get('stop') == '1':
        sy.finish()
        es.close()
        return nc

    sy.barrier()
    st = ExitStack()
    qTh = sb("qTh", [128, S], st=st)
    kTh = sb("kTh", [128, S], st=st)
    vh = sb("vh", [128, NT, 128], st=st)
    scp = [ps("scp%d" % i, [128, 512], st=st) for i in range(4)]
    dnp = [ps("dnp%d" % i, [128, 512], st=st) for i in range(2)]
    pvp = [ps("pvp%d" % i, [128, 512], st=st) for i in range(2)]
    ptl = [sb("ptl%d" % i, [128, 512], st=st) for i in range(4)]
    o_t = sb("o_t", [128, 512], st=st)
    t_t = sb("t_t", [128, 512], st=st)
    rd_t = sb("rd_t", [128, 512], st=st)
    sq_t = sb("sq_t", [128, 512], st=st)
    it = 0
    for h in range(HA):
        sy.dma("pool", "ld0", lambda e, h=h: e.dma_start(out=r_(qTh[:]), in_=(qT_s.ap()[h, :, 0:S])), r=["qT_s"], w=["qTh"])
        sy.dma("pool", "ld1", lambda e, h=h: e.dma_start(out=r_(kTh[:]), in_=(kT_s.ap()[h, :, 0:S])), r=["kT_s"], w=["kTh"])
        sy.dma("pool", "ld2", lambda e, h=h: e.dma_start(
            out=r_(vh[:]), in_=(v_s.ap()[0:S, h * 128:(h + 1) * 128].rearrange("(i p) d -> p i d", p=128))),
            r=["v_s"], w=["vh"])
        for qblk in range(NQB):
            q0 = qblk * QW
            nkt = (q0 + QW) // 128
            for kt in range(nkt):
                for c in range(2):
                    bi_ = it % 4
                    it += 1
                    sy.mm(lambda e, c=c, kt=kt, bi_=bi_: e.matmul(
                        scp[bi_][:, 0:QW], lhsT=r_(kTh[c * 64:(c + 1) * 64, kt * 128:(kt + 1) * 128]),
                        rhs=r_(qTh[c * 64:(c + 1) * 64, q0:q0 + QW]), start=True, stop=True),
                        r=["qTh", "kTh"], w=["scp%d" % bi_])
                    sy.op("act", lambda e, bi_=bi_: e.activation(out=r_(ptl[bi_][:, 0:QW]), in_=scp[bi_][:, 0:QW],
                                                                 func=AF.Exp, scale=DH ** -0.5),
                          r=["scp%d" % bi_], w=["ptl%d" % bi_])
                    off = kt - q0 // 128
                    if off >= 0:
                        sy.op("dve", lambda e, bi_=bi_, off=off: e.tensor_tensor(
                            out=r_(ptl[bi_][:, 0:QW]), in0=ptl[bi_][:, 0:QW], in1=cm[:, off, 0:QW], op=ALU.mult),
                            r=["ptl%d" % bi_, "cm"], w=["ptl%d" % bi_])
                    sy.mm(lambda e, c=c, kt=kt, bi_=bi_: e.matmul(
                        dnp[c][:, 0:QW], lhsT=r_(ones), rhs=r_(ptl[bi_][:, 0:QW]), start=(kt == 0), stop=(kt == nkt - 1)),
                        r=["cst", "ptl%d" % bi_], w=["dnp%d" % c], last=False)
                    sy.mm(lambda e, c=c, kt=kt, bi_=bi_: e.matmul(
                        pvp[c][:, 0:QW], lhsT=r_(vh[:, kt, :]), rhs=r_(ptl[bi_][:, 0:QW]), start=(kt == 0),
                        stop=(kt == nkt - 1)), r=["vh", "ptl%d" % bi_], w=["pvp%d" % c])
            W_ = QW
            sy.op("dve", lambda e: e.reciprocal(out=rd_t[:, 0:W_], in_=dnp[0][:, 0:W_]), r=["dnp0"], w=["rd_t"])
            sy.op("dve", lambda e: e.tensor_tensor(out=o_t[:, 0:W_], in0=pvp[0][:, 0:W_], in1=rd_t[:, 0:W_], op=ALU.mult),
                  r=["pvp0", "rd_t"], w=["o_t"])
            sy.op("dve", lambda e: e.reciprocal(out=rd_t[:, 0:W_], in_=dnp[1][:, 0:W_]), r=["dnp1"], w=["rd_t"])
            sy.op("dve", lambda e: e.tensor_tensor(out=t_t[:, 0:W_], in0=pvp[1][:, 0:W_], in1=rd_t[:, 0:W_], op=ALU.mult),
                  r=["pvp1", "rd_t"], w=["t_t"])
            sy.op("dve", lambda e: e.scalar_tensor_tensor(out=o_t[:, 0:W_], in0=t_t[:, 0:W_], scalar=lam[:, 1:2],
                                                          in1=o_t[:, 0:W_], op0=ALU.mult, op1=ALU.add),
                  r=["t_t", "o_t", "lam"], w=["o_t"])
            sy.op("act", lambda e: e.activation(out=r_(sq_t[:, 0:W_]), in_=o_t[:, 0:W_], func=AF.Square), r=["o_t"], w=["sq_t"])
            sy.mm(lambda e: e.matmul(dnp[0][:, 0:W_], lhsT=r_(ones), rhs=r_(sq_t[:, 0:W_]), start=True, stop=True),
                  r=["cst", "sq_t"], w=["dnp0"])
            sy.op("act", lambda e: e.activation(out=rd_t[:, 0:W_], in_=dnp[0][:, 0:W_], func=AF.Ln, bias=EPS,
                                                scale=1.0 / 128), r=["dnp0"], w=["rd_t"])
            sy.op("act", lambda e: e.activation(out=rd_t[:, 0:W_], in_=rd_t[:, 0:W_], func=AF.Exp, scale=-0.5),
                  r=["rd_t"], w=["rd_t"])
            sy.op("dve", lambda e: e.scalar_tensor_tensor(out=o_t[:, 0:W_], in0=o_t[:, 0:W_], scalar=gsc[:, 0:1],
                                                          in1=rd_t[:, 0:W_], op0=ALU.mult, op1=ALU.mult),
                  r=["o_t", "gsc", "rd_t"], w=["o_t"])
            sy.dma("sp", "st0", lambda e, h=h, q0=q0: e.dma_start(out=ainT_s.ap()[h, :, q0:q0 + W_], in_=o_t[:, 0:W_]),
                   r=["o_t"], wa=["ainT_s"])
    st.close()
    if cfg.get('stop') == '2':
        sy.finish()
        es.close()
        return nc

    sy.barrier()
    st = ExitStack()
    igt = sb("igt", [4, T], st=st)
    lft = sb("lft", [4, T], st=st)
    sy.dma("sp", "ld0", lambda e: e.dma_start(out=igt[:], in_=ig_s.ap()), r=["ig_s"], w=["igt"])
    sy.dma("sp", "ld1", lambda e: e.dma_start(out=lft[:], in_=lf_s.ap()), r=["lf_s"], w=["lft"])
    Bt = sb("Bt", [4, S], st=st)
    at = sb("at", [4, S], st=st)
    Mt = sb("Mt", [4, S], st=st)
    mt = sb("mtot", [4, S], st=st)
    o4 = sb("o4", [4, S], st=st)
    sy.op("pool", lambda e: e.memset(o4[:], 1.0), w=["o4"])
    sy.op("dve", lambda e: e.tensor_tensor_scan(out=Bt[:], data0=o4[:], data1=lft[:, 0:S],
                                                initial=0.0, op0=ALU.mult, op1=ALU.add), r=["lft", "o4"], w=["Bt"])
    sy.op("dve", lambda e: e.tensor_tensor(out=at[:], in0=igt[:, 0:S], in1=Bt[:], op=ALU.subtract),
          r=["igt", "Bt"], w=["at"])
    sy.op("dve", lambda e: e.tensor_tensor_scan(out=Mt[:], data0=at[:], data1=at[:], initial=0.0, op0=ALU.max,
                                                op1=ALU.max), r=["at"], w=["Mt"])
    sy.op("dve", lambda e: e.tensor_tensor(out=mt[:], in0=Bt[:], in1=Mt[:], op=ALU.add), r=["Bt", "Mt"], w=["mtot"])
    sy.dma("sp", "st0", lambda e: e.dma_start(out=mp_o.ap(), in_=mt[:, S - 1:S]), r=["mtot"], is_out=True)
    sy.dma("sp", "st1", lambda e: e.dma_start(out=M_s.ap(), in_=Mt[:]), r=["Mt"], w=["M_s"])
    MB = sb("MB", [128, 4, S], st=st)
    sy.dma("pool", "ldb", lambda e: e.dma_start(out=MB[:].rearrange("p h t -> p (h t)"),
                                              in_=M_s.ap().rearrange("h t -> (h t)").partition_broadcast(128)),
           r=["M_s"], w=["MB"])
    acol = sb("acol", [128, NT, 4], st=st)
    ecol = sb("ecol", [128, NT, 4], st=st)
    colp = ps("colp", [128, 2, NT * 4], st=st)
    for i in range(NT):
        sy.mm(lambda e, i=i: e.transpose(out=colp[:, 0, i * 4:(i + 1) * 4], in_=at[0:4, i * 128:(i + 1) * 128],
                                         identity=ident[0:4, 0:4]), r=["at", "cst"], w=["colp"], last=False)
        sy.mm(lambda e, i=i: e.transpose(out=colp[:, 1, i * 4:(i + 1) * 4], in_=mt[0:4, i * 128:(i + 1) * 128],
                                         identity=ident[0:4, 0:4]), r=["mtot", "cst"], w=["colp"], last=(i == NT - 1))
    sy.op("act", lambda e: e.copy(out=acol[:].rearrange("p i h -> p (i h)"), in_=colp[:, 0, :]), r=["colp"], w=["acol"])
    sy.op("act", lambda e: e.activation(out=ecol[:].rearrange("p i h -> p (i h)"), in_=colp[:, 1, :], func=AF.Exp,
                                        scale=-1.0), r=["colp"], w=["ecol"])
    qmc = [sb("qmc%d" % i, [64, 4, 128], st=st) for i in range(2)]
    kmc = [sb("kmc%d" % i, [64, 4, 128], st=st) for i in range(2)]
    kmt = [sb("kmt%d" % i, [128, 256], st=st) for i in range(2)]
    vm1 = [sb("vm1%d" % i, [128, 4, 129], st=st) for i in range(2)]
    for b in range(2):
        sy.op("pool", lambda e, b=b: e.memset(vm1[b][:], 1.0), w=["vm1%d" % b])

    def load_chunk(i):
        b = i % 2
        t0 = i * 128
        sy.dma("sp", "lq%d" % b, lambda e: e.dma_start(out=qmc[b][:], in_=qmT_s.ap()[:, :, t0:t0 + 128].rearrange("h p t -> p h t")),
               r=["qmT_s"], w=["qmc%d" % b])
        sy.dma("sp", "lk%d" % b, lambda e: e.dma_start(out=kmc[b][:], in_=kmT_s.ap()[:, :, t0:t0 + 128].rearrange("h p t -> p h t")),
               r=["kmT_s"], w=["kmc%d" % b])
        sy.dma("sp", "lt%d" % b, lambda e: e.dma_start(out=kmt[b][:], in_=km_s.ap()[t0:t0 + 128, :]), r=["km_s"], w=["kmt%d" % b])
        sy.dma("sp", "lv%d" % b, lambda e: e.dma_start(out=vm1[b][:, :, 0:128],
                                                       in_=vm_s.ap()[t0:t0 + 128, :].rearrange("p (h d) -> p h d", d=128)),
               r=["vm_s"], w=["vm1%d" % b])

    Cst = sb("Cst", [64, 4, 129], st=st)
    WtL = [sb("Wt%d" % i, [128, 128], st=st) for i in range(2)]
    WItL = [sb("WIt%d" % i, [128, 128], st=st) for i in range(2)]
    SttL = [sb("Stt%d" % i, [128, 128], st=st) for i in range(2)]
    qstL = [sb("qst%d" % i, [64, 128], st=st) for i in range(2)]
    kwtL = [sb("kwt%d" % i, [128, 64], st=st) for i in range(2)]
    hm = sb("hm", [128, 512], st=st)
    sogt = sb("sogt", [128, 512], st=st)
    smalL = [sb("smal%d" % i, [128, 16], st=st) for i in range(2)]
    st6L = [sb("st6%d" % i, [128, 6], st=st) for i in range(2)]
    scmL = [ps("scm%d" % i, [128, 128], st=st) for i in range(2)]
    ndpL = [ps("ndp%d" % i, [128, 129], st=st) for i in range(2)]
    uppL = [ps("upp%d" % i, [64, 129], st=st) for i in range(2)]
    PB = {"p": 0}
    mtp = ps("mtp", [128, 4, 128], st=st)
    mts = sb("mts", [128, 4, 128], st=st)

    def head_norm(src, npart, h, keys_r, emt, emt_keys):
        P = npart
        smal, st6 = smalL[PB["p"]], st6L[PB["p"]]
        SM, S6 = "smal%d" % PB["p"], "st6%d" % PB["p"]
        sy.op("dve", lambda e: e.tensor_scalar(out=smal[0:P, 9:10], in0=src[0:P, 128:129], scalar1=-1.0, scalar2=None,
                                               op0=ALU.mult), r=keys_r, w=[SM])
        sy.op("dve", lambda e: e.tensor_tensor(out=smal[0:P, 10:11], in0=src[0:P, 128:129], in1=smal[0:P, 9:10],
                                               op=ALU.max), r=keys_r + [SM], w=[SM])
        sy.op("dve", lambda e: e.tensor_scalar(out=smal[0:P, 0:1], in0=smal[0:P, 10:11], scalar1=emt, scalar2=None,
                                               op0=ALU.max), r=[SM] + emt_keys, w=[SM])
        sy.op("dve", lambda e: e.reciprocal(out=smal[0:P, 1:2], in_=smal[0:P, 0:1]), r=[SM], w=[SM])
        sy.op("dve", lambda e: e.bn_stats(out=st6[0:P, :], in_=src[0:P, 0:128]), r=keys_r, w=[S6])
        sy.op("dve", lambda e: e.bn_aggr(out=smal[0:P, 2:4], in_=st6[0:P, :]), r=[S6], w=[SM])
        sy.op("dve", lambda e: e.tensor_tensor(out=smal[0:P, 4:5], in0=smal[0:P, 1:2], in1=smal[0:P, 1:2], op=ALU.mult),
              r=[SM], w=[SM])
        sy.op("dve", lambda e: e.tensor_tensor(out=smal[0:P, 6:7], in0=smal[0:P, 4:5], in1=smal[0:P, 3:4], op=ALU.mult),
              r=[SM], w=[SM])
        sy.op("act", lambda e: e.activation(out=smal[0:P, 7:8], in_=smal[0:P, 6:7], func=AF.Ln, bias=EPS),
              r=[SM], w=[SM])
        sy.op("act", lambda e: e.activation(out=smal[0:P, 7:8], in_=smal[0:P, 7:8], func=AF.Exp, scale=-0.5),
              r=[SM], w=[SM])
        sy.op("dve", lambda e: e.tensor_tensor(out=smal[0:P, 5:6], in0=smal[0:P, 7:8], in1=smal[0:P, 1:2], op=ALU.mult),
              r=[SM], w=[SM])
        sy.op("dve", lambda e: e.tensor_scalar(out=hm[0:P, h * 128:(h + 1) * 128], in0=src[0:P, 0:128],
                                               scalar1=smal[0:P, 2:3], scalar2=smal[0:P, 5:6], op0=ALU.subtract,
                                               op1=ALU.mult), r=keys_r + [SM], w=["hm"])

    def gate_and_store(i):
        sy.dma("sp", "ld4", lambda e: e.dma_start(out=sogt[:], in_=sog_s.ap()[i * 128:(i + 1) * 128, :]),
               r=["sog_s"], w=["sogt"])
        sy.op("pool", lambda e: e.tensor_tensor(out=sogt[:], in0=sogt[:], in1=mhg[:], op=ALU.mult),
              r=["sogt", "mhg"], w=["sogt"])
        sy.op("dve", lambda e: e.tensor_tensor(out=hm[:], in0=hm[:], in1=sogt[:], op=ALU.mult), r=["hm", "sogt"], w=["hm"])
        for h in range(4):
            sy.mm(lambda e, h=h: e.transpose(out=mtp[:, h, :], in_=hm[:, h * 128:(h + 1) * 128], identity=ident),
                  r=["hm", "cst"], w=["mtp"], last=(h == 3))
        sy.op("act", lambda e: e.copy(out=mts[:], in_=mtp[:]), r=["mtp"], w=["mts"])
        sy.dma("sp", "st2", lambda e: e.dma_start(out=minT_s.ap()[:, :, i * 128:(i + 1) * 128].rearrange("h p t -> p h t"),
                                                  in_=mts[:]), r=["mts"], wa=["minT_s"])

    load_chunk(0)
    for i in range(NT):
        t0 = i * 128
        b = i % 2
        load_chunk(i + 1)
        qk_, kk_, tk_, vk_ = "qmc%d" % b, "kmc%d" % b, "kmt%d" % b, "vm1%d" % b
        for h in range(4):
            p_ = h % 2
            PB["p"] = p_
            Wt, WIt, Stt, qst, kwt = WtL[p_], WItL[p_], SttL[p_], qstL[p_], kwtL[p_]
            scm, ndp, upp = scmL[p_], ndpL[p_], uppL[p_]
            sy.op("act", lambda e, h=h: e.activation(out=Wt[:], in_=MB[:, h, t0:t0 + 128], func=AF.Exp,
                                                     bias=acol[:, i, h:h + 1], scale=-1.0), r=["MB", "acol"], w=["Wt%d" % p_])
            sy.op("pool", lambda e: e.tensor_tensor(out=Wt[:], in0=Wt[:], in1=maskD, op=ALU.mult), r=["Wt%d" % p_, "cst"], w=["Wt%d" % p_])
            if i > 0:
                sy.op("act", lambda e, h=h: e.activation(out=WIt[:], in_=MB[:, h, t0:t0 + 128], func=AF.Exp,
                                                         bias=MB[:, h, t0 - 1:t0], scale=-1.0), r=["MB"], w=["WIt%d" % p_])
                sy.op("dve", lambda e, h=h: e.tensor_tensor(out=qst[:], in0=qmc[b][:, h, :], in1=WIt[0:64, :],
                                                            op=ALU.mult), r=[qk_, "WIt%d" % p_], w=["qst%d" % p_])
            sy.mm(lambda e, h=h: e.matmul(scm[:], lhsT=kmc[b][:, h, :], rhs=qmc[b][:, h, :],
                                          start=True, stop=True), r=[kk_, qk_], w=["scm%d" % p_])
            sy.op("dve", lambda e: e.tensor_tensor(out=Stt[:], in0=scm[:], in1=Wt[:], op=ALU.mult), r=["scm%d" % p_, "Wt%d" % p_], w=["Stt%d" % p_])
            sy.mm(lambda e, h=h: e.matmul(ndp[:], lhsT=Stt[:], rhs=vm1[b][:, h, :], start=True, stop=(i == 0)),
                  r=["Stt%d" % p_, vk_], w=["ndp%d" % p_], last=(i == 0))
            if i > 0:
                sy.mm(lambda e, h=h: e.matmul(ndp[:], lhsT=qst[:], rhs=Cst[:, h, :], start=False, stop=True),
                      r=["qst%d" % p_, "Cst%d" % h], w=["ndp%d" % p_])
            head_norm(ndp, 128, h, ["ndp%d" % p_], ecol[:, i, h:h + 1], ["ecol"])
            sy.op("dve", lambda e, h=h: e.tensor_scalar(out=kwt[:], in0=kmt[b][:, h * 64:(h + 1) * 64],
                                                        scalar1=Wt[:, 127:128], scalar2=None, op0=ALU.mult),
                  r=[tk_, "Wt%d" % p_], w=["kwt%d" % p_])
            sy.mm(lambda e, h=h: e.matmul(upp[:], lhsT=kwt[:], rhs=vm1[b][:, h, :], start=True, stop=True),
                  r=["kwt%d" % p_, vk_], w=["upp%d" % p_])
            if i == 0:
                sy.op("act", lambda e, h=h: e.copy(out=Cst[:, h, :], in_=upp[:]), r=["upp%d" % p_], w=["Cst%d" % h])
            else:
                sy.op("dve", lambda e, h=h: e.scalar_tensor_tensor(out=Cst[:, h, :], in0=Cst[:, h, :],
                                                                   scalar=WIt[0:64, 127:128], in1=upp[:], op0=ALU.mult,
                                                                   op1=ALU.add), r=["Cst%d" % h, "WIt%d" % p_, "upp%d" % p_], w=["Cst%d" % h])
        gate_and_store(i)
    ck_ = ["Cst%d" % h for h in range(4)]
    sy.dma("sp", "st0", lambda e: e.dma_start(out=cp_o.ap().rearrange("h p d -> p h d"), in_=Cst[:, :, 0:128]),
           r=ck_, is_out=True)
    sy.dma("sp", "st1", lambda e: e.dma_start(out=np_o.ap().rearrange("h p -> p h"), in_=Cst[:, :, 128], allow_slow_non_contiguous=True),
           r=ck_, is_out=True)

    PB["p"] = 0
    scm, ndp, upp = scmL[0], ndpL[0], uppL[0]
    bS = NT % 2
    qS, kS, tS, vS = "qmc%d" % bS, "kmc%d" % bS, "kmt%d" % bS, "vm1%d" % bS
    C0 = sb("C0", [64, NSO * HM, 129], st=st)
    sy.dma("sp", "ld0", lambda e: e.dma_start(out=C0[:], in_=stc_d.ap()), w=["C0"])
    qms = sb("qms", [NSO, 256], st=st)
    sy.dma("sp", "ld1", lambda e: e.dma_start(out=qms[:], in_=qms_s.ap()[0:NSO, :]), r=["qms_s"], w=["qms"])
    sg = sb("sg", [NSO, 12, 4], st=st)
    sy.mm(lambda e: e.transpose(out=scm[0:NSO, 0:4], in_=igt[0:4, S:S + NSO], identity=ident[0:4, 0:4]),
          r=["igt", "cst"], w=["scm0"], last=False)
    sy.mm(lambda e: e.transpose(out=scm[0:NSO, 4:8], in_=lft[0:4, S:S + NSO], identity=ident[0:4, 0:4]),
          r=["lft", "cst"], w=["scm0"])
    sy.op("act", lambda e: e.copy(out=sg[:, 0:2, :].rearrange("p a b -> p (a b)"), in_=scm[0:NSO, 0:8]), r=["scm0"], w=["sg"])
    A = lambda k: sg[:, k, :]
    sy.op("dve", lambda e: e.tensor_tensor(out=A(8), in0=A(1), in1=stm[:], op=ALU.add), r=["sg", "stm"], w=["sg"])
    sy.op("dve", lambda e: e.tensor_tensor(out=A(2), in0=A(8), in1=A(0), op=ALU.max), r=["sg"], w=["sg"])
    sy.op("dve", lambda e: e.tensor_tensor(out=A(9), in0=A(8), in1=A(2), op=ALU.subtract), r=["sg"], w=["sg"])
    sy.op("act", lambda e: e.activation(out=A(3), in_=A(9), func=AF.Exp), r=["sg"], w=["sg"])
    sy.op("dve", lambda e: e.tensor_tensor(out=A(10), in0=A(0), in1=A(2), op=ALU.subtract), r=["sg"], w=["sg"])
    sy.op("act", lambda e: e.activation(out=A(4), in_=A(10), func=AF.Exp), r=["sg"], w=["sg"])
    sy.op("act", lambda e: e.activation(out=A(5), in_=A(2), func=AF.Exp, scale=-1.0), r=["sg"], w=["sg"])
    sy.dma("sp", "st2", lambda e: e.dma_start(out=ms_o.ap(), in_=A(2)), r=["sg"], is_out=True)
    qkt = sb("qkt", [NSO, 256], st=st)
    sy.op("dve", lambda e: e.tensor_tensor(out=qkt[:], in0=qms[:], in1=kmt[bS][0:NSO, :], op=ALU.mult),
          r=["qms", tS], w=["qkt"])
    sy.op("dve", lambda e: e.tensor_reduce(out=A(6), in_=qkt[:].rearrange("p (h d) -> p h d", d=64), axis=AX.X,
                                           op=ALU.add), r=["qkt"], w=["sg"])
    sy.op("dve", lambda e: e.tensor_tensor(out=A(7), in0=A(6), in1=A(4), op=ALU.mult), r=["sg"], w=["sg"])
    wdg = sb("wdg", [NSO, NSO, 4], st=st)
    sy.op("dve", lambda e: e.tensor_tensor(out=wdg[:], in0=ident[0:NSO, 0:NSO].unsqueeze(2).to_broadcast([NSO, NSO, 4]),
                                           in1=A(3).unsqueeze(1).to_broadcast([NSO, NSO, 4]), op=ALU.mult),
          r=["sg", "cst"], w=["wdg"])
    sy.mm(lambda e: e.matmul(scm[0:64, 0:NSO * 4], lhsT=ones[0:NSO, 0:64], rhs=wdg[:].rearrange("p a b -> p (a b)"), start=True,
                             stop=True), r=["cst", "wdg"], w=["scm0"])
    WB = sb("WB", [64, NSO * 4], st=st)
    sy.op("act", lambda e: e.copy(out=WB[:], in_=scm[0:64, 0:NSO * 4]), r=["scm0"], w=["WB"])
    Qd = sb("Qd", [64, 4, NSO, NSO], st=st)
    sy.op("dve", lambda e: e.tensor_tensor(out=Qd[:], in0=qmc[bS][:, :, 0:NSO].unsqueeze(2).to_broadcast([64, 4, NSO, NSO]),
                                           in1=i4[0:64, :, :].unsqueeze(1).to_broadcast([64, 4, NSO, NSO]), op=ALU.mult),
          r=[qS, "i4"], w=["Qd"])
    sy.op("pool", lambda e: e.memset(hm[:], 0.0), w=["hm"])
    nds = sb("nds", [NSO, 129], st=st)
    cj = sb("cj", [NSO, 1], st=st)
    kws = sb("kws", [NSO, 64], st=st)
    Cn = sb("Cn", [64, NSO * 4, 129], st=st)
    for h in range(4):
        for j in range(NSO):
            sy.mm(lambda e, h=h, j=j: e.matmul(ndp[0:NSO, :], lhsT=Qd[:, h, j, :], rhs=C0[:, j * 4 + h, :],
                                               start=(j == 0), stop=(j == NSO - 1)), r=["Qd", "C0"], w=["ndp0"],
                  last=(j == NSO - 1))
        sy.op("dve", lambda e, h=h: e.tensor_scalar(out=nds[:], in0=ndp[0:NSO, :], scalar1=sg[:, 3, h:h + 1], scalar2=None,
                                                    op0=ALU.mult), r=["ndp0", "sg"], w=["nds"])
        sy.op("dve", lambda e, h=h: e.scalar_tensor_tensor(out=nds[:], in0=vm1[bS][0:NSO, h, :], scalar=sg[:, 7, h:h + 1],
                                                           in1=nds[:], op0=ALU.mult, op1=ALU.add),
              r=[vS, "sg", "nds"], w=["nds"])
        head_norm(nds, NSO, h, ["nds"], sg[:, 5, h:h + 1], ["sg"])
        for j in range(NSO):
            sy.op("dve", lambda e, h=h, j=j: e.tensor_tensor(out=cj[:], in0=ident[0:NSO, j:j + 1], in1=sg[:, 4, h:h + 1],
                                                             op=ALU.mult), r=["cst", "sg"], w=["cj"])
            sy.op("dve", lambda e, h=h: e.tensor_scalar(out=kws[:], in0=kmt[bS][0:NSO, h * 64:(h + 1) * 64],
                                                        scalar1=cj[:, 0:1], scalar2=None, op0=ALU.mult),
                  r=[tS, "cj"], w=["kws"])
            sy.mm(lambda e, h=h: e.matmul(upp[:], lhsT=kws[:], rhs=vm1[bS][0:NSO, h, :], start=True, stop=True),
                  r=["kws", vS], w=["upp0"])
            jh = j * 4 + h
            sy.op("dve", lambda e, jh=jh: e.scalar_tensor_tensor(out=Cn[:, jh, :], in0=C0[:, jh, :],
                                                                 scalar=WB[:, jh:jh + 1], in1=upp[:], op0=ALU.mult,
                                                                 op1=ALU.add), r=["C0", "WB", "upp0"], w=["Cn"])
    gate_and_store(NT)
    sy.dma("sp", "st0", lambda e: e.dma_start(out=cs_o.ap().rearrange("a p d -> p a d"), in_=Cn[:, :, 0:128]),
           r=["Cn"], is_out=True)
    sy.dma("sp", "st1", lambda e: e.dma_start(out=ns_o.ap().rearrange("a p -> p a"), in_=Cn[:, :, 128], allow_slow_non_contiguous=True),
           r=["Cn"], is_out=True)
    st.close()
    if cfg.get('stop') == '3':
        sy.finish()
        es.close()
        return nc

    sy.barrier()
    st = ExitStack()
    wba = sb("wba", [128, 4, D], st=st)
    wbm = sb("wbm", [128, 4, D], st=st)
    wo = sb("wo", [128, KC, D], st=st)
    wr = sb("wr", [128, KC, E], st=st)
    sy.dma("pool", "ld0", lambda e: e.dma_start(out=r_(wba[:]), in_=(wba_d.ap())), w=["wba"])
    sy.dma("pool", "ld1", lambda e: e.dma_start(out=r_(wbm[:]), in_=(wbm_d.ap())), w=["wbm"])
    sy.dma("pool", "ld2", lambda e: e.dma_start(out=r_(wo[:]), in_=(wo_d.ap())), w=["wo"])
    sy.dma("sp", "ld3", lambda e: e.dma_start(out=wr[:], in_=wr_d.ap()), w=["wr"])
    ain = sb("ain", [128, 4, 512], st=st)
    mi_ = sb("mi_", [128, 4, 512], st=st)
    sgat = sb("sgat", [128, KC, 512], st=st)
    sgmt = sb("sgmt", [128, KC, 512], st=st)
    mg = sb("mg", [128, KC, 512], st=st)
    t1 = sb("t1", [128, 512], st=st)
    abp = [ps("abp%d" % i, [128, 512], st=st) for i in range(2)]
    mxp = [ps("mxp%d" % i, [128, 512], st=st) for i in range(2)]
    htp = [ps("htp%d" % i, [128, 4, 128], st=st) for i in range(2)]
    lgp = ps("lgp", [128, 2, E], st=st)
    xtk = sb("xtk", [128, D], st=st)
    pre2 = [sb("pre%d" % i, [128, D], st=st) for i in range(2)]
    hT = sb("hT", [128, KC, 128], st=st)
    st12 = sb("st12", [128, 2, 6], st=st)
    mv = sb("mv", [128, 4], st=st)
    lg = sb("lg", [128, E], st=st)
    top8 = sb("top8", [128, 8], st=st)
    rt_ = sb("rt_", [128, 8], st=st)
    selt = sb("selt", [128, E], st=st)
    cntb = sb("cntb", [128, E], st=st)
    slp = sb("slp", [128, E], st=st)
    tmpE = sb("tmpE", [128, E], st=st)
    slf = sb("slf", [128, 4], st=st)
    sy.op("pool", lambda e: e.memset(cntb[:], 0.0), w=["cntb"])
    bc_reg = nc.gpsimd.alloc_register("bc_reg")
    nc.gpsimd.reg_mov(bc_reg, E * CAP - 1)

    def layer_norm(src, dst, gi, keys):
        for hf in range(2):
            sy.op("dve", lambda e, hf=hf: e.bn_stats(out=st12[:, hf, :], in_=src[:, hf * 512:(hf + 1) * 512]),
                  r=keys, w=["st12"])
        sy.op("dve", lambda e: e.bn_aggr(out=mv[:, 0:2], in_=st12[:].rearrange("p a b -> p (a b)")), r=["st12"], w=["mv"])
        sy.op("act", lambda e: e.activation(out=mv[:, 2:3], in_=mv[:, 1:2], func=AF.Ln, bias=EPS), r=["mv"], w=["mv"])
        sy.op("act", lambda e: e.activation(out=mv[:, 2:3], in_=mv[:, 2:3], func=AF.Exp, scale=-0.5), r=["mv"], w=["mv"])
        sy.op("dve", lambda e: e.tensor_scalar(out=dst[:], in0=src[:], scalar1=mv[:, 0:1], scalar2=mv[:, 2:3],
                                               op0=ALU.subtract, op1=ALU.mult), r=keys + ["mv"], w=keys)
        sy.op("dve", lambda e: e.tensor_tensor(out=dst[:], in0=dst[:], in1=lnp[:, gi, :], op=ALU.mult),
              r=keys + ["lnp"], w=keys)
        sy.op("pool", lambda e: e.tensor_tensor(out=dst[:], in0=dst[:], in1=lnp[:, gi + 1, :], op=ALU.add),
              r=keys + ["lnp"], w=keys)

    for (g0, gn) in tgroups:
        for (dst_t, src_s, nm, nch) in ((ain, ainT_s, "ain", 4), (mi_, minT_s, "mi_", 4), (sgat, sga_s, "sgat", KC),
                                        (sgmt, sgm_s, "sgmt", KC)):
            sy.dma("pool", "l" + nm, lambda e, dst_t=dst_t, src_s=src_s: e.dma_start(
                out=r_(dst_t[:, :, 0:gn]), in_=(src_s.ap()[:, :, g0:g0 + gn].rearrange("h p t -> p h t"))),
                r=[src_s.name], w=[nm])
        for dc in range(KC):
            pa, pm = abp[dc % 2], mxp[dc % 2]
            for h in range(4):
                sy.mm(lambda e, h=h, dc=dc: e.matmul(pa[:, 0:gn], lhsT=r_(wba[:, h, dc * 128:(dc + 1) * 128]),
                                                     rhs=r_(ain[:, h, 0:gn]), start=(h == 0), stop=(h == 3)),
                      r=["wba", "ain"], w=["abp%d" % (dc % 2)], last=(h == 3))
            for h in range(4):
                sy.mm(lambda e, h=h, dc=dc: e.matmul(pm[:, 0:gn], lhsT=r_(wbm[:, h, dc * 128:(dc + 1) * 128]),
                                                     rhs=r_(mi_[:, h, 0:gn]), start=(h == 0), stop=(h == 3)),
                      r=["wbm", "mi_"], w=["mxp%d" % (dc % 2)], last=(h == 3))
            sy.op("dve", lambda e, dc=dc: e.tensor_tensor(out=t1[:, 0:gn], in0=pa[:, 0:gn], in1=sgat[:, dc, 0:gn],
                                                          op=ALU.mult), r=["abp%d" % (dc % 2), "sgat"], w=["t1"])
            sy.op("dve", lambda e, dc=dc: e.tensor_tensor(out=r_(mg[:, dc, 0:gn]), in0=pm[:, 0:gn], in1=sgmt[:, dc, 0:gn],
                                                          op=ALU.mult), r=["mxp%d" % (dc % 2), "sgmt"], w=["mg%d" % dc])
            sy.op("pool", lambda e, dc=dc: e.tensor_tensor(out=r_(mg[:, dc, 0:gn]), in0=mg[:, dc, 0:gn], in1=t1[:, 0:gn],
                                                           op=ALU.add), r=["mg%d" % dc, "t1"], w=["mg%d" % dc])
        mgk = ["mg%d" % dc for dc in range(KC)]
        for tl in range(gn // 128):
            i = (g0 // 128) + tl
            pre = pre2[i % 2]
            pk = "pre%d" % (i % 2)
            sy.dma("sp", "lx", lambda e, i=i: e.dma_start(out=xtk[:], in_=xtok_d.ap()[i * 128:(i + 1) * 128, :]), w=["xtk"])
            for hf in range(2):
                for dc in range(KC):
                    sy.mm(lambda e, dc=dc, hf=hf: e.matmul(abp[hf][:], lhsT=r_(mg[:, dc, tl * 128:(tl + 1) * 128]),
                                                           rhs=r_(wo[:, dc, hf * 512:(hf + 1) * 512]), start=(dc == 0),
                                                           stop=(dc == KC - 1)), r=mgk + ["wo"], w=["abp%d" % hf],
                          last=(dc == KC - 1))
                sy.op("dve", lambda e, hf=hf: e.scalar_tensor_tensor(out=pre[:, hf * 512:(hf + 1) * 512],
                                                                     in0=xtk[:, hf * 512:(hf + 1) * 512], scalar=ALPHA,
                                                                     in1=abp[hf][:], op0=ALU.mult, op1=ALU.add),
                      r=["xtk", "abp%d" % hf], w=[pk])
            layer_norm(pre, pre, 0, [pk])
            sy.dma("sp", "sh", lambda e, i=i: e.dma_start(out=hs_s.ap()[i * 128:(i + 1) * 128, :], in_=pre[:]),
                   r=[pk], wa=["hs_s"])
            for q4 in range(2):
                for k4 in range(4):
                    kc = q4 * 4 + k4
                    sy.mm(lambda e, kc=kc, k4=k4, q4=q4: e.transpose(out=htp[q4][:, k4, :],
                                                                    in_=pre[:, kc * 128:(kc + 1) * 128], identity=ident),
                          r=[pk, "cst"], w=["htp%d" % q4], last=(k4 == 3))
                sy.op("act", lambda e, q4=q4: e.copy(out=hT[:, q4 * 4:(q4 + 1) * 4, :], in_=htp[q4][:]),
                      r=["htp%d" % q4], w=["hT"])
            for kc in range(KC):
                sy.mm(lambda e, kc=kc: e.matmul(lgp[:, 0, :], lhsT=hT[:, kc, :], rhs=wr[:, kc, :], start=(kc == 0),
                                                stop=(kc == KC - 1)), r=["hT", "wr"], w=["lgp0"], last=(kc == KC - 1))
            sy.op("dve", lambda e: e.tensor_tensor(out=lg[:], in0=lgp[:, 0, :], in1=brb[:], op=ALU.add),
                  r=["lgp0", "brb"], w=["lg"])
            sy.op("dve", lambda e: e.max(out=top8[:], in_=lg[:]), r=["lg"], w=["top8"])
            sy.op("dve", lambda e: e.tensor_scalar(out=rt_[:, 0:1], in0=top8[:, 0:1], scalar1=-1.0, scalar2=None,
                                                   op0=ALU.mult), r=["top8"], w=["rt_"])
            sy.op("act", lambda e: e.activation(out=rt_[:, 4:8], in_=top8[:, 0:4], func=AF.Exp, bias=rt_[:, 0:1]),
                  r=["top8", "rt_"], w=["rt_"])
            sy.op("dve", lambda e: e.tensor_reduce(out=rt_[:, 1:2], in_=rt_[:, 4:8], axis=AX.X, op=ALU.add),
                  r=["rt_"], w=["rt_"])
            sy.op("dve", lambda e: e.reciprocal(out=rt_[:, 2:3], in_=rt_[:, 1:2]), r=["rt_"], w=["rt_"])
            sy.op("dve", lambda e, i=i: e.tensor_scalar(out=wts[:, i, :], in0=rt_[:, 4:8], scalar1=rt_[:, 2:3],
                                                        scalar2=None, op0=ALU.mult), r=["rt_"], w=["wts"])
            sy.op("dve", lambda e: e.tensor_scalar(out=selt[:], in0=lg[:], scalar1=top8[:, 3:4], scalar2=None,
                                                   op0=ALU.is_ge), r=["lg", "top8"], w=["selt"])
            if i == NT:
                sy.op("dve", lambda e: e.tensor_scalar(out=selt[:], in0=selt[:], scalar1=rowm, scalar2=None,
                                                       op0=ALU.mult), r=["selt", "cst"], w=["selt"])
            sy.mm(lambda e: e.matmul(lgp[:, 1, :], lhsT=ltri, rhs=selt[:], start=True, stop=True),
                  r=["cst", "selt"], w=["lgp1"])
            sy.op("dve", lambda e: e.tensor_tensor(out=slp[:], in0=lgp[:, 1, :], in1=cntb[:], op=ALU.add),
                  r=["lgp1", "cntb"], w=["slp"])
            sy.op("dve", lambda e: e.tensor_scalar(out=tmpE[:], in0=slp[:], scalar1=float(CAP) - 0.5, scalar2=BIG,
                                                   op0=ALU.is_ge, op1=ALU.mult), r=["slp"], w=["tmpE"])
            sy.op("dve", lambda e: e.tensor_tensor(out=slp[:], in0=slp[:], in1=tmpE[:], op=ALU.add),
                  r=["slp", "tmpE"], w=["slp"])
            sy.op("dve", lambda e: e.tensor_scalar(out=tmpE[:], in0=selt[:], scalar1=-BIG, scalar2=BIG, op0=ALU.mult,
                                                   op1=ALU.add), r=["selt"], w=["tmpE"])
            sy.op("dve", lambda e: e.tensor_tensor(out=slp[:], in0=slp[:], in1=tmpE[:], op=ALU.add),
                  r=["slp", "tmpE"], w=["slp"])
            sy.op("dve", lambda e: e.tensor_tensor(out=slp[:], in0=slp[:], in1=ecap[:], op=ALU.add),
                  r=["slp", "ecap"], w=["slp"])
            sy.mm(lambda e: e.matmul(lgp[:, 1, :], lhsT=ones, rhs=selt[:], start=True, stop=True),
                  r=["cst", "selt", "slp"], w=["lgp1"])
            sy.op("dve", lambda e: e.tensor_tensor(out=cntb[:], in0=cntb[:], in1=lgp[:, 1, :], op=ALU.add),
                  r=["cntb", "lgp1"], w=["cntb"])
            for j in range(4):
                sy.op("dve", lambda e, j=j: e.tensor_scalar(out=tmpE[:], in0=lg[:], scalar1=top8[:, j:j + 1], scalar2=None,
                                                            op0=ALU.is_equal), r=["lg", "top8"], w=["tmpE"])
                sy.op("dve", lambda e: e.tensor_tensor(out=tmpE[:], in0=tmpE[:], in1=slp[:], op=ALU.mult),
                      r=["tmpE", "slp"], w=["tmpE"])
                sy.op("dve", lambda e, j=j: e.tensor_reduce(out=slf[:, j:j + 1], in_=tmpE[:], axis=AX.X, op=ALU.add),
                      r=["tmpE"], w=["slf"])
            sy.op("dve", lambda e: e.tensor_scalar(out=slf[:], in0=slf[:], scalar1=float(E * CAP + 64), scalar2=None,
                                                   op0=ALU.min), r=["slf"], w=["slf"])
            sy.op("dve", lambda e, i=i: e.tensor_copy(out=sli[:, i, :], in_=slf[:]), r=["slf"], w=["sli"])
            for j in range(4):
                sy.dma("pool", "scat%d" % j, lambda e, i=i, j=j: e.indirect_dma_start(
                    out=xg_s.ap(), out_offset=bass.IndirectOffsetOnAxis(ap=sli[:, i, j:j + 1], axis=0), in_=pre[:],
                    in_offset=None, bounds_check=bc_reg, oob_is_err=False), r=[pk, "sli", "xg_zero"], wa=["xg_s"])
    st.close()
    if cfg.get('stop') == '4':
        sy.finish()
        es.close()
        return nc

    sy.barrier()
    st = ExitStack()
    NWB = 8
    wbuf = [sb("wbuf%d" % i, [128, KC, 512], BF16, st=st) for i in range(NWB)]
    xe2 = [sb("xe%d" % i, [128, CT, D], st=st) for i in range(2)]
    xeT2 = [sb("xeT%d" % i, [128, KC, CAP], BF16, st=st) for i in range(2)]
    hid = sb("hid", [128, FC, CAP], BF16, st=st)
    gt2 = [sb("gt%d" % i, [128, CAP], st=st) for i in range(2)]
    ut2 = [sb("ut%d" % i, [128, CAP], st=st) for i in range(2)]
    sgt2 = [sb("sgt%d" % i, [128, CAP], st=st) for i in range(2)]
    bdb = sb("bdb", [128, D], st=st)
    yst = [sb("yst%d" % i, [128, D], st=st) for i in range(2)]
    ep = [ps("ep%d" % i, [128, 512], st=st) for i in range(6)]
    tpp = [ps("tpp%d" % i, [128, 4, 128], st=st) for i in range(2)]
    wi = 0
    pi_ = 0
    ti_ = 0
    yi_ = 0
    assert FF % 512 == 0 or FF < 512
    FH = max(1, FF // 512)
    FW = min(512, FF)
    for e_ in range(E):
        xe, xeT = xe2[e_ % 2], xeT2[e_ % 2]
        xek, xetk = "xe%d" % (e_ % 2), "xeT%d" % (e_ % 2)
        sy.dma("sp", "lxe%d" % (e_ % 2), lambda e, e_=e_, xe=xe: e.dma_start(
            out=xe[:], in_=xg_s.ap()[e_ * CAP:(e_ + 1) * CAP, :].rearrange("(a p) d -> p a d", p=128)),
            r=["xg_s", "xg_zero"], w=[xek])
        sy.dma("pool", "lbd", lambda e, e_=e_: e.dma_start(out=bdb[:], in_=bd_d.ap()[e_].partition_broadcast(128)), w=["bdb"])
        for a in range(CT):
            for q4 in range(KC // 4):
                tb = ti_ % 2
                ti_ += 1
                for k4 in range(4):
                    kc = q4 * 4 + k4
                    sy.mm(lambda e, a=a, kc=kc, k4=k4, tb=tb, xe=xe: e.transpose(out=tpp[tb][:, k4, :],
                                                                                 in_=xe[:, a, kc * 128:(kc + 1) * 128],
                                                                                 identity=ident),
                          r=[xek, "cst"], w=["tpp%d" % tb], last=(k4 == 3))
                sy.op("act", lambda e, a=a, q4=q4, tb=tb, xeT=xeT: e.copy(out=xeT[:, q4 * 4:(q4 + 1) * 4, a * 128:(a + 1) * 128],
                                                                          in_=tpp[tb][:]), r=["tpp%d" % tb], w=[xetk])
        for fh in range(FH):
            wg_b = wi % NWB
            wi += 1
            wu_b = wi % NWB
            wi += 1
            sy.dma("pool", "lw%d" % wg_b, lambda e, e_=e_, fh=fh, wg_b=wg_b: e.dma_start(
                out=wbuf[wg_b][:, :, 0:FW], in_=(wg_d.ap()[e_, :, :, fh * FW:(fh + 1) * FW])), w=["wbuf%d" % wg_b])
            sy.dma("pool", "lw%d" % wu_b, lambda e, e_=e_, fh=fh, wu_b=wu_b: e.dma_start(
                out=wbuf[wu_b][:, :, 0:FW], in_=(wu_d.ap()[e_, :, :, fh * FW:(fh + 1) * FW])), w=["wbuf%d" % wu_b])
            for f4 in range(FW // 128):
                fc = fh * (FW // 128) + f4
                gt, ut, sgt = gt2[fc % 2], ut2[fc % 2], sgt2[fc % 2]
                gk, uk, sk = "gt%d" % (fc % 2), "ut%d" % (fc % 2), "sgt%d" % (fc % 2)
                pg = pi_ % 6
                pi_ += 1
                pu = pi_ % 6
                pi_ += 1
                for kc in range(KC):
                    sy.mm(lambda e, kc=kc, f4=f4, pg=pg, wg_b=wg_b, xeT=xeT: e.matmul(
                        ep[pg][:, 0:CAP], lhsT=wbuf[wg_b][:, kc, f4 * 128:(f4 + 1) * 128], rhs=xeT[:, kc, :],
                        start=(kc == 0), stop=(kc == KC - 1)), r=["wbuf%d" % wg_b, xetk], w=["ep%d" % pg],
                        last=(kc == KC - 1))
                for kc in range(KC):
                    sy.mm(lambda e, kc=kc, f4=f4, pu=pu, wu_b=wu_b, xeT=xeT: e.matmul(
                        ep[pu][:, 0:CAP], lhsT=wbuf[wu_b][:, kc, f4 * 128:(f4 + 1) * 128], rhs=xeT[:, kc, :],
                        start=(kc == 0), stop=(kc == KC - 1)), r=["wbuf%d" % wu_b, xetk], w=["ep%d" % pu],
                        last=(kc == KC - 1))
                sy.op("dve", lambda e, pg=pg, fc=fc, e_=e_, gt=gt: e.tensor_scalar(
                    out=gt[:], in0=ep[pg][:, 0:CAP], scalar1=bgu[:, 0, e_, fc:fc + 1], scalar2=LIMIT, op0=ALU.add,
                    op1=ALU.min), r=["ep%d" % pg, "bgu"], w=[gk])
                sy.op("act", lambda e, gt=gt, sgt=sgt: e.activation(out=sgt[:], in_=gt[:], func=AF.Sigmoid, scale=SALPHA),
                      r=[gk], w=[sk])
                sy.op("dve", lambda e, pu=pu, fc=fc, e_=e_, ut=ut: e.tensor_scalar(
                    out=ut[:], in0=ep[pu][:, 0:CAP], scalar1=bu1[:, e_, fc:fc + 1], scalar2=LIMIT + 1.0, op0=ALU.add,
                    op1=ALU.min), r=["ep%d" % pu, "bu1"], w=[uk])
                sy.op("dve", lambda e, gt=gt, sgt=sgt: e.tensor_tensor(out=gt[:], in0=gt[:], in1=sgt[:], op=ALU.mult),
                      r=[gk, sk], w=[gk])
                sy.op("dve", lambda e, fc=fc, gt=gt, ut=ut: e.scalar_tensor_tensor(out=hid[:, fc, :], in0=ut[:],
                                                                                   scalar=1.0 - LIMIT, in1=gt[:],
                                                                                   op0=ALU.max, op1=ALU.mult),
                      r=[gk, uk], w=["hid%d" % fc])
        hk = ["hid%d" % fc for fc in range(FC)]
        wds = []
        for dh in range(D // 512):
            wd_b = wi % NWB
            wi += 1
            wds.append(wd_b)
            sy.dma("pool", "lw%d" % wd_b, lambda e, e_=e_, dh=dh, wd_b=wd_b: e.dma_start(
                out=wbuf[wd_b][:, 0:FC, :], in_=(wd_d.ap()[e_, :, :, dh * 512:(dh + 1) * 512])), w=["wbuf%d" % wd_b])
            if dh == 0 and NWB < 4:
                pass
        for a in range(CT):
            yb = yi_ % 2
            yi_ += 1
            for dh in range(D // 512):
                pd = pi_ % 6
                pi_ += 1
                for fc in range(FC):
                    sy.mm(lambda e, fc=fc, a=a, dh=dh, pd=pd: e.matmul(
                        ep[pd][:], lhsT=hid[:, fc, a * 128:(a + 1) * 128], rhs=wbuf[wds[dh]][:, fc, :],
                        start=(fc == 0), stop=(fc == FC - 1)), r=hk + ["wbuf%d" % wds[dh]], w=["ep%d" % pd],
                        last=(fc == FC - 1))
                sy.op("dve", lambda e, dh=dh, pd=pd, yb=yb: e.tensor_tensor(
                    out=yst[yb][:, dh * 512:(dh + 1) * 512], in0=ep[pd][:], in1=bdb[:, dh * 512:(dh + 1) * 512],
                    op=ALU.add), r=["ep%d" % pd, "bdb"], w=["yst%d" % yb])
            sy.dma("sp", "sy%d" % yb, lambda e, e_=e_, a=a, yb=yb: e.dma_start(
                out=yx_s.ap()[e_ * CAP + a * 128:e_ * CAP + (a + 1) * 128, :], in_=yst[yb][:]),
                r=["yst%d" % yb], wa=["yx_s"])
    st.close()
    if cfg.get('stop') == '5':
        sy.finish()
        es.close()
        return nc

    sy.barrier()
    st = ExitStack()
    yg = [sb("yg%d" % i, [128, 4, D], st=st) for i in range(2)]
    hb = [sb("hb%d" % i, [128, D], st=st) for i in range(2)]
    st12 = sb("st12b", [128, 2, 6], st=st)
    mv = sb("mvb", [128, 4], st=st)
    for b in range(2):
        sy.op("pool", lambda e, b=b: e.memset(yg[b][:], 0.0), w=["yg%d" % b])
    for i in range(NTT):
        b = i % 2
        for j in range(4):
            sy.dma("pool", "gat%d_%d" % (b, j), lambda e, i=i, j=j, b=b: e.indirect_dma_start(
                out=yg[b][:, j, :], out_offset=None, in_=yx_s.ap(),
                in_offset=bass.IndirectOffsetOnAxis(ap=sli[:, i, j:j + 1], axis=0), bounds_check=bc_reg,
                oob_is_err=False), r=["yx_s", "sli"], w=["yg%d" % b])
        sy.dma("sp", "lh%d" % b, lambda e, i=i, b=b: e.dma_start(out=hb[b][:], in_=hs_s.ap()[i * 128:(i + 1) * 128, :]),
               r=["hs_s"], w=["hb%d" % b])
        sy.op("dve", lambda e, b=b: e.tensor_scalar(out=hb[b][:], in0=hb[b][:], scalar1=ALPHA, scalar2=None, op0=ALU.mult),
              r=["hb%d" % b], w=["hb%d" % b])
        for j in range(4):
            sy.op("dve", lambda e, b=b, j=j, i=i: e.scalar_tensor_tensor(out=hb[b][:], in0=yg[b][:, j, :],
                                                                         scalar=wts[:, i, j:j + 1], in1=hb[b][:],
                                                                         op0=ALU.mult, op1=ALU.add),
                  r=["yg%d" % b, "wts", "hb%d" % b], w=["hb%d" % b])
        layer_norm(hb[b], hb[b], 2, ["hb%d" % b])
        if i < NT:
            sy.dma("sp", "so%d" % b, lambda e, i=i, b=b: e.dma_start(out=y_o.ap()[i * 128:(i + 1) * 128, :], in_=hb[b][:]),
                   r=["hb%d" % b], is_out=True)
        else:
            sy.dma("sp", "so%d" % b, lambda e, b=b: e.dma_start(out=ys_o.ap(), in_=hb[b][0:NSO, :]),
                   r=["hb%d" % b], is_out=True)
    sy.finish()
    st.close()
    es.close()
    return nc


def consts(cfg):
    S, PAST, E, CAP = cfg["S"], cfg["PAST"], cfg["E"], cfg["CAP"]
    NT = S // 128
    c = np.zeros((128, 1024), np.float32)
    c[:, 0:128] = np.eye(128, dtype=np.float32)
    c[:, 128:256] = 1.0
    p = np.arange(128)
    c[:, 256:384] = (p[:, None] < p[None, :]).astype(np.float32)
    c[:, 384:512] = (p[:, None] <= p[None, :]).astype(np.float32)
    c[:NSO, 512] = 1.0
    c[:PAST // 128, 513] = 1.0
    q = np.arange(512)
    cm = np.zeros((128, 4, 512), np.float32)
    for off in range(4):
        cm[:, off, :] = (q[None, :] >= off * 128 + p[:, None]).astype(np.float32)
    inv = (np.float32(THETA) ** (-np.arange(0, ROT, 2, dtype=np.float32) / np.float32(ROT))).astype(np.float32)
    pos = np.concatenate([np.arange(S), np.full(128, PAST)]).astype(np.float32)
    ang = pos[:, None] * inv[None, :]
    tab = np.concatenate([np.cos(ang), np.sin(ang)], axis=1).astype(np.float32)
    rope = np.ascontiguousarray(tab.reshape(NT + 1, 128, 16).transpose(1, 0, 2))
    ropes = np.ascontiguousarray(np.broadcast_to(tab[S], (128, 16))).astype(np.float32)
    ecap = np.ascontiguousarray(np.broadcast_to((np.arange(E) * CAP).astype(np.float32), (128, E)))
    i4 = np.ascontiguousarray(np.broadcast_to(np.eye(4, dtype=np.float32), (128, 4, 4)))
    return dict(cst=c, cm=cm, rope=rope, ropes=ropes, ecap=ecap, i4=i4)


def prep(cfg, inp):
    S, PAST, NPOOL, E, FF, CAP, D = (cfg[k] for k in ("S", "PAST", "NPOOL", "E", "FF", "CAP", "D"))
    KC, FC = D // 128, FF // 128
    T = S + 128
    f = lambda a: np.ascontiguousarray(np.asarray(a, dtype=np.float32))
    cs = consts(cfg)
    w_in = np.asarray(inp["w_in"][0], np.float32)
    win = f(w_in.reshape(KC, 128, -1).transpose(1, 0, 2))
    shared = dict(
        win=win,
        bigate=f(np.concatenate([inp["b_igate"][0], inp["b_fgate"][0]]).reshape(8, 1)),
        lamp=f(np.stack([inp["lambda_q1"][0], inp["lambda_k1"][0], inp["lambda_q2"][0], inp["lambda_k2"][0]])),
        subg=f(np.asarray(inp["subln_g"][0]).reshape(128, 1)),
        mhg=f(inp["mh_norm_g"][0]),
        wba=f(np.asarray(inp["w_ba"][0]).reshape(4, 128, D).transpose(1, 0, 2)),
        wbm=f(np.asarray(inp["w_bm"][0]).reshape(4, 128, D).transpose(1, 0, 2)),
        wo=f(np.asarray(inp["w_o"][0]).reshape(KC, 128, D).transpose(1, 0, 2)),
        lnp=f(np.stack([inp["ln1_g"][0], inp["ln1_b"][0], inp["ln2_g"][0], inp["ln2_b"][0]])),
        wr=f(np.asarray(inp["w_router"][0]).reshape(KC, 128, E).transpose(1, 0, 2)),
        br=f(inp["b_router"][0]),
        wg=f(np.asarray(inp["w_gate"][0]).reshape(E, KC, 128, FF).transpose(0, 2, 1, 3)),
        wu=f(np.asarray(inp["w_up"][0]).reshape(E, KC, 128, FF).transpose(0, 2, 1, 3)),
        wd=f(np.asarray(inp["w_down"][0]).reshape(E, FC, 128, D).transpose(0, 2, 1, 3)),
        bgu=f(np.stack([np.asarray(inp["b_gate"][0]).reshape(E, FC, 128), np.asarray(inp["b_up"][0]).reshape(E, FC, 128)])
              .transpose(3, 0, 1, 2)),
        bd=f(inp["b_down"][0]),
        **cs,
    )
    xp = np.asarray(inp["x_prompt"], np.float32)
    xs = np.asarray(inp["x_sample"], np.float32)[:, 0, :]
    ck = np.asarray(inp["cache_k"][0])
    cv = np.asarray(inp["cache_v"][0])
    pt = np.asarray(inp["page_table"]).astype(np.int32)
    ckh, cvh = {}, {}
    for h in range(HA):
        ckh[h] = np.ascontiguousarray(ck[:, :, h, :].reshape(NPOOL, 128 // 32, 32 * 128).transpose(1, 0, 2))
        cvh[h] = np.ascontiguousarray(cv[:, :, h, :].reshape(NPOOL, 128 // 32, 32 * 128).transpose(1, 0, 2))
    cols = {}
    for h in range(HA):
        cols[h] = np.concatenate([w_in[:, h * 128:(h + 1) * 128], w_in[:, 512 + h * 128:512 + (h + 1) * 128],
                                  w_in[:, 1024 + h * 128:1024 + (h + 1) * 128]], axis=1)
    whs_all = f(np.stack([cols[hh].reshape(KC, 128, 384).transpose(1, 0, 2) for hh in range(4)], axis=1))
    maps = []
    for c in range(8):
        h, g = c % 4, c // 4
        xt = np.zeros((T, D), np.float32)
        xt[:S] = xp[c]
        xt[S:S + NSO] = xs[4 * c:4 * c + 4]
        xT = f(xt.T.reshape(KC, 128, T).transpose(1, 0, 2))
        xo = xs[4 * c:4 * c + 4]
        xgT = np.zeros((128, 4, KC, NSG), np.float32)
        xoT = xo.T.reshape(KC, 128, NSO).transpose(1, 0, 2)
        for hh in range(4):
            xgT[:, hh, :, hh * 4:hh * 4 + 4] = xoT
        ptg = np.zeros((128, NSG), np.int32)
        for hh in range(4):
            ptg[:pt.shape[1], hh * 4:hh * 4 + 4] = pt[4 * c:4 * c + 4].T
        sel = np.zeros((128, 16), np.float32)
        for hh in range(4):
            for j in range(4):
                rank = g * 4 + hh
                sel[rank * 16 + (c % 4) * 4 + j, hh * 4 + j] = 1.0
        sc = np.asarray(inp["state_c"][0][4 * c:4 * c + 4], np.float32)
        sn = np.asarray(inp["state_n"][0][4 * c:4 * c + 4], np.float32)
        stc = np.concatenate([sc, sn[..., None]], axis=-1).reshape(NSO * HM, 64, 129).transpose(1, 0, 2)
        m = dict(shared)
        m.update(xT=xT, xtok=f(xt), xgT=xgT, whs=whs_all,
                 pt=ptg, selm=sel, stc=f(stc),
                 stm=f(inp["state_m"][0][4 * c:4 * c + 4]))
        for hh in range(4):
            for i in range(4):
                m["ck%d_%d" % (hh, i)] = ckh[hh][i]
                m["cv%d_%d" % (hh, i)] = cvh[hh][i]
        maps.append(m)
    return maps


def assemble(cfg, res):
    S, D = cfg["S"], cfg["D"]
    g = lambda n: np.stack([np.asarray(r[n]) for r in res])
    y_p = g("y")
    y_s = g("ysamp").reshape(32, 1, D)
    k_p = g("k").reshape(1, 8, S, HA, 128)
    v_p = g("v").reshape(1, 8, S, HA, 128)
    c_p = g("cp").reshape(1, 8, HM, 64, 128)
    n_p = g("npr").reshape(1, 8, HM, 64)
    m_p = g("mp").reshape(1, 8, HM)
    k_s = g("ksamp").reshape(1, 32, 1, HA, 128)
    v_s = g("vsamp").reshape(1, 32, 1, HA, 128)
    c_s = g("csamp").reshape(1, 32, HM, 64, 128)
    n_s = g("nsamp").reshape(1, 32, HM, 64)
    m_s = g("msamp").reshape(1, 32, HM)
    return tuple(np.ascontiguousarray(a, dtype=np.float32) for a in
                 (y_p, y_s, k_p, v_p, c_p, n_p, m_p, k_s, v_s, c_s, n_s, m_s))


def run(cfg, inp):
    nc = build(cfg)
    maps = prep(cfg, inp)
    res = run_bass_kernel_spmd(nc, maps, core_ids=list(range(8)))
    return assemble(cfg, res.results)


def kernel(**inputs):
    return run(FULL, inputs)
```

```python
import math
from contextlib import ExitStack

import numpy as np
import concourse.bass as bass
import concourse.mybir as mybir
from concourse.bass_utils import run_bass_kernel_spmd

F32 = mybir.dt.float32
F32R = mybir.dt.float32r
BF16 = mybir.dt.bfloat16
I32 = mybir.dt.int32
ALU = mybir.AluOpType
AF = mybir.ActivationFunctionType
AX = mybir.AxisListType

FULL = dict(S=2048, PAST=16384, NPOOL=5120, E=32, FF=1024, CAP=384, D=1024)

HA, DH = 4, 64
HM, DK, DV = 4, 64, 128
ROT = 16
THETA = 500000.0
SOFTCAP = 15.0
LIMIT = 7.0
SALPHA = 1.702
EPS = 1e-5
DEPTH = 1
ALPHA = (2.0 * DEPTH) ** 0.25
LAM_INIT = 0.8 - 0.6 * math.exp(-0.3 * 0)
NSO = 4
NSG = 16
BIG = 1.0e6


def r_(ap):
    return ap.bitcast(F32R)


class Sync:
    def __init__(self, nc, es):
        self.nc = nc
        self.es = es
        self.engs = {"pe": nc.tensor, "act": nc.scalar, "dve": nc.vector, "pool": nc.gpsimd, "sp": nc.sync}
        self.sem = {}
        self.cnt = {}
        for n in ("pe", "act", "dve", "pool"):
            self.sem[n] = es.enter_context(nc.semaphore("s_" + n))
            self.cnt[n] = 0
        self.seen = {n: {} for n in self.engs}
        self.last_w = {}
        self.readers = {}
        self.dsem = {}
        self.dcnt = {}
        self.out_tokens = []
        self.extra_tokens = []

    def _deps(self, r, w):
        deps = []
        for k in r:
            deps += self.last_w.get(k, [])
        for k in w:
            deps += self.readers.get(k, [])
            deps += self.last_w.get(k, [])
        return deps

    def barrier(self):
        toks = [("s_" + n, self.sem[n], self.cnt[n]) for n in self.sem if self.cnt[n] > 0]
        toks += [("d_" + k, self.dsem[k], self.dcnt[k]) for k in self.dsem if self.dcnt[k] > 0]
        toks += list(self.extra_tokens)
        for eng in self.engs:
            self._wait(eng, toks)

    def _wait(self, eng, deps):
        best = {}
        for (sn, sem, val) in deps:
            if best.get(sn, (None, 0))[1] < val:
                best[sn] = (sem, val)
        for sn, (sem, val) in best.items():
            if self.seen[eng].get(sn, 0) < val:
                self.engs[eng].wait_ge(sem, val)
                self.seen[eng][sn] = val

    def _record(self, tok, r, w, wa=()):
        for k in w:
            self.last_w[k] = [tok]
            self.readers[k] = []
        for k in wa:
            self.last_w.setdefault(k, []).append(tok)
        for k in r:
            if k not in w:
                self.readers.setdefault(k, []).append(tok)

    def op(self, eng, fn, r=(), w=()):
        self._wait(eng, self._deps(r, w))
        ins = fn(self.engs[eng])
        self.cnt[eng] += 1
        ins.then_inc(self.sem[eng], 1)
        tok = ("s_" + eng, self.sem[eng], self.cnt[eng])
        self._record(tok, r, w)
        return tok

    def mm(self, fn, r=(), w=(), last=True):
        self._wait("pe", self._deps(r, w))
        ins = fn(self.engs["pe"])
        if last:
            self.cnt["pe"] += 1
            ins.then_inc(self.sem["pe"], 1)
            tok = ("s_pe", self.sem["pe"], self.cnt["pe"])
            pr, pw = getattr(self, "_pend", ([], []))
            self._record(tok, list(r) + pr, list(w) + pw)
            self._pend = ([], [])
        else:
            pr, pw = getattr(self, "_pend", ([], []))
            self._pend = (pr + list(r), pw + list(w))

    def dma(self, q, key, fn, r=(), w=(), wa=(), is_out=False):
        if key not in self.dsem:
            self.dsem[key] = self.es.enter_context(self.nc.semaphore("d_" + key))
            self.dcnt[key] = 0
        self._wait(q, self._deps(r, w))
        ins = fn(self.engs[q])
        self.dcnt[key] += 16
        ins.then_inc(self.dsem[key], 16)
        tok = ("d_" + key, self.dsem[key], self.dcnt[key])
        self._record(tok, r, w, wa)
        if is_out:
            self.out_tokens.append(tok)
        return tok

    def finish(self):
        best = {}
        for (sn, sem, val) in self.out_tokens:
            if best.get(sn, (None, 0))[1] < val:
                best[sn] = (sem, val)
        for sn, (sem, val) in best.items():
            self.engs["sp"].wait_ge(sem, val)


def build(cfg):
    S, PAST, NPOOL, E, FF, CAP, D = (cfg[k] for k in ("S", "PAST", "NPOOL", "E", "FF", "CAP", "D"))
    KC = D // 128
    FC = FF // 128
    NT = S // 128
    T = S + 128
    NTT = NT + 1
    PAGES = PAST // 128
    assert PAGES <= 128
    NIN = 5128
    NQB = S // 512 if S >= 512 else 1
    QW = 512 if S >= 512 else S
    CT = CAP // 128
    RB = 32
    NRB = 128 // RB

    nc = bass.Bass("TRN2", target_bir_lowering=False)

    def din(name, shape, dt=F32):
        return nc.dram_tensor(name, list(shape), dt, kind="ExternalInput")

    def dout(name, shape, dt=F32):
        return nc.dram_tensor(name, list(shape), dt, kind="ExternalOutput")

    def dscr(name, shape, dt=F32):
        return nc.dram_tensor(name, list(shape), dt)

    xT_d = din("xT", [128, KC, T])
    xtok_d = din("xtok", [T, D])
    xgT_d = din("xgT", [128, 4, KC, NSG])
    win_d = din("win", [128, KC, NIN])
    whs_d = din("whs", [128, 4, KC, 384])
    ck_d = [[din("ck%d_%d" % (h, i), [NPOOL, RB * 128]) for i in range(NRB)] for h in range(HA)]
    cv_d = [[din("cv%d_%d" % (h, i), [NPOOL, RB * 128]) for i in range(NRB)] for h in range(HA)]
    pt_d = din("pt", [128, NSG], I32)
    sel_d = din("selm", [128, 16])
    stc_d = din("stc", [64, NSO * HM, 129])
    stm_d = din("stm", [NSO, HM])
    big_d = din("bigate", [8, 1])
    lamp_d = din("lamp", [4, 64])
    subg_d = din("subg", [128, 1])
    mhg_d = din("mhg", [512])
    wba_d = din("wba", [128, 4, D])
    wbm_d = din("wbm", [128, 4, D])
    wo_d = din("wo", [128, KC, D])
    ln_d = din("lnp", [4, D])
    wr_d = din("wr", [128, KC, E])
    br_d = din("br", [E])
    wg_d = din("wg", [E, 128, KC, FF])
    wu_d = din("wu", [E, 128, KC, FF])
    wd_d = din("wd", [E, 128, FC, D])
    bgu_d = din("bgu", [128, 2, E, FC])
    bd_d = din("bd", [E, D])
    cst_d = din("cst", [128, 1024])
    cm_d = din("cm", [128, 4, 512])
    rope_d = din("rope", [128, NTT, 16])
    ropes_d = din("ropes", [128, 16])
    ecap_d = din("ecap", [128, E])
    i4_d = din("i4", [128, 4, 4])

    y_o = dout("y", [S, D])
    ys_o = dout("ysamp", [NSO, D])
    k_o = dout("k", [S, 512])
    v_o = dout("v", [S, 512])
    cp_o = dout("cp", [HM, 64, 128])
    np_o = dout("npr", [HM, 64])
    mp_o = dout("mp", [HM, 1])
    ks_o = dout("ksamp", [NSO, 512])
    vs_o = dout("vsamp", [NSO, 512])
    cs_o = dout("csamp", [NSO * HM, 64, 128])
    ns_o = dout("nsamp", [NSO * HM, 64])
    ms_o = dout("msamp", [NSO, HM])

    qT_s = dscr("qT_s", [HA, 128, T])
    kT_s = dscr("kT_s", [HA, 128, T])
    v_s = dscr("v_s", [T, 512])
    qmT_s = dscr("qmT_s", [HM, 64, T])
    kmT_s = dscr("kmT_s", [HM, 64, T])
    km_s = dscr("km_s", [T, 256])
    qms_s = dscr("qms_s", [128, 256])
    vm_s = dscr("vm_s", [T, 512])
    ig_s = dscr("ig_s", [4, T])
    lf_s = dscr("lf_s", [4, T])
    sog_s = dscr("sog_s", [T, 512])
    sga_s = dscr("sga_s", [KC, 128, T])
    sgm_s = dscr("sgm_s", [KC, 128, T])
    ainT_s = dscr("ainT_s", [HA, 128, T])
    minT_s = dscr("minT_s", [4, 128, T])
    M_s = dscr("M_s", [4, S])
    qsb_s = dscr("qsb_s", [NSG, 128])
    agi_s = dscr("agi_s", [NSG, 128])
    ago_s = dscr("ago_s", [8 * NSG, 128])
    hs_s = dscr("hs_s", [T, D])
    xg_s = dscr("xg_s", [E * CAP, D])
    yx_s = dscr("yx_s", [E * CAP, D])

    es = ExitStack()
    sy = Sync(nc, es)

    def sb(name, shape, dt=F32, st=None):
        return (st or es).enter_context(nc.sbuf_tensor("t_" + name, list(shape), dt))

    def ps(name, shape, dt=F32, st=None):
        return (st or es).enter_context(nc.psum_tensor("p_" + name, list(shape), dt))

    cst = sb("cst", [128, 1024])
    ident = cst[:, 0:128]
    ones = cst[:, 128:256]
    ltri = cst[:, 256:384]
    maskD = cst[:, 384:512]
    rowm = cst[:, 512:513]
    zeros = cst[:, 640:1024]
    cm = sb("cm", [128, 4, 512])
    rope = sb("rope", [128, NTT, 16])
    ropes = sb("ropes", [128, 16])
    ecap = sb("ecap", [128, E])
    i4 = sb("i4", [128, 4, 4])
    lnp = sb("lnp", [128, 4, D])
    brb = sb("brb", [128, E])
    mhg = sb("mhg", [128, 512])
    subg = sb("subg", [128, 1])
    bigt = sb("bigt", [8, 1])
    lamp = sb("lamp", [128, 4, 64])
    bgu = sb("bgu", [128, 2, E, FC])
    stm = sb("stm", [NSO, HM])
    selm = sb("selm", [128, 16])
    ptt = sb("ptt", [128, NSG], I32)
    ainS = sb("ainS", [128, 16])
    wts = sb("wts", [128, NTT, 4])
    sli = sb("sli", [128, NTT, 4], I32)

    CK = ["cst", "cm", "rope", "ropes", "ecap", "i4", "lnp", "brb", "mhg", "subg", "bigt", "lamp", "bgu",
          "stm", "selm", "ptt"]
    loads = [
        (r_(cst[:]), r_(cst_d.ap())), (cm[:], cm_d.ap()), (rope[:], rope_d.ap()), (ropes[:], ropes_d.ap()),
        (ecap[:], ecap_d.ap()), (i4[:], i4_d.ap()),
        (lnp[:].rearrange("p a d -> p (a d)"), ln_d.ap().rearrange("a d -> (a d)").partition_broadcast(128)),
        (brb[:], br_d.ap().partition_broadcast(128)), (mhg[:], mhg_d.ap().partition_broadcast(128)),
        (subg[:], subg_d.ap()), (bigt[:], big_d.ap()),
        (lamp[:].rearrange("p a d -> p (a d)"), lamp_d.ap().rearrange("a d -> (a d)").partition_broadcast(128)),
        (bgu[:], bgu_d.ap()), (stm[:], stm_d.ap()), (selm[:], sel_d.ap()), (ptt[:], pt_d.ap()),
    ]
    for li, (o, i) in enumerate(loads):
        bc = li in (0, 6, 7, 8, 11)
        sy.dma("pool" if bc else "sp", "constb" if bc else "const", lambda e, o=o, i=i: e.dma_start(out=o, in_=i), w=[])
    ctok = [("d_const", sy.dsem["const"], sy.dcnt["const"]), ("d_constb", sy.dsem["constb"], sy.dcnt["constb"])]
    for k in CK:
        sy.last_w[k] = list(ctok)

    lam = sb("lam", [128, 4])
    ltmp = sb("ltmp", [128, 2, 64])
    sy.op("dve", lambda e: e.tensor_tensor(out=ltmp[:, 0, :], in0=lamp[:, 0, :], in1=lamp[:, 1, :], op=ALU.mult),
          r=["lamp"], w=["ltmp"])
    sy.op("dve", lambda e: e.tensor_tensor(out=ltmp[:, 1, :], in0=lamp[:, 2, :], in1=lamp[:, 3, :], op=ALU.mult),
          r=["lamp"], w=["ltmp"])
    sy.op("dve", lambda e: e.tensor_reduce(out=lam[:, 2:4], in_=ltmp[:], axis=AX.X, op=ALU.add),
          r=["ltmp"], w=["lam"])
    sy.op("act", lambda e: e.activation(out=lam[:, 2:4], in_=lam[:, 2:4], func=AF.Exp), r=["lam"], w=["lam"])
    sy.op("dve", lambda e: e.tensor_tensor(out=lam[:, 0:1], in0=lam[:, 2:3], in1=lam[:, 3:4], op=ALU.subtract),
          r=["lam"], w=["lam"])
    sy.op("dve", lambda e: e.tensor_scalar(out=lam[:, 0:1], in0=lam[:, 0:1], scalar1=LAM_INIT, scalar2=None,
                                           op0=ALU.add), r=["lam"], w=["lam"])
    sy.op("dve", lambda e: e.tensor_scalar(out=lam[:, 1:2], in0=lam[:, 0:1], scalar1=-1.0, scalar2=None,
                                           op0=ALU.mult), r=["lam"], w=["lam"])
    gsc = sb("gsc", [128, 1])
    sy.op("dve", lambda e: e.tensor_scalar(out=gsc[:], in0=subg[:], scalar1=(1.0 - LAM_INIT), scalar2=None,
                                           op0=ALU.mult), r=["subg"], w=["gsc"])
    bu1 = sb("bu1", [128, E, FC])
    sy.op("dve", lambda e: e.tensor_scalar(out=bu1[:], in0=bgu[:, 1, :, :], scalar1=1.0, scalar2=None, op0=ALU.add),
          r=["bgu"], w=["bu1"])
    bsc = sb("bsc", [8, 1])
    sy.op("dve", lambda e: e.tensor_scalar(out=bsc[:], in0=bigt[:], scalar1=1.0 / SOFTCAP, scalar2=None,
                                           op0=ALU.mult), r=["bigt"], w=["bsc"])

    zt = sb("zt", [128, D])
    sy.op("pool", lambda e: e.memset(zt[:], 0.0), w=["zt"])
    nz = (E * CAP) // 128
    for z0 in range(0, nz, 32):
        z1 = min(nz, z0 + 32)
        sy.dma("pool", "zero", lambda e, z0=z0, z1=z1: e.dma_start(
            out=xg_s.ap()[z0 * 128:z1 * 128, :].rearrange("(a p) d -> p a d", p=128),
            in_=zt[:].unsqueeze(1).to_broadcast([128, z1 - z0, D])), r=["zt"], wa=["xg_zero"])

    for h in range(HA):
        sy.dma("pool", "zero", lambda e, h=h: e.dma_start(out=ainT_s.ap()[h, :, S + NSO:T], in_=zt[:, 0:128 - NSO]),
               r=["zt"], wa=["ainT_s"])

    st = ExitStack()
    xgT = sb("xgT", [128, 4, KC, NSG], st=st)
    whs = sb("whs", [128, 4, KC, 384], st=st)
    sy.dma("sp", "ld0", lambda e: e.dma_start(out=xgT[:], in_=xgT_d.ap()), w=["xgT"])
    sy.dma("sp", "ld1", lambda e: e.dma_start(out=whs[:], in_=whs_d.ap()), w=["whs"])
    zs_ps = ps("zs_ps", [NSG, 384], st=st)
    for h in range(4):
        for kc in range(KC):
            sy.mm(lambda e, kc=kc, h=h: e.matmul(zs_ps[:], lhsT=xgT[:, h, kc, :], rhs=whs[:, h, kc, :],
                                                 start=(h == 0 and kc == 0), stop=(h == 3 and kc == KC - 1)),
                  r=["xgT", "whs"], w=["zs_ps"], last=(h == 3 and kc == KC - 1))
    zs = sb("zs", [NSG, 384], st=st)
    zv = zs_ps[:].rearrange("p (g d) -> p g d", d=64)
    zo = zs[:].rearrange("p (g d) -> p g d", d=64)
    cosb = ropes[0:NSG, 0:8].unsqueeze(1).to_broadcast([NSG, 4, 8])
    sinb = ropes[0:NSG, 8:16].unsqueeze(1).to_broadcast([NSG, 4, 8])
    rt = sb("rt", [NSG, 4, 4, 8], st=st)

    def rope_ops(src, dst, cosv, sinv, tmp, n, rk, wk_, tk):
        sy.op("dve", lambda e: e.tensor_tensor(out=tmp[:, 0], in0=src[:, :, 0:8], in1=cosv, op=ALU.mult),
              r=[rk], w=[tk])
        sy.op("dve", lambda e: e.tensor_tensor(out=tmp[:, 1], in0=src[:, :, 8:16], in1=sinv, op=ALU.mult),
              r=[rk], w=[tk])
        sy.op("dve", lambda e: e.tensor_tensor(out=tmp[:, 2], in0=src[:, :, 8:16], in1=cosv, op=ALU.mult),
              r=[rk], w=[tk])
        sy.op("dve", lambda e: e.tensor_tensor(out=tmp[:, 3], in0=src[:, :, 0:8], in1=sinv, op=ALU.mult),
              r=[rk], w=[tk])
        sy.op("dve", lambda e: e.tensor_tensor(out=dst[:, :, 0:8], in0=tmp[:, 0], in1=tmp[:, 1], op=ALU.subtract),
              r=[tk], w=[wk_])
        sy.op("dve", lambda e: e.tensor_tensor(out=dst[:, :, 8:16], in0=tmp[:, 2], in1=tmp[:, 3], op=ALU.add),
              r=[tk], w=[wk_])
        sy.op("act", lambda e: e.copy(out=dst[:, :, 16:64], in_=src[:, :, 16:64]), r=[rk], w=[wk_])

    rope_ops(zv[:, 0:4, :], zo[:, 0:4, :], cosb, sinb, rt[:], NSG, "zs_ps", "zs", "rt")
    sy.op("act", lambda e: e.copy(out=zs[:, 256:384], in_=zs_ps[:, 256:384]), r=["zs_ps"], w=["zs"])
    sy.dma("sp", "ld0", lambda e: e.dma_start(out=qsb_s.ap(), in_=zs[:, 0:128]), r=["zs"], w=["qsb_s"])
    qb = sb("qb", [128, NSG, 128], st=st)
    sy.dma("pool", "ldb", lambda e: e.dma_start(
        out=qb[:].rearrange("p j d -> p (j d)"),
        in_=qsb_s.ap().rearrange("j d -> (j d)").partition_broadcast(128)), r=["qsb_s"], w=["qb"])
    pn = sb("pn", [NSG, 2], st=st)
    pnt = sb("pnt", [NSG, 128], st=st)
    sy.op("dve", lambda e: e.tensor_tensor(out=pnt[:], in0=zs[:, 0:128], in1=zs[:, 128:256], op=ALU.mult),
          r=["zs"], w=["pnt"])
    sy.op("dve", lambda e: e.tensor_reduce(out=pn[:], in_=pnt[:].rearrange("p (c d) -> p c d", d=64), axis=AX.X,
                                           op=ALU.add), r=["pnt"], w=["pn"])
    sy.op("act", lambda e: e.activation(out=pn[:], in_=pn[:], func=AF.Exp, scale=DH ** -0.5), r=["pn"], w=["pn"])
    pnd = sb("pnd", [NSG, NSG, 2], st=st)
    sy.op("dve", lambda e: e.tensor_tensor(out=pnd[:], in0=ident[0:NSG, 0:NSG].unsqueeze(2).to_broadcast([NSG, NSG, 2]),
                                           in1=pn[:].unsqueeze(1).to_broadcast([NSG, NSG, 2]), op=ALU.mult),
          r=["pn", "cst"], w=["pnd"])

    kb = [sb("kb%d" % i, [128, RB, 128], st=st) for i in range(2)]
    vb = [sb("vb%d" % i, [128, RB, 128], st=st) for i in range(2)]
    ktmp = sb("ktmp", [128, RB, 128], st=st)
    vbb = [sb("vbb%d" % i, [128, RB, 128], BF16, st=st) for i in range(2)]
    scb = sb("scb", [128, 128, 2], BF16, st=st)
    sc_s = sb("sc_s", [128, 128, 2], st=st)
    prs = sb("prs", [128, 2], st=st)
    pv_ps = ps("pv_ps", [128, 2], st=st)
    dn_ps = ps("dn_ps", [128, 2], st=st)
    oS = sb("oS", [128, NSG], st=st)
    osm = sb("osm", [128, 8], st=st)
    blk = 0
    for j in range(NSG):
        for rb in range(NRB):
            b = blk % 2
            blk += 1
            sy.dma("pool", "kb%d" % b, lambda e, b=b, rb=rb, j=j: e.indirect_dma_start(
                out=kb[b][:].rearrange("p r d -> p (r d)"), out_offset=None, in_=ck_d[j // 4][rb].ap(),
                in_offset=bass.IndirectOffsetOnAxis(ap=ptt[:, j:j + 1], axis=0)), r=["ptt"], w=["kb%d" % b])
            sy.dma("pool", "vb%d" % b, lambda e, b=b, rb=rb, j=j: e.indirect_dma_start(
                out=vb[b][:].rearrange("p r d -> p (r d)"), out_offset=None, in_=cv_d[j // 4][rb].ap(),
                in_offset=bass.IndirectOffsetOnAxis(ap=ptt[:, j:j + 1], axis=0)), r=["ptt"], w=["vb%d" % b])
            sy.op("act", lambda e, b=b: e.copy(out=vbb[b][:], in_=vb[b][:]), r=["vb%d" % b], w=["vbb%d" % b])
            sy.op("dve", lambda e, b=b, j=j: e.tensor_tensor(
                out=ktmp[:], in0=kb[b][:], in1=qb[:, j, :].unsqueeze(1).to_broadcast([128, RB, 128]), op=ALU.mult),
                r=["kb%d" % b, "qb"], w=["ktmp"])
            sy.op("dve", lambda e, rb=rb: e.tensor_reduce(
                out=sc_s[:, rb * RB:(rb + 1) * RB, :], in_=ktmp[:].rearrange("p r (c d) -> p r c d", d=64),
                axis=AX.X, op=ALU.add), r=["ktmp"], w=["sc_s%d" % rb])
            sy.op("act", lambda e, rb=rb: e.activation(
                out=sc_s[:, rb * RB:(rb + 1) * RB, :], in_=sc_s[:, rb * RB:(rb + 1) * RB, :], func=AF.Exp,
                scale=DH ** -0.5), r=["sc_s%d" % rb], w=["sc_s%d" % rb])
            if PAGES < 128:
                sy.op("dve", lambda e, rb=rb: e.tensor_scalar(
                    out=sc_s[:, rb * RB:(rb + 1) * RB, :], in0=sc_s[:, rb * RB:(rb + 1) * RB, :],
                    scalar1=cst[:, 513:514], scalar2=None, op0=ALU.mult), r=["sc_s%d" % rb, "cst"], w=["sc_s%d" % rb])
            sy.op("act", lambda e, rb=rb: e.copy(out=scb[:, rb * RB:(rb + 1) * RB, :],
                                                 in_=sc_s[:, rb * RB:(rb + 1) * RB, :]),
                  r=["sc_s%d" % rb], w=["scb%d" % rb])
            for rr in range(RB):
                row = rb * RB + rr
                sy.mm(lambda e, b=b, rr=rr, row=row: e.matmul(
                    pv_ps[:], lhsT=vbb[b][:, rr, :], rhs=scb[:, row, :], start=(row == 0), stop=False),
                    r=["vbb%d" % b, "scb%d" % rb], w=["pv_ps"], last=(rr == RB - 1))
        sy.mm(lambda e, j=j: e.matmul(pv_ps[:], lhsT=zs[:, 256:384], rhs=pnd[:, j, :], start=False, stop=True),
              r=["zs", "pnd"], w=["pv_ps"])
        sy.op("dve", lambda e: e.tensor_reduce(out=prs[:], in_=sc_s[:].rearrange("p r c -> p c r"), axis=AX.X,
                                               op=ALU.add), r=["sc_s%d" % i for i in range(NRB)], w=["prs"])
        sy.mm(lambda e: e.matmul(dn_ps[:], lhsT=ones, rhs=prs[:], start=True, stop=False),
              r=["cst", "prs"], w=["dn_ps"], last=False)
        sy.mm(lambda e, j=j: e.matmul(dn_ps[:], lhsT=ones[0:NSG, :], rhs=pnd[:, j, :], start=False, stop=True),
              r=["cst", "pnd"], w=["dn_ps"])
        sy.op("dve", lambda e: e.reciprocal(out=osm[:, 0:2], in_=dn_ps[:]), r=["dn_ps"], w=["osm"])
        sy.op("dve", lambda e: e.tensor_tensor(out=osm[:, 2:4], in0=pv_ps[:], in1=osm[:, 0:2], op=ALU.mult),
              r=["pv_ps", "osm"], w=["osm"])
        sy.op("dve", lambda e, j=j: e.scalar_tensor_tensor(
            out=oS[:, j:j + 1], in0=osm[:, 3:4], scalar=lam[:, 1:2], in1=osm[:, 2:3], op0=ALU.mult, op1=ALU.add),
            r=["osm", "lam"], w=["oS"])
    osq = sb("osq", [128, NSG], st=st)
    ss_ps = ps("ss_ps", [128, NSG], st=st)
    sy.op("act", lambda e: e.activation(out=osq[:], in_=oS[:], func=AF.Square), r=["oS"], w=["osq"])
    sy.mm(lambda e: e.matmul(ss_ps[:], lhsT=ones, rhs=osq[:], start=True, stop=True), r=["cst", "osq"], w=["ss_ps"])
    sy.op("act", lambda e: e.activation(out=osq[:], in_=ss_ps[:], func=AF.Ln, bias=EPS, scale=1.0 / 128),
          r=["ss_ps"], w=["osq"])
    sy.op("act", lambda e: e.activation(out=osq[:], in_=osq[:], func=AF.Exp, scale=-0.5), r=["osq"], w=["osq"])
    sy.op("dve", lambda e: e.scalar_tensor_tensor(out=oS[:], in0=oS[:], scalar=gsc[:, 0:1], in1=osq[:],
                                                  op0=ALU.mult, op1=ALU.mult), r=["oS", "gsc", "osq"], w=["oS"])
    sy.op("act", lambda e: e.copy(out=ainS[:], in_=oS[:]), r=["oS"], w=["ainS"])
    st.close()
    if cfg.get('stop') == 'S':
        sy.finish()
        es.close()
        return nc
    for h in range(HA):
        sy.dma("sp", "st0", lambda e, h=h: e.dma_start(out=ainT_s.ap()[h, :, S:S + NSO], in_=ainS[:, h * 4:h * 4 + 4]),
               r=["ainS"], wa=["ainT_s"])

    sy.barrier()
    st = ExitStack()
    xT = sb("xT", [128, KC, T], st=st)
    sy.dma("pool", "ld0", lambda e: e.dma_start(out=r_(xT[:]), in_=(xT_d.ap())), w=["xT"])
    BW = 520
    wb = [sb("wb%d" % i, [128, KC, BW], st=st) for i in range(2)]
    blocks = [(0, 512), (512, 512), (1024, 512), (1536, 512), (2048, 512), (2560, 520), (3080, 512), (3592, 512),
              (4104, 512), (4616, 512)]
    zp = [ps("zp%d" % i, [128, 512], st=st) for i in range(4)]
    zsb = [sb("zsb%d" % i, [128, 512], st=st) for i in range(4)]
    rtt = sb("rtt", [128, 4, 8, 8], st=st)
    tp = [ps("tp%d" % i, [128, 4, 128], st=st) for i in range(2)]
    tsb = [sb("tsb%d" % i, [128, 4, 128], st=st) for i in range(2)]
    cnt = {"z": 0, "t": 0}
    tgroups = [(g * 512, min(512, S - g * 512)) for g in range((S + 511) // 512)] + [(S, 128)]

    def tok_major(bi, i, c0, n, post):
        zi = cnt["z"] % 4
        cnt["z"] += 1
        for kc in range(KC):
            sy.mm(lambda e, kc=kc: e.matmul(zp[zi][:, 0:n], lhsT=r_(xT[:, kc, i * 128:(i + 1) * 128]),
                                            rhs=r_(wb[bi % 2][:, kc, c0:c0 + n]), start=(kc == 0), stop=(kc == KC - 1)),
                  r=["xT", "wb%d" % (bi % 2)], w=["zp%d" % zi], last=(kc == KC - 1))
        post(zp[zi], zi)

    def feat_major(bi, c0, m, t0, n, post, cast=r_):
        zi = cnt["z"] % 4
        cnt["z"] += 1
        for kc in range(KC):
            sy.mm(lambda e, kc=kc: e.matmul(zp[zi][0:m, 0:n], lhsT=cast(wb[bi % 2][:, kc, c0:c0 + m]),
                                            rhs=cast(xT[:, kc, t0:t0 + n]), start=(kc == 0), stop=(kc == KC - 1)),
                  r=["xT", "wb%d" % (bi % 2)], w=["zp%d" % zi], last=(kc == KC - 1))
        post(zp[zi], zi)

    def transposes_out(src_tile, zi, dst, i):
        ti = cnt["t"] % 2
        cnt["t"] += 1
        for h in range(4):
            sy.mm(lambda e, h=h: e.transpose(out=tp[ti][:, h, :], in_=src_tile[:, h * 128:(h + 1) * 128],
                                             identity=ident), r=["zsb%d" % zi, "cst"], w=["tp%d" % ti], last=(h == 3))
        sy.op("act", lambda e: e.copy(out=tsb[ti][:], in_=tp[ti][:]), r=["tp%d" % ti], w=["tsb%d" % ti])
        sy.dma("sp", "st%d" % ti, lambda e: e.dma_start(
            out=dst.ap()[:, :, i * 128:(i + 1) * 128].rearrange("h p t -> p h t"), in_=tsb[ti][:]),
            r=["tsb%d" % ti], wa=[dst.name])

    for bi, (c_off, c_w) in enumerate(blocks):
        if bi >= cfg.get('nblk', 99):
            break
        sy.dma("pool", "wb%d" % (bi % 2), lambda e, bi=bi, c_off=c_off, c_w=c_w: e.dma_start(
            out=r_(wb[bi % 2][:, :, 0:c_w]), in_=(win_d.ap()[:, :, c_off:c_off + c_w])), w=["wb%d" % (bi % 2)])
        if bi in (0, 1):
            for i in range(NTT):
                def post(zpt, zi, i=i, bi=bi):
                    src = zpt[:].rearrange("p (g d) -> p g d", d=64)
                    dst = zsb[zi][:].rearrange("p (g d) -> p g d", d=64)
                    cosv = rope[:, i, 0:8].unsqueeze(1).to_broadcast([128, 8, 8])
                    sinv = rope[:, i, 8:16].unsqueeze(1).to_broadcast([128, 8, 8])
                    rope_ops(src, dst, cosv, sinv, rtt[:], 128, "zp%d" % zi, "zsb%d" % zi, "rtt")
                    if bi == 1:
                        if i < NT:
                            sy.dma("sp", "zo%d" % zi, lambda e: e.dma_start(out=k_o.ap()[i * 128:(i + 1) * 128, :],
                                                                            in_=zsb[zi][:]), r=["zsb%d" % zi], is_out=True)
                        else:
                            sy.dma("sp", "zo%d" % zi, lambda e: e.dma_start(out=ks_o.ap(), in_=zsb[zi][0:NSO, :]),
                                   r=["zsb%d" % zi], is_out=True)
                    transposes_out(zsb[zi], zi, qT_s if bi == 0 else kT_s, i)
                tok_major(bi, i, 0, 512, post)
        elif bi in (2, 4):
            for i in range(NTT):
                def post(zpt, zi, i=i, bi=bi):
                    sy.op("act", lambda e: e.copy(out=zsb[zi][:], in_=zpt[:]), r=["zp%d" % zi], w=["zsb%d" % zi])
                    dst = v_s if bi == 2 else vm_s
                    sy.dma("sp", "zo%d" % zi, lambda e: e.dma_start(out=dst.ap()[i * 128:(i + 1) * 128, :], in_=zsb[zi][:]),
                           r=["zsb%d" % zi], wa=[dst.name])
                    if bi == 2:
                        if i < NT:
                            sy.dma("sp", "zq%d" % zi, lambda e: e.dma_start(out=v_o.ap()[i * 128:(i + 1) * 128, :],
                                                                            in_=zsb[zi][:]), r=["zsb%d" % zi], is_out=True)
                        else:
                            sy.dma("sp", "zq%d" % zi, lambda e: e.dma_start(out=vs_o.ap(), in_=zsb[zi][0:NSO, :]),
                                   r=["zsb%d" % zi], is_out=True)
                tok_major(bi, i, 0, 512, post)
        elif bi == 3:
            for (t0, n) in tgroups:
                for ch in range(4):
                    def post(zpt, zi, ch=ch, t0=t0, n=n):
                        if ch < 2:
                            sy.op("act", lambda e: e.copy(out=zsb[zi][:, 0:n], in_=zpt[:, 0:n]), r=["zp%d" % zi],
                                  w=["zsb%d" % zi])
                            dst = qmT_s
                        else:
                            sy.op("act", lambda e: e.mul(out=zsb[zi][:, 0:n], in_=zpt[:, 0:n], mul=DK ** -0.5),
                                  r=["zp%d" % zi], w=["zsb%d" % zi])
                            dst = kmT_s
                        hh = (ch % 2) * 2
                        sy.dma("sp", "zo%d" % zi, lambda e: e.dma_start(
                            out=dst.ap()[hh:hh + 2, :, t0:t0 + n].rearrange("h p t -> (h p) t"), in_=zsb[zi][:, 0:n]),
                            r=["zsb%d" % zi], wa=[dst.name])
                    feat_major(bi, ch * 128, 128, t0, n, post)
            for i in range(NTT):
                def post(zpt, zi, i=i):
                    sy.op("act", lambda e: e.mul(out=zsb[zi][:, 0:256], in_=zpt[:, 0:256], mul=DK ** -0.5),
                          r=["zp%d" % zi], w=["zsb%d" % zi])
                    sy.dma("sp", "zo%d" % zi, lambda e: e.dma_start(out=km_s.ap()[i * 128:(i + 1) * 128, :],
                                                                    in_=zsb[zi][:, 0:256]), r=["zsb%d" % zi], wa=["km_s"])
                tok_major(bi, i, 256, 256, post)

            def post(zpt, zi):
                sy.op("act", lambda e: e.copy(out=zsb[zi][:, 0:256], in_=zpt[:, 0:256]), r=["zp%d" % zi], w=["zsb%d" % zi])
                sy.dma("sp", "zo%d" % zi, lambda e: e.dma_start(out=qms_s.ap(), in_=zsb[zi][:, 0:256]),
                       r=["zsb%d" % zi], wa=["qms_s"])
            tok_major(bi, NT, 0, 256, post)
        elif bi == 5:
            for (t0, n) in tgroups:
                def post(zpt, zi, t0=t0, n=n):
                    a = zsb[zi]
                    sy.op("act", lambda e: e.activation(out=a[0:8, 0:n], in_=zpt[0:8, 0:n], func=AF.Tanh,
                                                        bias=bsc[:, 0:1], scale=1.0 / SOFTCAP),
                          r=["zp%d" % zi, "bsc"], w=["zsb%d" % zi])
                    sy.op("dve", lambda e: e.tensor_scalar(out=a[0:8, 0:n], in0=a[0:8, 0:n], scalar1=SOFTCAP,
                                                           scalar2=None, op0=ALU.mult), r=["zsb%d" % zi], w=["zsb%d" % zi])
                    sy.dma("sp", "zo%d" % zi, lambda e: e.dma_start(out=ig_s.ap()[:, t0:t0 + n], in_=a[0:4, 0:n]),
                           r=["zsb%d" % zi], wa=["ig_s"])
                    sy.op("act", lambda e: e.activation(out=a[0:8, 0:n], in_=a[0:8, 0:n], func=AF.Exp, scale=-1.0),
                          r=["zsb%d" % zi], w=["zsb%d" % zi])
                    sy.op("act", lambda e: e.activation(out=a[0:8, 0:n], in_=a[0:8, 0:n], func=AF.Ln, bias=1.0),
                          r=["zsb%d" % zi], w=["zsb%d" % zi])
                    sy.op("dve", lambda e: e.tensor_scalar(out=a[0:8, 0:n], in0=a[0:8, 0:n], scalar1=-1.0,
                                                           scalar2=None, op0=ALU.mult), r=["zsb%d" % zi], w=["zsb%d" % zi])
                    sy.dma("sp", "zq%d" % zi, lambda e: e.dma_start(out=lf_s.ap()[:, t0:t0 + n], in_=a[4:8, 0:n]),
                           r=["zsb%d" % zi], wa=["lf_s"])
                feat_major(bi, 0, 8, t0, n, post, cast=lambda a: a)
            for i in range(NTT):
                def post(zpt, zi, i=i):
                    sy.op("act", lambda e: e.activation(out=zsb[zi][:], in_=zpt[:], func=AF.Sigmoid),
                          r=["zp%d" % zi], w=["zsb%d" % zi])
                    sy.dma("sp", "zo%d" % zi, lambda e: e.dma_start(out=sog_s.ap()[i * 128:(i + 1) * 128, :],
                                                                    in_=zsb[zi][:]), r=["zsb%d" % zi], wa=["sog_s"])
                tok_major(bi, i, 8, 512, post)
        else:
            dst = sga_s if bi in (6, 7) else sgm_s
            base = ((bi - 6) % 2) * 4
            for (t0, n) in tgroups:
                for ch in range(4):
                    def post(zpt, zi, ch=ch, t0=t0, n=n, dst=dst, base=base):
                        sy.op("act", lambda e: e.activation(out=zsb[zi][:, 0:n], in_=zpt[:, 0:n], func=AF.Sigmoid),
                              r=["zp%d" % zi], w=["zsb%d" % zi])
                        sy.dma("sp", "zo%d" % zi, lambda e: e.dma_start(out=dst.ap()[base + ch, :, t0:t0 + n],
                                                                        in_=zsb[zi][:, 0:n]), r=["zsb%d" % zi], wa=[dst.name])
                    feat_major(bi, ch * 128, 128, t0, n, post)
    st.close()
    if cfg.get('stop') == '1':
        sy.finish()
        es.close()
        return nc

    sy.barrier()
    st = ExitStack()
    qTh = sb("qTh", [128, S], st=st)
    kTh = sb("kTh", [128, S], st=st)
    vh = sb("vh", [128, NT, 128], st=st)
    scp = [ps("scp%d" % i, [128, 512], st=st) for i in range(4)]
    dnp = [ps("dnp%d" % i, [128, 512], st=st) for i in range(2)]
    pvp = [ps("pvp%d" % i, [128, 512], st=st) for i in range(2)]
    ptl = [sb("ptl%d" % i, [128, 512], st=st) for i in range(4)]
    o_t = sb("o_t", [128, 512], st=st)
    t_t = sb("t_t", [128, 512], st=st)
    rd_t = sb("rd_t", [128, 512], st=st)
    sq_t = sb("sq_t", [128, 512], st=st)
    it = 0
    for h in range(HA):
        sy.dma("pool", "ld0", lambda e, h=h: e.dma_start(out=r_(qTh[:]), in_=(qT_s.ap()[h, :, 0:S])), r=["qT_s"], w=["qTh"])
        sy.dma("pool", "ld1", lambda e, h=h: e.dma_start(out=r_(kTh[:]), in_=(kT_s.ap()[h, :, 0:S])), r=["kT_s"], w=["kTh"])
        sy.dma("pool", "ld2", lambda e, h=h: e.dma_start(
            out=r_(vh[:]), in_=(v_s.ap()[0:S, h * 128:(h + 1) * 128].rearrange("(i p) d -> p i d", p=128))),
            r=["v_s"], w=["vh"])
        for qblk in range(NQB):
            q0 = qblk * QW
            nkt = (q0 + QW) // 128
            for kt in range(nkt):
                for c in range(2):
                    bi_ = it % 4
                    it += 1
                    sy.mm(lambda e, c=c, kt=kt, bi_=bi_: e.matmul(
                        scp[bi_][:, 0:QW], lhsT=r_(kTh[c * 64:(c + 1) * 64, kt * 128:(kt + 1) * 128]),
                        rhs=r_(qTh[c * 64:(c + 1) * 64, q0:q0 + QW]), start=True, stop=True),
                        r=["qTh", "kTh"], w=["scp%d" % bi_])
                    sy.op("act", lambda e, bi_=bi_: e.activation(out=r_(ptl[bi_][:, 0:QW]), in_=scp[bi_][:, 0:QW],
                                                                 func=AF.Exp, scale=DH ** -0.5),
                          r=["scp%d" % bi_], w=["ptl%d" % bi_])
                    off = kt - q0 // 128
                    if off >= 0:
                        sy.op("dve", lambda e, bi_=bi_, off=off: e.tensor_tensor(
                            out=r_(ptl[bi_][:, 0:QW]), in0=ptl[bi_][:, 0:QW], in1=cm[:, off, 0:QW], op=ALU.mult),
                            r=["ptl%d" % bi_, "cm"], w=["ptl%d" % bi_])
                    sy.mm(lambda e, c=c, kt=kt, bi_=bi_: e.matmul(
                        dnp[c][:, 0:QW], lhsT=r_(ones), rhs=r_(ptl[bi_][:, 0:QW]), start=(kt == 0), stop=(kt == nkt - 1)),
                        r=["cst", "ptl%d" % bi_], w=["dnp%d" % c], last=False)
                    sy.mm(lambda e, c=c, kt=kt, bi_=bi_: e.matmul(
                        pvp[c][:, 0:QW], lhsT=r_(vh[:, kt, :]), rhs=r_(ptl[bi_][:, 0:QW]), start=(kt == 0),
                        stop=(kt == nkt - 1)), r=["vh", "ptl%d" % bi_], w=["pvp%d" % c])
            W_ = QW
            sy.op("dve", lambda e: e.reciprocal(out=rd_t[:, 0:W_], in_=dnp[0][:, 0:W_]), r=["dnp0"], w=["rd_t"])
            sy.op("dve", lambda e: e.tensor_tensor(out=o_t[:, 0:W_], in0=pvp[0][:, 0:W_], in1=rd_t[:, 0:W_], op=ALU.mult),
                  r=["pvp0", "rd_t"], w=["o_t"])
            sy.op("dve", lambda e: e.reciprocal(out=rd_t[:, 0:W_], in_=dnp[1][:, 0:W_]), r=["dnp1"], w=["rd_t"])
            sy.op("dve", lambda e: e.tensor_tensor(out=t_t[:, 0:W_], in0=pvp[1][:, 0:W_], in1=rd_t[:, 0:W_], op=ALU.mult),
                  r=["pvp1", "rd_t"], w=["t_t"])
            sy.op("dve", lambda e: e.scalar_tensor_tensor(out=o_t[:, 0:W_], in0=t_t[:, 0:W_], scalar=lam[:, 1:2],
                                                          in1=o_t[:, 0:W_], op0=ALU.mult, op1=ALU.add),
                  r=["t_t", "o_t", "lam"], w=["o_t"])
            sy.op("act", lambda e: e.activation(out=r_(sq_t[:, 0:W_]), in_=o_t[:, 0:W_], func=AF.Square), r=["o_t"], w=["sq_t"])
            sy.mm(lambda e: e.matmul(dnp[0][:, 0:W_], lhsT=r_(ones), rhs=r_(sq_t[:, 0:W_]), start=True, stop=True),
                  r=["cst", "sq_t"], w=["dnp0"])
            sy.op("act", lambda e: e.activation(out=rd_t[:, 0:W_], in_=dnp[0][:, 0:W_], func=AF.Ln, bias=EPS,
                                                scale=1.0 / 128), r=["dnp0"], w=["rd_t"])
            sy.op("act", lambda e: e.activation(out=rd_t[:, 0:W_], in_=rd_t[:, 0:W_], func=AF.Exp, scale=-0.5),
                  r=["rd_t"], w=["rd_t"])
            sy.op("dve", lambda e: e.scalar_tensor_tensor(out=o_t[:, 0:W_], in0=o_t[:, 0:W_], scalar=gsc[:, 0:1],
                                                          in1=rd_t[:, 0:W_], op0=ALU.mult, op1=ALU.mult),
                  r=["o_t", "gsc", "rd_t"], w=["o_t"])
            sy.dma("sp", "st0", lambda e, h=h, q0=q0: e.dma_start(out=ainT_s.ap()[h, :, q0:q0 + W_], in_=o_t[:, 0:W_]),
                   r=["o_t"], wa=["ainT_s"])
    st.close()
    if cfg.get('stop') == '2':
        sy.finish()
        es.close()
        return nc

    sy.barrier()
    st = ExitStack()
    igt = sb("igt", [4, T], st=st)
    lft = sb("lft", [4, T], st=st)
    sy.dma("sp", "ld0", lambda e: e.dma_start(out=igt[:], in_=ig_s.ap()), r=["ig_s"], w=["igt"])
    sy.dma("sp", "ld1", lambda e: e.dma_start(out=lft[:], in_=lf_s.ap()), r=["lf_s"], w=["lft"])
    Bt = sb("Bt", [4, S], st=st)
    at = sb("at", [4, S], st=st)
    Mt = sb("Mt", [4, S], st=st)
    mt = sb("mtot", [4, S], st=st)
    o4 = sb("o4", [4, S], st=st)
    sy.op("pool", lambda e: e.memset(o4[:], 1.0), w=["o4"])
    sy.op("dve", lambda e: e.tensor_tensor_scan(out=Bt[:], data0=o4[:], data1=lft[:, 0:S],
                                                initial=0.0, op0=ALU.mult, op1=ALU.add), r=["lft", "o4"], w=["Bt"])
    sy.op("dve", lambda e: e.tensor_tensor(out=at[:], in0=igt[:, 0:S], in1=Bt[:], op=ALU.subtract),
          r=["igt", "Bt"], w=["at"])
    sy.op("dve", lambda e: e.tensor_tensor_scan(out=Mt[:], data0=at[:], data1=at[:], initial=0.0, op0=ALU.max,
                                                op1=ALU.max), r=["at"], w=["Mt"])
    sy.op("dve", lambda e: e.tensor_tensor(out=mt[:], in0=Bt[:], in1=Mt[:], op=ALU.add), r=["Bt", "Mt"], w=["mtot"])
    sy.dma("sp", "st0", lambda e: e.dma_start(out=mp_o.ap(), in_=mt[:, S - 1:S]), r=["mtot"], is_out=True)
    sy.dma("sp", "st1", lambda e: e.dma_start(out=M_s.ap(), in_=Mt[:]), r=["Mt"], w=["M_s"])
    MB = sb("MB", [128, 4, S], st=st)
    sy.dma("pool", "ldb", lambda e: e.dma_start(out=MB[:].rearrange("p h t -> p (h t)"),
                                              in_=M_s.ap().rearrange("h t -> (h t)").partition_broadcast(128)),
           r=["M_s"], w=["MB"])
    acol = sb("acol", [128, NT, 4], st=st)
    ecol = sb("ecol", [128, NT, 4], st=st)
    colp = ps("colp", [128, 2, NT * 4], st=st)
    for i in range(NT):
        sy.mm(lambda e, i=i: e.transpose(out=colp[:, 0, i * 4:(i + 1) * 4], in_=at[0:4, i * 128:(i + 1) * 128],
                                         identity=ident[0:4, 0:4]), r=["at", "cst"], w=["colp"], last=False)
        sy.mm(lambda e, i=i: e.transpose(out=colp[:, 1, i * 4:(i + 1) * 4], in_=mt[0:4, i * 128:(i + 1) * 128],
                                         identity=ident[0:4, 0:4]), r=["mtot", "cst"], w=["colp"], last=(i == NT - 1))
    sy.op("act", lambda e: e.copy(out=acol[:].rearrange("p i h -> p (i h)"), in_=colp[:, 0, :]), r=["colp"], w=["acol"])
    sy.op("act", lambda e: e.activation(out=ecol[:].rearrange("p i h -> p (i h)"), in_=colp[:, 1, :], func=AF.Exp,
                                        scale=-1.0), r=["colp"], w=["ecol"])
    qmc = [sb("qmc%d" % i, [64, 4, 128], st=st) for i in range(2)]
    kmc = [sb("kmc%d" % i, [64, 4, 128], st=st) for i in range(2)]
    kmt = [sb("kmt%d" % i, [128, 256], st=st) for i in range(2)]
    vm1 = [sb("vm1%d" % i, [128, 4, 129], st=st) for i in range(2)]
    for b in range(2):
        sy.op("pool", lambda e, b=b: e.memset(vm1[b][:], 1.0), w=["vm1%d" % b])

    def load_chunk(i):
        b = i % 2
        t0 = i * 128
        sy.dma("sp", "lq%d" % b, lambda e: e.dma_start(out=qmc[b][:], in_=qmT_s.ap()[:, :, t0:t0 + 128].rearrange("h p t -> p h t")),
               r=["qmT_s"], w=["qmc%d" % b])
        sy.dma("sp", "lk%d" % b, lambda e: e.dma_start(out=kmc[b][:], in_=kmT_s.ap()[:, :, t0:t0 + 128].rearrange("h p t -> p h t")),
               r=["kmT_s"], w=["kmc%d" % b])
        sy.dma("sp", "lt%d" % b, lambda e: e.dma_start(out=kmt[b][:], in_=km_s.ap()[t0:t0 + 128, :]), r=["km_s"], w=["kmt%d" % b])
        sy.dma("sp", "lv%d" % b, lambda e: e.dma_start(out=vm1[b][:, :, 0:128],
                                                       in_=vm_s.ap()[t0:t0 + 128, :].rearrange("p (h d) -> p h d", d=128)),
               r=["vm_s"], w=["vm1%d" % b])

    Cst = sb("Cst", [64, 4, 129], st=st)
    WtL = [sb("Wt%d" % i, [128, 128], st=st) for i in range(2)]
    WItL = [sb("WIt%d" % i, [128, 128], st=st) for i in range(2)]
    SttL = [sb("Stt%d" % i, [128, 128], st=st) for i in range(2)]
    qstL = [sb("qst%d" % i, [64, 128], st=st) for i in range(2)]
    kwtL = [sb("kwt%d" % i, [128, 64], st=st) for i in range(2)]
    hm = sb("hm", [128, 512], st=st)
    sogt = sb("sogt", [128, 512], st=st)
    smalL = [sb("smal%d" % i, [128, 16], st=st) for i in range(2)]
    st6L = [sb("st6%d" % i, [128, 6], st=st) for i in range(2)]
    scmL = [ps("scm%d" % i, [128, 128], st=st) for i in range(2)]
    ndpL = [ps("ndp%d" % i, [128, 129], st=st) for i in range(2)]
    uppL = [ps("upp%d" % i, [64, 129], st=st) for i in range(2)]
    PB = {"p": 0}
    mtp = ps("mtp", [128, 4, 128], st=st)
    mts = sb("mts", [128, 4, 128], st=st)

    def head_norm(src, npart, h, keys_r, emt, emt_keys):
        P = npart
        smal, st6 = smalL[PB["p"]], st6L[PB["p"]]
        SM, S6 = "smal%d" % PB["p"], "st6%d" % PB["p"]
        sy.op("dve", lambda e: e.tensor_scalar(out=smal[0:P, 9:10], in0=src[0:P, 128:129], scalar1=-1.0, scalar2=None,
                                               op0=ALU.mult), r=keys_r, w=[SM])
        sy.op("dve", lambda e: e.tensor_tensor(out=smal[0:P, 10:11], in0=src[0:P, 128:129], in1=smal[0:P, 9:10],
                                               op=ALU.max), r=keys_r + [SM], w=[SM])
        sy.op("dve", lambda e: e.tensor_scalar(out=smal[0:P, 0:1], in0=smal[0:P, 10:11], scalar1=emt, scalar2=None,
                                               op0=ALU.max), r=[SM] + emt_keys, w=[SM])
        sy.op("dve", lambda e: e.reciprocal(out=smal[0:P, 1:2], in_=smal[0:P, 0:1]), r=[SM], w=[SM])
        sy.op("dve", lambda e: e.bn_stats(out=st6[0:P, :], in_=src[0:P, 0:128]), r=keys_r, w=[S6])
        sy.op("dve", lambda e: e.bn_aggr(out=smal[0:P, 2:4], in_=st6[0:P, :]), r=[S6], w=[SM])
        sy.op("dve", lambda e: e.tensor_tensor(out=smal[0:P, 4:5], in0=smal[0:P, 1:2], in1=smal[0:P, 1:2], op=ALU.mult),
              r=[SM], w=[SM])
        sy.op("dve", lambda e: e.tensor_tensor(out=smal[0:P, 6:7], in0=smal[0:P, 4:5], in1=smal[0:P, 3:4], op=ALU.mult),
              r=[SM], w=[SM])
        sy.op("act", lambda e: e.activation(out=smal[0:P, 7:8], in_=smal[0:P, 6:7], func=AF.Ln, bias=EPS),
              r=[SM], w=[SM])
        sy.op("act", lambda e: e.activation(out=smal[0:P, 7:8], in_=smal[0:P, 7:8], func=AF.Exp, scale=-0.5),
              r=[SM], w=[SM])
        sy.op("dve", lambda e: e.tensor_tensor(out=smal[0:P, 5:6], in0=smal[0:P, 7:8], in1=smal[0:P, 1:2], op=ALU.mult),
              r=[SM], w=[SM])
        sy.op("dve", lambda e: e.tensor_scalar(out=hm[0:P, h * 128:(h + 1) * 128], in0=src[0:P, 0:128],
                                               scalar1=smal[0:P, 2:3], scalar2=smal[0:P, 5:6], op0=ALU.subtract,
                                               op1=ALU.mult), r=keys_r + [SM], w=["hm"])

    def gate_and_store(i):
        sy.dma("sp", "ld4", lambda e: e.dma_start(out=sogt[:], in_=sog_s.ap()[i * 128:(i + 1) * 128, :]),
               r=["sog_s"], w=["sogt"])
        sy.op("pool", lambda e: e.tensor_tensor(out=sogt[:], in0=sogt[:], in1=mhg[:], op=ALU.mult),
              r=["sogt", "mhg"], w=["sogt"])
        sy.op("dve", lambda e: e.tensor_tensor(out=hm[:], in0=hm[:], in1=sogt[:], op=ALU.mult), r=["hm", "sogt"], w=["hm"])
        for h in range(4):
            sy.mm(lambda e, h=h: e.transpose(out=mtp[:, h, :], in_=hm[:, h * 128:(h + 1) * 128], identity=ident),
                  r=["hm", "cst"], w=["mtp"], last=(h == 3))
        sy.op("act", lambda e: e.copy(out=mts[:], in_=mtp[:]), r=["mtp"], w=["mts"])
        sy.dma("sp", "st2", lambda e: e.dma_start(out=minT_s.ap()[:, :, i * 128:(i + 1) * 128].rearrange("h p t -> p h t"),
                                                  in_=mts[:]), r=["mts"], wa=["minT_s"])

    load_chunk(0)
    for i in range(NT):
        t0 = i * 128
        b = i % 2
        load_chunk(i + 1)
        qk_, kk_, tk_, vk_ = "qmc%d" % b, "kmc%d" % b, "kmt%d" % b, "vm1%d" % b
        for h in range(4):
            p_ = h % 2
            PB["p"] = p_
            Wt, WIt, Stt, qst, kwt = WtL[p_], WItL[p_], SttL[p_], qstL[p_], kwtL[p_]
            scm, ndp, upp = scmL[p_], ndpL[p_], uppL[p_]
            sy.op("act", lambda e, h=h: e.activation(out=Wt[:], in_=MB[:, h, t0:t0 + 128], func=AF.Exp,
                                                     bias=acol[:, i, h:h + 1], scale=-1.0), r=["MB", "acol"], w=["Wt%d" % p_])
            sy.op("pool", lambda e: e.tensor_tensor(out=Wt[:], in0=Wt[:], in1=maskD, op=ALU.mult), r=["Wt%d" % p_, "cst"], w=["Wt%d" % p_])
            if i > 0:
                sy.op("act", lambda e, h=h: e.activation(out=WIt[:], in_=MB[:, h, t0:t0 + 128], func=AF.Exp,
                                                         bias=MB[:, h, t0 - 1:t0], scale=-1.0), r=["MB"], w=["WIt%d" % p_])
                sy.op("dve", lambda e, h=h: e.tensor_tensor(out=qst[:], in0=qmc[b][:, h, :], in1=WIt[0:64, :],
                                                            op=ALU.mult), r=[qk_, "WIt%d" % p_], w=["qst%d" % p_])
            sy.mm(lambda e, h=h: e.matmul(scm[:], lhsT=kmc[b][:, h, :], rhs=qmc[b][:, h, :],
                                          start=True, stop=True), r=[kk_, qk_], w=["scm%d" % p_])
            sy.op("dve", lambda e: e.tensor_tensor(out=Stt[:], in0=scm[:], in1=Wt[:], op=ALU.mult), r=["scm%d" % p_, "Wt%d" % p_], w=["Stt%d" % p_])
            sy.mm(lambda e, h=h: e.matmul(ndp[:], lhsT=Stt[:], rhs=vm1[b][:, h, :], start=True, stop=(i == 0)),
                  r=["Stt%d" % p_, vk_], w=["ndp%d" % p_], last=(i == 0))
            if i > 0:
                sy.mm(lambda e, h=h: e.matmul(ndp[:], lhsT=qst[:], rhs=Cst[:, h, :], start=False, stop=True),
                      r=["qst%d" % p_, "Cst%d" % h], w=["ndp%d" % p_])
            head_norm(ndp, 128, h, ["ndp%d" % p_], ecol[:, i, h:h + 1], ["ecol"])
            sy.op("dve", lambda e, h=h: e.tensor_scalar(out=kwt[:], in0=kmt[b][:, h * 64:(h + 1) * 64],
                                                        scalar1=Wt[:, 127:128], scalar2=None, op0=ALU.mult),
                  r=[tk_, "Wt%d" % p_], w=["kwt%d" % p_])
            sy.mm(lambda e, h=h: e.matmul(upp[:], lhsT=kwt[:], rhs=vm1[b][:, h, :], start=True, stop=True),
                  r=["kwt%d" % p_, vk_], w=["upp%d" % p_])
            if i == 0:
                sy.op("act", lambda e, h=h: e.copy(out=Cst[:, h, :], in_=upp[:]), r=["upp%d" % p_], w=["Cst%d" % h])
            else:
                sy.op("dve", lambda e, h=h: e.scalar_tensor_tensor(out=Cst[:, h, :], in0=Cst[:, h, :],
                                                                   scalar=WIt[0:64, 127:128], in1=upp[:], op0=ALU.mult,
                                                                   op1=ALU.add), r=["Cst%d" % h, "WIt%d" % p_, "upp%d" % p_], w=["Cst%d" % h])
        gate_and_store(i)
    ck_ = ["Cst%d" % h for h in range(4)]
    sy.dma("sp", "st0", lambda e: e.dma_start(out=cp_o.ap().rearrange("h p d -> p h d"), in_=Cst[:, :, 0:128]),
           r=ck_, is_out=True)
    sy.dma("sp", "st1", lambda e: e.dma_start(out=np_o.ap().rearrange("h p -> p h"), in_=Cst[:, :, 128], allow_slow_non_contiguous=True),
           r=ck_, is_out=True)

    PB["p"] = 0
    scm, ndp, upp = scmL[0], ndpL[0], uppL[0]
    bS = NT % 2
    qS, kS, tS, vS = "qmc%d" % bS, "kmc%d" % bS, "kmt%d" % bS, "vm1%d" % bS
    C0 = sb("C0", [64, NSO * HM, 129], st=st)
    sy.dma("sp", "ld0", lambda e: e.dma_start(out=C0[:], in_=stc_d.ap()), w=["C0"])
    qms = sb("qms", [NSO, 256], st=st)
    sy.dma("sp", "ld1", lambda e: e.dma_start(out=qms[:], in_=qms_s.ap()[0:NSO, :]), r=["qms_s"], w=["qms"])
    sg = sb("sg", [NSO, 12, 4], st=st)
    sy.mm(lambda e: e.transpose(out=scm[0:NSO, 0:4], in_=igt[0:4, S:S + NSO], identity=ident[0:4, 0:4]),
          r=["igt", "cst"], w=["scm0"], last=False)
    sy.mm(lambda e: e.transpose(out=scm[0:NSO, 4:8], in_=lft[0:4, S:S + NSO], identity=ident[0:4, 0:4]),
          r=["lft", "cst"], w=["scm0"])
    sy.op("act", lambda e: e.copy(out=sg[:, 0:2, :].rearrange("p a b -> p (a b)"), in_=scm[0:NSO, 0:8]), r=["scm0"], w=["sg"])
    A = lambda k: sg[:, k, :]
    sy.op("dve", lambda e: e.tensor_tensor(out=A(8), in0=A(1), in1=stm[:], op=ALU.add), r=["sg", "stm"], w=["sg"])
    sy.op("dve", lambda e: e.tensor_tensor(out=A(2), in0=A(8), in1=A(0), op=ALU.max), r=["sg"], w=["sg"])
    sy.op("dve", lambda e: e.tensor_tensor(out=A(9), in0=A(8), in1=A(2), op=ALU.subtract), r=["sg"], w=["sg"])
    sy.op("act", lambda e: e.activation(out=A(3), in_=A(9), func=AF.Exp), r=["sg"], w=["sg"])
    sy.op("dve", lambda e: e.tensor_tensor(out=A(10), in0=A(0), in1=A(2), op=ALU.subtract), r=["sg"], w=["sg"])
    sy.op("act", lambda e: e.activation(out=A(4), in_=A(10), func=AF.Exp), r=["sg"], w=["sg"])
    sy.op("act", lambda e: e.activation(out=A(5), in_=A(2), func=AF.Exp, scale=-1.0), r=["sg"], w=["sg"])
    sy.dma("sp", "st2", lambda e: e.dma_start(out=ms_o.ap(), in_=A(2)), r=["sg"], is_out=True)
    qkt = sb("qkt", [NSO, 256], st=st)
    sy.op("dve", lambda e: e.tensor_tensor(out=qkt[:], in0=qms[:], in1=kmt[bS][0:NSO, :], op=ALU.mult),
          r=["qms", tS], w=["qkt"])
    sy.op("dve", lambda e: e.tensor_reduce(out=A(6), in_=qkt[:].rearrange("p (h d) -> p h d", d=64), axis=AX.X,
                                           op=ALU.add), r=["qkt"], w=["sg"])
    sy.op("dve", lambda e: e.tensor_tensor(out=A(7), in0=A(6), in1=A(4), op=ALU.mult), r=["sg"], w=["sg"])
    wdg = sb("wdg", [NSO, NSO, 4], st=st)
    sy.op("dve", lambda e: e.tensor_tensor(out=wdg[:], in0=ident[0:NSO, 0:NSO].unsqueeze(2).to_broadcast([NSO, NSO, 4]),
                                           in1=A(3).unsqueeze(1).to_broadcast([NSO, NSO, 4]), op=ALU.mult),
          r=["sg", "cst"], w=["wdg"])
    sy.mm(lambda e: e.matmul(scm[0:64, 0:NSO * 4], lhsT=ones[0:NSO, 0:64], rhs=wdg[:].rearrange("p a b -> p (a b)"), start=True,
                             stop=True), r=["cst", "wdg"], w=["scm0"])
    WB = sb("WB", [64, NSO * 4], st=st)
    sy.op("act", lambda e: e.copy(out=WB[:], in_=scm[0:64, 0:NSO * 4]), r=["scm0"], w=["WB"])
    Qd = sb("Qd", [64, 4, NSO, NSO], st=st)
    sy.op("dve", lambda e: e.tensor_tensor(out=Qd[:], in0=qmc[bS][:, :, 0:NSO].unsqueeze(2).to_broadcast([64, 4, NSO, NSO]),
                                           in1=i4[0:64, :, :].unsqueeze(1).to_broadcast([64, 4, NSO, NSO]), op=ALU.mult),
          r=[qS, "i4"], w=["Qd"])
    sy.op("pool", lambda e: e.memset(hm[:], 0.0), w=["hm"])
    nds = sb("nds", [NSO, 129], st=st)
    cj = sb("cj", [NSO, 1], st=st)
    kws = sb("kws", [NSO, 64], st=st)
    Cn = sb("Cn", [64, NSO * 4, 129], st=st)
    for h in range(4):
        for j in range(NSO):
            sy.mm(lambda e, h=h, j=j: e.matmul(ndp[0:NSO, :], lhsT=Qd[:, h, j, :], rhs=C0[:, j * 4 + h, :],
                                               start=(j == 0), stop=(j == NSO - 1)), r=["Qd", "C0"], w=["ndp0"],
                  last=(j == NSO - 1))
        sy.op("dve", lambda e, h=h: e.tensor_scalar(out=nds[:], in0=ndp[0:NSO, :], scalar1=sg[:, 3, h:h + 1], scalar2=None,
                                                    op0=ALU.mult), r=["ndp0", "sg"], w=["nds"])
        sy.op("dve", lambda e, h=h: e.scalar_tensor_tensor(out=nds[:], in0=vm1[bS][0:NSO, h, :], scalar=sg[:, 7, h:h + 1],
                                                           in1=nds[:], op0=ALU.mult, op1=ALU.add),
              r=[vS, "sg", "nds"], w=["nds"])
        head_norm(nds, NSO, h, ["nds"], sg[:, 5, h:h + 1], ["sg"])
        for j in range(NSO):
            sy.op("dve", lambda e, h=h, j=j: e.tensor_tensor(out=cj[:], in0=ident[0:NSO, j:j + 1], in1=sg[:, 4, h:h + 1],
                                                             op=ALU.mult), r=["cst", "sg"], w=["cj"])
            sy.op("dve", lambda e, h=h: e.tensor_scalar(out=kws[:], in0=kmt[bS][0:NSO, h * 64:(h + 1) * 64],
                                                        scalar1=cj[:, 0:1], scalar2=None, op0=ALU.mult),
                  r=[tS, "cj"], w=["kws"])
            sy.mm(lambda e, h=h: e.matmul(upp[:], lhsT=kws[:], rhs=vm1[bS][0:NSO, h, :], start=True, stop=True),
                  r=["kws", vS], w=["upp0"])
            jh = j * 4 + h
            sy.op("dve", lambda e, jh=jh: e.scalar_tensor_tensor(out=Cn[:, jh, :], in0=C0[:, jh, :],
                                                                 scalar=WB[:, jh:jh + 1], in1=upp[:], op0=ALU.mult,
                                                                 op1=ALU.add), r=["C0", "WB", "upp0"], w=["Cn"])
    gate_and_store(NT)
    sy.dma("sp", "st0", lambda e: e.dma_start(out=cs_o.ap().rearrange("a p d -> p a d"), in_=Cn[:, :, 0:128]),
           r=["Cn"], is_out=True)
    sy.dma("sp", "st1", lambda e: e.dma_start(out=ns_o.ap().rearrange("a p -> p a"), in_=Cn[:, :, 128], allow_slow_non_contiguous=True),
           r=["Cn"], is_out=True)
    st.close()
    if cfg.get('stop') == '3':
        sy.finish()
        es.close()
        return nc

    sy.barrier()
    st = ExitStack()
    wba = sb("wba", [128, 4, D], st=st)
    wbm = sb("wbm", [128, 4, D], st=st)
    wo = sb("wo", [128, KC, D], st=st)
    wr = sb("wr", [128, KC, E], st=st)
    sy.dma("pool", "ld0", lambda e: e.dma_start(out=r_(wba[:]), in_=(wba_d.ap())), w=["wba"])
    sy.dma("pool", "ld1", lambda e: e.dma_start(out=r_(wbm[:]), in_=(wbm_d.ap())), w=["wbm"])
    sy.dma("pool", "ld2", lambda e: e.dma_start(out=r_(wo[:]), in_=(wo_d.ap())), w=["wo"])
    sy.dma("sp", "ld3", lambda e: e.dma_start(out=wr[:], in_=wr_d.ap()), w=["wr"])
    ain = sb("ain", [128, 4, 512], st=st)
    mi_ = sb("mi_", [128, 4, 512], st=st)
    sgat = sb("sgat", [128, KC, 512], st=st)
    sgmt = sb("sgmt", [128, KC, 512], st=st)
    mg = sb("mg", [128, KC, 512], st=st)
    t1 = sb("t1", [128, 512], st=st)
    abp = [ps("abp%d" % i, [128, 512], st=st) for i in range(2)]
    mxp = [ps("mxp%d" % i, [128, 512], st=st) for i in range(2)]
    htp = [ps("htp%d" % i, [128, 4, 128], st=st) for i in range(2)]
    lgp = ps("lgp", [128, 2, E], st=st)
    xtk = sb("xtk", [128, D], st=st)
    pre2 = [sb("pre%d" % i, [128, D], st=st) for i in range(2)]
    hT = sb("hT", [128, KC, 128], st=st)
    st12 = sb("st12", [128, 2, 6], st=st)
    mv = sb("mv", [128, 4], st=st)
    lg = sb("lg", [128, E], st=st)
    top8 = sb("top8", [128, 8], st=st)
    rt_ = sb("rt_", [128, 8], st=st)
    selt = sb("selt", [128, E], st=st)
    cntb = sb("cntb", [128, E], st=st)
    slp = sb("slp", [128, E], st=st)
    tmpE = sb("tmpE", [128, E], st=st)
    slf = sb("slf", [128, 4], st=st)
    sy.op("pool", lambda e: e.memset(cntb[:], 0.0), w=["cntb"])
    bc_reg = nc.gpsimd.alloc_register("bc_reg")
    nc.gpsimd.reg_mov(bc_reg, E * CAP - 1)

    def layer_norm(src, dst, gi, keys):
        for hf in range(2):
            sy.op("dve", lambda e, hf=hf: e.bn_stats(out=st12[:, hf, :], in_=src[:, hf * 512:(hf + 1) * 512]),
                  r=keys, w=["st12"])
        sy.op("dve", lambda e: e.bn_aggr(out=mv[:, 0:2], in_=st12[:].rearrange("p a b -> p (a b)")), r=["st12"], w=["mv"])
        sy.op("act", lambda e: e.activation(out=mv[:, 2:3], in_=mv[:, 1:2], func=AF.Ln, bias=EPS), r=["mv"], w=["mv"])
        sy.op("act", lambda e: e.activation(out=mv[:, 2:3], in_=mv[:, 2:3], func=AF.Exp, scale=-0.5), r=["mv"], w=["mv"])
        sy.op("dve", lambda e: e.tensor_scalar(out=dst[:], in0=src[:], scalar1=mv[:, 0:1], scalar2=mv[:, 2:3],
                                               op0=ALU.subtract, op1=ALU.mult), r=keys + ["mv"], w=keys)
        sy.op("pool", lambda e: e.tensor_tensor(out=dst[:], in0=dst[:], in1=lnp[:, gi, :], op=ALU.mult),
              r=keys + ["lnp"], w=keys)
        sy.op("pool", lambda e: e.tensor_tensor(out=dst[:], in0=dst[:], in1=lnp[:, gi + 1, :], op=ALU.add),
              r=keys + ["lnp"], w=keys)

    for (g0, gn) in tgroups:
        for (dst_t, src_s, nm, nch) in ((ain, ainT_s, "ain", 4), (mi_, minT_s, "mi_", 4), (sgat, sga_s, "sgat", KC),
                                        (sgmt, sgm_s, "sgmt", KC)):
            sy.dma("pool", "l" + nm, lambda e, dst_t=dst_t, src_s=src_s: e.dma_start(
                out=r_(dst_t[:, :, 0:gn]), in_=(src_s.ap()[:, :, g0:g0 + gn].rearrange("h p t -> p h t"))),
                r=[src_s.name], w=[nm])
        for dc in range(KC):
            pa, pm = abp[dc % 2], mxp[dc % 2]
            for h in range(4):
                sy.mm(lambda e, h=h, dc=dc: e.matmul(pa[:, 0:gn], lhsT=r_(wba[:, h, dc * 128:(dc + 1) * 128]),
                                                     rhs=r_(ain[:, h, 0:gn]), start=(h == 0), stop=(h == 3)),
                      r=["wba", "ain"], w=["abp%d" % (dc % 2)], last=(h == 3))
            for h in range(4):
                sy.mm(lambda e, h=h, dc=dc: e.matmul(pm[:, 0:gn], lhsT=r_(wbm[:, h, dc * 128:(dc + 1) * 128]),
                                                     rhs=r_(mi_[:, h, 0:gn]), start=(h == 0), stop=(h == 3)),
                      r=["wbm", "mi_"], w=["mxp%d" % (dc % 2)], last=(h == 3))
            sy.op("dve", lambda e, dc=dc: e.tensor_tensor(out=t1[:, 0:gn], in0=pa[:, 0:gn], in1=sgat[:, dc, 0:gn],
                                                          op=ALU.mult), r=["abp%d" % (dc % 2), "sgat"], w=["t1"])
            sy.op("dve", lambda e, dc=dc: e.tensor_tensor(out=r_(mg[:, dc, 0:gn]), in0=pm[:, 0:gn], in1=sgmt[:, dc, 0:gn],
                                                          op=ALU.mult), r=["mxp%d" % (dc % 2), "sgmt"], w=["mg%d" % dc])
            sy.op("pool", lambda e, dc=dc: e.tensor_tensor(out=r_(mg[:, dc, 0:gn]), in0=mg[:, dc, 0:gn], in1=t1[:, 0:gn],
                                                           op=ALU.add), r=["mg%d" % dc, "t1"], w=["mg%d" % dc])
        mgk = ["mg%d" % dc for dc in range(KC)]
        for tl in range(gn // 128):
            i = (g0 // 128) + tl
            pre = pre2[i % 2]
            pk = "pre%d" % (i % 2)
            sy.dma("sp", "lx", lambda e, i=i: e.dma_start(out=xtk[:], in_=xtok_d.ap()[i * 128:(i + 1) * 128, :]), w=["xtk"])
            for hf in range(2):
                for dc in range(KC):
                    sy.mm(lambda e, dc=dc, hf=hf: e.matmul(abp[hf][:], lhsT=r_(mg[:, dc, tl * 128:(tl + 1) * 128]),
                                                           rhs=r_(wo[:, dc, hf * 512:(hf + 1) * 512]), start=(dc == 0),
                                                           stop=(dc == KC - 1)), r=mgk + ["wo"], w=["abp%d" % hf],
                          last=(dc == KC - 1))
                sy.op("dve", lambda e, hf=hf: e.scalar_tensor_tensor(out=pre[:, hf * 512:(hf + 1) * 512],
                                                                     in0=xtk[:, hf * 512:(hf + 1) * 512], scalar=ALPHA,
                                                                     in1=abp[hf][:], op0=ALU.mult, op1=ALU.add),
                      r=["xtk", "abp%d" % hf], w=[pk])
            layer_norm(pre, pre, 0, [pk])
            sy.dma("sp", "sh", lambda e, i=i: e.dma_start(out=hs_s.ap()[i * 128:(i + 1) * 128, :], in_=pre[:]),
                   r=[pk], wa=["hs_s"])
            for q4 in range(2):
                for k4 in range(4):
                    kc = q4 * 4 + k4
                    sy.mm(lambda e, kc=kc, k4=k4, q4=q4: e.transpose(out=htp[q4][:, k4, :],
                                                                    in_=pre[:, kc * 128:(kc + 1) * 128], identity=ident),
                          r=[pk, "cst"], w=["htp%d" % q4], last=(k4 == 3))
                sy.op("act", lambda e, q4=q4: e.copy(out=hT[:, q4 * 4:(q4 + 1) * 4, :], in_=htp[q4][:]),
                      r=["htp%d" % q4], w=["hT"])
            for kc in range(KC):
                sy.mm(lambda e, kc=kc: e.matmul(lgp[:, 0, :], lhsT=hT[:, kc, :], rhs=wr[:, kc, :], start=(kc == 0),
                                                stop=(kc == KC - 1)), r=["hT", "wr"], w=["lgp0"], last=(kc == KC - 1))
            sy.op("dve", lambda e: e.tensor_tensor(out=lg[:], in0=lgp[:, 0, :], in1=brb[:], op=ALU.add),
                  r=["lgp0", "brb"], w=["lg"])
            sy.op("dve", lambda e: e.max(out=top8[:], in_=lg[:]), r=["lg"], w=["top8"])
            sy.op("dve", lambda e: e.tensor_scalar(out=rt_[:, 0:1], in0=top8[:, 0:1], scalar1=-1.0, scalar2=None,
                                                   op0=ALU.mult), r=["top8"], w=["rt_"])
            sy.op("act", lambda e: e.activation(out=rt_[:, 4:8], in_=top8[:, 0:4], func=AF.Exp, bias=rt_[:, 0:1]),
                  r=["top8", "rt_"], w=["rt_"])
            sy.op("dve", lambda e: e.tensor_reduce(out=rt_[:, 1:2], in_=rt_[:, 4:8], axis=AX.X, op=ALU.add),
                  r=["rt_"], w=["rt_"])
            sy.op("dve", lambda e: e.reciprocal(out=rt_[:, 2:3], in_=rt_[:, 1:2]), r=["rt_"], w=["rt_"])
            sy.op("dve", lambda e, i=i: e.tensor_scalar(out=wts[:, i, :], in0=rt_[:, 4:8], scalar1=rt_[:, 2:3],
                                                        scalar2=None, op0=ALU.mult), r=["rt_"], w=["wts"])
            sy.op("dve", lambda e: e.tensor_scalar(out=selt[:], in0=lg[:], scalar1=top8[:, 3:4], scalar2=None,
                                                   op0=ALU.is_ge), r=["lg", "top8"], w=["selt"])
            if i == NT:
                sy.op("dve", lambda e: e.tensor_scalar(out=selt[:], in0=selt[:], scalar1=rowm, scalar2=None,
                                                       op0=ALU.mult), r=["selt", "cst"], w=["selt"])
            sy.mm(lambda e: e.matmul(lgp[:, 1, :], lhsT=ltri, rhs=selt[:], start=True, stop=True),
                  r=["cst", "selt"], w=["lgp1"])
            sy.op("dve", lambda e: e.tensor_tensor(out=slp[:], in0=lgp[:, 1, :], in1=cntb[:], op=ALU.add),
                  r=["lgp1", "cntb"], w=["slp"])
            sy.op("dve", lambda e: e.tensor_scalar(out=tmpE[:], in0=slp[:], scalar1=float(CAP) - 0.5, scalar2=BIG,
                                                   op0=ALU.is_ge, op1=ALU.mult), r=["slp"], w=["tmpE"])
            sy.op("dve", lambda e: e.tensor_tensor(out=slp[:], in0=slp[:], in1=tmpE[:], op=ALU.add),
                  r=["slp", "tmpE"], w=["slp"])
            sy.op("dve", lambda e: e.tensor_scalar(out=tmpE[:], in0=selt[:], scalar1=-BIG, scalar2=BIG, op0=ALU.mult,
                                                   op1=ALU.add), r=["selt"], w=["tmpE"])
            sy.op("dve", lambda e: e.tensor_tensor(out=slp[:], in0=slp[:], in1=tmpE[:], op=ALU.add),
                  r=["slp", "tmpE"], w=["slp"])
            sy.op("dve", lambda e: e.tensor_tensor(out=slp[:], in0=slp[:], in1=ecap[:], op=ALU.add),
                  r=["slp", "ecap"], w=["slp"])
            sy.mm(lambda e: e.matmul(lgp[:, 1, :], lhsT=ones, rhs=selt[:], start=True, stop=True),
                  r=["cst", "selt", "slp"], w=["lgp1"])
            sy.op("dve", lambda e: e.tensor_tensor(out=cntb[:], in0=cntb[:], in1=lgp[:, 1, :], op=ALU.add),
                  r=["cntb", "lgp1"], w=["cntb"])
            for j in range(4):
                sy.op("dve", lambda e, j=j: e.tensor_scalar(out=tmpE[:], in0=lg[:], scalar1=top8[:, j:j + 1], scalar2=None,
                                                            op0=ALU.is_equal), r=["lg", "top8"], w=["tmpE"])
                sy.op("dve", lambda e: e.tensor_tensor(out=tmpE[:], in0=tmpE[:], in1=slp[:], op=ALU.mult),
                      r=["tmpE", "slp"], w=["tmpE"])
                sy.op("dve", lambda e, j=j: e.tensor_reduce(out=slf[:, j:j + 1], in_=tmpE[:], axis=AX.X, op=ALU.add),
                      r=["tmpE"], w=["slf"])
            sy.op("dve", lambda e: e.tensor_scalar(out=slf[:], in0=slf[:], scalar1=float(E * CAP + 64), scalar2=None,
                                                   op0=ALU.min), r=["slf"], w=["slf"])
            sy.op("dve", lambda e, i=i: e.tensor_copy(out=sli[:, i, :], in_=slf[:]), r=["slf"], w=["sli"])
            for j in range(4):
                sy.dma("pool", "scat%d" % j, lambda e, i=i, j=j: e.indirect_dma_start(
                    out=xg_s.ap(), out_offset=bass.IndirectOffsetOnAxis(ap=sli[:, i, j:j + 1], axis=0), in_=pre[:],
                    in_offset=None, bounds_check=bc_reg, oob_is_err=False), r=[pk, "sli", "xg_zero"], wa=["xg_s"])
    st.close()
    if cfg.get('stop') == '4':
        sy.finish()
        es.close()
        return nc

    sy.barrier()
    st = ExitStack()
    NWB = 6
    wbuf = [sb("wbuf%d" % i, [128, KC, 512], BF16, st=st) for i in range(NWB)]
    xe2 = [sb("xe%d" % i, [128, CT, D], st=st) for i in range(2)]
    xeT2 = [sb("xeT%d" % i, [128, KC, CAP], BF16, st=st) for i in range(2)]
    hid = sb("hid", [128, FC, CAP], BF16, st=st)
    gt2 = [sb("gt%d" % i, [128, CAP], st=st) for i in range(2)]
    ut2 = [sb("ut%d" % i, [128, CAP], st=st) for i in range(2)]
    sgt2 = [sb("sgt%d" % i, [128, CAP], st=st) for i in range(2)]
    bdb = sb("bdb", [128, D], st=st)
    yst = [sb("yst%d" % i, [128, D], st=st) for i in range(2)]
    ep = [ps("ep%d" % i, [128, 512], st=st) for i in range(6)]
    tpp = [ps("tpp%d" % i, [128, 4, 128], st=st) for i in range(2)]
    wi = 0
    pi_ = 0
    ti_ = 0
    yi_ = 0
    assert FF % 512 == 0 or FF < 512
    FH = max(1, FF // 512)
    FW = min(512, FF)
    for e_ in range(E):
        xe, xeT = xe2[e_ % 2], xeT2[e_ % 2]
        xek, xetk = "xe%d" % (e_ % 2), "xeT%d" % (e_ % 2)
        sy.dma("sp", "lxe%d" % (e_ % 2), lambda e, e_=e_, xe=xe: e.dma_start(
            out=xe[:], in_=xg_s.ap()[e_ * CAP:(e_ + 1) * CAP, :].rearrange("(a p) d -> p a d", p=128)),
            r=["xg_s", "xg_zero"], w=[xek])
        sy.dma("pool", "lbd", lambda e, e_=e_: e.dma_start(out=bdb[:], in_=bd_d.ap()[e_].partition_broadcast(128)), w=["bdb"])
        for a in range(CT):
            for q4 in range(KC // 4):
                tb = ti_ % 2
                ti_ += 1
                for k4 in range(4):
                    kc = q4 * 4 + k4
                    sy.mm(lambda e, a=a, kc=kc, k4=k4, tb=tb, xe=xe: e.transpose(out=tpp[tb][:, k4, :],
                                                                                 in_=xe[:, a, kc * 128:(kc + 1) * 128],
                                                                                 identity=ident),
                          r=[xek, "cst"], w=["tpp%d" % tb], last=(k4 == 3))
                sy.op("act", lambda e, a=a, q4=q4, tb=tb, xeT=xeT: e.copy(out=xeT[:, q4 * 4:(q4 + 1) * 4, a * 128:(a + 1) * 128],
                                                                          in_=tpp[tb][:]), r=["tpp%d" % tb], w=[xetk])
        for fh in range(FH):
            wg_b = wi % NWB
            wi += 1
            wu_b = wi % NWB
            wi += 1
            sy.dma("pool", "lw%d" % wg_b, lambda e, e_=e_, fh=fh, wg_b=wg_b: e.dma_start(
                out=wbuf[wg_b][:, :, 0:FW], in_=(wg_d.ap()[e_, :, :, fh * FW:(fh + 1) * FW])), w=["wbuf%d" % wg_b])
            sy.dma("pool", "lw%d" % wu_b, lambda e, e_=e_, fh=fh, wu_b=wu_b: e.dma_start(
                out=wbuf[wu_b][:, :, 0:FW], in_=(wu_d.ap()[e_, :, :, fh * FW:(fh + 1) * FW])), w=["wbuf%d" % wu_b])
            for f4 in range(FW // 128):
                fc = fh * (FW // 128) + f4
                gt, ut, sgt = gt2[fc % 2], ut2[fc % 2], sgt2[fc % 2]
                gk, uk, sk = "gt%d" % (fc % 2), "ut%d" % (fc % 2), "sgt%d" % (fc % 2)
                pg = pi_ % 6
                pi_ += 1
                pu = pi_ % 6
                pi_ += 1
                for kc in range(KC):
                    sy.mm(lambda e, kc=kc, f4=f4, pg=pg, wg_b=wg_b, xeT=xeT: e.matmul(
                        ep[pg][:, 0:CAP], lhsT=wbuf[wg_b][:, kc, f4 * 128:(f4 + 1) * 128], rhs=xeT[:, kc, :],
                        start=(kc == 0), stop=(kc == KC - 1)), r=["wbuf%d" % wg_b, xetk], w=["ep%d" % pg],
                        last=(kc == KC - 1))
                for kc in range(KC):
                    sy.mm(lambda e, kc=kc, f4=f4, pu=pu, wu_b=wu_b, xeT=xeT: e.matmul(
                        ep[pu][:, 0:CAP], lhsT=wbuf[wu_b][:, kc, f4 * 128:(f4 + 1) * 128], rhs=xeT[:, kc, :],
                        start=(kc == 0), stop=(kc == KC - 1)), r=["wbuf%d" % wu_b, xetk], w=["ep%d" % pu],
                        last=(kc == KC - 1))
                sy.op("dve", lambda e, pg=pg, fc=fc, e_=e_, gt=gt: e.tensor_scalar(
                    out=gt[:], in0=ep[pg][:, 0:CAP], scalar1=bgu[:, 0, e_, fc:fc + 1], scalar2=LIMIT, op0=ALU.add,
                    op1=ALU.min), r=["ep%d" % pg, "bgu"], w=[gk])
                sy.op("act", lambda e, gt=gt, sgt=sgt: e.activation(out=sgt[:], in_=gt[:], func=AF.Sigmoid, scale=SALPHA),
                      r=[gk], w=[sk])
                sy.op("dve", lambda e, pu=pu, fc=fc, e_=e_, ut=ut: e.tensor_scalar(
                    out=ut[:], in0=ep[pu][:, 0:CAP], scalar1=bu1[:, e_, fc:fc + 1], scalar2=LIMIT + 1.0, op0=ALU.add,
                    op1=ALU.min), r=["ep%d" % pu, "bu1"], w=[uk])
                sy.op("dve", lambda e, gt=gt, sgt=sgt: e.tensor_tensor(out=gt[:], in0=gt[:], in1=sgt[:], op=ALU.mult),
                      r=[gk, sk], w=[gk])
                sy.op("dve", lambda e, fc=fc, gt=gt, ut=ut: e.scalar_tensor_tensor(out=hid[:, fc, :], in0=ut[:],
                                                                                   scalar=1.0 - LIMIT, in1=gt[:],
                                                                                   op0=ALU.max, op1=ALU.mult),
                      r=[gk, uk], w=["hid%d" % fc])
        hk = ["hid%d" % fc for fc in range(FC)]
        wds = []
        for dh in range(D // 512):
            wd_b = wi % NWB
            wi += 1
            wds.append(wd_b)
            sy.dma("pool", "lw%d" % wd_b, lambda e, e_=e_, dh=dh, wd_b=wd_b: e.dma_start(
                out=wbuf[wd_b][:, 0:FC, :], in_=(wd_d.ap()[e_, :, :, dh * 512:(dh + 1) * 512])), w=["wbuf%d" % wd_b])
            if dh == 0 and NWB < 4:
                pass
        for a in range(CT):
            yb = yi_ % 2
            yi_ += 1
            for dh in range(D // 512):
                pd = pi_ % 6
                pi_ += 1
                for fc in range(FC):
                    sy.mm(lambda e, fc=fc, a=a, dh=dh, pd=pd: e.matmul(
                        ep[pd][:], lhsT=hid[:, fc, a * 128:(a + 1) * 128], rhs=wbuf[wds[dh]][:, fc, :],
                        start=(fc == 0), stop=(fc == FC - 1)), r=hk + ["wbuf%d" % wds[dh]], w=["ep%d" % pd],
                        last=(fc == FC - 1))
                sy.op("dve", lambda e, dh=dh, pd=pd, yb=yb: e.tensor_tensor(
                    out=yst[yb][:, dh * 512:(dh + 1) * 512], in0=ep[pd][:], in1=bdb[:, dh * 512:(dh + 1) * 512],
                    op=ALU.add), r=["ep%d" % pd, "bdb"], w=["yst%d" % yb])
            sy.dma("sp", "sy%d" % yb, lambda e, e_=e_, a=a, yb=yb: e.dma_start(
                out=yx_s.ap()[e_ * CAP + a * 128:e_ * CAP + (a + 1) * 128, :], in_=yst[yb][:]),
                r=["yst%d" % yb], wa=["yx_s"])
    st.close()
    if cfg.get('stop') == '5':
        sy.finish()
        es.close()
        return nc

    sy.barrier()
    st = ExitStack()
    yg = [sb("yg%d" % i, [128, 4, D], st=st) for i in range(2)]
    hb = [sb("hb%d" % i, [128, D], st=st) for i in range(2)]
    st12 = sb("st12b", [128, 2, 6], st=st)
    mv = sb("mvb", [128, 4], st=st)
    for b in range(2):
        sy.op("pool", lambda e, b=b: e.memset(yg[b][:], 0.0), w=["yg%d" % b])
    for i in range(NTT):
        b = i % 2
        for j in range(4):
            sy.dma("pool", "gat%d_%d" % (b, j), lambda e, i=i, j=j, b=b: e.indirect_dma_start(
                out=yg[b][:, j, :], out_offset=None, in_=yx_s.ap(),
                in_offset=bass.IndirectOffsetOnAxis(ap=sli[:, i, j:j + 1], axis=0), bounds_check=bc_reg,
                oob_is_err=False), r=["yx_s", "sli"], w=["yg%d" % b])
        sy.dma("sp", "lh%d" % b, lambda e, i=i, b=b: e.dma_start(out=hb[b][:], in_=hs_s.ap()[i * 128:(i + 1) * 128, :]),
               r=["hs_s"], w=["hb%d" % b])
        sy.op("dve", lambda e, b=b: e.tensor_scalar(out=hb[b][:], in0=hb[b][:], scalar1=ALPHA, scalar2=None, op0=ALU.mult),
              r=["hb%d" % b], w=["hb%d" % b])
        for j in range(4):
            sy.op("dve", lambda e, b=b, j=j, i=i: e.scalar_tensor_tensor(out=hb[b][:], in0=yg[b][:, j, :],
                                                                         scalar=wts[:, i, j:j + 1], in1=hb[b][:],
                                                                         op0=ALU.mult, op1=ALU.add),
                  r=["yg%d" % b, "wts", "hb%d" % b], w=["hb%d" % b])
        layer_norm(hb[b], hb[b], 2, ["hb%d" % b])
        if i < NT:
            sy.dma("sp", "so%d" % b, lambda e, i=i, b=b: e.dma_start(out=y_o.ap()[i * 128:(i + 1) * 128, :], in_=hb[b][:]),
                   r=["hb%d" % b], is_out=True)
        else:
            sy.dma("sp", "so%d" % b, lambda e, b=b: e.dma_start(out=ys_o.ap(), in_=hb[b][0:NSO, :]),
                   r=["hb%d" % b], is_out=True)
    sy.finish()
    st.close()
    es.close()
    return nc


def consts(cfg):
    S, PAST, E, CAP = cfg["S"], cfg["PAST"], cfg["E"], cfg["CAP"]
    NT = S // 128
    c = np.zeros((128, 1024), np.float32)
    c[:, 0:128] = np.eye(128, dtype=np.float32)
    c[:, 128:256] = 1.0
    p = np.arange(128)
    c[:, 256:384] = (p[:, None] < p[None, :]).astype(np.float32)
    c[:, 384:512] = (p[:, None] <= p[None, :]).astype(np.float32)
    c[:NSO, 512] = 1.0
    c[:PAST // 128, 513] = 1.0
    q = np.arange(512)
    cm = np.zeros((128, 4, 512), np.float32)
    for off in range(4):
        cm[:, off, :] = (q[None, :] >= off * 128 + p[:, None]).astype(np.float32)
    inv = (np.float32(THETA) ** (-np.arange(0, ROT, 2, dtype=np.float32) / np.float32(ROT))).astype(np.float32)
    pos = np.concatenate([np.arange(S), np.full(128, PAST)]).astype(np.float32)
    ang = pos[:, None] * inv[None, :]
    tab = np.concatenate([np.cos(ang), np.sin(ang)], axis=1).astype(np.float32)
    rope = np.ascontiguousarray(tab.reshape(NT + 1, 128, 16).transpose(1, 0, 2))
    ropes = np.ascontiguousarray(np.broadcast_to(tab[S], (128, 16))).astype(np.float32)
    ecap = np.ascontiguousarray(np.broadcast_to((np.arange(E) * CAP).astype(np.float32), (128, E)))
    i4 = np.ascontiguousarray(np.broadcast_to(np.eye(4, dtype=np.float32), (128, 4, 4)))
    return dict(cst=c, cm=cm, rope=rope, ropes=ropes, ecap=ecap, i4=i4)


def prep(cfg, inp):
    S, PAST, NPOOL, E, FF, CAP, D = (cfg[k] for k in ("S", "PAST", "NPOOL", "E", "FF", "CAP", "D"))
    KC, FC = D // 128, FF // 128
    T = S + 128
    f = lambda a: np.ascontiguousarray(np.asarray(a, dtype=np.float32))
    cs = consts(cfg)
    w_in = np.asarray(inp["w_in"][0], np.float32)
    win = f(w_in.reshape(KC, 128, -1).transpose(1, 0, 2))
    shared = dict(
        win=win,
        bigate=f(np.concatenate([inp["b_igate"][0], inp["b_fgate"][0]]).reshape(8, 1)),
        lamp=f(np.stack([inp["lambda_q1"][0], inp["lambda_k1"][0], inp["lambda_q2"][0], inp["lambda_k2"][0]])),
        subg=f(np.asarray(inp["subln_g"][0]).reshape(128, 1)),
        mhg=f(inp["mh_norm_g"][0]),
        wba=f(np.asarray(inp["w_ba"][0]).reshape(4, 128, D).transpose(1, 0, 2)),
        wbm=f(np.asarray(inp["w_bm"][0]).reshape(4, 128, D).transpose(1, 0, 2)),
        wo=f(np.asarray(inp["w_o"][0]).reshape(KC, 128, D).transpose(1, 0, 2)),
        lnp=f(np.stack([inp["ln1_g"][0], inp["ln1_b"][0], inp["ln2_g"][0], inp["ln2_b"][0]])),
        wr=f(np.asarray(inp["w_router"][0]).reshape(KC, 128, E).transpose(1, 0, 2)),
        br=f(inp["b_router"][0]),
        wg=f(np.asarray(inp["w_gate"][0]).reshape(E, KC, 128, FF).transpose(0, 2, 1, 3)),
        wu=f(np.asarray(inp["w_up"][0]).reshape(E, KC, 128, FF).transpose(0, 2, 1, 3)),
        wd=f(np.asarray(inp["w_down"][0]).reshape(E, FC, 128, D).transpose(0, 2, 1, 3)),
        bgu=f(np.stack([np.asarray(inp["b_gate"][0]).reshape(E, FC, 128), np.asarray(inp["b_up"][0]).reshape(E, FC, 128)])
              .transpose(3, 0, 1, 2)),
        bd=f(inp["b_down"][0]),
        **cs,
    )
    xp = np.asarray(inp["x_prompt"], np.float32)
    xs = np.asarray(inp["x_sample"], np.float32)[:, 0, :]
    ck = np.asarray(inp["cache_k"][0])
    cv = np.asarray(inp["cache_v"][0])
    pt = np.asarray(inp["page_table"]).astype(np.int32)
    ckh, cvh = {}, {}
    for h in range(HA):
        ckh[h] = np.ascontiguousarray(ck[:, :, h, :].reshape(NPOOL, 128 // 32, 32 * 128).transpose(1, 0, 2))
        cvh[h] = np.ascontiguousarray(cv[:, :, h, :].reshape(NPOOL, 128 // 32, 32 * 128).transpose(1, 0, 2))
    cols = {}
    for h in range(HA):
        cols[h] = np.concatenate([w_in[:, h * 128:(h + 1) * 128], w_in[:, 512 + h * 128:512 + (h + 1) * 128],
                                  w_in[:, 1024 + h * 128:1024 + (h + 1) * 128]], axis=1)
    whs_all = f(np.stack([cols[hh].reshape(KC, 128, 384).transpose(1, 0, 2) for hh in range(4)], axis=1))
    maps = []
    for c in range(8):
        h, g = c % 4, c // 4
        xt = np.zeros((T, D), np.float32)
        xt[:S] = xp[c]
        xt[S:S + NSO] = xs[4 * c:4 * c + 4]
        xT = f(xt.T.reshape(KC, 128, T).transpose(1, 0, 2))
        xo = xs[4 * c:4 * c + 4]
        xgT = np.zeros((128, 4, KC, NSG), np.float32)
        xoT = xo.T.reshape(KC, 128, NSO).transpose(1, 0, 2)
        for hh in range(4):
            xgT[:, hh, :, hh * 4:hh * 4 + 4] = xoT
        ptg = np.zeros((128, NSG), np.int32)
        for hh in range(4):
            ptg[:pt.shape[1], hh * 4:hh * 4 + 4] = pt[4 * c:4 * c + 4].T
        sel = np.zeros((128, 16), np.float32)
        for hh in range(4):
            for j in range(4):
                rank = g * 4 + hh
                sel[rank * 16 + (c % 4) * 4 + j, hh * 4 + j] = 1.0
        sc = np.asarray(inp["state_c"][0][4 * c:4 * c + 4], np.float32)
        sn = np.asarray(inp["state_n"][0][4 * c:4 * c + 4], np.float32)
        stc = np.concatenate([sc, sn[..., None]], axis=-1).reshape(NSO * HM, 64, 129).transpose(1, 0, 2)
        m = dict(shared)
        m.update(xT=xT, xtok=f(xt), xgT=xgT, whs=whs_all,
                 pt=ptg, selm=sel, stc=f(stc),
                 stm=f(inp["state_m"][0][4 * c:4 * c + 4]))
        for hh in range(4):
            for i in range(4):
                m["ck%d_%d" % (hh, i)] = ckh[hh][i]
                m["cv%d_%d" % (hh, i)] = cvh[hh][i]
        maps.append(m)
    return maps


def assemble(cfg, res):
    S, D = cfg["S"], cfg["D"]
    g = lambda n: np.stack([np.asarray(r[n]) for r in res])
    y_p = g("y")
    y_s = g("ysamp").reshape(32, 1, D)
    k_p = g("k").reshape(1, 8, S, HA, 128)
    v_p = g("v").reshape(1, 8, S, HA, 128)
    c_p = g("cp").reshape(1, 8, HM, 64, 128)
    n_p = g("npr").reshape(1, 8, HM, 64)
    m_p = g("mp").reshape(1, 8, HM)
    k_s = g("ksamp").reshape(1, 32, 1, HA, 128)
    v_s = g("vsamp").reshape(1, 32, 1, HA, 128)
    c_s = g("csamp").reshape(1, 32, HM, 64, 128)
    n_s = g("nsamp").reshape(1, 32, HM, 64)
    m_s = g("msamp").reshape(1, 32, HM)
    return tuple(np.ascontiguousarray(a, dtype=np.float32) for a in
                 (y_p, y_s, k_p, v_p, c_p, n_p, m_p, k_s, v_s, c_s, n_s, m_s))


def run(cfg, inp):
    nc = build(cfg)
    maps = prep(cfg, inp)
    res = run_bass_kernel_spmd(nc, maps, core_ids=list(range(8)))
    return assemble(cfg, res.results)


def kernel(**inputs):
    return run(FULL, inputs)
```
